# Optimizing a Trainium2 kernel written in Bass

```python
import numpy as np
import jax
import jax.numpy as jnp
from jax import lax


D_MODEL = 1024
BATCH = 4
SEQ = 4096
DEPTH = 2

HEAD_DIM = 64
N_HEADS = D_MODEL // HEAD_DIM
N_KV_HEADS = 4
GROUP = N_HEADS // N_KV_HEADS
ROPE_THETA = 10000.0
RMS_EPS = 1e-6
NEG_INF = -1e30
POS_INF = 1e30
IDX_HEADS = 8
IDX_DIM = HEAD_DIM
DSA_TOPK = 256
DSA_Q_BLOCK = 128
CMP_LEN = 32
CMP_STRIDE = 16
CMP_HIDDEN = 256
SLC_LEN = 64
SLC_TOPN = 16
WINDOW = 512
NSA_Q_BLOCK = 64
N_NSA_BRANCH = 3
D_FF = 256 * ((8 * D_MODEL // 3 + 255) // 256)
N_EXPERTS = 8
TOP_K_EXPERTS = 2
D_FF_EXPERT = 7 * D_MODEL // 2
N_A_LAYERS = max(1, DEPTH // 2)
N_B_LAYERS = DEPTH - N_A_LAYERS
N_DENSE_LAYERS = (DEPTH + 1) // 2
N_MOE_LAYERS = DEPTH // 2
A_WIDTHS = (N_HEADS * HEAD_DIM, N_KV_HEADS * HEAD_DIM, N_KV_HEADS * HEAD_DIM, IDX_HEADS * IDX_DIM, IDX_DIM, IDX_HEADS)
A_IN = sum(A_WIDTHS)
B_IN = N_HEADS * HEAD_DIM + N_NSA_BRANCH * N_HEADS
KV_WIDTH = 2 * N_NSA_BRANCH * N_KV_HEADS * HEAD_DIM

kernel_name = 'yoco_dsa_nsa_adaln_moe_trunk'

F32 = jnp.float32


def rmsnorm(x, g):
    xf = x.astype(F32)
    y = xf * lax.rsqrt(jnp.mean(xf * xf, axis=-1, keepdims=True) + RMS_EPS)
    return (y * g.astype(F32)).astype(x.dtype)


def modulate(u, shift, scale):
    return u * (1.0 + scale[:, None, :]) + shift[:, None, :]


def rope_tables(positions, dim):
    inv = 1.0 / (ROPE_THETA ** (jnp.arange(0, dim, 2, dtype=F32) / dim))
    ang = positions.astype(F32)[..., None] * inv
    return jnp.cos(ang), jnp.sin(ang)


def apply_rope(x, cos, sin):
    x1, x2 = jnp.split(x.astype(F32), 2, axis=-1)
    c = cos[:, :, None, :]
    s = sin[:, :, None, :]
    return jnp.concatenate([x1 * c - x2 * s, x2 * c + x1 * s], axis=-1).astype(x.dtype)


def masked_softmax(s, mask):
    p = jax.nn.softmax(jnp.where(mask, s, NEG_INF), axis=-1)
    return jnp.where(mask, p, 0.0)


def swiglu(u, wg, wu, wd):
    return (jax.nn.silu(u @ wg) * (u @ wu)) @ wd


def moe_swiglu(u, w_router, wg, wu, wd):
    bsz, seq, d = u.shape
    xf = u.reshape(bsz * seq, d)
    logits = (xf @ w_router).astype(F32)
    vals, idx = lax.top_k(logits, TOP_K_EXPERTS)
    w = jax.nn.softmax(vals, axis=-1)
    gate = jnp.sum(jax.nn.one_hot(idx, N_EXPERTS, dtype=F32) * w[..., None], axis=1)
    y = jnp.zeros_like(xf)
    for e in range(N_EXPERTS):
        y = y + gate[:, e:e + 1].astype(xf.dtype) * swiglu(xf, wg[e], wu[e], wd[e])
    return y.reshape(bsz, seq, d)


def dsa_mixer(u, cos, sin, w_in, w_out):
    bsz, seq, _ = u.shape
    splits = [int(i) for i in np.cumsum(A_WIDTHS[:-1])]
    q, k, v, iq, ik, iw = jnp.split(u @ w_in, splits, axis=-1)
    q = apply_rope(q.reshape(bsz, seq, N_HEADS, HEAD_DIM), cos, sin)
    k = apply_rope(k.reshape(bsz, seq, N_KV_HEADS, HEAD_DIM), cos, sin)
    v = v.reshape(bsz, seq, N_KV_HEADS, HEAD_DIM)
    iq = apply_rope(iq.reshape(bsz, seq, IDX_HEADS, IDX_DIM), cos, sin).astype(F32)
    ik = apply_rope(ik.reshape(bsz, seq, 1, IDX_DIM), cos, sin)[:, :, 0].astype(F32)
    iw = iw.astype(F32) * (IDX_HEADS ** -0.5 * IDX_DIM ** -0.5)
    n_keep = min(DSA_TOPK, seq // 4)
    key_pos = jnp.arange(seq)
    scale = HEAD_DIM ** -0.5

    def block(i):
        q0 = i * DSA_Q_BLOCK
        t = q0 + jnp.arange(DSA_Q_BLOCK)
        qb = lax.dynamic_slice_in_dim(q, q0, DSA_Q_BLOCK, axis=1)
        iqb = lax.dynamic_slice_in_dim(iq, q0, DSA_Q_BLOCK, axis=1)
        iwb = lax.dynamic_slice_in_dim(iw, q0, DSA_Q_BLOCK, axis=1)
        rel = jax.nn.relu(jnp.einsum('bqhd,bsd->bqhs', iqb, ik))
        score = jnp.einsum('bqh,bqhs->bqs', iwb, rel)
        score = jnp.where(key_pos[None, None, :] <= t[None, :, None], score, NEG_INF)
        _, idx = lax.top_k(score, n_keep)
        kg = jax.vmap(lambda kb, ib: kb[ib])(k, idx)
        vg = jax.vmap(lambda vb, ib: vb[ib])(v, idx)
        qg = qb.reshape(bsz, DSA_Q_BLOCK, N_KV_HEADS, GROUP, HEAD_DIM)
        s = jnp.einsum('bqgrd,bqkgd->bgrqk', qg, kg).astype(F32) * scale
        valid = (idx <= t[None, :, None])[:, None, None]
        p = masked_softmax(s, valid).astype(vg.dtype)
        o = jnp.einsum('bgrqk,bqkgd->bqgrd', p, vg)
        return o.reshape(bsz, DSA_Q_BLOCK, N_HEADS * HEAD_DIM)

    o = lax.map(block, jnp.arange(seq // DSA_Q_BLOCK))
    o = jnp.transpose(o, (1, 0, 2, 3)).reshape(bsz, seq, N_HEADS * HEAD_DIM)
    return o @ w_out


def compress_blocks(x, blk_idx, pe, w1, w2):
    bsz = x.shape[0]
    n_cmp = blk_idx.shape[0]
    xb = x[:, blk_idx] + pe[None, None, :, None, :]
    xb = jnp.transpose(xb, (0, 1, 3, 2, 4)).reshape(bsz, n_cmp, N_KV_HEADS, CMP_LEN * HEAD_DIM)
    return jax.nn.gelu(xb @ w1) @ w2


def nsa_shared_kv(h, c_act, kv_gain, w_kv_ada, b_kv_ada, w_kv, pe_k, w1_k, w2_k, pe_v, w1_v, w2_v, cos, sin):
    bsz, seq, _ = h.shape
    shift, scale = jnp.split(c_act @ w_kv_ada + b_kv_ada, 2, axis=-1)
    u = modulate(rmsnorm(h, kv_gain), shift, scale)
    kv = (u @ w_kv).reshape(bsz, seq, 2 * N_NSA_BRANCH, N_KV_HEADS, HEAD_DIM)
    k_cmp = apply_rope(kv[:, :, 0], cos, sin)
    v_cmp = kv[:, :, 1]
    k_slc = apply_rope(kv[:, :, 2], cos, sin)
    v_slc = kv[:, :, 3]
    k_win = apply_rope(kv[:, :, 4], cos, sin)
    v_win = kv[:, :, 5]
    n_cmp = (seq - CMP_LEN) // CMP_STRIDE + 1
    blk_idx = np.arange(n_cmp)[:, None] * CMP_STRIDE + np.arange(CMP_LEN)[None, :]
    kc = compress_blocks(k_cmp, blk_idx, pe_k, w1_k, w2_k)
    vc = compress_blocks(v_cmp, blk_idx, pe_v, w1_v, w2_v)
    return kc, vc, k_slc, v_slc, k_win, v_win


def nsa_mixer(u, cos, sin, kc, vc, k_slc, v_slc, k_win, v_win, w_q, w_out):
    bsz, seq, _ = u.shape
    proj = u @ w_q
    q = apply_rope(proj[..., :N_HEADS * HEAD_DIM].reshape(bsz, seq, N_HEADS, HEAD_DIM), cos, sin)
    gates = jax.nn.sigmoid(proj[..., N_HEADS * HEAD_DIM:].astype(F32)).reshape(bsz, seq, N_HEADS, N_NSA_BRANCH)
    n_cmp = kc.shape[1]
    n_slc = seq // SLC_LEN
    n_sel = min(SLC_TOPN, n_slc)
    cmp_start = np.arange(n_cmp) * CMP_STRIDE
    cmp_end = jnp.asarray(cmp_start + CMP_LEN - 1)
    slc_start = np.arange(n_slc) * SLC_LEN
    ov = np.minimum(cmp_start[:, None] + CMP_LEN, slc_start[None, :] + SLC_LEN) - np.maximum(cmp_start[:, None], slc_start[None, :])
    agg = jnp.asarray(np.clip(ov, 0, None) / CMP_LEN, dtype=F32)
    ks_blk = jnp.transpose(k_slc.reshape(bsz, n_slc, SLC_LEN, N_KV_HEADS, HEAD_DIM), (0, 3, 1, 2, 4))
    vs_blk = jnp.transpose(v_slc.reshape(bsz, n_slc, SLC_LEN, N_KV_HEADS, HEAD_DIM), (0, 3, 1, 2, 4))
    kw_pad = jnp.pad(k_win, ((0, 0), (WINDOW, 0), (0, 0), (0, 0)))
    vw_pad = jnp.pad(v_win, ((0, 0), (WINDOW, 0), (0, 0), (0, 0)))
    blk = jnp.arange(n_slc)
    in_blk = jnp.arange(SLC_LEN)
    scale = HEAD_DIM ** -0.5

    def block(i):
        q0 = i * NSA_Q_BLOCK
        t = q0 + jnp.arange(NSA_Q_BLOCK)
        qb = lax.dynamic_slice_in_dim(q, q0, NSA_Q_BLOCK, axis=1)
        qb = jnp.transpose(qb.reshape(bsz, NSA_Q_BLOCK, N_KV_HEADS, GROUP, HEAD_DIM), (0, 2, 3, 1, 4))
        gb = lax.dynamic_slice_in_dim(gates, q0, NSA_Q_BLOCK, axis=1)
        gb = jnp.transpose(gb.reshape(bsz, NSA_Q_BLOCK, N_KV_HEADS, GROUP, N_NSA_BRANCH), (0, 2, 3, 1, 4))
        sc = jnp.einsum('bgrqd,bngd->bgrqn', qb, kc).astype(F32) * scale
        pc = masked_softmax(sc, cmp_end[None, :] <= t[:, None])
        oc = jnp.einsum('bgrqn,bngd->bgrqd', pc.astype(vc.dtype), vc)
        imp = jnp.einsum('bgrqn,nj->bgqj', pc, agg)
        jt = t // SLC_LEN
        valid_b = blk[None, :] * SLC_LEN <= t[:, None]
        forced = (blk[None, :] == 0) | (blk[None, :] == jt[:, None]) | (blk[None, :] == jt[:, None] - 1)
        imp = jnp.where(valid_b, jnp.where(forced, POS_INF, imp), NEG_INF)
        _, sel = lax.top_k(imp, n_sel)
        kg = jax.vmap(jax.vmap(lambda kb, sb: kb[sb]))(ks_blk, sel)
        vg = jax.vmap(jax.vmap(lambda vb, sb: vb[sb]))(vs_blk, sel)
        tok = sel[..., None] * SLC_LEN + in_blk
        ss = jnp.einsum('bgrqd,bgqnld->bgrqnl', qb, kg).astype(F32) * scale
        ss = ss.reshape(bsz, N_KV_HEADS, GROUP, NSA_Q_BLOCK, n_sel * SLC_LEN)
        ms = (tok <= t[None, None, :, None, None]).reshape(bsz, N_KV_HEADS, 1, NSA_Q_BLOCK, n_sel * SLC_LEN)
        ps = masked_softmax(ss, ms)
        os_ = jnp.einsum('bgrqm,bgqmd->bgrqd', ps.astype(vg.dtype), vg.reshape(bsz, N_KV_HEADS, NSA_Q_BLOCK, n_sel * SLC_LEN, HEAD_DIM))
        kwb = lax.dynamic_slice_in_dim(kw_pad, q0, NSA_Q_BLOCK + WINDOW, axis=1)
        vwb = lax.dynamic_slice_in_dim(vw_pad, q0, NSA_Q_BLOCK + WINDOW, axis=1)
        s_pos = q0 - WINDOW + jnp.arange(NSA_Q_BLOCK + WINDOW)
        mw = (s_pos[None, :] >= 0) & (s_pos[None, :] <= t[:, None]) & (s_pos[None, :] > t[:, None] - WINDOW)
        sw = jnp.einsum('bgrqd,bkgd->bgrqk', qb, kwb).astype(F32) * scale
        pw = masked_softmax(sw, mw)
        ow = jnp.einsum('bgrqk,bkgd->bgrqd', pw.astype(vwb.dtype), vwb)
        o = gb[..., 0:1] * oc.astype(F32) + gb[..., 1:2] * os_.astype(F32) + gb[..., 2:3] * ow.astype(F32)
        return jnp.transpose(o, (0, 3, 1, 2, 4)).reshape(bsz, NSA_Q_BLOCK, N_HEADS * HEAD_DIM).astype(u.dtype)

    o = lax.map(block, jnp.arange(seq // NSA_Q_BLOCK))
    o = jnp.transpose(o, (1, 0, 2, 3)).reshape(bsz, seq, N_HEADS * HEAD_DIM)
    return o @ w_out


def setup_inputs(seed: int = 0) -> dict:
    key = jax.random.key(seed)
    ks = jax.random.split(key, 32)
    d = D_MODEL

    def nrm(k, shape, scale):
        return jax.random.normal(k, shape, F32) * scale

    offset = jax.random.randint(ks[2], (BATCH, 1), 0, 1024, dtype=jnp.int32)
    positions = (offset + jnp.arange(SEQ, dtype=jnp.int32)[None, :]).astype(jnp.int32)
    return {
        'x': nrm(ks[0], (BATCH, SEQ, d), 1.0),
        'c': nrm(ks[1], (BATCH, d), 1.0),
        'positions': positions,
        'attn_gain': 1.0 + nrm(ks[3], (DEPTH, d), 0.05),
        'ffn_gain': 1.0 + nrm(ks[4], (DEPTH, d), 0.05),
        'w_ada': nrm(ks[5], (DEPTH, d, 6 * d), 0.5 * d ** -0.5),
        'b_ada': nrm(ks[6], (DEPTH, 6 * d), 0.01),
        'a_w_in': nrm(ks[7], (N_A_LAYERS, d, A_IN), d ** -0.5),
        'a_w_out': nrm(ks[8], (N_A_LAYERS, N_HEADS * HEAD_DIM, d), (N_HEADS * HEAD_DIM) ** -0.5),
        'b_w_q': nrm(ks[9], (N_B_LAYERS, d, B_IN), d ** -0.5),
        'b_w_out': nrm(ks[10], (N_B_LAYERS, N_HEADS * HEAD_DIM, d), (N_HEADS * HEAD_DIM) ** -0.5),
        'kv_gain': 1.0 + nrm(ks[11], (d,), 0.05),
        'w_kv_ada': nrm(ks[12], (d, 2 * d), 0.5 * d ** -0.5),
        'b_kv_ada': nrm(ks[13], (2 * d,), 0.01),
        'w_kv': nrm(ks[14], (d, KV_WIDTH), d ** -0.5),
        'cmp_pe_k': nrm(ks[15], (CMP_LEN, HEAD_DIM), 0.5),
        'cmp_w1_k': nrm(ks[16], (CMP_LEN * HEAD_DIM, CMP_HIDDEN), (CMP_LEN * HEAD_DIM) ** -0.5),
        'cmp_w2_k': nrm(ks[17], (CMP_HIDDEN, HEAD_DIM), CMP_HIDDEN ** -0.5),
        'cmp_pe_v': nrm(ks[18], (CMP_LEN, HEAD_DIM), 0.5),
        'cmp_w1_v': nrm(ks[19], (CMP_LEN * HEAD_DIM, CMP_HIDDEN), (CMP_LEN * HEAD_DIM) ** -0.5),
        'cmp_w2_v': nrm(ks[20], (CMP_HIDDEN, HEAD_DIM), CMP_HIDDEN ** -0.5),
        'ffn_w_gate': nrm(ks[21], (N_DENSE_LAYERS, d, D_FF), d ** -0.5),
        'ffn_w_up': nrm(ks[22], (N_DENSE_LAYERS, d, D_FF), d ** -0.5),
        'ffn_w_down': nrm(ks[23], (N_DENSE_LAYERS, D_FF, d), D_FF ** -0.5),
        'moe_w_router': nrm(ks[24], (N_MOE_LAYERS, d, N_EXPERTS), d ** -0.5),
        'moe_w_gate': nrm(ks[25], (N_MOE_LAYERS, N_EXPERTS, d, D_FF_EXPERT), d ** -0.5),
        'moe_w_up': nrm(ks[26], (N_MOE_LAYERS, N_EXPERTS, d, D_FF_EXPERT), d ** -0.5),
        'moe_w_down': nrm(ks[27], (N_MOE_LAYERS, N_EXPERTS, D_FF_EXPERT, d), D_FF_EXPERT ** -0.5),
        'final_gain': 1.0 + nrm(ks[28], (d,), 0.05),
    }


def reference(x, c, positions, attn_gain, ffn_gain, w_ada, b_ada, a_w_in, a_w_out, b_w_q, b_w_out,
              kv_gain, w_kv_ada, b_kv_ada, w_kv, cmp_pe_k, cmp_w1_k, cmp_w2_k, cmp_pe_v, cmp_w1_v, cmp_w2_v,
              ffn_w_gate, ffn_w_up, ffn_w_down, moe_w_router, moe_w_gate, moe_w_up, moe_w_down, final_gain):
    cos, sin = rope_tables(positions, HEAD_DIM)
    c_act = jax.nn.silu(c)
    h = x
    kc = vc = k_slc = v_slc = k_win = v_win = None
    for layer in range(DEPTH):
        mod = c_act @ w_ada[layer] + b_ada[layer]
        a_shift, a_scale, a_gate, f_shift, f_scale, f_gate = jnp.split(mod, 6, axis=-1)
        if layer == N_A_LAYERS:
            kc, vc, k_slc, v_slc, k_win, v_win = nsa_shared_kv(
                h, c_act, kv_gain, w_kv_ada, b_kv_ada, w_kv,
                cmp_pe_k, cmp_w1_k, cmp_w2_k, cmp_pe_v, cmp_w1_v, cmp_w2_v, cos, sin)
        u = modulate(rmsnorm(h, attn_gain[layer]), a_shift, a_scale)
        if layer < N_A_LAYERS:
            mix = dsa_mixer(u, cos, sin, a_w_in[layer], a_w_out[layer])
        else:
            j = layer - N_A_LAYERS
            mix = nsa_mixer(u, cos, sin, kc, vc, k_slc, v_slc, k_win, v_win, b_w_q[j], b_w_out[j])
        h = h + a_gate[:, None, :] * mix
        u = modulate(rmsnorm(h, ffn_gain[layer]), f_shift, f_scale)
        if layer % 2 == 0:
            ff = swiglu(u, ffn_w_gate[layer // 2], ffn_w_up[layer // 2], ffn_w_down[layer // 2])
        else:
            m = layer // 2
            ff = moe_swiglu(u, moe_w_router[m], moe_w_gate[m], moe_w_up[m], moe_w_down[m])
        h = h + f_gate[:, None, :] * ff
    return rmsnorm(h, final_gain)
```

```python
import contextlib
import numpy as np
import concourse.bass as bass
import concourse.mybir as mybir
from concourse.bass_utils import run_bass_kernel_spmd

F32 = mybir.dt.float32
BF16 = mybir.dt.bfloat16
I32 = mybir.dt.int32
AF = mybir.ActivationFunctionType
ALU = mybir.AluOpType
AX = mybir.AxisListType

ENGS = ['pe', 'act', 'dve', 'pool', 'sp']
NEG = -1.0e30
MASKV = -30000.0
TWO_PI = 6.283185


class Buf:
    __slots__ = ('w', 'r', 'name', 'dsem')

    def __init__(self, name=''):
        self.name = name
        self.w = None
        self.r = []
        self.dsem = None


class Prog:
    def __init__(self, nc, same_engine_sync=True):
        self.nc = nc
        self.ops = {e: [] for e in ENGS}
        self.cnt = {e: 0 for e in ENGS}
        self.known = {e: {} for e in ENGS}
        self.ndsem = 0
        self.dsem_val = {}
        self.same_engine_sync = same_engine_sync

    def _deps(self, eng, reads, writes):
        toks = []
        for b in reads:
            if b.w is not None:
                toks.append(b.w)
        for b in writes:
            if b.w is not None:
                toks.append(b.w)
            toks.extend(b.r)
        need = {}
        for (k, v) in toks:
            if k == eng and (eng == 'pe' or not self.same_engine_sync):
                continue
            if self.known[eng].get(k, 0) >= v:
                continue
            if need.get(k, 0) < v:
                need[k] = v
        for k, v in need.items():
            self.known[eng][k] = v
        return list(need.items())

    def _commit(self, tok, reads, writes):
        for b in reads:
            b.r.append(tok)
        for b in writes:
            b.w = tok
            b.r = []

    def op(self, eng, fn, reads=(), writes=()):
        waits = self._deps(eng, reads, writes)
        self.cnt[eng] += 1
        tok = (eng, self.cnt[eng])
        self.ops[eng].append((waits, fn, (eng, 1)))
        self._commit(tok, reads, writes)
        return tok

    def dma(self, q, items, reads=(), writes=(), sem_buf=None, fns=None, inc=16, **kw):
        sb = sem_buf if sem_buf is not None else writes[0]
        if sb.dsem is None:
            sb.dsem = ('d', self.ndsem)
            self.dsem_val[sb.dsem] = 0
            self.ndsem += 1
        key = sb.dsem
        waits = self._deps(q, reads, writes)
        if fns is None:
            fns = []
            for (o, a) in items:
                def fn(e, o=o, a=a):
                    return e.dma_start(out=o, in_=a, **kw)
                fns.append(fn)
        for i, fn in enumerate(fns):
            self.dsem_val[key] += inc
            self.ops[q].append((waits if i == 0 else [], fn, (key, inc)))
        tok = (key, self.dsem_val[key])
        self._commit(tok, reads, writes)
        return tok

    def barrier(self):
        for e in ENGS:
            waits = []
            for k in ENGS:
                if k != e and self.cnt[k] > self.known[e].get(k, 0):
                    waits.append((k, self.cnt[k]))
                    self.known[e][k] = self.cnt[k]
            for k, v in self.dsem_val.items():
                if v > self.known[e].get(k, 0):
                    waits.append((k, v))
                    self.known[e][k] = v
            if e != 'pe' and self.cnt[e] > self.known[e].get(e, 0):
                waits.append((e, self.cnt[e]))
                self.known[e][e] = self.cnt[e]
            self.ops[e].append((waits, None, None))

    def emit(self):
        nc = self.nc
        st = self.st
        if not hasattr(self, 'sems'):
            self.sems = {}
        sems = self.sems
        for e in ENGS:
            if e not in sems:
                sems[e] = st.enter_context(nc.semaphore('s_' + e))
        for i in range(self.ndsem):
            if ('d', i) not in sems:
                sems[('d', i)] = st.enter_context(nc.semaphore('d_%d' % i))
        with nc.Block() as block:
            def replay(ename):
                def run(e):
                    for (waits, fn, inc) in self.ops[ename]:
                        for (k, v) in waits:
                            e.wait_ge(sems[k], v)
                        if fn is not None:
                            ins = fn(e)
                            ins.then_inc(sems[inc[0]], inc[1])
                return run
            block.tensor(replay('pe'))
            block.scalar(replay('act'))
            block.vector(replay('dve'))
            block.gpsimd(replay('pool'))
            block.sync(replay('sp'))
        self.ops = {e: [] for e in ENGS}


class Arena:
    def __init__(self, t, n):
        self.t = t
        self.n = n
        self.off = 0

    def alloc(self, ncols):
        o = self.off
        self.off += ncols
        assert self.off <= self.n, (self.off, self.n)
        return self.t[:, o:o + ncols]

    def mark(self):
        return self.off

    def reset(self, m=0):
        self.off = m


class Ctx:
    pass


C_IDENT = 0
C_TRI = 128
C_PAD = 256
C_INV = 384
C_SSC = 385
C_CSC = 386
C_ONES = 387
NCST = 392


def make_consts(j):
    c = np.zeros((128, NCST), np.float32)
    c[:, C_IDENT:C_IDENT + 128] = np.eye(128, dtype=np.float32)
    q = np.arange(128)[:, None]
    k = np.arange(128)[None, :]
    c[:, C_TRI:C_TRI + 128] = np.where(k <= q, 0.0, NEG)
    c[:, C_PAD:C_PAD + 128] = NEG if j == 0 else 0.0
    inv = 1.0 / (10000.0 ** (np.arange(0, 64, 2, dtype=np.float32) / np.float32(64)))
    inv = inv.astype(np.float32)
    c[0:64, C_INV] = np.concatenate([inv, inv])
    c[0:32, C_SSC] = -TWO_PI
    c[32:64, C_SSC] = TWO_PI
    c[:, C_CSC] = TWO_PI
    c[:, C_ONES] = 1.0
    return c


NBIG = 52600


def setup_ctx(nc):
    X = Ctx()
    X.nc = nc
    X.st = contextlib.ExitStack()
    X.big = X.st.enter_context(nc.sbuf_tensor("big", [128, NBIG], F32))

    def carve(n32):
        X.a32 = Arena(X.big[:, 0:n32], n32)
        X.a16 = Arena(X.big[:, n32:NBIG].bitcast(BF16), 2 * (NBIG - n32))
    X.carve = carve
    X.ai = X.st.enter_context(nc.sbuf_tensor("ai32", [128, 512], I32))
    X.ps = [X.st.enter_context(nc.psum_tensor("ps%d" % i, [128, 512], F32)) for i in range(8)]
    X.psb = [Buf('ps%d' % i) for i in range(8)]
    X.P = Prog(nc)
    X.P.st = X.st
    return X


def load_consts(X, cst_dram):
    P = X.P
    X.cst = X.a32.alloc(cst_dram.shape[1])
    X.Bcst = Buf('cst')
    P.dma('sp', [(X.cst, cst_dram[:, :])], writes=[X.Bcst])
    X.ident = X.cst[:, C_IDENT:C_IDENT + 128]
    X.irep = X.a16.alloc(512)
    X.Birep = Buf('irep')
    for r in range(4):
        P.op('dve', lambda e, r=r: e.tensor_copy(out=X.irep[:, r * 128:(r + 1) * 128], in_=X.ident),
             reads=[X.Bcst], writes=[X.Birep])


def rope_tables(X, pos_i_dram_row, n, Ct, St, Bt, tmp32, tmpi, Btmp):
    P = X.P
    cst = X.cst
    pi_ = tmpi[0:64, 0:n]
    y = tmp32[0:64, 0:n]
    f = tmp32[0:64, n:2 * n]
    g = tmp32[0:64, 2 * n:3 * n]
    P.dma('sp', [(pi_, pos_i_dram_row.partition_broadcast(64))], writes=[Btmp])
    P.op('dve', lambda e: e.tensor_copy(out=y, in_=pi_), reads=[Btmp], writes=[Btmp])
    P.op('dve', lambda e: e.tensor_scalar(out=y, in0=y, scalar1=cst[0:64, C_INV:C_INV + 1], scalar2=float(1.0 / (2 * np.pi)),
                                           op0=ALU.mult, op1=ALU.mult), reads=[Btmp, X.Bcst], writes=[Btmp])

    def frac_to(dst, src, addc):
        if addc != 0.0:
            P.op('dve', lambda e: e.tensor_scalar_add(out=dst, in0=src, scalar1=addc), reads=[Btmp], writes=[Btmp])
            s2 = dst
        else:
            s2 = src
        P.op('dve', lambda e: e.tensor_copy(out=pi_, in_=s2), reads=[Btmp], writes=[Btmp])
        P.op('dve', lambda e: e.tensor_copy(out=g, in_=pi_), reads=[Btmp], writes=[Btmp])
        P.op('dve', lambda e: e.tensor_tensor(out=dst, in0=s2, in1=g, op=ALU.subtract), reads=[Btmp], writes=[Btmp])
        P.op('dve', lambda e: e.tensor_single_scalar(out=g, in_=dst, scalar=0.5, op=ALU.is_gt), reads=[Btmp], writes=[Btmp])
        P.op('dve', lambda e: e.tensor_tensor(out=dst, in0=dst, in1=g, op=ALU.subtract), reads=[Btmp], writes=[Btmp])
        P.op('dve', lambda e: e.tensor_single_scalar(out=g, in_=dst, scalar=-0.5, op=ALU.is_lt), reads=[Btmp], writes=[Btmp])
        P.op('dve', lambda e: e.tensor_tensor(out=dst, in0=dst, in1=g, op=ALU.add), reads=[Btmp], writes=[Btmp])

    frac_to(f, y, 0.0)
    P.op('act', lambda e: e.activation(out=St, in_=f, func=AF.Sin, scale=cst[0:64, C_SSC:C_SSC + 1]),
         reads=[Btmp, X.Bcst], writes=[Bt])
    frac_to(f, y, 0.25)
    P.op('act', lambda e: e.activation(out=Ct, in_=f, func=AF.Sin, scale=cst[0:64, C_CSC:C_CSC + 1]),
         reads=[Btmp, X.Bcst], writes=[Bt])


def mod_vectors(X, cT_dram, w_ada_dram, b_adaT_dram, ncols, wbuf, Bw, psum_idx, cact, modT, bT):
    P = X.P
    nj = ncols // 128
    Bc = Buf('cact')
    P.dma('sp', [(cact, cT_dram[:, :])], writes=[Bc])
    P.op('act', lambda e: e.activation(out=cact, in_=cact, func=AF.Silu), reads=[Bc], writes=[Bc])
    Bm = Buf('modT')
    P.dma('sp', [(bT, b_adaT_dram[:, :])], writes=[Bm])
    ps = X.ps[psum_idx]
    Bps = X.psb[psum_idx]
    ngrp = ncols // 512
    for jg in range(ngrp):
        s = jg % 2
        w3 = wbuf[s].rearrange("p (k n) -> p k n", k=8)
        P.dma('sp', [(w3, w_ada_dram[:, jg * 512:(jg + 1) * 512].rearrange("(k p) n -> p k n", p=128))], writes=[Bw[s]])
        for jc in range(4):
            J = jg * 4 + jc
            for k in range(8):
                P.op('pe', lambda e, J=J, k=k, jc=jc, w3=w3: e.matmul(ps[:, J:J + 1], lhsT=w3[:, k, jc * 128:(jc + 1) * 128],
                                                                      rhs=cact[:, k:k + 1], start=(k == 0), stop=(k == 7)),
                     reads=[Bw[s], Bc], writes=[Bps])
    P.op('dve', lambda e: e.tensor_tensor(out=modT, in0=ps[:, 0:nj], in1=bT, op=ALU.add), reads=[Bps, Bm], writes=[Bm])
    return modT, Bm, cact, Bc


def bcast_row_vec(X, cact, Bc, w_dram_cols, b_dram_row, out_bc, Bout, wbuf, Bw, psA, psB):
    P = X.P
    crep = X.a32.alloc(8 * 128)
    Bcr = Buf('crep')
    crep3 = crep.rearrange("p (k n) -> p k n", k=8)
    for k in range(8):
        P.op('dve', lambda e, k=k: e.tensor_copy(out=crep3[:, k, :], in_=cact[:, k:k + 1].to_broadcast([128, 128])),
             reads=[Bc], writes=[Bcr])
    P.dma('sp', [(out_bc, b_dram_row.partition_broadcast(128))], writes=[Bout])
    for half in range(2):
        s = half % 2
        w3 = wbuf[s].rearrange("p (k n) -> p k n", k=8)
        P.dma('sp', [(w3, w_dram_cols[:, half * 512:(half + 1) * 512].rearrange("(k p) n -> p k n", p=128))], writes=[Bw[s]])
        pi = psA if half == 0 else psB
        for k in range(8):
            P.op('pe', lambda e, k=k, w3=w3, pi=pi: e.matmul(X.ps[pi][:, :], lhsT=crep3[:, k, :], rhs=w3[:, k, :],
                                                            start=(k == 0), stop=(k == 7)),
                 reads=[Bw[s], Bcr], writes=[X.psb[pi]])
        P.op('dve', lambda e, half=half, pi=pi: e.tensor_tensor(out=out_bc[:, half * 512:(half + 1) * 512], in0=X.ps[pi][:, :],
                                                                in1=out_bc[:, half * 512:(half + 1) * 512], op=ALU.add),
             reads=[X.psb[pi], Bout], writes=[Bout])


def norm_modT(X, xt, Bx, G1, SH, Bmod, uT_dst, Buo, xn, Bxn, ss, psA, psB, junk, u32_dst=None, Bu32=None):
    P = X.P
    P.op('act', lambda e: e.activation(out=junk, in_=xt, func=AF.Square, accum_out=ss[:, 0:1]), reads=[Bx], writes=[Bxn])
    P.op('act', lambda e: e.activation(out=ss[:, 1:2], in_=ss[:, 0:1], func=AF.Sqrt, scale=1.0 / 1024.0, bias=X.eps_col),
         reads=[Bxn, X.Bcst2], writes=[Bxn])
    P.op('dve', lambda e: e.reciprocal(out=ss[:, 2:3], in_=ss[:, 1:2]), reads=[Bxn], writes=[Bxn])
    P.op('dve', lambda e: e.tensor_scalar(out=xn, in0=xt, scalar1=ss[:, 2:3], scalar2=None, op0=ALU.mult),
         reads=[Bx, Bxn], writes=[Bxn])
    for k in range(8):
        pi = psA if k < 4 else psB
        P.op('pe', lambda e, k=k, pi=pi: e.transpose(out=X.ps[pi][:, (k % 4) * 128:(k % 4 + 1) * 128],
                                                     in_=xn[:, k * 128:(k + 1) * 128], identity=X.ident),
             reads=[Bxn, X.Bcst], writes=[X.psb[pi]])
    for k in range(8):
        pi = psA if k < 4 else psB
        P.op('act', lambda e, k=k, pi=pi: e.activation(out=uT_dst(k), in_=X.ps[pi][:, (k % 4) * 128:(k % 4 + 1) * 128],
                                                       func=AF.Identity, scale=G1[:, k:k + 1], bias=SH[:, k:k + 1]),
             reads=[X.psb[pi], Bmod], writes=[Buo])
        if u32_dst is not None:
            P.op('act', lambda e, k=k, pi=pi: e.activation(out=u32_dst(k), in_=X.ps[pi][:, (k % 4) * 128:(k % 4 + 1) * 128],
                                                           func=AF.Identity, scale=G1[:, k:k + 1], bias=SH[:, k:k + 1]),
                 reads=[X.psb[pi], Bmod], writes=[Bu32])


def load_w_bf16(X, dst3, w_dram_cols, Bw, nsplit=1):
    src = w_dram_cols.rearrange("(k p) n -> p k n", p=128)
    items = []
    for s in range(nsplit):
        k0 = s * 8 // nsplit
        k1 = (s + 1) * 8 // nsplit
        items.append((dst3[:, k0:k1, :], src[:, k0:k1, :]))
    X.P.dma('pool', items, writes=[Bw])


NQB = 16
NKC = 32
NBIS = 26


def phase_att0(X, T, nqb=NQB, nbis=NBIS):
    xk = T['xk']
    posk = T['posk']
    cst_d = T['cst_d']
    cT = T['cT']
    w_ada = T['w_ada']
    b_adaT = T['b_adaT']
    b_gate = T['b_gate']
    gainT = T['gainT']
    w_in = T['w_in']
    w_out = T['w_out']
    hmid = T['hmid']
    X.carve(15750)
    P = X.P
    a32, a16 = X.a32, X.a16
    if True:
        load_consts(X, cst_d)
        X.eps_col = a32.alloc(1)
        X.Bcst2 = Buf('cst2')
        P.op('pool', lambda e: e.memset(X.eps_col, 1e-6), writes=[X.Bcst2])

        gate_bc = a32.alloc(1024)
        Bgate = Buf('gate')
        G1 = a32.alloc(8)
        gT = a32.alloc(8)
        cact = a32.alloc(8)
        modT = a32.alloc(16)
        bT = a32.alloc(16)
        m32 = a32.mark()
        wbuf = [a32.alloc(8 * 512), a32.alloc(8 * 512)]
        Bw = [Buf('wa0'), Buf('wa1')]
        modT, Bmod, cact, Bc = mod_vectors(X, cT, w_ada[:, 0:2048], b_adaT, 2048, wbuf, Bw, 0, cact, modT, bT)
        bcast_row_vec(X, cact, Bc, w_ada[:, 2048:3072], b_gate, gate_bc, Bgate, wbuf, Bw, 1, 2)
        P.dma('sp', [(gT, gainT[:, :])], writes=[Bmod])
        P.op('dve', lambda e: e.scalar_tensor_tensor(out=G1, in0=modT[:, 8:16], scalar=1.0, in1=gT, op0=ALU.add, op1=ALU.mult),
             reads=[Bmod], writes=[Bmod])
        SH = modT[:, 0:8]
        P.barrier()
        a32.reset(m32)

        KT = a16.alloc(4 * NKC * 128).rearrange("p (g t) -> p g t", g=4)
        IKT = a16.alloc(NKC * 128)
        VAf = a16.alloc(NKC * 260)
        VA = VAf.rearrange("p (c n) -> p c n", c=NKC)
        BKV = Buf('kv')
        P.op('pool', lambda e: e.memset(VAf, 1.0), writes=[BKV])
        m16 = a16.mark()

        WA = a16.alloc(8 * 576).rearrange("p (k n) -> p k n", k=8)
        WAp = a16.alloc(8 * 320).rearrange("p (k n) -> p k n", k=8)
        BW = Buf('WA')
        BWp = Buf('WAp')
        w_in3 = w_in.rearrange("(k p) n -> p k n", p=128)
        P.dma('pool', [(WA[:, :, 0:512], w_in3[:, :, 1024:1536]), (WA[:, :, 512:576], w_in3[:, :, 2048:2112])], writes=[BW])

        def perm_copy(Wsrc, Wdst, pairs, Bs, Bd):
            for (s0, d0, n) in pairs:
                nh = n // 64
                for k in range(8):
                    src = Wsrc[:, k, s0:s0 + n].rearrange("p (h t i) -> p h t i", h=nh, t=2)
                    dst = Wdst[:, k, d0:d0 + n].rearrange("p (h t i) -> p h t i", h=nh, t=2)
                    eng = 'pool' if k % 2 == 0 else 'dve'
                    P.op(eng, lambda e, src=src, dst=dst: e.tensor_copy(out=dst[:, :, 0, :], in_=src[:, :, 1, :]), reads=[Bs], writes=[Bd])
                    P.op(eng, lambda e, src=src, dst=dst: e.tensor_copy(out=dst[:, :, 1, :], in_=src[:, :, 0, :]), reads=[Bs], writes=[Bd])
        perm_copy(WA, WAp, [(0, 0, 256), (512, 256, 64)], BW, BWp)
        uT = a16.alloc(8 * 512).rearrange("p (k t) -> p k t", k=8)
        BuT = Buf('uT')

        xt = [a32.alloc(1024), a32.alloc(1024)]
        Bxt = [Buf('xt0'), Buf('xt1')]
        xn = a32.alloc(1024)
        Bxn = Buf('xn')
        ss = a32.alloc(4)
        junk = xn
        Ct = a32.alloc(512)
        St = a32.alloc(512)
        Btab = Buf('tab')
        tmp32 = a32.alloc(3 * 512)
        Btmp = Buf('ttmp')
        r1 = a32.alloc(512)
        r2 = a32.alloc(512)
        Br = Buf('ropetmp')

        def rope_combine(psA_i, psB_i, n, nh, dst):
            A = X.ps[psA_i][0:64, 0:nh * n].rearrange("p (h t) -> p h t", h=nh)
            B = X.ps[psB_i][0:64, 0:nh * n].rearrange("p (h t) -> p h t", h=nh)
            c_b = Ct[0:64, 0:n].unsqueeze(1).to_broadcast([64, nh, n])
            s_b = St[0:64, 0:n].unsqueeze(1).to_broadcast([64, nh, n])
            t1 = r1[0:64, 0:nh * n].rearrange("p (h t) -> p h t", h=nh)
            t2 = r2[0:64, 0:nh * n].rearrange("p (h t) -> p h t", h=nh)
            P.op('dve', lambda e: e.tensor_tensor(out=t1, in0=A, in1=c_b, op=ALU.mult), reads=[X.psb[psA_i], Btab], writes=[Br])
            P.op('dve', lambda e: e.tensor_tensor(out=t2, in0=B, in1=s_b, op=ALU.mult), reads=[X.psb[psB_i], Btab], writes=[Br])
            return t1, t2

        for grp in range(NKC // 4):
            t0 = grp * 512
            rope_tables(X, posk[:, t0:t0 + 512], 512, Ct[0:64, :], St[0:64, :], Btab, tmp32, X.ai, Btmp)
            for cc in range(4):
                ch = grp * 4 + cc
                s = ch % 2
                P.dma('sp', [(xt[s], xk[ch * 128:(ch + 1) * 128, :])], writes=[Bxt[s]])
                norm_modT(X, xt[s], Bxt[s], G1, SH, Bmod, lambda k, cc=cc: uT[:, k, cc * 128:(cc + 1) * 128], BuT,
                          xn, Bxn, ss, 0, 1, junk)
                for k in range(8):
                    P.op('pe', lambda e, k=k, cc=cc: e.matmul(X.ps[2][:, 0:256], lhsT=uT[:, k, cc * 128:(cc + 1) * 128],
                                                              rhs=WA[:, k, 256:512], start=(k == 0), stop=(k == 7)),
                         reads=[BuT, BW], writes=[X.psb[2]])
                P.op('act', lambda e, ch=ch: e.copy(out=VA[:, ch, :].rearrange("p (g d) -> p g d", g=4)[:, :, 0:64],
                                                    in_=X.ps[2][:, 0:256].rearrange("p (g d) -> p g d", g=4)),
                     reads=[X.psb[2]], writes=[BKV])
            for (kind, c0, c0p, nh) in [('k', 0, 0, 4), ('ik', 512, 256, 1)]:
                for hh in range(nh):
                    for (W_, pi, cb_) in [(WA, 3, c0), (WAp, 4, c0p)]:
                        for k in range(8):
                            P.op('pe', lambda e, k=k, W_=W_, pi=pi, cbase=cb_ + hh * 64: e.matmul(
                                X.ps[pi][0:64, :], lhsT=W_[:, k, cbase:cbase + 64], rhs=uT[:, k, :],
                                start=(k == 0), stop=(k == 7)),
                                reads=[BuT, BW, BWp], writes=[X.psb[pi]])
                    t1, t2 = rope_combine(3, 4, 512, 1, None)
                    if kind == 'k':
                        dst = KT[0:64, hh, t0:t0 + 512]
                    else:
                        dst = IKT[0:64, t0:t0 + 512]
                    P.op('pool', lambda e, dst=dst, t1=t1, t2=t2: e.tensor_tensor(out=dst, in0=t1[:, 0, :], in1=t2[:, 0, :], op=ALU.add),
                         reads=[Br], writes=[BKV])
        P.barrier()

        a16.reset(m16)
        WB = a16.alloc(8 * 1544).rearrange("p (k n) -> p k n", k=8)
        WBp = a16.alloc(8 * 1536).rearrange("p (k n) -> p k n", k=8)
        BW = Buf('WB')
        BWp = Buf('WBp')
        P.dma('pool', [(WB[:, 0:4, 0:1024], w_in3[:, 0:4, 0:1024]), (WB[:, 4:8, 0:1024], w_in3[:, 4:8, 0:1024]),
                       (WB[:, :, 1024:1536], w_in3[:, :, 1536:2048]), (WB[:, :, 1536:1544], w_in3[:, :, 2112:2120])], writes=[BW])
        perm_copy(WB, WBp, [(0, 0, 1024), (1024, 1024, 512)], BW, BWp)
        Wo = a16.alloc(8 * 1024).rearrange("p (k n) -> p k n", k=8)
        BWo = Buf('Wout')
        load_w_bf16(X, Wo, w_out, BWo, nsplit=2)
        QT = a16.alloc(16 * 128).rearrange("p (h t) -> p h t", h=16)
        IQT = a16.alloc(8 * 128).rearrange("p (h t) -> p h t", h=8)
        BQ = Buf('QT')
        iw = a32.alloc(24)
        Biw = Buf('iw')
        Isc = a32.alloc(NKC * 128)
        BI = Buf('I')
        rl = [a32.alloc(512), a32.alloc(512)]
        Brl = [Buf('rl0'), Buf('rl1')]
        bs = a32.alloc(16)
        Bbs = Buf('bs')
        Mb = a16.alloc(NKC * 128)
        cjunk = Mb
        BMb = Buf('Mb')
        PT = [a16.alloc(512), a16.alloc(512)]
        BPT = [Buf('pt0'), Buf('pt1')]
        On = a32.alloc(1024)
        BOn = Buf('On')
        rden = a32.alloc(16)
        OnT = a16.alloc(8 * 128).rearrange("p (k t) -> p k t", k=8)
        BOnT = Buf('OnT')
        hm_ = a32.alloc(1024)
        hm = [hm_, hm_]
        Bhm_ = Buf('hm')
        Bhm = [Bhm_, Bhm_]
        Bout = Buf('hmid_out')
        uq = a16.alloc(8 * 128).rearrange("p (k t) -> p k t", k=8)
        Buq = Buf('uq')

        for m in range(nqb):
            sc = 2 * m + 1
            nk = sc + 1
            nkeys = nk * 128
            s = m % 2
            P.dma('sp', [(xt[s], xk[sc * 128:(sc + 1) * 128, :])], writes=[Bxt[s]])
            rope_tables(X, posk[:, sc * 128:(sc + 1) * 128], 128, Ct[0:64, 0:128], St[0:64, 0:128], Btab, tmp32, X.ai, Btmp)
            norm_modT(X, xt[s], Bxt[s], G1, SH, Bmod, lambda k: uq[:, k, :], Buq, xn, Bxn, ss, 0, 1, junk)
            for (c0, nb, dstT) in [(0, 4, QT), (1024, 2, IQT)]:
                for b4 in range(nb):
                    for (W_, pi) in [(WB, 3), (WBp, 4)]:
                        for hh in range(4):
                            cbase = c0 + (b4 * 4 + hh) * 64
                            for k in range(8):
                                P.op('pe', lambda e, k=k, W_=W_, pi=pi, cbase=cbase, hh=hh: e.matmul(
                                    X.ps[pi][0:64, hh * 128:(hh + 1) * 128], lhsT=W_[:, k, cbase:cbase + 64], rhs=uq[:, k, :],
                                    start=(k == 0), stop=(k == 7)),
                                    reads=[Buq, BW, BWp], writes=[X.psb[pi]])
                    t1, t2 = rope_combine(3, 4, 128, 4, None)
                    P.op('pool', lambda e, dstT=dstT, b4=b4, t1=t1, t2=t2: e.tensor_tensor(
                        out=dstT[0:64, b4 * 4:(b4 + 1) * 4, :], in0=t1, in1=t2, op=ALU.add), reads=[Br], writes=[BQ])
            for k in range(8):
                P.op('pe', lambda e, k=k: e.matmul(X.ps[2][:, 0:8], lhsT=uq[:, k, :], rhs=WB[:, k, 1536:1544],
                                                   start=(k == 0), stop=(k == 7)), reads=[Buq, BW], writes=[X.psb[2]])
            P.op('act', lambda e: e.activation(out=iw[:, 0:8], in_=X.ps[2][:, 0:8], func=AF.Abs),
                 reads=[X.psb[2]], writes=[Biw])
            P.op('dve', lambda e: e.tensor_scalar(out=iw[:, 8:16], in0=X.ps[2][:, 0:8], scalar1=0.0, scalar2=0.5,
                                                   op0=ALU.is_ge, op1=ALU.subtract), reads=[X.psb[2]], writes=[Biw])
            ngr = (nkeys + 511) // 512
            it = 0
            for kg in range(ngr):
                k0 = kg * 512
                wdt = min(512, nkeys - k0)
                for h in range(8):
                    pi = 5 + (it % 2)
                    rs = it % 2
                    it += 1
                    P.op('pe', lambda e, pi=pi, h=h, k0=k0, wdt=wdt: e.matmul(X.ps[pi][:, 0:wdt], lhsT=IQT[0:64, h, :],
                                                                              rhs=IKT[0:64, k0:k0 + wdt], start=True, stop=True),
                         reads=[BQ], writes=[X.psb[pi]])
                    P.op('act', lambda e, pi=pi, h=h, rs=rs, wdt=wdt: e.activation(out=rl[rs][:, 0:wdt], in_=X.ps[pi][:, 0:wdt],
                                                                                   func=AF.Relu, scale=iw[:, h:h + 1]),
                         reads=[X.psb[pi], Biw], writes=[Brl[rs]])
                    if h == 0:
                        P.op('dve', lambda e, rs=rs, k0=k0, wdt=wdt: e.tensor_scalar(
                            out=Isc[:, k0:k0 + wdt], in0=rl[rs][:, 0:wdt], scalar1=iw[:, 8:9], scalar2=None, op0=ALU.mult),
                            reads=[Brl[rs], Biw], writes=[BI])
                    else:
                        P.op('dve', lambda e, rs=rs, k0=k0, wdt=wdt, h=h: e.scalar_tensor_tensor(
                            out=Isc[:, k0:k0 + wdt], in0=rl[rs][:, 0:wdt], scalar=iw[:, 8 + h:9 + h], in1=Isc[:, k0:k0 + wdt],
                            op0=ALU.mult, op1=ALU.add), reads=[Brl[rs], Biw, BI], writes=[BI])
            Iv = Isc[:, 0:nkeys]
            if m >= 1:
                P.op('dve', lambda e, Iv=Iv: e.tensor_reduce(out=bs[:, 0:1], in_=Iv, axis=AX.X, op=ALU.max), reads=[BI], writes=[Bbs])
                P.op('dve', lambda e, Iv=Iv: e.tensor_reduce(out=bs[:, 1:2], in_=Iv, axis=AX.X, op=ALU.min), reads=[BI], writes=[Bbs])
            P.op('dve', lambda e, sc=sc: e.tensor_tensor(out=Isc[:, sc * 128:(sc + 1) * 128], in0=Isc[:, sc * 128:(sc + 1) * 128],
                                                         in1=X.cst[:, C_TRI:C_TRI + 128], op=ALU.add), reads=[BI, X.Bcst], writes=[BI])
            P.op('dve', lambda e: e.tensor_tensor(out=Isc[:, 0:128], in0=Isc[:, 0:128], in1=X.cst[:, C_PAD:C_PAD + 128], op=ALU.add),
                 reads=[BI, X.Bcst], writes=[BI])
            if m >= 1:
                P.op('dve', lambda e: e.tensor_tensor(out=bs[:, 2:3], in0=bs[:, 0:1], in1=bs[:, 1:2], op=ALU.subtract), reads=[Bbs], writes=[Bbs])
                P.op('dve', lambda e: e.tensor_copy(out=bs[:, 3:4], in_=bs[:, 1:2]), reads=[Bbs], writes=[Bbs])
                for it_b in range(1, nbis + 1):
                    ck = float(2.0 ** (-it_b))
                    P.op('dve', lambda e, ck=ck: e.scalar_tensor_tensor(out=bs[:, 4:5], in0=bs[:, 2:3], scalar=ck, in1=bs[:, 3:4],
                                                                        op0=ALU.mult, op1=ALU.add), reads=[Bbs], writes=[Bbs])
                    P.op('dve', lambda e, Iv=Iv, nkeys=nkeys: e.tensor_scalar(out=cjunk[:, 0:nkeys], in0=Iv, scalar1=bs[:, 4:5], scalar2=None,
                                                                              op0=ALU.is_ge, op1=ALU.add, accum_out=bs[:, 5:6]),
                         reads=[Bbs, BI], writes=[Bbs])
                    P.op('dve', lambda e: e.tensor_scalar(out=bs[:, 6:7], in0=bs[:, 5:6], scalar1=255.5, scalar2=bs[:, 2:3],
                                                           op0=ALU.is_ge, op1=ALU.mult), reads=[Bbs], writes=[Bbs])
                    P.op('dve', lambda e, ck=ck: e.scalar_tensor_tensor(out=bs[:, 3:4], in0=bs[:, 6:7], scalar=ck, in1=bs[:, 3:4],
                                                                        op0=ALU.mult, op1=ALU.add), reads=[Bbs], writes=[Bbs])
            else:
                P.op('dve', lambda e: e.memset(bs[:, 3:4], -1.0e29), writes=[Bbs])
            P.op('dve', lambda e, Iv=Iv, nkeys=nkeys: e.tensor_scalar(out=Mb[:, 0:nkeys], in0=Iv, scalar1=bs[:, 3:4], scalar2=MASKV,
                                                                      op0=ALU.is_lt, op1=ALU.mult), reads=[BI, Bbs], writes=[BMb])
            it = 0
            for g in range(4):
                po = 2 + (g % 2) * 0
                for c in range(nk):
                    pi = 5 + (it % 2)
                    ps_ = it % 2
                    it += 1
                    P.op('pe', lambda e, pi=pi, g=g, c=c: e.matmul(X.ps[pi][:, :], lhsT=KT[0:64, g, c * 128:(c + 1) * 128],
                                                                   rhs=QT[0:64, g * 4:(g + 1) * 4, :], start=True, stop=False),
                         reads=[BQ], writes=[X.psb[pi]])
                    P.op('pe', lambda e, pi=pi, c=c: e.matmul(X.ps[pi][:, :], lhsT=Mb[:, c * 128:(c + 1) * 128], rhs=X.irep,
                                                              start=False, stop=True),
                         reads=[BMb, X.Birep], writes=[X.psb[pi]])
                    P.op('act', lambda e, pi=pi, ps_=ps_: e.activation(out=PT[ps_], in_=X.ps[pi][:, :], func=AF.Exp, scale=0.125),
                         reads=[X.psb[pi]], writes=[BPT[ps_]])
                    for r in range(4):
                        P.op('pe', lambda e, r=r, ps_=ps_, c=c, g=g: e.matmul(X.ps[2][:, r * 65:(r + 1) * 65],
                                                                              lhsT=PT[ps_][:, r * 128:(r + 1) * 128],
                                                                              rhs=VA[:, c, g * 65:(g + 1) * 65],
                                                                              start=(c == 0 and r == 0), stop=(c == nk - 1),
                                                                              skip_group_check=True),
                             reads=[BPT[ps_]], writes=[X.psb[2]])
                O3 = X.ps[2][:, 0:260].rearrange("p (r d) -> p r d", r=4)
                P.op('dve', lambda e, O3=O3, g=g: e.reciprocal(out=rden[:, g * 4:(g + 1) * 4], in_=O3[:, :, 64]), reads=[X.psb[2]], writes=[BOn])
                P.op('dve', lambda e, O3=O3, g=g: e.tensor_tensor(
                    out=On[:, g * 256:(g + 1) * 256].rearrange("p (r d) -> p r d", r=4), in0=O3[:, :, 0:64],
                    in1=rden[:, g * 4:(g + 1) * 4].unsqueeze(2).to_broadcast([128, 4, 64]), op=ALU.mult),
                    reads=[X.psb[2], BOn], writes=[BOn])
            for k in range(8):
                pi = 0 if k < 4 else 1
                P.op('pe', lambda e, k=k, pi=pi: e.transpose(out=X.ps[pi][:, (k % 4) * 128:(k % 4 + 1) * 128],
                                                             in_=On[:, k * 128:(k + 1) * 128], identity=X.ident),
                     reads=[BOn, X.Bcst], writes=[X.psb[pi]])
            for half in range(2):
                P.op('act', lambda e, half=half: e.copy(out=OnT[:, half * 4:(half + 1) * 4, :],
                                                        in_=X.ps[half][:, :].rearrange("p (k t) -> p k t", k=4)),
                     reads=[X.psb[half]], writes=[BOnT])
            for half in range(2):
                pi = 3 + half
                for k in range(8):
                    P.op('pe', lambda e, k=k, pi=pi, half=half: e.matmul(X.ps[pi][:, :], lhsT=OnT[:, k, :],
                                                                         rhs=Wo[:, k, half * 512:(half + 1) * 512],
                                                                         start=(k == 0), stop=(k == 7)),
                         reads=[BOnT, BWo], writes=[X.psb[pi]])
                P.op('dve', lambda e, pi=pi, half=half, s=s: e.tensor_tensor(out=hm[s][:, half * 512:(half + 1) * 512], in0=X.ps[pi][:, :],
                                                                             in1=gate_bc[:, half * 512:(half + 1) * 512], op=ALU.mult),
                     reads=[X.psb[pi], Bgate], writes=[Bhm[s]])
            P.op('pool', lambda e, s=s: e.tensor_tensor(out=hm[s], in0=hm[s], in1=xt[s], op=ALU.add), reads=[Bhm[s], Bxt[s]], writes=[Bhm[s]])
            P.dma('sp', [(hmid[m * 128:(m + 1) * 128, :], hm[s])], reads=[Bhm[s]], writes=[Buf('o')], sem_buf=Bhm[s])
        P.barrier()


def build_att0(nqb=NQB, nbis=NBIS):
    nc = bass.Bass("TRN2", target_bir_lowering=False)

    def din(name, shape, dt=F32):
        return nc.dram_tensor(name, shape, dt, kind="ExternalInput").ap()
    xk = din("xk", [NKC * 128, 1024])
    posk = din("posk", [1, NKC * 128], I32)
    cst_d = din("cst", [128, NCST])
    cT = din("cT", [128, 8])
    w_ada = din("w_ada", [1024, 3072])
    b_adaT = din("b_adaT", [128, 16])
    b_gate = din("b_gate", [1, 1024])
    gainT = din("gainT", [128, 8])
    w_in = din("w_in", [1024, 2120])
    w_out = din("w_out", [1024, 1024])
    hmid = nc.dram_tensor("hmid", [nqb * 128, 1024], F32, kind="ExternalOutput").ap()

    T = dict(xk=xk, posk=posk, cst_d=cst_d, cT=cT, w_ada=w_ada, b_adaT=b_adaT, b_gate=b_gate, gainT=gainT, w_in=w_in, w_out=w_out, hmid=hmid)
    X = setup_ctx(nc)
    with X.st:
        phase_att0(X, T, nqb, nbis)
        X.P.emit()
    return nc


def att0_inputs(inputs, layer=0):
    x = np.asarray(inputs['x'], np.float32)
    pos = np.asarray(inputs['positions'], np.int32)
    maps = []
    for c in range(8):
        b, j = c // 2, c % 2
        if j == 0:
            xk = np.concatenate([np.zeros((128, 1024), np.float32), x[b, 0:31 * 128]], axis=0)
            pk = np.concatenate([np.zeros((128,), np.int32), pos[b, 0:31 * 128]])
        else:
            xk = x[b]
            pk = pos[b]
        maps.append({
            "xk": np.ascontiguousarray(xk), "posk": np.ascontiguousarray(pk[None, :]),
            "cst": make_consts(j),
            "cT": np.ascontiguousarray(inputs['c'][b].reshape(8, 128).T),
            "w_ada": np.ascontiguousarray(inputs['w_ada'][layer][:, 0:3072]),
            "b_adaT": np.ascontiguousarray(inputs['b_ada'][layer][0:2048].reshape(16, 128).T),
            "b_gate": np.ascontiguousarray(inputs['b_ada'][layer][2048:3072][None, :]),
            "gainT": np.ascontiguousarray(inputs['attn_gain'][layer].reshape(8, 128).T),
            "w_in": np.ascontiguousarray(inputs['a_w_in'][0]),
            "w_out": np.ascontiguousarray(inputs['a_w_out'][0]),
        })
    return maps


def gather_blocks(res, key, nqb=NQB):
    out = np.zeros((4, 4096, 1024), np.float32)
    for c in range(8):
        b, j = c // 2, c % 2
        r = res[c][key]
        for m in range(nqb):
            i = 2 * m + j
            out[b, i * 128:(i + 1) * 128] = r[m * 128:(m + 1) * 128]
    return out


def ffn_phase0(X, cT, w_ada_f, b_adaT, b_gate, gainT):
    P = X.P
    a32 = X.a32
    gate_bc = a32.alloc(1024)
    Bgate = Buf('gate')
    G1 = a32.alloc(8)
    gT = a32.alloc(8)
    cact = a32.alloc(8)
    modT = a32.alloc(16)
    bT = a32.alloc(16)
    m32 = a32.mark()
    wbuf = [a32.alloc(8 * 512), a32.alloc(8 * 512)]
    Bw = [Buf('wa0'), Buf('wa1')]
    modT, Bmod, cact, Bc = mod_vectors(X, cT, w_ada_f[:, 0:2048], b_adaT, 2048, wbuf, Bw, 0, cact, modT, bT)
    bcast_row_vec(X, cact, Bc, w_ada_f[:, 2048:3072], b_gate, gate_bc, Bgate, wbuf, Bw, 1, 2)
    P.dma('sp', [(gT, gainT[:, :])], writes=[Bmod])
    P.op('dve', lambda e: e.scalar_tensor_tensor(out=G1, in0=modT[:, 8:16], scalar=1.0, in1=gT, op0=ALU.add, op1=ALU.mult),
         reads=[Bmod], writes=[Bmod])
    SH = modT[:, 0:8]
    P.barrier()
    a32.reset(m32)
    return G1, SH, Bmod, gate_bc, Bgate


def phase_ffn0(X, T, ntok=2048, dff=2816):
    hin = T['hin']
    cst_d = T['cst_d']
    cT = T['cT']
    w_ada = T['w_ada']
    b_adaT = T['b_adaT']
    b_gate = T['b_gate']
    gainT = T['gainT']
    wg = T['wg']
    wu = T['wu']
    wd = T['wd']
    hout = T['hout']
    NF = dff // 128
    GT = 1024
    ngrp = ntok // GT
    NT = GT // 128
    X.carve(15000)
    P = X.P
    a32, a16 = X.a32, X.a16
    if True:
        load_consts(X, cst_d)
        X.eps_col = a32.alloc(1)
        X.Bcst2 = Buf('cst2')
        P.op('pool', lambda e: e.memset(X.eps_col, 1e-6), writes=[X.Bcst2])
        G1, SH, Bmod, gate_bc, Bgate = ffn_phase0(X, cT, w_ada, b_adaT, b_gate, gainT)

        Wd = a16.alloc(NF * 1024).rearrange("p (f n) -> p f n", f=NF)
        BWd = Buf('Wd')
        wd3 = wd.rearrange("(f p) n -> p f n", p=128)
        nsp = 4
        P.dma('pool', [(Wd[:, (i * NF) // nsp:((i + 1) * NF) // nsp, :], wd3[:, (i * NF) // nsp:((i + 1) * NF) // nsp, :]) for i in range(nsp)],
              writes=[BWd])
        wg3 = wg.rearrange("(k p) n -> p k n", p=128)
        wu3 = wu.rearrange("(k p) n -> p k n", p=128)
        NWB = 3
        Wgb = [a16.alloc(8 * 128).rearrange("p (k n) -> p k n", k=8) for _ in range(NWB)]
        Wub = [a16.alloc(8 * 128).rearrange("p (k n) -> p k n", k=8) for _ in range(NWB)]
        BWg = [Buf('wg%d' % i) for i in range(NWB)]
        u2T = a16.alloc(8 * GT).rearrange("p (k t) -> p k t", k=8)
        Bu2 = Buf('u2T')
        actT = a16.alloc(NF * GT).rearrange("p (f t) -> p f t", f=NF)
        Bact = Buf('actT')
        hres = a32.alloc(NT * 1024).rearrange("p (t n) -> p t n", t=NT)
        Bh = [Buf('h%d' % i) for i in range(NT)]
        xn = a32.alloc(1024)
        Bxn = Buf('xn')
        ss = a32.alloc(4)
        sg = [a32.alloc(512), a32.alloc(512)]
        Bsg = [Buf('sg0'), Buf('sg1')]
        ot = [a32.alloc(1024), a32.alloc(1024)]
        Bot = [Buf('ot0'), Buf('ot1')]

        wi = 0
        for grp in range(ngrp):
            g0 = grp * GT
            for t in range(NT):
                P.dma('sp', [(hres[:, t, :], hin[g0 + t * 128:g0 + (t + 1) * 128, :])], writes=[Bh[t]])
                norm_modT(X, hres[:, t, :], Bh[t], G1, SH, Bmod, lambda k, t=t: u2T[:, k, t * 128:(t + 1) * 128], Bu2,
                          xn, Bxn, ss, 0, 1, xn)
            it = 0
            for f in range(NF):
                wb = wi % NWB
                wi += 1
                P.dma('pool', [(Wgb[wb], wg3[:, :, f * 128:(f + 1) * 128]), (Wub[wb], wu3[:, :, f * 128:(f + 1) * 128])], writes=[BWg[wb]])
                for half in range(GT // 512):
                    pg = 2 + 2 * (it % 2)
                    pu = pg + 1
                    sgi = it % 2
                    it += 1
                    for (W_, pi) in [(Wgb[wb], pg), (Wub[wb], pu)]:
                        for k in range(8):
                            P.op('pe', lambda e, k=k, W_=W_, pi=pi, half=half: e.matmul(X.ps[pi][:, :], lhsT=W_[:, k, :],
                                                                                       rhs=u2T[:, k, half * 512:(half + 1) * 512],
                                                                                       start=(k == 0), stop=(k == 7)),
                                 reads=[BWg[wb], Bu2], writes=[X.psb[pi]])
                    P.op('act', lambda e, pg=pg, sgi=sgi: e.activation(out=sg[sgi], in_=X.ps[pg][:, :], func=AF.Silu),
                         reads=[X.psb[pg]], writes=[Bsg[sgi]])
                    P.op('dve', lambda e, pu=pu, sgi=sgi, f=f, half=half: e.tensor_tensor(out=actT[:, f, half * 512:(half + 1) * 512], in0=X.ps[pu][:, :],
                                                                                        in1=sg[sgi], op=ALU.mult),
                         reads=[X.psb[pu], Bsg[sgi]], writes=[Bact])
            it = 0
            for t in range(NT):
                o = t % 2
                for half in range(2):
                    pi = 6 + (it % 2)
                    it += 1
                    for f in range(NF):
                        P.op('pe', lambda e, f=f, pi=pi, t=t, half=half: e.matmul(X.ps[pi][:, :], lhsT=actT[:, f, t * 128:(t + 1) * 128],
                                                                                 rhs=Wd[:, f, half * 512:(half + 1) * 512],
                                                                                 start=(f == 0), stop=(f == NF - 1)),
                             reads=[Bact, BWd], writes=[X.psb[pi]])
                    P.op('dve', lambda e, pi=pi, o=o, half=half: e.tensor_tensor(out=ot[o][:, half * 512:(half + 1) * 512], in0=X.ps[pi][:, :],
                                                                                in1=gate_bc[:, half * 512:(half + 1) * 512], op=ALU.mult),
                         reads=[X.psb[pi], Bgate], writes=[Bot[o]])
                P.op('pool', lambda e, o=o, t=t: e.tensor_tensor(out=ot[o], in0=ot[o], in1=hres[:, t, :], op=ALU.add),
                     reads=[Bot[o], Bh[t]], writes=[Bot[o]])
                P.dma('sp', [(hout[g0 + t * 128:g0 + (t + 1) * 128, :], ot[o])], reads=[Bot[o]], writes=[Buf('o')], sem_buf=Bot[o])
        P.barrier()


def build_ffn0(ntok=2048, dff=2816):
    nc = bass.Bass("TRN2", target_bir_lowering=False)

    def din(name, shape, dt=F32):
        return nc.dram_tensor(name, shape, dt, kind="ExternalInput").ap()
    hin = din("hin", [ntok, 1024])
    cst_d = din("cst", [128, NCST])
    cT = din("cT", [128, 8])
    w_ada = din("w_ada", [1024, 3072])
    b_adaT = din("b_adaT", [128, 16])
    b_gate = din("b_gate", [1, 1024])
    gainT = din("gainT", [128, 8])
    wg = din("wg", [1024, dff])
    wu = din("wu", [1024, dff])
    wd = din("wd", [dff, 1024])
    hout = nc.dram_tensor("hout", [ntok, 1024], F32, kind="ExternalOutput").ap()
    NF = dff // 128
    GT = 1024
    ngrp = ntok // GT
    NT = GT // 128

    T = dict(hin=hin, cst_d=cst_d, cT=cT, w_ada=w_ada, b_adaT=b_adaT, b_gate=b_gate, gainT=gainT, wg=wg, wu=wu, wd=wd, hout=hout)
    X = setup_ctx(nc)
    with X.st:
        phase_ffn0(X, T, ntok, dff)
        X.P.emit()
    return nc


def ffn0_inputs(inputs, hmid_cores, layer=0):
    maps = []
    for c in range(8):
        b, j = c // 2, c % 2
        maps.append({
            "hin": np.ascontiguousarray(hmid_cores[c]),
            "cst": make_consts(j),
            "cT": np.ascontiguousarray(inputs['c'][b].reshape(8, 128).T),
            "w_ada": np.ascontiguousarray(inputs['w_ada'][layer][:, 3072:6144]),
            "b_adaT": np.ascontiguousarray(inputs['b_ada'][layer][3072:5120].reshape(16, 128).T),
            "b_gate": np.ascontiguousarray(inputs['b_ada'][layer][5120:6144][None, :]),
            "gainT": np.ascontiguousarray(inputs['ffn_gain'][layer].reshape(8, 128).T),
            "wg": np.ascontiguousarray(inputs['ffn_w_gate'][0]),
            "wu": np.ascontiguousarray(inputs['ffn_w_up'][0]),
            "wd": np.ascontiguousarray(inputs['ffn_w_down'][0]),
        })
    return maps


def split_blocks(full, nqb=NQB):
    outs = []
    for c in range(8):
        b, j = c // 2, c % 2
        outs.append(np.concatenate([full[b, (2 * m + j) * 128:(2 * m + j + 1) * 128] for m in range(nqb)], axis=0))
    return outs


NCMP = 255


def phase_kv1(X, T):
    hk = T['hk']
    posk = T['posk']
    cst_d = T['cst_d']
    cT = T['cT']
    w_ada = T['w_ada']
    b_adaT = T['b_adaT']
    gainT = T['gainT']
    w_kv = T['w_kv']
    w1k = T['w1k']
    w1v = T['w1v']
    w2k = T['w2k']
    w2v = T['w2v']
    peTk = T['peTk']
    peTv = T['peTv']
    o_kslcT = T['o_kslcT']
    o_kwinT = T['o_kwinT']
    o_vslc = T['o_vslc']
    o_vwin = T['o_vwin']
    o_kcT = T['o_kcT']
    o_vc = T['o_vc']
    X.carve(14000)
    P = X.P
    a32, a16 = X.a32, X.a16
    if True:
        load_consts(X, cst_d)
        X.eps_col = a32.alloc(1)
        X.Bcst2 = Buf('cst2')
        P.op('pool', lambda e: e.memset(X.eps_col, 1e-6), writes=[X.Bcst2])
        G1 = a32.alloc(8)
        gT = a32.alloc(8)
        cact = a32.alloc(8)
        modT = a32.alloc(16)
        bT = a32.alloc(16)
        m32 = a32.mark()
        wbuf = [a32.alloc(8 * 512), a32.alloc(8 * 512)]
        Bw = [Buf('wa0'), Buf('wa1')]
        modT, Bmod, cact, Bc = mod_vectors(X, cT, w_ada, b_adaT, 2048, wbuf, Bw, 0, cact, modT, bT)
        P.dma('sp', [(gT, gainT[:, :])], writes=[Bmod])
        P.op('dve', lambda e: e.scalar_tensor_tensor(out=G1, in0=modT[:, 8:16], scalar=1.0, in1=gT, op0=ALU.add, op1=ALU.mult),
             reads=[Bmod], writes=[Bmod])
        SH = modT[:, 0:8]
        P.barrier()
        a32.reset(m32)

        W = a16.alloc(8 * 1536).rearrange("p (k n) -> p k n", k=8)
        Wp = a16.alloc(8 * 768).rearrange("p (k n) -> p k n", k=8)
        BW = Buf('W')
        BWp = Buf('Wp')
        load_w_bf16(X, W, w_kv, BW, nsplit=2)
        for (s0, d0) in [(0, 0), (512, 256), (1024, 512)]:
            for k in range(8):
                src = W[:, k, s0:s0 + 256].rearrange("p (h t i) -> p h t i", h=4, t=2)
                dst = Wp[:, k, d0:d0 + 256].rearrange("p (h t i) -> p h t i", h=4, t=2)
                eng = 'pool' if k % 2 == 0 else 'dve'
                P.op(eng, lambda e, src=src, dst=dst: e.tensor_copy(out=dst[:, :, 0, :], in_=src[:, :, 1, :]), reads=[BW], writes=[BWp])
                P.op(eng, lambda e, src=src, dst=dst: e.tensor_copy(out=dst[:, :, 1, :], in_=src[:, :, 0, :]), reads=[BW], writes=[BWp])
        KcT = a16.alloc(4 * NKC * 128).rearrange("p (g t) -> p g t", g=4)
        VcT = a16.alloc(4 * NKC * 128).rearrange("p (g t) -> p g t", g=4)
        Bcmp = Buf('cmpstore')
        xt = [a32.alloc(1024), a32.alloc(1024)]
        Bxt = [Buf('xt0'), Buf('xt1')]
        xn = a32.alloc(1024)
        Bxn = Buf('xn')
        ss = a32.alloc(4)
        uT = a16.alloc(8 * 512).rearrange("p (k t) -> p k t", k=8)
        BuT = Buf('uT')
        Ct = a32.alloc(512)
        St = a32.alloc(512)
        Btab = Buf('tab')
        tmp32 = a32.alloc(3 * 512)
        Btmp = Buf('ttmp')
        r1 = a32.alloc(512)
        r2 = a32.alloc(512)
        Br = Buf('ropetmp')
        kst = [a16.alloc(512), a16.alloc(512)]
        Bkst = [Buf('kst0'), Buf('kst1')]
        vst = [a16.alloc(512), a16.alloc(512)]
        Bvst = [Buf('vst0'), Buf('vst1')]
        ridx_t = None
        if T.get('ridx') is not None:
            ridx_t = a32.alloc(NKC).bitcast(I32)
            Bridx = Buf('ridx')
            P.dma('sp', [(ridx_t, T['ridx'][:, :])], writes=[Bridx])
        ki = 0
        for grp in range(NKC // 4):
            t0 = grp * 512
            rope_tables(X, posk[:, t0:t0 + 512], 512, Ct[0:64, :], St[0:64, :], Btab, tmp32, X.ai, Btmp)
            for cc in range(4):
                ch = grp * 4 + cc
                s = ch % 2
                if ridx_t is None:
                    P.dma('sp', [(xt[s], hk[ch * 128:(ch + 1) * 128, :])], writes=[Bxt[s]])
                else:
                    P.dma('pool', None, reads=[Bridx], writes=[Bxt[s]],
                          fns=[lambda e, s=s, ch=ch: e.indirect_dma_start(out=xt[s], out_offset=None, in_=hk[:, :],
                                                                          in_offset=bass.IndirectOffsetOnAxis(ap=ridx_t[:, ch:ch + 1], axis=0))])
                norm_modT(X, xt[s], Bxt[s], G1, SH, Bmod, lambda k, cc=cc: uT[:, k, cc * 128:(cc + 1) * 128], BuT,
                          xn, Bxn, ss, 0, 1, xn)
                for (vi, c0) in [(0, 768), (1, 1280)]:
                    for k in range(8):
                        P.op('pe', lambda e, k=k, cc=cc, vi=vi, c0=c0: e.matmul(X.ps[2][:, vi * 256:(vi + 1) * 256],
                                                                               lhsT=uT[:, k, cc * 128:(cc + 1) * 128],
                                                                               rhs=W[:, k, c0:c0 + 256], start=(k == 0 and vi == 0), stop=(k == 7),
                                                                               skip_group_check=True),
                             reads=[BuT, BW], writes=[X.psb[2]])
                P.op('act', lambda e, s=s: e.copy(out=vst[s], in_=X.ps[2][:, :]), reads=[X.psb[2]], writes=[Bvst[s]])
                P.dma('sp', [(o_vslc[ch * 128:(ch + 1) * 128, :], vst[s][:, 0:256]), (o_vwin[ch * 128:(ch + 1) * 128, :], vst[s][:, 256:512])],
                      reads=[Bvst[s]], writes=[Buf('o')], sem_buf=Bvst[s])
            for (kind, c0, c0p) in [('kcmp', 0, 0), ('kslc', 512, 256), ('kwin', 1024, 512), ('vcmp', 256, None)]:
                for hh in range(4):
                    srcs = [(W, 3, c0)] if c0p is None else [(W, 3, c0), (Wp, 4, c0p)]
                    for (W_, pi, cb_) in srcs:
                        for k in range(8):
                            P.op('pe', lambda e, k=k, W_=W_, pi=pi, cbase=cb_ + hh * 64: e.matmul(
                                X.ps[pi][0:64, :], lhsT=W_[:, k, cbase:cbase + 64], rhs=uT[:, k, :],
                                start=(k == 0), stop=(k == 7)),
                                reads=[BuT, BW, BWp], writes=[X.psb[pi]])
                    if kind == 'vcmp':
                        P.op('act', lambda e, hh=hh, t0=t0: e.copy(out=VcT[0:64, hh, t0:t0 + 512], in_=X.ps[3][0:64, :]),
                             reads=[X.psb[3]], writes=[Bcmp])
                        continue
                    t1 = r1[0:64, :]
                    t2 = r2[0:64, :]
                    P.op('dve', lambda e, t1=t1: e.tensor_tensor(out=t1, in0=X.ps[3][0:64, :], in1=Ct[0:64, :], op=ALU.mult),
                         reads=[X.psb[3], Btab], writes=[Br])
                    P.op('dve', lambda e, t2=t2: e.tensor_tensor(out=t2, in0=X.ps[4][0:64, :], in1=St[0:64, :], op=ALU.mult),
                         reads=[X.psb[4], Btab], writes=[Br])
                    if kind == 'kcmp':
                        P.op('pool', lambda e, hh=hh, t0=t0, t1=t1, t2=t2: e.tensor_tensor(out=KcT[0:64, hh, t0:t0 + 512], in0=t1, in1=t2, op=ALU.add),
                             reads=[Br], writes=[Bcmp])
                    else:
                        ks = ki % 2
                        ki += 1
                        P.op('pool', lambda e, ks=ks, t1=t1, t2=t2: e.tensor_tensor(out=kst[ks][0:64, :], in0=t1, in1=t2, op=ALU.add),
                             reads=[Br], writes=[Bkst[ks]])
                        od = o_kslcT if kind == 'kslc' else o_kwinT
                        P.dma('sp', [(od[:, hh * NKC * 128 + t0:hh * NKC * 128 + t0 + 512], kst[ks][0:64, :])],
                              reads=[Bkst[ks]], writes=[Buf('o')], sem_buf=Bkst[ks])
        P.barrier()
        w1s = a16.alloc(32 * 256).rearrange("p (l n) -> p l n", l=32)
        w2s = a16.alloc(2 * 64).rearrange("p (c n) -> p c n", c=2)
        peT = a16.alloc(32)
        Bwc = Buf('wc')
        bvec = a32.alloc(2)
        Bbv = Buf('bvec')
        xh = a32.alloc(256)
        x2 = a32.alloc(256)
        Bxh = Buf('xh')
        actT = a16.alloc(2 * 256).rearrange("p (c n) -> p c n", c=2)
        Bat = Buf('actT')
        ost = a16.alloc(1024)
        Bost = Buf('ost')
        ostv = a16.alloc(2 * 256).rearrange("p (c n) -> p c n", c=2)
        Bostv = Buf('ostv')
        P.op('pool', lambda e: e.memset(ost, 0.0), writes=[Bost])
        P.op('pool', lambda e: e.memset(ostv, 0.0), writes=[Bostv])
        for (kv, w1d, w2d, ped, SRC) in [('k', w1k, w2k, peTk, KcT), ('v', w1v, w2v, peTv, VcT)]:
            P.dma('pool', [(w1s[0:64, :, :], w1d.rearrange("(l d) n -> d l n", d=64)), (w2s, w2d.rearrange("(c p) n -> p c n", p=128)),
                           (peT[0:64, :], ped[:, :])], writes=[Bwc])
            for hc in range(2):
                for l in range(32):
                    P.op('pe', lambda e, hc=hc, l=l: e.matmul(X.ps[0][:, hc:hc + 1], lhsT=w1s[0:64, l, hc * 128:(hc + 1) * 128],
                                                              rhs=peT[0:64, l:l + 1], start=(l == 0), stop=(l == 31)),
                         reads=[Bwc], writes=[X.psb[0]])
            P.op('dve', lambda e: e.tensor_copy(out=bvec, in_=X.ps[0][:, 0:2]), reads=[X.psb[0]], writes=[Bbv])
            for g in range(4):
                for hc in range(2):
                    pi = 1 + hc
                    for l in range(32):
                        rhs = SRC[0:64, g, l:l + 16 * (NCMP - 1) + 1:16]
                        P.op('pe', lambda e, hc=hc, l=l, pi=pi, rhs=rhs: e.matmul(X.ps[pi][:, 0:NCMP], lhsT=w1s[0:64, l, hc * 128:(hc + 1) * 128],
                                                                                  rhs=rhs, start=(l == 0), stop=(l == 31)),
                             reads=[Bwc, Bcmp], writes=[X.psb[pi]])
                    xv = xh[:, 0:NCMP]
                    x2v = x2[:, 0:NCMP]
                    P.op('act', lambda e, pi=pi, hc=hc, xv=xv: e.activation(out=xv, in_=X.ps[pi][:, 0:NCMP], func=AF.Identity, bias=bvec[:, hc:hc + 1]),
                         reads=[X.psb[pi], Bbv], writes=[Bxh])
                    P.op('dve', lambda e, xv=xv, x2v=x2v: e.tensor_tensor(out=x2v, in0=xv, in1=xv, op=ALU.mult), reads=[Bxh], writes=[Bxh])
                    P.op('dve', lambda e, x2v=x2v: e.tensor_scalar(out=x2v, in0=x2v, scalar1=0.044715, scalar2=1.0, op0=ALU.mult, op1=ALU.add),
                         reads=[Bxh], writes=[Bxh])
                    P.op('dve', lambda e, xv=xv, x2v=x2v: e.tensor_tensor(out=x2v, in0=x2v, in1=xv, op=ALU.mult), reads=[Bxh], writes=[Bxh])
                    P.op('act', lambda e, x2v=x2v: e.activation(out=x2v, in_=x2v, func=AF.Sigmoid, scale=1.5957691216), reads=[Bxh], writes=[Bxh])
                    P.op('dve', lambda e, hc=hc, xv=xv, x2v=x2v: e.tensor_tensor(out=actT[:, hc, 0:NCMP], in0=x2v, in1=xv, op=ALU.mult),
                         reads=[Bxh], writes=[Bat])
                if kv == 'k':
                    for hc in range(2):
                        P.op('pe', lambda e, hc=hc: e.matmul(X.ps[3][0:64, 0:NCMP], lhsT=w2s[:, hc, :], rhs=actT[:, hc, 0:NCMP],
                                                             start=(hc == 0), stop=(hc == 1)), reads=[Bat, Bwc], writes=[X.psb[3]])
                    P.op('act', lambda e, g=g: e.copy(out=ost[0:64, g * 256:g * 256 + NCMP], in_=X.ps[3][0:64, 0:NCMP]),
                         reads=[X.psb[3]], writes=[Bost])
                else:
                    for nchk in range(2):
                        n0 = nchk * 128
                        nn = min(128, NCMP - n0)
                        for hc in range(2):
                            P.op('pe', lambda e, hc=hc, n0=n0, nn=nn: e.matmul(X.ps[4][0:nn, 0:64], lhsT=actT[:, hc, n0:n0 + nn], rhs=w2s[:, hc, :],
                                                                               start=(hc == 0), stop=(hc == 1)), reads=[Bat, Bwc], writes=[X.psb[4]])
                        P.op('act', lambda e, g=g, nchk=nchk, nn=nn: e.copy(out=ostv[0:nn, nchk, g * 64:(g + 1) * 64], in_=X.ps[4][0:nn, 0:64]),
                             reads=[X.psb[4]], writes=[Bostv])
        P.dma('sp', [(o_kcT[:, :], ost[0:64, :])], reads=[Bost], writes=[Buf('o')], sem_buf=Bost)
        P.dma('sp', [(o_vc.rearrange("(c p) n -> p c n", p=128), ostv)], reads=[Bostv], writes=[Buf('o')], sem_buf=Bostv)
        P.barrier()


def build_kv1():
    nc = bass.Bass("TRN2", target_bir_lowering=False)

    def din(name, shape, dt=F32):
        return nc.dram_tensor(name, shape, dt, kind="ExternalInput").ap()

    def dout(name, shape, dt=BF16):
        return nc.dram_tensor(name, shape, dt, kind="ExternalOutput").ap()
    hk = din("hk", [NKC * 128, 1024])
    posk = din("posk", [1, NKC * 128], I32)
    cst_d = din("cst", [128, NCST])
    cT = din("cT", [128, 8])
    w_ada = din("w_ada", [1024, 2048])
    b_adaT = din("b_adaT", [128, 16])
    gainT = din("gainT", [128, 8])
    w_kv = din("w_kv", [1024, 1536])
    w1k = din("w1k", [2048, 256])
    w1v = din("w1v", [2048, 256])
    w2k = din("w2k", [256, 64])
    w2v = din("w2v", [256, 64])
    peTk = din("peTk", [64, 32])
    peTv = din("peTv", [64, 32])
    o_kslcT = dout("kslcT", [64, 4 * NKC * 128])
    o_kwinT = dout("kwinT", [64, 4 * NKC * 128])
    o_vslc = dout("vslc", [NKC * 128, 256])
    o_vwin = dout("vwin", [NKC * 128, 256])
    o_kcT = dout("kcT", [64, 4 * 256])
    o_vc = dout("vc", [256, 256])

    T = dict(hk=hk, posk=posk, cst_d=cst_d, cT=cT, w_ada=w_ada, b_adaT=b_adaT, gainT=gainT, w_kv=w_kv, w1k=w1k, w1v=w1v, w2k=w2k, w2v=w2v, peTk=peTk, peTv=peTv, o_kslcT=o_kslcT, o_kwinT=o_kwinT, o_vslc=o_vslc, o_vwin=o_vwin, o_kcT=o_kcT, o_vc=o_vc)
    X = setup_ctx(nc)
    with X.st:
        phase_kv1(X, T)
        X.P.emit()
    return nc


def storage_order(full_b, j):
    if j == 0:
        pad = np.zeros((128,) + full_b.shape[1:], full_b.dtype)
        return np.ascontiguousarray(np.concatenate([pad, full_b[0:31 * 128]], axis=0))
    return np.ascontiguousarray(full_b)


def kv1_inputs(inputs, h1_full):
    maps = []
    for c in range(8):
        b, j = c // 2, c % 2
        maps.append({
            "hk": storage_order(h1_full[b], j),
            "posk": np.ascontiguousarray(storage_order(np.asarray(inputs['positions'][b], np.int32), j)[None, :]),
            "cst": make_consts(j),
            "cT": np.ascontiguousarray(inputs['c'][b].reshape(8, 128).T),
            "w_ada": np.ascontiguousarray(inputs['w_kv_ada']),
            "b_adaT": np.ascontiguousarray(inputs['b_kv_ada'].reshape(16, 128).T),
            "gainT": np.ascontiguousarray(inputs['kv_gain'].reshape(8, 128).T),
            "w_kv": np.ascontiguousarray(inputs['w_kv']),
            "w1k": np.ascontiguousarray(inputs['cmp_w1_k']), "w1v": np.ascontiguousarray(inputs['cmp_w1_v']),
            "w2k": np.ascontiguousarray(inputs['cmp_w2_k']), "w2v": np.ascontiguousarray(inputs['cmp_w2_v']),
            "peTk": np.ascontiguousarray(inputs['cmp_pe_k'].T), "peTv": np.ascontiguousarray(inputs['cmp_pe_v'].T),
        })
    return maps


C_TRIU = NCST
NCST1 = NCST + 128


def make_consts1(j):
    c = np.zeros((128, NCST1), np.float32)
    c[:, 0:NCST] = make_consts(j)
    q = np.arange(128)[:, None]
    k = np.arange(128)[None, :]
    c[:, C_TRIU:C_TRIU + 128] = np.where(k > q, 0.0, NEG)
    return c


def make_tables1(j, nqb=NQB):
    import ml_dtypes
    cmpMb = np.zeros((nqb, 128, 256), np.float32)
    vm = np.zeros((nqb, 128, 64), np.float32)
    va = np.zeros((nqb, 128, 64), np.float32)
    shift = 128 * (1 - j)
    n = np.arange(256)[None, :]
    jb_st = np.arange(64)[None, :]
    for m in range(nqb):
        t_st = (2 * m + 1) * 128 + np.arange(128)[:, None]
        valid = (n <= 254) & (n >= 8 * (1 - j)) & (16 * n + 31 <= t_st)
        cmpMb[m] = np.where(valid, 0.0, MASKV)
        t_g = t_st - shift
        jb = jb_st - 2 * (1 - j)
        jt = t_g // 64
        valid_b = (jb >= 0) & (jb * 64 <= t_g)
        f0 = (jb == 0)
        f1 = (jb == jt)
        f2 = (jb == jt - 1)
        forced = f0 | f1 | f2
        vm[m] = np.where(valid_b & ~forced, 1.0, 0.0)
        a = np.where(f0, 1.0e30, np.where(f1, 0.9e30, np.where(f2, 0.8e30, 0.0)))
        va[m] = np.where(valid_b, a, NEG)
    cs = np.arange(256)[:, None] * 16
    ss_ = np.arange(64)[None, :] * 64
    ov = np.minimum(cs + 32, ss_ + 64) - np.maximum(cs, ss_)
    agg = (np.clip(ov, 0, None) / 32.0).astype(np.float32)
    agg[255] = 0.0
    return {"cmpMb": cmpMb.astype(ml_dtypes.bfloat16), "selvm": vm, "selva": va, "agg": agg.astype(ml_dtypes.bfloat16)}


def phase_att1(X, T, nqb=NQB):
    hq = T['hq']
    posq = T['posq']
    cst_d = T['cst_d']
    cT = T['cT']
    w_ada = T['w_ada']
    b_adaT = T['b_adaT']
    b_gate = T['b_gate']
    gainT = T['gainT']
    w_q = T['w_q']
    w_out = T['w_out']
    kslcT_d = T['kslcT_d']
    kwinT_d = T['kwinT_d']
    vslc_d = T['vslc_d']
    vwin_d = T['vwin_d']
    kcT_d = T['kcT_d']
    vc_d = T['vc_d']
    cmpMb_d = T['cmpMb_d']
    selvm_d = T['selvm_d']
    selva_d = T['selva_d']
    agg_d = T['agg_d']
    hmid = T['hmid']
    X.carve(15000)
    P = X.P
    a32, a16 = X.a32, X.a16
    if True:
        X.cst = a32.alloc(NCST1)
        X.Bcst = Buf('cst')
        P.dma('sp', [(X.cst, cst_d[:, :])], writes=[X.Bcst])
        X.ident = X.cst[:, C_IDENT:C_IDENT + 128]
        X.irep = a16.alloc(512)
        X.Birep = Buf('irep')
        for r in range(4):
            P.op('dve', lambda e, r=r: e.tensor_copy(out=X.irep[:, r * 128:(r + 1) * 128], in_=X.ident), reads=[X.Bcst], writes=[X.Birep])
        trib = a16.alloc(128)
        triub = a16.alloc(128)
        P.op('dve', lambda e: e.tensor_copy(out=trib, in_=X.cst[:, C_TRI:C_TRI + 128]), reads=[X.Bcst], writes=[X.Birep])
        P.op('dve', lambda e: e.tensor_copy(out=triub, in_=X.cst[:, C_TRIU:C_TRIU + 128]), reads=[X.Bcst], writes=[X.Birep])
        X.eps_col = a32.alloc(1)
        tiny = a32.alloc(1)
        X.Bcst2 = Buf('cst2')
        P.op('pool', lambda e: e.memset(X.eps_col, 1e-6), writes=[X.Bcst2])
        P.op('pool', lambda e: e.memset(tiny, 1e-30), writes=[X.Bcst2])
        G1, SH, Bmod, gate_bc, Bgate = ffn_phase0(X, cT, w_ada, b_adaT, b_gate, gainT)

        KsT = a16.alloc(4 * NKC * 128).rearrange("p (g t) -> p g t", g=4)
        VsAf = a16.alloc(NKC * 260)
        VsA = VsAf.rearrange("p (c n) -> p c n", c=NKC)
        kcT = a16.alloc(1024).rearrange("p (g n) -> p g n", g=4)
        vcxf = a16.alloc(2 * 260)
        vcx = vcxf.rearrange("p (c n) -> p c n", c=2)
        agg = a16.alloc(128).rearrange("p (c n) -> p c n", c=2)
        BKV = Buf('kv')
        P.op('pool', lambda e: e.memset(VsAf, 1.0), writes=[BKV])
        P.op('pool', lambda e: e.memset(vcxf, 1.0), writes=[BKV])
        ks3 = kslcT_d.rearrange("p (g t) -> p g t", g=4)
        P.dma('sp', [(KsT[0:64, g, :], ks3[:, g, :]) for g in range(4)], writes=[BKV])
        vs4 = vslc_d.rearrange("(c p) (g d) -> p c g d", p=128, g=4)
        VsA4 = VsAf.rearrange("p (c g d) -> p c g d", c=NKC, g=4)
        P.dma('sp', [(VsA4[:, :, g, 0:64], vs4[:, :, g, :]) for g in range(4)], writes=[BKV])
        P.dma('sp', [(kcT[0:64, :, :], kcT_d.rearrange("p (g n) -> p g n", g=4))], writes=[BKV])
        vcx4 = vcxf.rearrange("p (c g d) -> p c g d", c=2, g=4)
        vc4 = vc_d.rearrange("(c p) (g d) -> p c g d", p=128, g=4)
        P.dma('sp', [(vcx4[:, :, g, 0:64], vc4[:, :, g, :]) for g in range(4)], writes=[BKV])
        P.dma('sp', [(agg, agg_d.rearrange("(c p) n -> p c n", p=128))], writes=[BKV])
        WB = a16.alloc(8 * 1072).rearrange("p (k n) -> p k n", k=8)
        WBp = a16.alloc(8 * 1024).rearrange("p (k n) -> p k n", k=8)
        BW = Buf('WB')
        BWp = Buf('WBp')
        load_w_bf16(X, WB, w_q, BW, nsplit=2)
        for k in range(8):
            src = WB[:, k, 0:1024].rearrange("p (h t i) -> p h t i", h=16, t=2)
            dst = WBp[:, k, 0:1024].rearrange("p (h t i) -> p h t i", h=16, t=2)
            eng = 'pool' if k % 2 == 0 else 'dve'
            P.op(eng, lambda e, src=src, dst=dst: e.tensor_copy(out=dst[:, :, 0, :], in_=src[:, :, 1, :]), reads=[BW], writes=[BWp])
            P.op(eng, lambda e, src=src, dst=dst: e.tensor_copy(out=dst[:, :, 1, :], in_=src[:, :, 0, :]), reads=[BW], writes=[BWp])
        Wo = a16.alloc(8 * 1024).rearrange("p (k n) -> p k n", k=8)
        BWo = Buf('Wout')
        load_w_bf16(X, Wo, w_out, BWo, nsplit=2)
        xt = [a32.alloc(1024), a32.alloc(1024)]
        Bxt = [Buf('xt0'), Buf('xt1')]
        xn = a32.alloc(1024)
        Bxn = Buf('xn')
        ss = a32.alloc(4)
        Ct = a32.alloc(128)
        St = a32.alloc(128)
        Btab = Buf('tab')
        tmp32 = a32.alloc(3 * 128)
        Btmp = Buf('ttmp')
        r1 = a32.alloc(512)
        r2 = a32.alloc(512)
        Br = Buf('ropetmp')
        uq = a16.alloc(8 * 128).rearrange("p (k t) -> p k t", k=8)
        Buq = Buf('uq')
        QT = a16.alloc(16 * 128).rearrange("p (h t) -> p h t", h=16)
        BQ = Buf('QT')
        gts = a32.alloc(48)
        Bgts = Buf('gts')
        gts3 = gts.rearrange("p (h b) -> p h b", b=3)
        KwT = [a16.alloc(4 * 640).rearrange("p (g t) -> p g t", g=4) for _ in range(2)]
        VwAf = [a16.alloc(5 * 260) for _ in range(2)]
        BKw = [Buf('kw0'), Buf('kw1')]
        for i in range(2):
            P.op('pool', lambda e, i=i: e.memset(VwAf[i], 1.0), writes=[BKw[i]])
        cmb = [a16.alloc(256), a16.alloc(256)]
        svm = [a32.alloc(64), a32.alloc(64)]
        sva = [a32.alloc(64), a32.alloc(64)]
        Btb = [Buf('tb0'), Buf('tb1')]
        PT = [a16.alloc(512), a16.alloc(512)]
        BPT = [Buf('pt0'), Buf('pt1')]
        rden = a32.alloc(8)
        coef = a32.alloc(8)
        Brd = Buf('rden')
        imp = a32.alloc(64)
        imp2 = a32.alloc(64)
        mx = a32.alloc(16)
        Bimp = Buf('imp')
        selb = a16.alloc(64)
        Bselb = Buf('selb')
        Mbs = [a16.alloc(NKC * 128), a16.alloc(NKC * 128)]
        BMbs = [Buf('mbs0'), Buf('mbs1')]
        On = a32.alloc(1024)
        BOn = Buf('On')
        otmp = a32.alloc(256)
        Botmp = Buf('otmp')
        OnT = a16.alloc(8 * 128).rearrange("p (k t) -> p k t", k=8)
        BOnT = Buf('OnT')
        hm = a32.alloc(1024)
        Bhm = Buf('hm')
        kw3 = kwinT_d.rearrange("p (g t) -> p g t", g=4)

        state = {'it': 0, 'ob': 0}

        def attend(g, chunks, out_slot_fn):
            ob = 2 if state['ob'] % 2 == 0 else 7
            state['ob'] += 1
            n = len(chunks)
            for ci, (kl, ml, bias, vr) in enumerate(chunks):
                pi = 5 + (state['it'] % 2)
                ps_ = state['it'] % 2
                state['it'] += 1
                P.op('pe', lambda e, pi=pi, kl=kl, ml=ml, g=g: e.matmul(X.ps[pi][:, :], lhsT=kl, rhs=QT[0:64, g * 4:(g + 1) * 4, :],
                                                                       start=True, stop=(ml is None)),
                     reads=[BQ], writes=[X.psb[pi]])
                if ml is not None:
                    P.op('pe', lambda e, pi=pi, ml=ml: e.matmul(X.ps[pi][:, :], lhsT=ml, rhs=X.irep, start=False, stop=True),
                         reads=[X.Birep, BMbs[0], BMbs[1], Btb[0], Btb[1]], writes=[X.psb[pi]])
                if bias is None:
                    P.op('act', lambda e, pi=pi, ps_=ps_: e.activation(out=PT[ps_], in_=X.ps[pi][:, :], func=AF.Exp, scale=0.125),
                         reads=[X.psb[pi]], writes=[BPT[ps_]])
                else:
                    P.op('act', lambda e, pi=pi, ps_=ps_, bias=bias: e.activation(out=PT[ps_], in_=X.ps[pi][:, :], func=AF.Exp, scale=0.125, bias=bias),
                         reads=[X.psb[pi], X.Bcst], writes=[BPT[ps_]])
                for r in range(4):
                    P.op('pe', lambda e, r=r, ps_=ps_, vr=vr, ob=ob, ci=ci, n=n: e.matmul(X.ps[ob][:, r * 65:(r + 1) * 65],
                                                                                       lhsT=PT[ps_][:, r * 128:(r + 1) * 128], rhs=vr,
                                                                                       start=(ci == 0 and r == 0), stop=(ci == n - 1),
                                                                                       skip_group_check=True),
                         reads=[BPT[ps_], BKw[0], BKw[1]], writes=[X.psb[ob]])
                out_slot_fn(ci, ps_)
            return ob

        for m in range(nqb):
            sc = 2 * m + 1
            nk = sc + 1
            nkeys = nk * 128
            s = m % 2
            P.dma('sp', [(xt[s], hq[m * 128:(m + 1) * 128, :])], writes=[Bxt[s]])
            P.dma('sp', [(cmb[s], cmpMb_d[m, :, :]), (svm[s], selvm_d[m, :, :]), (sva[s], selva_d[m, :, :])], writes=[Btb[s]])
            c_lo = max(0, sc - 4)
            nwc = sc - c_lo + 1
            VwA = VwAf[s].rearrange("p (c n) -> p c n", c=5)
            VwA4 = VwAf[s].rearrange("p (c g d) -> p c g d", c=5, g=4)
            P.dma('sp', [(KwT[s][0:64, g, 0:nwc * 128], kw3[:, g, c_lo * 128:(sc + 1) * 128]) for g in range(4)] +
                  [(VwA4[:, 0:nwc, g, 0:64], vwin_d[c_lo * 128:(sc + 1) * 128, :].rearrange("(c p) (g d) -> p c g d", p=128, g=4)[:, :, g, :]) for g in range(4)],
                  writes=[BKw[s]])
            rope_tables(X, posq[:, m * 128:(m + 1) * 128], 128, Ct[0:64, :], St[0:64, :], Btab, tmp32, X.ai, Btmp)
            norm_modT(X, xt[s], Bxt[s], G1, SH, Bmod, lambda k: uq[:, k, :], Buq, xn, Bxn, ss, 0, 1, xn)
            for b4 in range(4):
                for (W_, pi) in [(WB, 3), (WBp, 4)]:
                    for hh in range(4):
                        cbase = (b4 * 4 + hh) * 64
                        for k in range(8):
                            P.op('pe', lambda e, k=k, W_=W_, pi=pi, cbase=cbase, hh=hh: e.matmul(
                                X.ps[pi][0:64, hh * 128:(hh + 1) * 128], lhsT=W_[:, k, cbase:cbase + 64], rhs=uq[:, k, :],
                                start=(k == 0), stop=(k == 7)),
                                reads=[Buq, BW, BWp], writes=[X.psb[pi]])
                A = X.ps[3][0:64, :].rearrange("p (h t) -> p h t", h=4)
                B = X.ps[4][0:64, :].rearrange("p (h t) -> p h t", h=4)
                c_b = Ct[0:64, :].unsqueeze(1).to_broadcast([64, 4, 128])
                s_b = St[0:64, :].unsqueeze(1).to_broadcast([64, 4, 128])
                t1 = r1[0:64, :].rearrange("p (h t) -> p h t", h=4)
                t2 = r2[0:64, :].rearrange("p (h t) -> p h t", h=4)
                P.op('dve', lambda e, A=A, c_b=c_b, t1=t1: e.tensor_tensor(out=t1, in0=A, in1=c_b, op=ALU.mult), reads=[X.psb[3], Btab], writes=[Br])
                P.op('dve', lambda e, B=B, s_b=s_b, t2=t2: e.tensor_tensor(out=t2, in0=B, in1=s_b, op=ALU.mult), reads=[X.psb[4], Btab], writes=[Br])
                P.op('pool', lambda e, b4=b4, t1=t1, t2=t2: e.tensor_tensor(out=QT[0:64, b4 * 4:(b4 + 1) * 4, :], in0=t1, in1=t2, op=ALU.add),
                     reads=[Br], writes=[BQ])
            for k in range(8):
                P.op('pe', lambda e, k=k: e.matmul(X.ps[2][:, 0:48], lhsT=uq[:, k, :], rhs=WB[:, k, 1024:1072],
                                                   start=(k == 0), stop=(k == 7)), reads=[Buq, BW], writes=[X.psb[2]])
            P.op('act', lambda e: e.activation(out=gts, in_=X.ps[2][:, 0:48], func=AF.Sigmoid), reads=[X.psb[2]], writes=[Bgts])

            for g in range(4):
                ms = g % 2
                def cmp_extra(ci, ps_, g=g):
                    for r in range(4):
                        P.op('pe', lambda e, r=r, ps_=ps_, ci=ci: e.matmul(X.ps[3][:, r * 64:(r + 1) * 64], lhsT=PT[ps_][:, r * 128:(r + 1) * 128],
                                                                          rhs=agg[:, ci, :], start=(ci == 0 and r == 0), stop=(ci == 1),
                                                                          skip_group_check=True),
                             reads=[BPT[ps_], BKV], writes=[X.psb[3]])
                chunks = [(kcT[0:64, g, ci * 128:(ci + 1) * 128], cmb[s][:, ci * 128:(ci + 1) * 128], None, vcx[:, ci, g * 65:(g + 1) * 65])
                          for ci in range(2)]
                ob = attend(g, chunks, cmp_extra)
                O3 = X.ps[ob][:, 0:260].rearrange("p (r d) -> p r d", r=4)
                P.op('dve', lambda e, O3=O3: e.tensor_scalar(out=rden[:, 0:4], in0=O3[:, :, 64], scalar1=tiny[:, 0:1], scalar2=None, op0=ALU.add),
                     reads=[X.psb[ob], X.Bcst2], writes=[Brd])
                P.op('dve', lambda e: e.reciprocal(out=rden[:, 0:4], in_=rden[:, 0:4]), reads=[Brd], writes=[Brd])
                P.op('dve', lambda e, g=g: e.tensor_tensor(out=coef[:, 0:4], in0=rden[:, 0:4], in1=gts3[:, g * 4:(g + 1) * 4, 0], op=ALU.mult),
                     reads=[Brd, Bgts], writes=[Brd])
                P.op('dve', lambda e, O3=O3, g=g: e.tensor_tensor(
                    out=On[:, g * 256:(g + 1) * 256].rearrange("p (r d) -> p r d", r=4), in0=O3[:, :, 0:64],
                    in1=coef[:, 0:4].unsqueeze(2).to_broadcast([128, 4, 64]), op=ALU.mult),
                    reads=[X.psb[ob], Brd], writes=[BOn])
                for r in range(4):
                    if r == 0:
                        P.op('dve', lambda e: e.tensor_scalar(out=imp, in0=X.ps[3][:, 0:64], scalar1=rden[:, 0:1], scalar2=None, op0=ALU.mult),
                             reads=[X.psb[3], Brd], writes=[Bimp])
                    else:
                        P.op('dve', lambda e, r=r: e.scalar_tensor_tensor(out=imp, in0=X.ps[3][:, r * 64:(r + 1) * 64], scalar=rden[:, r:r + 1],
                                                                          in1=imp, op0=ALU.mult, op1=ALU.add),
                             reads=[X.psb[3], Brd, Bimp], writes=[Bimp])
                P.op('dve', lambda e, s=s: e.tensor_tensor(out=imp, in0=imp, in1=svm[s], op=ALU.mult), reads=[Bimp, Btb[s]], writes=[Bimp])
                P.op('dve', lambda e, s=s: e.tensor_tensor(out=imp, in0=imp, in1=sva[s], op=ALU.add), reads=[Bimp, Btb[s]], writes=[Bimp])
                P.op('dve', lambda e: e.max(out=mx[:, 0:8], in_=imp), reads=[Bimp], writes=[Bimp])
                P.op('dve', lambda e: e.match_replace(out=imp2, in_to_replace=mx[:, 0:8], in_values=imp, imm_value=-3.0e38), reads=[Bimp], writes=[Bimp])
                P.op('dve', lambda e: e.max(out=mx[:, 8:16], in_=imp2), reads=[Bimp], writes=[Bimp])
                P.op('dve', lambda e: e.tensor_reduce(out=mx[:, 0:1], in_=mx[:, 8:16], axis=AX.X, op=ALU.min), reads=[Bimp], writes=[Bimp])
                P.op('dve', lambda e: e.tensor_scalar(out=selb, in0=imp, scalar1=mx[:, 0:1], scalar2=MASKV, op0=ALU.is_lt, op1=ALU.mult),
                     reads=[Bimp], writes=[Bselb])
                nblk = 2 * nk
                P.op('pool', lambda e, ms=ms, nblk=nblk, nkeys=nkeys: e.tensor_copy(
                    out=Mbs[ms][:, 0:nkeys].rearrange("p (b l) -> p b l", l=64),
                    in_=selb[:, 0:nblk].unsqueeze(2).to_broadcast([128, nblk, 64])), reads=[Bselb], writes=[BMbs[ms]])
                P.op('pool', lambda e, ms=ms, sc=sc: e.tensor_tensor(out=Mbs[ms][:, sc * 128:(sc + 1) * 128], in0=Mbs[ms][:, sc * 128:(sc + 1) * 128],
                                                                    in1=trib, op=ALU.add), reads=[BMbs[ms], X.Birep], writes=[BMbs[ms]])
                chunks = [(KsT[0:64, g, c * 128:(c + 1) * 128], Mbs[ms][:, c * 128:(c + 1) * 128],
                           (X.cst[:, C_PAD:C_PAD + 1] if c == 0 else None), VsA[:, c, g * 65:(g + 1) * 65]) for c in range(nk)]
                ob = attend(g, chunks, lambda ci, ps_: None)

                def accum_branch(ob, br, g=g):
                    O3 = X.ps[ob][:, 0:260].rearrange("p (r d) -> p r d", r=4)
                    P.op('dve', lambda e, O3=O3: e.reciprocal(out=rden[:, 4:8], in_=O3[:, :, 64]), reads=[X.psb[ob]], writes=[Brd])
                    P.op('dve', lambda e: e.tensor_tensor(out=coef[:, 4:8], in0=rden[:, 4:8], in1=gts3[:, g * 4:(g + 1) * 4, br], op=ALU.mult),
                         reads=[Brd, Bgts], writes=[Brd])
                    P.op('dve', lambda e, O3=O3: e.tensor_tensor(out=otmp.rearrange("p (r d) -> p r d", r=4), in0=O3[:, :, 0:64],
                                                                in1=coef[:, 4:8].unsqueeze(2).to_broadcast([128, 4, 64]), op=ALU.mult),
                         reads=[X.psb[ob], Brd], writes=[Botmp])
                    P.op('pool', lambda e: e.tensor_tensor(out=On[:, g * 256:(g + 1) * 256], in0=On[:, g * 256:(g + 1) * 256], in1=otmp, op=ALU.add),
                         reads=[Botmp, BOn], writes=[BOn])
                accum_branch(ob, 1)
                chunks = []
                for wi_, c in enumerate(range(c_lo, sc + 1)):
                    if c == sc:
                        ml = trib
                    elif c == sc - 4:
                        ml = triub
                    else:
                        ml = None
                    chunks.append((KwT[s][0:64, g, wi_ * 128:(wi_ + 1) * 128], ml,
                                   (X.cst[:, C_PAD:C_PAD + 1] if c == 0 else None), VwA[:, wi_, g * 65:(g + 1) * 65]))
                ob = attend(g, chunks, lambda ci, ps_: None)
                accum_branch(ob, 2)
            for k in range(8):
                pi = 0 if k < 4 else 1
                P.op('pe', lambda e, k=k, pi=pi: e.transpose(out=X.ps[pi][:, (k % 4) * 128:(k % 4 + 1) * 128],
                                                             in_=On[:, k * 128:(k + 1) * 128], identity=X.ident),
                     reads=[BOn, X.Bcst], writes=[X.psb[pi]])
            for half in range(2):
                P.op('act', lambda e, half=half: e.copy(out=OnT[:, half * 4:(half + 1) * 4, :],
                                                        in_=X.ps[half][:, :].rearrange("p (k t) -> p k t", k=4)),
                     reads=[X.psb[half]], writes=[BOnT])
            for half in range(2):
                pi = 3 + half
                for k in range(8):
                    P.op('pe', lambda e, k=k, pi=pi, half=half: e.matmul(X.ps[pi][:, :], lhsT=OnT[:, k, :],
                                                                         rhs=Wo[:, k, half * 512:(half + 1) * 512],
                                                                         start=(k == 0), stop=(k == 7)),
                         reads=[BOnT, BWo], writes=[X.psb[pi]])
                P.op('dve', lambda e, pi=pi, half=half: e.tensor_tensor(out=hm[:, half * 512:(half + 1) * 512], in0=X.ps[pi][:, :],
                                                                        in1=gate_bc[:, half * 512:(half + 1) * 512], op=ALU.mult),
                     reads=[X.psb[pi], Bgate], writes=[Bhm])
            P.op('pool', lambda e, s=s: e.tensor_tensor(out=hm, in0=hm, in1=xt[s], op=ALU.add), reads=[Bhm, Bxt[s]], writes=[Bhm])
            P.dma('sp', [(hmid[m * 128:(m + 1) * 128, :], hm)], reads=[Bhm], writes=[Buf('o')], sem_buf=Bhm)
        P.barrier()


def build_att1(nqb=NQB):
    nc = bass.Bass("TRN2", target_bir_lowering=False)

    def din(name, shape, dt=F32):
        return nc.dram_tensor(name, shape, dt, kind="ExternalInput").ap()
    hq = din("hq", [nqb * 128, 1024])
    posq = din("posq", [1, nqb * 128], I32)
    cst_d = din("cst", [128, NCST1])
    cT = din("cT", [128, 8])
    w_ada = din("w_ada", [1024, 3072])
    b_adaT = din("b_adaT", [128, 16])
    b_gate = din("b_gate", [1, 1024])
    gainT = din("gainT", [128, 8])
    w_q = din("w_q", [1024, 1072])
    w_out = din("w_out", [1024, 1024])
    kslcT_d = din("kslcT", [64, 4 * NKC * 128], BF16)
    kwinT_d = din("kwinT", [64, 4 * NKC * 128], BF16)
    vslc_d = din("vslc", [NKC * 128, 256], BF16)
    vwin_d = din("vwin", [NKC * 128, 256], BF16)
    kcT_d = din("kcT", [64, 1024], BF16)
    vc_d = din("vc", [256, 256], BF16)
    cmpMb_d = din("cmpMb", [nqb, 128, 256], BF16)
    selvm_d = din("selvm", [nqb, 128, 64])
    selva_d = din("selva", [nqb, 128, 64])
    agg_d = din("agg", [256, 64], BF16)
    hmid = nc.dram_tensor("hmid", [nqb * 128, 1024], F32, kind="ExternalOutput").ap()

    T = dict(hq=hq, posq=posq, cst_d=cst_d, cT=cT, w_ada=w_ada, b_adaT=b_adaT, b_gate=b_gate, gainT=gainT, w_q=w_q, w_out=w_out, kslcT_d=kslcT_d, kwinT_d=kwinT_d, vslc_d=vslc_d, vwin_d=vwin_d, kcT_d=kcT_d, vc_d=vc_d, cmpMb_d=cmpMb_d, selvm_d=selvm_d, selva_d=selva_d, agg_d=agg_d, hmid=hmid)
    X = setup_ctx(nc)
    with X.st:
        phase_att1(X, T, nqb)
        X.P.emit()
    return nc


def att1_inputs(inputs, h1_cores, kv_res, nqb=NQB):
    maps = []
    for c in range(8):
        b, j = c // 2, c % 2
        pos = np.asarray(inputs['positions'][b], np.int32)
        posq = np.concatenate([pos[(2 * m + j) * 128:(2 * m + j + 1) * 128] for m in range(nqb)])
        mp = {
            "hq": np.ascontiguousarray(h1_cores[c][0:nqb * 128]),
            "posq": np.ascontiguousarray(posq[None, :]),
            "cst": make_consts1(j),
            "cT": np.ascontiguousarray(inputs['c'][b].reshape(8, 128).T),
            "w_ada": np.ascontiguousarray(inputs['w_ada'][1][:, 0:3072]),
            "b_adaT": np.ascontiguousarray(inputs['b_ada'][1][0:2048].reshape(16, 128).T),
            "b_gate": np.ascontiguousarray(inputs['b_ada'][1][2048:3072][None, :]),
            "gainT": np.ascontiguousarray(inputs['attn_gain'][1].reshape(8, 128).T),
            "w_q": np.ascontiguousarray(inputs['b_w_q'][0]),
            "w_out": np.ascontiguousarray(inputs['b_w_out'][0]),
        }
        for k in ["kslcT", "kwinT", "vslc", "vwin", "kcT", "vc"]:
            mp[k] = kv_res[c][k]
        mp.update(make_tables1(j, nqb))
        maps.append(mp)
    return maps


def phase_ffn1(X, T, ntok=2048, dff=3584, nexp=8):
    hin = T['hin']
    cst_d = T['cst_d']
    cT = T['cT']
    w_ada = T['w_ada']
    b_adaT = T['b_adaT']
    b_gate = T['b_gate']
    gainT = T['gainT']
    fgain = T['fgain']
    wr = T['wr']
    wg = T['wg']
    wu = T['wu']
    wd = T['wd']
    hout = T['hout']
    NF = dff // 128
    FB = 4
    GT = 1024
    ngrp = ntok // GT
    NT = GT // 128
    DQ = 256
    X.carve(16300)
    P = X.P
    a32, a16 = X.a32, X.a16
    if True:
        load_consts(X, cst_d)
        X.eps_col = a32.alloc(1)
        X.Bcst2 = Buf('cst2')
        P.op('pool', lambda e: e.memset(X.eps_col, 1e-6), writes=[X.Bcst2])
        G1, SH, Bmod, gate_bc, Bgate = ffn_phase0(X, cT, w_ada, b_adaT, b_gate, gainT)
        fg_bc = a32.alloc(1024)
        P.dma('sp', [(fg_bc, fgain.partition_broadcast(128))], writes=[Bgate])
        Wr = a32.alloc(8 * nexp).rearrange("p (k n) -> p k n", k=8)
        BWr = Buf('Wr')
        P.dma('sp', [(Wr, wr.rearrange("(k p) n -> p k n", p=128))], writes=[BWr])

        NWB = 2
        Wgb = [a16.alloc(8 * 128 * FB).rearrange("p (k n) -> p k n", k=8) for _ in range(NWB)]
        Wub = [a16.alloc(8 * 128 * FB).rearrange("p (k n) -> p k n", k=8) for _ in range(NWB)]
        BWg = [Buf('wg%d' % i) for i in range(NWB)]
        Wdb = [a16.alloc(NF * DQ).rearrange("p (f n) -> p f n", f=NF) for _ in range(2)]
        BWd = [Buf('wd0'), Buf('wd1')]
        u2T = a16.alloc(8 * GT).rearrange("p (k t) -> p k t", k=8)
        Bu2 = Buf('u2T')
        actT = a16.alloc(NF * GT).rearrange("p (f t) -> p f t", f=NF)
        Bact = Buf('actT')
        yacc = a32.alloc(NT * 1024).rearrange("p (t n) -> p t n", t=NT)
        By = [Buf('y%d' % i) for i in range(NT)]
        xt = [a32.alloc(1024), a32.alloc(1024)]
        Bxt = [Buf('xt0'), Buf('xt1')]
        xn = a32.alloc(1024)
        Bxn = Buf('xn')
        ss = a32.alloc(4)
        u32 = a32.alloc(8 * 128).rearrange("p (k t) -> p k t", k=8)
        Bu32 = Buf('u32')
        gall = a32.alloc(NT * nexp).rearrange("p (t n) -> p t n", t=NT)
        Bgall = Buf('gall')
        rt = a32.alloc(64)
        Brt = Buf('rt')
        sg = [a32.alloc(512), a32.alloc(512)]
        Bsg = [Buf('sg0'), Buf('sg1')]

        wi = 0
        di = 0
        for grp in range(ngrp):
            g0 = grp * GT
            for t in range(NT):
                s = t % 2
                P.dma('sp', [(xt[s], hin[g0 + t * 128:g0 + (t + 1) * 128, :])], writes=[Bxt[s]])
                norm_modT(X, xt[s], Bxt[s], G1, SH, Bmod, lambda k, t=t: u2T[:, k, t * 128:(t + 1) * 128], Bu2,
                          xn, Bxn, ss, 0, 1, xn, u32_dst=lambda k: u32[:, k, :], Bu32=Bu32)
                for k in range(8):
                    P.op('pe', lambda e, k=k: e.matmul(X.ps[2][:, 0:nexp], lhsT=u32[:, k, :], rhs=Wr[:, k, :], start=(k == 0), stop=(k == 7)),
                         reads=[Bu32, BWr], writes=[X.psb[2]])
                lg = rt[:, 0:8]
                e1 = rt[:, 8:16]
                lg2 = rt[:, 16:24]
                e2 = rt[:, 24:32]
                m1 = rt[:, 32:33]
                m2 = rt[:, 33:34]
                dl = rt[:, 34:35]
                w1_ = rt[:, 35:36]
                w2_ = rt[:, 36:37]
                gt_ = gall[:, t, :]
                P.op('dve', lambda e, lg=lg: e.tensor_copy(out=lg, in_=X.ps[2][:, 0:nexp]), reads=[X.psb[2]], writes=[Brt])
                P.op('dve', lambda e, lg=lg, m1=m1: e.tensor_reduce(out=m1, in_=lg, axis=AX.X, op=ALU.max), reads=[Brt], writes=[Brt])
                P.op('dve', lambda e, lg=lg, m1=m1, e1=e1: e.tensor_scalar(out=e1, in0=lg, scalar1=m1, scalar2=None, op0=ALU.is_equal), reads=[Brt], writes=[Brt])
                P.op('dve', lambda e, lg=lg, e1=e1, lg2=lg2: e.scalar_tensor_tensor(out=lg2, in0=e1, scalar=NEG, in1=lg, op0=ALU.mult, op1=ALU.add),
                     reads=[Brt], writes=[Brt])
                P.op('dve', lambda e, lg2=lg2, m2=m2: e.tensor_reduce(out=m2, in_=lg2, axis=AX.X, op=ALU.max), reads=[Brt], writes=[Brt])
                P.op('dve', lambda e, lg2=lg2, m2=m2, e2=e2: e.tensor_scalar(out=e2, in0=lg2, scalar1=m2, scalar2=None, op0=ALU.is_equal), reads=[Brt], writes=[Brt])
                P.op('dve', lambda e, m1=m1, m2=m2, dl=dl: e.tensor_tensor(out=dl, in0=m1, in1=m2, op=ALU.subtract), reads=[Brt], writes=[Brt])
                P.op('act', lambda e, dl=dl, w1_=w1_: e.activation(out=w1_, in_=dl, func=AF.Sigmoid), reads=[Brt], writes=[Brt])
                P.op('act', lambda e, dl=dl, w2_=w2_: e.activation(out=w2_, in_=dl, func=AF.Sigmoid, scale=-1.0), reads=[Brt], writes=[Brt])
                P.op('dve', lambda e, e1=e1, w1_=w1_, gt_=gt_: e.tensor_scalar(out=gt_, in0=e1, scalar1=w1_, scalar2=None, op0=ALU.mult),
                     reads=[Brt], writes=[Bgall])
                P.op('dve', lambda e, e2=e2, w2_=w2_, gt_=gt_: e.scalar_tensor_tensor(out=gt_, in0=e2, scalar=w2_, in1=gt_, op0=ALU.mult, op1=ALU.add),
                     reads=[Brt, Bgall], writes=[Bgall])
            for ex in range(nexp):
                wg3 = wg[ex].rearrange("(k p) n -> p k n", p=128)
                wu3 = wu[ex].rearrange("(k p) n -> p k n", p=128)
                wd3 = wd[ex].rearrange("(f p) n -> p f n", p=128)
                it = 0
                for fb in range(NF // FB):
                    wb = wi % NWB
                    wi += 1
                    f0 = fb * FB
                    P.dma('pool', [(Wgb[wb], wg3[:, :, f0 * 128:(f0 + FB) * 128]), (Wub[wb], wu3[:, :, f0 * 128:(f0 + FB) * 128])], writes=[BWg[wb]])
                    for fi in range(FB):
                        f = f0 + fi
                        for half in range(GT // 512):
                            pg = 2 + 2 * (it % 2)
                            pu = pg + 1
                            sgi = it % 2
                            it += 1
                            for (W_, pi) in [(Wgb[wb], pg), (Wub[wb], pu)]:
                                for k in range(8):
                                    P.op('pe', lambda e, k=k, W_=W_, pi=pi, half=half, fi=fi: e.matmul(
                                        X.ps[pi][:, :], lhsT=W_[:, k, fi * 128:(fi + 1) * 128], rhs=u2T[:, k, half * 512:(half + 1) * 512],
                                        start=(k == 0), stop=(k == 7)), reads=[BWg[wb], Bu2], writes=[X.psb[pi]])
                            P.op('act', lambda e, pg=pg, sgi=sgi: e.activation(out=sg[sgi], in_=X.ps[pg][:, :], func=AF.Silu),
                                 reads=[X.psb[pg]], writes=[Bsg[sgi]])
                            P.op('dve', lambda e, pu=pu, sgi=sgi, f=f, half=half: e.tensor_tensor(
                                out=actT[:, f, half * 512:(half + 1) * 512], in0=X.ps[pu][:, :], in1=sg[sgi], op=ALU.mult),
                                reads=[X.psb[pu], Bsg[sgi]], writes=[Bact])
                it = 0
                for q in range(1024 // DQ):
                    db = di % 2
                    di += 1
                    P.dma('pool', [(Wdb[db][:, 0:NF // 2, :], wd3[:, 0:NF // 2, q * DQ:(q + 1) * DQ]),
                                   (Wdb[db][:, NF // 2:NF, :], wd3[:, NF // 2:NF, q * DQ:(q + 1) * DQ])], writes=[BWd[db]])
                    for t in range(NT):
                        pi = 6 + (it % 2)
                        it += 1
                        for f in range(NF):
                            P.op('pe', lambda e, f=f, pi=pi, t=t, db=db: e.matmul(X.ps[pi][:, 0:DQ], lhsT=actT[:, f, t * 128:(t + 1) * 128],
                                                                                 rhs=Wdb[db][:, f, :], start=(f == 0), stop=(f == NF - 1)),
                                 reads=[Bact, BWd[db]], writes=[X.psb[pi]])
                        if ex == 0:
                            P.op('dve', lambda e, pi=pi, t=t, q=q, ex=ex: e.tensor_scalar(out=yacc[:, t, q * DQ:(q + 1) * DQ], in0=X.ps[pi][:, 0:DQ],
                                                                                         scalar1=gall[:, t, ex:ex + 1], scalar2=None, op0=ALU.mult),
                                 reads=[X.psb[pi], Bgall], writes=[By[t]])
                        else:
                            P.op('dve', lambda e, pi=pi, t=t, q=q, ex=ex: e.scalar_tensor_tensor(
                                out=yacc[:, t, q * DQ:(q + 1) * DQ], in0=X.ps[pi][:, 0:DQ], scalar=gall[:, t, ex:ex + 1],
                                in1=yacc[:, t, q * DQ:(q + 1) * DQ], op0=ALU.mult, op1=ALU.add),
                                reads=[X.psb[pi], Bgall, By[t]], writes=[By[t]])
            for t in range(NT):
                s = t % 2
                P.dma('sp', [(xt[s], hin[g0 + t * 128:g0 + (t + 1) * 128, :])], writes=[Bxt[s]])
                P.op('pool', lambda e, t=t: e.tensor_tensor(out=yacc[:, t, :], in0=yacc[:, t, :], in1=gate_bc, op=ALU.mult),
                     reads=[By[t], Bgate], writes=[By[t]])
                P.op('pool', lambda e, t=t, s=s: e.tensor_tensor(out=yacc[:, t, :], in0=yacc[:, t, :], in1=xt[s], op=ALU.add),
                     reads=[By[t], Bxt[s]], writes=[By[t]])
                P.op('act', lambda e, t=t: e.activation(out=xn, in_=yacc[:, t, :], func=AF.Square, accum_out=ss[:, 0:1]), reads=[By[t]], writes=[Bxn])
                P.op('act', lambda e: e.activation(out=ss[:, 1:2], in_=ss[:, 0:1], func=AF.Sqrt, scale=1.0 / 1024.0, bias=X.eps_col),
                     reads=[Bxn, X.Bcst2], writes=[Bxn])
                P.op('dve', lambda e: e.reciprocal(out=ss[:, 2:3], in_=ss[:, 1:2]), reads=[Bxn], writes=[Bxn])
                P.op('dve', lambda e, t=t: e.scalar_tensor_tensor(out=yacc[:, t, :], in0=yacc[:, t, :], scalar=ss[:, 2:3], in1=fg_bc,
                                                                  op0=ALU.mult, op1=ALU.mult), reads=[By[t], Bxn, Bgate], writes=[By[t]])
                P.dma('sp', [(hout[g0 + t * 128:g0 + (t + 1) * 128, :], yacc[:, t, :])], reads=[By[t]], writes=[Buf('o')], sem_buf=By[t])
        P.barrier()


def build_ffn1(ntok=2048, dff=3584, nexp=8):
    nc = bass.Bass("TRN2", target_bir_lowering=False)

    def din(name, shape, dt=F32):
        return nc.dram_tensor(name, shape, dt, kind="ExternalInput").ap()
    hin = din("hin", [ntok, 1024])
    cst_d = din("cst", [128, NCST])
    cT = din("cT", [128, 8])
    w_ada = din("w_ada", [1024, 3072])
    b_adaT = din("b_adaT", [128, 16])
    b_gate = din("b_gate", [1, 1024])
    gainT = din("gainT", [128, 8])
    fgain = din("fgain", [1, 1024])
    wr = din("wr", [1024, nexp])
    wg = din("wg", [nexp, 1024, dff])
    wu = din("wu", [nexp, 1024, dff])
    wd = din("wd", [nexp, dff, 1024])
    hout = nc.dram_tensor("hout", [ntok, 1024], F32, kind="ExternalOutput").ap()
    NF = dff // 128
    FB = 4
    GT = 1024
    ngrp = ntok // GT
    NT = GT // 128
    DQ = 256

    T = dict(hin=hin, cst_d=cst_d, cT=cT, w_ada=w_ada, b_adaT=b_adaT, b_gate=b_gate, gainT=gainT, fgain=fgain, wr=wr, wg=wg, wu=wu, wd=wd, hout=hout)
    X = setup_ctx(nc)
    with X.st:
        phase_ffn1(X, T, ntok, dff, nexp)
        X.P.emit()
    return nc


def ffn1_inputs(inputs, hmid_cores):
    maps = []
    for c in range(8):
        b, j = c // 2, c % 2
        maps.append({
            "hin": np.ascontiguousarray(hmid_cores[c]),
            "cst": make_consts(j),
            "cT": np.ascontiguousarray(inputs['c'][b].reshape(8, 128).T),
            "w_ada": np.ascontiguousarray(inputs['w_ada'][1][:, 3072:6144]),
            "b_adaT": np.ascontiguousarray(inputs['b_ada'][1][3072:5120].reshape(16, 128).T),
            "b_gate": np.ascontiguousarray(inputs['b_ada'][1][5120:6144][None, :]),
            "gainT": np.ascontiguousarray(inputs['ffn_gain'][1].reshape(8, 128).T),
            "fgain": np.ascontiguousarray(inputs['final_gain'][None, :]),
            "wr": np.ascontiguousarray(inputs['moe_w_router'][0]),
            "wg": np.ascontiguousarray(inputs['moe_w_gate'][0]),
            "wu": np.ascontiguousarray(inputs['moe_w_up'][0]),
            "wd": np.ascontiguousarray(inputs['moe_w_down'][0]),
        })
    return maps


def _run(nc, maps):
    res = run_bass_kernel_spmd(nc, maps, core_ids=list(range(8)))
    return res.results


def _kernel_unfused_impl(**inputs):
    inputs = {k: np.asarray(v) for k, v in inputs.items()}
    r0 = _run(build_att0(), att0_inputs(inputs))
    hmid0 = [np.asarray(r0[c]["hmid"]) for c in range(8)]
    r1 = _run(build_ffn0(), ffn0_inputs(inputs, hmid0))
    h1c = [np.asarray(r1[c]["hout"]) for c in range(8)]
    h1_full = gather_blocks(r1, "hout")
    r2 = _run(build_kv1(), kv1_inputs(inputs, h1_full))
    kv_res = [{k: np.asarray(r2[c][k]) for k in ["kslcT", "kwinT", "vslc", "vwin", "kcT", "vc"]} for c in range(8)]
    r3 = _run(build_att1(), att1_inputs(inputs, h1c, kv_res))
    hmid1 = [np.asarray(r3[c]["hmid"]) for c in range(8)]
    r4 = _run(build_ffn1(), ffn1_inputs(inputs, hmid1))
    return gather_blocks(r4, "hout").astype(np.float32)


def build_fused(stop_after=None):
    nc = bass.Bass("TRN2", target_bir_lowering=False)

    def din(name, shape, dt=F32):
        return nc.dram_tensor(name, shape, dt, kind="ExternalInput").ap()

    def dint(name, shape, dt=F32):
        return nc.dram_tensor(name, shape, dt, kind="Internal").ap()
    xk = din("xk", [NKC * 128, 1024])
    posk = din("posk", [1, NKC * 128], I32)
    posq = din("posq", [1, NQB * 128], I32)
    cst = din("cst", [128, NCST1])
    cT = din("cT", [128, 8])
    w_ada = din("w_ada", [2, 1024, 6144])
    b_adaT = din("b_adaT", [2, 128, 48])
    b_row = din("b_row", [2, 1, 6144])
    agT = din("agT", [2, 128, 8])
    fgT = din("fgT", [2, 128, 8])
    kvgT = din("kvgT", [128, 8])
    w_kv_ada = din("w_kv_ada", [1024, 2048])
    b_kvT = din("b_kvT", [128, 16])
    a_w_in = din("a_w_in", [1024, 2120])
    a_w_out = din("a_w_out", [1024, 1024])
    b_w_q = din("b_w_q", [1024, 1072])
    b_w_out = din("b_w_out", [1024, 1024])
    w_kv = din("w_kv", [1024, 1536])
    w1k = din("w1k", [2048, 256])
    w1v = din("w1v", [2048, 256])
    w2k = din("w2k", [256, 64])
    w2v = din("w2v", [256, 64])
    peTk = din("peTk", [64, 32])
    peTv = din("peTv", [64, 32])
    fwg = din("fwg", [1024, 2816])
    fwu = din("fwu", [1024, 2816])
    fwd = din("fwd", [2816, 1024])
    mwr = din("mwr", [1024, 8])
    if stop_after is None:
        mwg = din("mwg", [8, 1024, 3584])
        mwu = din("mwu", [8, 1024, 3584])
        mwd = din("mwd", [8, 3584, 1024])
    fgain = din("fgain", [1, 1024])
    cmpMb = din("cmpMb", [NQB, 128, 256], BF16)
    selvm = din("selvm", [NQB, 128, 64])
    selva = din("selva", [NQB, 128, 64])
    agg = din("agg", [256, 64], BF16)
    ridx = din("ridx", [128, NKC], I32)
    out = nc.dram_tensor("out", [NQB * 128, 1024], F32, kind="ExternalOutput").ap()
    hmid0 = dint("hmid0", [NQB * 128, 1024])
    h1own = dint("h1own", [NQB * 128, 1024])
    h1pair = dint("h1pair", [2 * NQB * 128, 1024])
    hmid1 = dint("hmid1", [NQB * 128, 1024])
    i_kslcT = dint("i_kslcT", [64, 4 * NKC * 128], BF16)
    i_kwinT = dint("i_kwinT", [64, 4 * NKC * 128], BF16)
    i_vslc = dint("i_vslc", [NKC * 128, 256], BF16)
    i_vwin = dint("i_vwin", [NKC * 128, 256], BF16)
    i_kcT = dint("i_kcT", [64, 1024], BF16)
    i_vc = dint("i_vc", [256, 256], BF16)

    X = setup_ctx(nc)
    P = X.P
    with X.st:
        def dbg_out(src, rows):
            dbg = nc.dram_tensor("dbg", [rows, 1024], F32, kind="ExternalOutput").ap()
            X.carve(15000)
            tl = X.a32.alloc(1024)
            for r in range(rows // 128):
                Bt = Buf('dbg')
                P.dma('sp', [(tl, src[r * 128:(r + 1) * 128, :])], writes=[Bt])
                P.dma('sp', [(dbg[r * 128:(r + 1) * 128, :], tl)], reads=[Bt], writes=[Buf('o')], sem_buf=Bt)
                P.barrier()
            P.emit()
            return nc
        phase_att0(X, dict(xk=xk, posk=posk, cst_d=cst, cT=cT, w_ada=w_ada[0][:, 0:3072], b_adaT=b_adaT[0][:, 0:16],
                           b_gate=b_row[0][:, 2048:3072], gainT=agT[0], w_in=a_w_in, w_out=a_w_out, hmid=hmid0))
        P.emit()
        if stop_after == 'att0':
            return dbg_out(hmid0, NQB * 128)
        if stop_after in ('att0q1', 'att0q2', 'att0q3'):
            X.carve(20000)
            tq = X.a32.alloc(4096)
            if stop_after == 'att0q1':
                P.dma('sp', [(tq[:, 0:1024], b_row[0][:, 5120:6144].partition_broadcast(128))], writes=[Buf('q')])
            elif stop_after == 'att0q2':
                P.dma('sp', [(tq.rearrange("p (k n) -> p k n", k=8), w_ada[0][:, 3072:3584].rearrange("(k p) n -> p k n", p=128))], writes=[Buf('q')])
            else:
                P.dma('sp', [(tq[:, 0:8], cT[:, :]), (tq[:, 8:24], b_adaT[0][:, 24:40]), (tq[:, 24:32], fgT[0][:, :])], writes=[Buf('q')])
            P.barrier()
            stop_after = 'att0r'
        if stop_after == 'att0m':
            for i in range(0, NBIG, 2000):
                P.op('pool', lambda e, i=i: e.memset(X.big[:, i:min(i + 2000, NBIG)], 12345.0), writes=[Buf('z')])
            P.barrier()
            stop_after = 'att0r'
        if stop_after == 'att0p':
            dbgA = nc.dram_tensor("dbgA", [NQB * 128, 1024], F32, kind="ExternalOutput").ap()
            X.carve(52000)
            X.a32.alloc(34000)
            hrA = X.a32.alloc(16 * 1024).rearrange("p (t n) -> p t n", t=16)
            BsA = [Buf('rA%d' % t) for t in range(16)]
            for t in range(16):
                P.dma('sp', [(hrA[:, t, :], hmid0[t * 128:(t + 1) * 128, :])], writes=[BsA[t]])
            for t in range(16):
                P.dma('sp', [(dbgA[t * 128:(t + 1) * 128, :], hrA[:, t, :])], reads=[BsA[t]], writes=[Buf('o')], sem_buf=BsA[t])
            P.barrier()
            X.carve(15000)
            load_consts(X, cst)
            X.eps_col = X.a32.alloc(1)
            X.Bcst2 = Buf('cst2')
            P.op('pool', lambda e: e.memset(X.eps_col, 1e-6), writes=[X.Bcst2])
            ffn_phase0(X, cT, w_ada[0][:, 3072:6144], b_adaT[0][:, 24:40], b_row[0][:, 5120:6144], fgT[0])
            P.barrier()
            stop_after = 'att0r'
        if stop_after == 'att0r':
            dbg = nc.dram_tensor("dbg", [NQB * 128, 1024], F32, kind="ExternalOutput").ap()
            X.carve(52000)
            X.a32.alloc(34000)
            hr = X.a32.alloc(16 * 1024).rearrange("p (t n) -> p t n", t=16)
            Bs = [Buf('r%d' % t) for t in range(16)]
            for t in range(16):
                P.dma('sp', [(hr[:, t, :], hmid0[t * 128:(t + 1) * 128, :])], writes=[Bs[t]])
            for t in range(16):
                P.dma('sp', [(dbg[t * 128:(t + 1) * 128, :], hr[:, t, :])], reads=[Bs[t]], writes=[Buf('o')], sem_buf=Bs[t])
            P.barrier()
            P.emit()
            return nc
        if stop_after == 'att0s':
            X.carve(15000)
            Wt = X.a16.alloc(22 * 1024).rearrange("p (f n) -> p f n", f=22)
            P.dma('pool', [(Wt, fwd.rearrange("(f p) n -> p f n", p=128))], writes=[Buf('wt')])
            P.barrier()
            return dbg_out(hmid0, NQB * 128)
        phase_ffn0(X, dict(hin=(hmid0 if stop_after != 'ffn0y' else din('hin_dbg', [NQB * 128, 1024])), cst_d=cst, cT=cT, w_ada=w_ada[0][:, 3072:6144], b_adaT=b_adaT[0][:, 24:40],
                           b_gate=b_row[0][:, 5120:6144], gainT=fgT[0], wg=fwg, wu=fwu, wd=fwd,
                           hout=(h1own if stop_after not in ('ffn0x', 'ffn0y') else nc.dram_tensor("dbg", [NQB * 128, 1024], F32, kind="ExternalOutput").ap())))
        P.emit()
        if stop_after in ('ffn0x', 'ffn0y'):
            return nc
        if stop_after == 'ffn0':
            return dbg_out(h1own, NQB * 128)
        Bcc = Buf('cc')
        P.dma('pool', None, writes=[Bcc], inc=1,
              fns=[lambda e, q=q: e.collective_compute("AllGather", ALU.bypass, replica_groups=[[0, 1], [2, 3], [4, 5], [6, 7]],
                                                       ins=[h1own[q * 512:(q + 1) * 512, :]], outs=[h1pair[q * 1024:(q + 1) * 1024, :]])
                   for q in range(4)])
        P.barrier()
        P.emit()
        if stop_after == 'cc':
            return dbg_out(h1pair, 2 * NQB * 128)
        phase_kv1(X, dict(hk=h1pair, posk=posk, cst_d=cst, cT=cT, w_ada=w_kv_ada, b_adaT=b_kvT, gainT=kvgT, w_kv=w_kv,
                          w1k=w1k, w1v=w1v, w2k=w2k, w2v=w2v, peTk=peTk, peTv=peTv, ridx=ridx,
                          o_kslcT=i_kslcT, o_kwinT=i_kwinT, o_vslc=i_vslc, o_vwin=i_vwin, o_kcT=i_kcT, o_vc=i_vc))
        P.emit()
        phase_att1(X, dict(hq=h1own, posq=posq, cst_d=cst, cT=cT, w_ada=w_ada[1][:, 0:3072], b_adaT=b_adaT[1][:, 0:16],
                           b_gate=b_row[1][:, 2048:3072], gainT=agT[1], w_q=b_w_q, w_out=b_w_out,
                           kslcT_d=i_kslcT, kwinT_d=i_kwinT, vslc_d=i_vslc, vwin_d=i_vwin, kcT_d=i_kcT, vc_d=i_vc,
                           cmpMb_d=cmpMb, selvm_d=selvm, selva_d=selva, agg_d=agg, hmid=hmid1))
        P.emit()
        phase_ffn1(X, dict(hin=hmid1, cst_d=cst, cT=cT, w_ada=w_ada[1][:, 3072:6144], b_adaT=b_adaT[1][:, 24:40],
                           b_gate=b_row[1][:, 5120:6144], gainT=fgT[1], fgain=fgain, wr=mwr, wg=mwg, wu=mwu, wd=mwd, hout=out))
        P.emit()
    return nc


def fused_inputs(inputs):
    A = lambda a: np.ascontiguousarray(a)
    maps = []
    shared = {
        "w_ada": A(inputs['w_ada']),
        "b_adaT": A(inputs['b_ada'].reshape(2, 48, 128).transpose(0, 2, 1)),
        "b_row": A(inputs['b_ada'][:, None, :]),
        "agT": A(inputs['attn_gain'].reshape(2, 8, 128).transpose(0, 2, 1)),
        "fgT": A(inputs['ffn_gain'].reshape(2, 8, 128).transpose(0, 2, 1)),
        "kvgT": A(inputs['kv_gain'].reshape(8, 128).T),
        "w_kv_ada": A(inputs['w_kv_ada']),
        "b_kvT": A(inputs['b_kv_ada'].reshape(16, 128).T),
        "a_w_in": A(inputs['a_w_in'][0]), "a_w_out": A(inputs['a_w_out'][0]),
        "b_w_q": A(inputs['b_w_q'][0]), "b_w_out": A(inputs['b_w_out'][0]),
        "w_kv": A(inputs['w_kv']),
        "w1k": A(inputs['cmp_w1_k']), "w1v": A(inputs['cmp_w1_v']), "w2k": A(inputs['cmp_w2_k']), "w2v": A(inputs['cmp_w2_v']),
        "peTk": A(inputs['cmp_pe_k'].T), "peTv": A(inputs['cmp_pe_v'].T),
        "fwg": A(inputs['ffn_w_gate'][0]), "fwu": A(inputs['ffn_w_up'][0]), "fwd": A(inputs['ffn_w_down'][0]),
        "mwr": A(inputs['moe_w_router'][0]), "mwg": A(inputs['moe_w_gate'][0]), "mwu": A(inputs['moe_w_up'][0]),
        "mwd": A(inputs['moe_w_down'][0]),
        "fgain": A(inputs['final_gain'][None, :]),
    }
    tabs = [make_tables1(0), make_tables1(1)]
    for c in range(8):
        b, j = c // 2, c % 2
        pos = np.asarray(inputs['positions'][b], np.int32)
        posq = np.concatenate([pos[(2 * m + j) * 128:(2 * m + j + 1) * 128] for m in range(NQB)])
        ridx = np.zeros((128, NKC), np.int32)
        for sc in range(NKC):
            gc = max(0, sc - (1 - j))
            o_ = (gc // 2) * 128 + np.arange(128)
            ridx[:, sc] = (o_ // 512) * 1024 + (gc % 2) * 512 + (o_ % 512)
        mp = dict(shared)
        mp.update({
            "xk": storage_order(np.asarray(inputs['x'][b], np.float32), j),
            "posk": A(storage_order(pos, j)[None, :]),
            "posq": A(posq[None, :]),
            "cst": make_consts1(j),
            "cT": A(inputs['c'][b].reshape(8, 128).T),
            "ridx": ridx,
        })
        mp.update(tabs[j])
        maps.append(mp)
    return maps


def kernel_unfused(**inputs):
    return _kernel_unfused_impl(**inputs)


def kernel(**inputs):
    inputs = {k: np.asarray(v) for k, v in inputs.items()}
    res = _run(build_fused(), fused_inputs(inputs))
    return gather_blocks(res, "out").astype(np.float32)
```

```python
import contextlib
import numpy as np
import concourse.bass as bass
import concourse.mybir as mybir
from concourse.bass_utils import run_bass_kernel_spmd

F32 = mybir.dt.float32
BF16 = mybir.dt.bfloat16
I32 = mybir.dt.int32
AF = mybir.ActivationFunctionType
ALU = mybir.AluOpType
AX = mybir.AxisListType

ENGS = ['pe', 'act', 'dve', 'pool', 'sp']
NEG = -1.0e30
MASKV = -30000.0
TWO_PI = 6.283185


class Buf:
    __slots__ = ('w', 'r', 'name', 'dsem')

    def __init__(self, name=''):
        self.name = name
        self.w = None
        self.r = []
        self.dsem = None


class Prog:
    def __init__(self, nc, same_engine_sync=True):
        self.nc = nc
        self.ops = {e: [] for e in ENGS}
        self.cnt = {e: 0 for e in ENGS}
        self.known = {e: {} for e in ENGS}
        self.ndsem = 0
        self.dsem_val = {}
        self.same_engine_sync = same_engine_sync

    def _deps(self, eng, reads, writes):
        toks = []
        for b in reads:
            if b.w is not None:
                toks.append(b.w)
        for b in writes:
            if b.w is not None:
                toks.append(b.w)
            toks.extend(b.r)
        need = {}
        for (k, v) in toks:
            if k == eng and (eng == 'pe' or not self.same_engine_sync):
                continue
            if self.known[eng].get(k, 0) >= v:
                continue
            if need.get(k, 0) < v:
                need[k] = v
        for k, v in need.items():
            self.known[eng][k] = v
        return list(need.items())

    def _commit(self, tok, reads, writes):
        for b in reads:
            b.r.append(tok)
        for b in writes:
            b.w = tok
            b.r = []

    def op(self, eng, fn, reads=(), writes=()):
        waits = self._deps(eng, reads, writes)
        self.cnt[eng] += 1
        tok = (eng, self.cnt[eng])
        self.ops[eng].append((waits, fn, (eng, 1)))
        self._commit(tok, reads, writes)
        return tok

    def dma(self, q, items, reads=(), writes=(), sem_buf=None, fns=None, inc=16, **kw):
        sb = sem_buf if sem_buf is not None else writes[0]
        if sb.dsem is None:
            sb.dsem = ('d', self.ndsem)
            self.dsem_val[sb.dsem] = 0
            self.ndsem += 1
        key = sb.dsem
        waits = self._deps(q, reads, writes)
        if fns is None:
            fns = []
            for (o, a) in items:
                def fn(e, o=o, a=a):
                    return e.dma_start(out=o, in_=a, **kw)
                fns.append(fn)
        for i, fn in enumerate(fns):
            self.dsem_val[key] += inc
            self.ops[q].append((waits if i == 0 else [], fn, (key, inc)))
        tok = (key, self.dsem_val[key])
        self._commit(tok, reads, writes)
        return tok

    def barrier(self):
        for e in ENGS:
            waits = []
            for k in ENGS:
                if k != e and self.cnt[k] > self.known[e].get(k, 0):
                    waits.append((k, self.cnt[k]))
                    self.known[e][k] = self.cnt[k]
            for k, v in self.dsem_val.items():
                if v > self.known[e].get(k, 0):
                    waits.append((k, v))
                    self.known[e][k] = v
            if e != 'pe' and self.cnt[e] > self.known[e].get(e, 0):
                waits.append((e, self.cnt[e]))
                self.known[e][e] = self.cnt[e]
            self.ops[e].append((waits, None, None))

    def emit(self):
        nc = self.nc
        st = self.st
        if not hasattr(self, 'sems'):
            self.sems = {}
        sems = self.sems
        for e in ENGS:
            if e not in sems:
                sems[e] = st.enter_context(nc.semaphore('s_' + e))
        for i in range(self.ndsem):
            if ('d', i) not in sems:
                sems[('d', i)] = st.enter_context(nc.semaphore('d_%d' % i))
        with nc.Block() as block:
            def replay(ename):
                def run(e):
                    for (waits, fn, inc) in self.ops[ename]:
                        for (k, v) in waits:
                            e.wait_ge(sems[k], v)
                        if fn is not None:
                            ins = fn(e)
                            ins.then_inc(sems[inc[0]], inc[1])
                return run
            block.tensor(replay('pe'))
            block.scalar(replay('act'))
            block.vector(replay('dve'))
            block.gpsimd(replay('pool'))
            block.sync(replay('sp'))
        self.ops = {e: [] for e in ENGS}


class Arena:
    def __init__(self, t, n):
        self.t = t
        self.n = n
        self.off = 0

    def alloc(self, ncols):
        o = self.off
        self.off += ncols
        assert self.off <= self.n, (self.off, self.n)
        return self.t[:, o:o + ncols]

    def mark(self):
        return self.off

    def reset(self, m=0):
        self.off = m


class Ctx:
    pass


C_IDENT = 0
C_TRI = 128
C_PAD = 256
C_INV = 384
C_SSC = 385
C_CSC = 386
C_ONES = 387
NCST = 392


def make_consts(j):
    c = np.zeros((128, NCST), np.float32)
    c[:, C_IDENT:C_IDENT + 128] = np.eye(128, dtype=np.float32)
    q = np.arange(128)[:, None]
    k = np.arange(128)[None, :]
    c[:, C_TRI:C_TRI + 128] = np.where(k <= q, 0.0, NEG)
    c[:, C_PAD:C_PAD + 128] = NEG if j == 0 else 0.0
    inv = 1.0 / (10000.0 ** (np.arange(0, 64, 2, dtype=np.float32) / np.float32(64)))
    inv = inv.astype(np.float32)
    c[0:64, C_INV] = np.concatenate([inv, inv])
    c[0:32, C_SSC] = -TWO_PI
    c[32:64, C_SSC] = TWO_PI
    c[:, C_CSC] = TWO_PI
    c[:, C_ONES] = 1.0
    return c


NBIG = 52600


def setup_ctx(nc):
    X = Ctx()
    X.nc = nc
    X.st = contextlib.ExitStack()
    X.big = X.st.enter_context(nc.sbuf_tensor("big", [128, NBIG], F32))

    def carve(n32):
        X.a32 = Arena(X.big[:, 0:n32], n32)
        X.a16 = Arena(X.big[:, n32:NBIG].bitcast(BF16), 2 * (NBIG - n32))
    X.carve = carve
    X.ai = X.st.enter_context(nc.sbuf_tensor("ai32", [128, 512], I32))
    X.ps = [X.st.enter_context(nc.psum_tensor("ps%d" % i, [128, 512], F32)) for i in range(8)]
    X.psb = [Buf('ps%d' % i) for i in range(8)]
    X.P = Prog(nc)
    X.P.st = X.st
    return X


def load_consts(X, cst_dram):
    P = X.P
    X.cst = X.a32.alloc(cst_dram.shape[1])
    X.Bcst = Buf('cst')
    P.dma('sp', [(X.cst, cst_dram[:, :])], writes=[X.Bcst])
    X.ident = X.cst[:, C_IDENT:C_IDENT + 128]
    X.irep = X.a16.alloc(512)
    X.Birep = Buf('irep')
    for r in range(4):
        P.op('dve', lambda e, r=r: e.tensor_copy(out=X.irep[:, r * 128:(r + 1) * 128], in_=X.ident),
             reads=[X.Bcst], writes=[X.Birep])


def rope_tables(X, pos_i_dram_row, n, Ct, St, Bt, tmp32, tmpi, Btmp):
    P = X.P
    cst = X.cst
    pi_ = tmpi[0:64, 0:n]
    y = tmp32[0:64, 0:n]
    f = tmp32[0:64, n:2 * n]
    g = tmp32[0:64, 2 * n:3 * n]
    P.dma('sp', [(pi_, pos_i_dram_row.partition_broadcast(64))], writes=[Btmp])
    P.op('dve', lambda e: e.tensor_copy(out=y, in_=pi_), reads=[Btmp], writes=[Btmp])
    P.op('dve', lambda e: e.tensor_scalar(out=y, in0=y, scalar1=cst[0:64, C_INV:C_INV + 1], scalar2=float(1.0 / (2 * np.pi)),
                                           op0=ALU.mult, op1=ALU.mult), reads=[Btmp, X.Bcst], writes=[Btmp])

    def frac_to(dst, src, addc):
        if addc != 0.0:
            P.op('dve', lambda e: e.tensor_scalar_add(out=dst, in0=src, scalar1=addc), reads=[Btmp], writes=[Btmp])
            s2 = dst
        else:
            s2 = src
        P.op('dve', lambda e: e.tensor_copy(out=pi_, in_=s2), reads=[Btmp], writes=[Btmp])
        P.op('dve', lambda e: e.tensor_copy(out=g, in_=pi_), reads=[Btmp], writes=[Btmp])
        P.op('dve', lambda e: e.tensor_tensor(out=dst, in0=s2, in1=g, op=ALU.subtract), reads=[Btmp], writes=[Btmp])
        P.op('dve', lambda e: e.tensor_single_scalar(out=g, in_=dst, scalar=0.5, op=ALU.is_gt), reads=[Btmp], writes=[Btmp])
        P.op('dve', lambda e: e.tensor_tensor(out=dst, in0=dst, in1=g, op=ALU.subtract), reads=[Btmp], writes=[Btmp])
        P.op('dve', lambda e: e.tensor_single_scalar(out=g, in_=dst, scalar=-0.5, op=ALU.is_lt), reads=[Btmp], writes=[Btmp])
        P.op('dve', lambda e: e.tensor_tensor(out=dst, in0=dst, in1=g, op=ALU.add), reads=[Btmp], writes=[Btmp])

    frac_to(f, y, 0.0)
    P.op('act', lambda e: e.activation(out=St, in_=f, func=AF.Sin, scale=cst[0:64, C_SSC:C_SSC + 1]),
         reads=[Btmp, X.Bcst], writes=[Bt])
    frac_to(f, y, 0.25)
    P.op('act', lambda e: e.activation(out=Ct, in_=f, func=AF.Sin, scale=cst[0:64, C_CSC:C_CSC + 1]),
         reads=[Btmp, X.Bcst], writes=[Bt])


def mod_vectors(X, cT_dram, w_ada_dram, b_adaT_dram, ncols, wbuf, Bw, psum_idx, cact, modT, bT):
    P = X.P
    nj = ncols // 128
    Bc = Buf('cact')
    P.dma('sp', [(cact, cT_dram[:, :])], writes=[Bc])
    P.op('act', lambda e: e.activation(out=cact, in_=cact, func=AF.Silu), reads=[Bc], writes=[Bc])
    Bm = Buf('modT')
    P.dma('sp', [(bT, b_adaT_dram[:, :])], writes=[Bm])
    ps = X.ps[psum_idx]
    Bps = X.psb[psum_idx]
    ngrp = ncols // 512
    for jg in range(ngrp):
        s = jg % 2
        w3 = wbuf[s].rearrange("p (k n) -> p k n", k=8)
        P.dma('sp', [(w3, w_ada_dram[:, jg * 512:(jg + 1) * 512].rearrange("(k p) n -> p k n", p=128))], writes=[Bw[s]])
        for jc in range(4):
            J = jg * 4 + jc
            for k in range(8):
                P.op('pe', lambda e, J=J, k=k, jc=jc, w3=w3: e.matmul(ps[:, J:J + 1], lhsT=w3[:, k, jc * 128:(jc + 1) * 128],
                                                                      rhs=cact[:, k:k + 1], start=(k == 0), stop=(k == 7)),
                     reads=[Bw[s], Bc], writes=[Bps])
    P.op('dve', lambda e: e.tensor_tensor(out=modT, in0=ps[:, 0:nj], in1=bT, op=ALU.add), reads=[Bps, Bm], writes=[Bm])
    return modT, Bm, cact, Bc


def bcast_row_vec(X, cact, Bc, w_dram_cols, b_dram_row, out_bc, Bout, wbuf, Bw, psA, psB):
    P = X.P
    crep = X.a32.alloc(8 * 128)
    Bcr = Buf('crep')
    crep3 = crep.rearrange("p (k n) -> p k n", k=8)
    for k in range(8):
        P.op('dve', lambda e, k=k: e.tensor_copy(out=crep3[:, k, :], in_=cact[:, k:k + 1].to_broadcast([128, 128])),
             reads=[Bc], writes=[Bcr])
    P.dma('sp', [(out_bc, b_dram_row.partition_broadcast(128))], writes=[Bout])
    for half in range(2):
        s = half % 2
        w3 = wbuf[s].rearrange("p (k n) -> p k n", k=8)
        P.dma('sp', [(w3, w_dram_cols[:, half * 512:(half + 1) * 512].rearrange("(k p) n -> p k n", p=128))], writes=[Bw[s]])
        pi = psA if half == 0 else psB
        for k in range(8):
            P.op('pe', lambda e, k=k, w3=w3, pi=pi: e.matmul(X.ps[pi][:, :], lhsT=crep3[:, k, :], rhs=w3[:, k, :],
                                                            start=(k == 0), stop=(k == 7)),
                 reads=[Bw[s], Bcr], writes=[X.psb[pi]])
        P.op('dve', lambda e, half=half, pi=pi: e.tensor_tensor(out=out_bc[:, half * 512:(half + 1) * 512], in0=X.ps[pi][:, :],
                                                                in1=out_bc[:, half * 512:(half + 1) * 512], op=ALU.add),
             reads=[X.psb[pi], Bout], writes=[Bout])


def norm_modT(X, xt, Bx, G1, SH, Bmod, uT_dst, Buo, xn, Bxn, ss, psA, psB, junk, u32_dst=None, Bu32=None):
    P = X.P
    P.op('act', lambda e: e.activation(out=junk, in_=xt, func=AF.Square, accum_out=ss[:, 0:1]), reads=[Bx], writes=[Bxn])
    P.op('act', lambda e: e.activation(out=ss[:, 1:2], in_=ss[:, 0:1], func=AF.Sqrt, scale=1.0 / 1024.0, bias=X.eps_col),
         reads=[Bxn, X.Bcst2], writes=[Bxn])
    P.op('dve', lambda e: e.reciprocal(out=ss[:, 2:3], in_=ss[:, 1:2]), reads=[Bxn], writes=[Bxn])
    P.op('dve', lambda e: e.tensor_scalar(out=xn, in0=xt, scalar1=ss[:, 2:3], scalar2=None, op0=ALU.mult),
         reads=[Bx, Bxn], writes=[Bxn])
    for k in range(8):
        pi = psA if k < 4 else psB
        P.op('pe', lambda e, k=k, pi=pi: e.transpose(out=X.ps[pi][:, (k % 4) * 128:(k % 4 + 1) * 128],
                                                     in_=xn[:, k * 128:(k + 1) * 128], identity=X.ident),
             reads=[Bxn, X.Bcst], writes=[X.psb[pi]])
    for k in range(8):
        pi = psA if k < 4 else psB
        P.op('act', lambda e, k=k, pi=pi: e.activation(out=uT_dst(k), in_=X.ps[pi][:, (k % 4) * 128:(k % 4 + 1) * 128],
                                                       func=AF.Identity, scale=G1[:, k:k + 1], bias=SH[:, k:k + 1]),
             reads=[X.psb[pi], Bmod], writes=[Buo])
        if u32_dst is not None:
            P.op('act', lambda e, k=k, pi=pi: e.activation(out=u32_dst(k), in_=X.ps[pi][:, (k % 4) * 128:(k % 4 + 1) * 128],
                                                           func=AF.Identity, scale=G1[:, k:k + 1], bias=SH[:, k:k + 1]),
                 reads=[X.psb[pi], Bmod], writes=[Bu32])


def load_w_bf16(X, dst3, w_dram_cols, Bw, nsplit=1):
    src = w_dram_cols.rearrange("(k p) n -> p k n", p=128)
    items = []
    for s in range(nsplit):
        k0 = s * 8 // nsplit
        k1 = (s + 1) * 8 // nsplit
        items.append((dst3[:, k0:k1, :], src[:, k0:k1, :]))
    X.P.dma('pool', items, writes=[Bw])


NQB = 16
NKC = 32
NBIS = 26


def phase_att0(X, T, nqb=NQB, nbis=NBIS):
    xk = T['xk']
    posk = T['posk']
    cst_d = T['cst_d']
    cT = T['cT']
    w_ada = T['w_ada']
    b_adaT = T['b_adaT']
    b_gate = T['b_gate']
    gainT = T['gainT']
    w_in = T['w_in']
    w_out = T['w_out']
    hmid = T['hmid']
    X.carve(15750)
    P = X.P
    a32, a16 = X.a32, X.a16
    if True:
        load_consts(X, cst_d)
        X.eps_col = a32.alloc(1)
        X.Bcst2 = Buf('cst2')
        P.op('pool', lambda e: e.memset(X.eps_col, 1e-6), writes=[X.Bcst2])

        gate_bc = a32.alloc(1024)
        Bgate = Buf('gate')
        G1 = a32.alloc(8)
        gT = a32.alloc(8)
        cact = a32.alloc(8)
        modT = a32.alloc(16)
        bT = a32.alloc(16)
        m32 = a32.mark()
        wbuf = [a32.alloc(8 * 512), a32.alloc(8 * 512)]
        Bw = [Buf('wa0'), Buf('wa1')]
        modT, Bmod, cact, Bc = mod_vectors(X, cT, w_ada[:, 0:2048], b_adaT, 2048, wbuf, Bw, 0, cact, modT, bT)
        bcast_row_vec(X, cact, Bc, w_ada[:, 2048:3072], b_gate, gate_bc, Bgate, wbuf, Bw, 1, 2)
        P.dma('sp', [(gT, gainT[:, :])], writes=[Bmod])
        P.op('dve', lambda e: e.scalar_tensor_tensor(out=G1, in0=modT[:, 8:16], scalar=1.0, in1=gT, op0=ALU.add, op1=ALU.mult),
             reads=[Bmod], writes=[Bmod])
        SH = modT[:, 0:8]
        P.barrier()
        a32.reset(m32)

        KT = a16.alloc(4 * NKC * 128).rearrange("p (g t) -> p g t", g=4)
        IKT = a16.alloc(NKC * 128)
        VAf = a16.alloc(NKC * 260)
        VA = VAf.rearrange("p (c n) -> p c n", c=NKC)
        BKV = Buf('kv')
        P.op('pool', lambda e: e.memset(VAf, 1.0), writes=[BKV])
        m16 = a16.mark()

        WA = a16.alloc(8 * 576).rearrange("p (k n) -> p k n", k=8)
        WAp = a16.alloc(8 * 320).rearrange("p (k n) -> p k n", k=8)
        BW = Buf('WA')
        BWp = Buf('WAp')
        w_in3 = w_in.rearrange("(k p) n -> p k n", p=128)
        P.dma('pool', [(WA[:, :, 0:512], w_in3[:, :, 1024:1536]), (WA[:, :, 512:576], w_in3[:, :, 2048:2112])], writes=[BW])

        def perm_copy(Wsrc, Wdst, pairs, Bs, Bd):
            for (s0, d0, n) in pairs:
                nh = n // 64
                for k in range(8):
                    src = Wsrc[:, k, s0:s0 + n].rearrange("p (h t i) -> p h t i", h=nh, t=2)
                    dst = Wdst[:, k, d0:d0 + n].rearrange("p (h t i) -> p h t i", h=nh, t=2)
                    eng = 'pool' if k % 2 == 0 else 'dve'
                    P.op(eng, lambda e, src=src, dst=dst: e.tensor_copy(out=dst[:, :, 0, :], in_=src[:, :, 1, :]), reads=[Bs], writes=[Bd])
                    P.op(eng, lambda e, src=src, dst=dst: e.tensor_copy(out=dst[:, :, 1, :], in_=src[:, :, 0, :]), reads=[Bs], writes=[Bd])
        perm_copy(WA, WAp, [(0, 0, 256), (512, 256, 64)], BW, BWp)
        uT = a16.alloc(8 * 512).rearrange("p (k t) -> p k t", k=8)
        BuT = Buf('uT')

        xt = [a32.alloc(1024), a32.alloc(1024)]
        Bxt = [Buf('xt0'), Buf('xt1')]
        xn = a32.alloc(1024)
        Bxn = Buf('xn')
        ss = a32.alloc(4)
        junk = xn
        Ct = a32.alloc(512)
        St = a32.alloc(512)
        Btab = Buf('tab')
        tmp32 = a32.alloc(3 * 512)
        Btmp = Buf('ttmp')
        r1 = a32.alloc(512)
        r2 = a32.alloc(512)
        Br = Buf('ropetmp')

        def rope_combine(psA_i, psB_i, n, nh, dst):
            A = X.ps[psA_i][0:64, 0:nh * n].rearrange("p (h t) -> p h t", h=nh)
            B = X.ps[psB_i][0:64, 0:nh * n].rearrange("p (h t) -> p h t", h=nh)
            c_b = Ct[0:64, 0:n].unsqueeze(1).to_broadcast([64, nh, n])
            s_b = St[0:64, 0:n].unsqueeze(1).to_broadcast([64, nh, n])
            t1 = r1[0:64, 0:nh * n].rearrange("p (h t) -> p h t", h=nh)
            t2 = r2[0:64, 0:nh * n].rearrange("p (h t) -> p h t", h=nh)
            P.op('dve', lambda e: e.tensor_tensor(out=t1, in0=A, in1=c_b, op=ALU.mult), reads=[X.psb[psA_i], Btab], writes=[Br])
            P.op('dve', lambda e: e.tensor_tensor(out=t2, in0=B, in1=s_b, op=ALU.mult), reads=[X.psb[psB_i], Btab], writes=[Br])
            return t1, t2

        for grp in range(NKC // 4):
            t0 = grp * 512
            rope_tables(X, posk[:, t0:t0 + 512], 512, Ct[0:64, :], St[0:64, :], Btab, tmp32, X.ai, Btmp)
            for cc in range(4):
                ch = grp * 4 + cc
                s = ch % 2
                P.dma('sp', [(xt[s], xk[ch * 128:(ch + 1) * 128, :])], writes=[Bxt[s]])
                norm_modT(X, xt[s], Bxt[s], G1, SH, Bmod, lambda k, cc=cc: uT[:, k, cc * 128:(cc + 1) * 128], BuT,
                          xn, Bxn, ss, 0, 1, junk)
                for k in range(8):
                    P.op('pe', lambda e, k=k, cc=cc: e.matmul(X.ps[2][:, 0:256], lhsT=uT[:, k, cc * 128:(cc + 1) * 128],
                                                              rhs=WA[:, k, 256:512], start=(k == 0), stop=(k == 7)),
                         reads=[BuT, BW], writes=[X.psb[2]])
                P.op('act', lambda e, ch=ch: e.copy(out=VA[:, ch, :].rearrange("p (g d) -> p g d", g=4)[:, :, 0:64],
                                                    in_=X.ps[2][:, 0:256].rearrange("p (g d) -> p g d", g=4)),
                     reads=[X.psb[2]], writes=[BKV])
            for (kind, c0, c0p, nh) in [('k', 0, 0, 4), ('ik', 512, 256, 1)]:
                for hh in range(nh):
                    for (W_, pi, cb_) in [(WA, 3, c0), (WAp, 4, c0p)]:
                        for k in range(8):
                            P.op('pe', lambda e, k=k, W_=W_, pi=pi, cbase=cb_ + hh * 64: e.matmul(
                                X.ps[pi][0:64, :], lhsT=W_[:, k, cbase:cbase + 64], rhs=uT[:, k, :],
                                start=(k == 0), stop=(k == 7)),
                                reads=[BuT, BW, BWp], writes=[X.psb[pi]])
                    t1, t2 = rope_combine(3, 4, 512, 1, None)
                    if kind == 'k':
                        dst = KT[0:64, hh, t0:t0 + 512]
                    else:
                        dst = IKT[0:64, t0:t0 + 512]
                    P.op('pool', lambda e, dst=dst, t1=t1, t2=t2: e.tensor_tensor(out=dst, in0=t1[:, 0, :], in1=t2[:, 0, :], op=ALU.add),
                         reads=[Br], writes=[BKV])
        P.barrier()

        a16.reset(m16)
        WB = a16.alloc(8 * 1544).rearrange("p (k n) -> p k n", k=8)
        WBp = a16.alloc(8 * 1536).rearrange("p (k n) -> p k n", k=8)
        BW = Buf('WB')
        BWp = Buf('WBp')
        P.dma('pool', [(WB[:, 0:4, 0:1024], w_in3[:, 0:4, 0:1024]), (WB[:, 4:8, 0:1024], w_in3[:, 4:8, 0:1024]),
                       (WB[:, :, 1024:1536], w_in3[:, :, 1536:2048]), (WB[:, :, 1536:1544], w_in3[:, :, 2112:2120])], writes=[BW])
        perm_copy(WB, WBp, [(0, 0, 1024), (1024, 1024, 512)], BW, BWp)
        Wo = a16.alloc(8 * 1024).rearrange("p (k n) -> p k n", k=8)
        BWo = Buf('Wout')
        load_w_bf16(X, Wo, w_out, BWo, nsplit=2)
        QT = a16.alloc(16 * 128).rearrange("p (h t) -> p h t", h=16)
        IQT = a16.alloc(8 * 128).rearrange("p (h t) -> p h t", h=8)
        BQ = Buf('QT')
        iw = a32.alloc(24)
        Biw = Buf('iw')
        Isc = a32.alloc(NKC * 128)
        BI = Buf('I')
        rl = [a32.alloc(512), a32.alloc(512)]
        Brl = [Buf('rl0'), Buf('rl1')]
        bs = a32.alloc(16)
        Bbs = Buf('bs')
        Mb = a16.alloc(NKC * 128)
        cjunk = Mb
        BMb = Buf('Mb')
        PT = [a16.alloc(512), a16.alloc(512)]
        BPT = [Buf('pt0'), Buf('pt1')]
        On = a32.alloc(1024)
        BOn = Buf('On')
        rden = a32.alloc(16)
        OnT = a16.alloc(8 * 128).rearrange("p (k t) -> p k t", k=8)
        BOnT = Buf('OnT')
        hm_ = a32.alloc(1024)
        hm = [hm_, hm_]
        Bhm_ = Buf('hm')
        Bhm = [Bhm_, Bhm_]
        Bout = Buf('hmid_out')
        uq = a16.alloc(8 * 128).rearrange("p (k t) -> p k t", k=8)
        Buq = Buf('uq')

        for m in range(nqb):
            sc = 2 * m + 1
            nk = sc + 1
            nkeys = nk * 128
            s = m % 2
            P.dma('sp', [(xt[s], xk[sc * 128:(sc + 1) * 128, :])], writes=[Bxt[s]])
            rope_tables(X, posk[:, sc * 128:(sc + 1) * 128], 128, Ct[0:64, 0:128], St[0:64, 0:128], Btab, tmp32, X.ai, Btmp)
            norm_modT(X, xt[s], Bxt[s], G1, SH, Bmod, lambda k: uq[:, k, :], Buq, xn, Bxn, ss, 0, 1, junk)
            for (c0, nb, dstT) in [(0, 4, QT), (1024, 2, IQT)]:
                for b4 in range(nb):
                    for (W_, pi) in [(WB, 3), (WBp, 4)]:
                        for hh in range(4):
                            cbase = c0 + (b4 * 4 + hh) * 64
                            for k in range(8):
                                P.op('pe', lambda e, k=k, W_=W_, pi=pi, cbase=cbase, hh=hh: e.matmul(
                                    X.ps[pi][0:64, hh * 128:(hh + 1) * 128], lhsT=W_[:, k, cbase:cbase + 64], rhs=uq[:, k, :],
                                    start=(k == 0), stop=(k == 7)),
                                    reads=[Buq, BW, BWp], writes=[X.psb[pi]])
                    t1, t2 = rope_combine(3, 4, 128, 4, None)
                    P.op('pool', lambda e, dstT=dstT, b4=b4, t1=t1, t2=t2: e.tensor_tensor(
                        out=dstT[0:64, b4 * 4:(b4 + 1) * 4, :], in0=t1, in1=t2, op=ALU.add), reads=[Br], writes=[BQ])
            for k in range(8):
                P.op('pe', lambda e, k=k: e.matmul(X.ps[2][:, 0:8], lhsT=uq[:, k, :], rhs=WB[:, k, 1536:1544],
                                                   start=(k == 0), stop=(k == 7)), reads=[Buq, BW], writes=[X.psb[2]])
            P.op('act', lambda e: e.activation(out=iw[:, 0:8], in_=X.ps[2][:, 0:8], func=AF.Abs),
                 reads=[X.psb[2]], writes=[Biw])
            P.op('dve', lambda e: e.tensor_scalar(out=iw[:, 8:16], in0=X.ps[2][:, 0:8], scalar1=0.0, scalar2=0.5,
                                                   op0=ALU.is_ge, op1=ALU.subtract), reads=[X.psb[2]], writes=[Biw])
            ngr = (nkeys + 511) // 512
            it = 0
            for kg in range(ngr):
                k0 = kg * 512
                wdt = min(512, nkeys - k0)
                for h in range(8):
                    pi = 5 + (it % 2)
                    rs = it % 2
                    it += 1
                    P.op('pe', lambda e, pi=pi, h=h, k0=k0, wdt=wdt: e.matmul(X.ps[pi][:, 0:wdt], lhsT=IQT[0:64, h, :],
                                                                              rhs=IKT[0:64, k0:k0 + wdt], start=True, stop=True),
                         reads=[BQ], writes=[X.psb[pi]])
                    P.op('act', lambda e, pi=pi, h=h, rs=rs, wdt=wdt: e.activation(out=rl[rs][:, 0:wdt], in_=X.ps[pi][:, 0:wdt],
                                                                                   func=AF.Relu, scale=iw[:, h:h + 1]),
                         reads=[X.psb[pi], Biw], writes=[Brl[rs]])
                    if h == 0:
                        P.op('dve', lambda e, rs=rs, k0=k0, wdt=wdt: e.tensor_scalar(
                            out=Isc[:, k0:k0 + wdt], in0=rl[rs][:, 0:wdt], scalar1=iw[:, 8:9], scalar2=None, op0=ALU.mult),
                            reads=[Brl[rs], Biw], writes=[BI])
                    else:
                        P.op('dve', lambda e, rs=rs, k0=k0, wdt=wdt, h=h: e.scalar_tensor_tensor(
                            out=Isc[:, k0:k0 + wdt], in0=rl[rs][:, 0:wdt], scalar=iw[:, 8 + h:9 + h], in1=Isc[:, k0:k0 + wdt],
                            op0=ALU.mult, op1=ALU.add), reads=[Brl[rs], Biw, BI], writes=[BI])
            Iv = Isc[:, 0:nkeys]
            if m >= 1:
                P.op('dve', lambda e, Iv=Iv: e.tensor_reduce(out=bs[:, 0:1], in_=Iv, axis=AX.X, op=ALU.max), reads=[BI], writes=[Bbs])
                P.op('dve', lambda e, Iv=Iv: e.tensor_reduce(out=bs[:, 1:2], in_=Iv, axis=AX.X, op=ALU.min), reads=[BI], writes=[Bbs])
            P.op('dve', lambda e, sc=sc: e.tensor_tensor(out=Isc[:, sc * 128:(sc + 1) * 128], in0=Isc[:, sc * 128:(sc + 1) * 128],
                                                         in1=X.cst[:, C_TRI:C_TRI + 128], op=ALU.add), reads=[BI, X.Bcst], writes=[BI])
            P.op('dve', lambda e: e.tensor_tensor(out=Isc[:, 0:128], in0=Isc[:, 0:128], in1=X.cst[:, C_PAD:C_PAD + 128], op=ALU.add),
                 reads=[BI, X.Bcst], writes=[BI])
            if m >= 1:
                P.op('dve', lambda e: e.tensor_tensor(out=bs[:, 2:3], in0=bs[:, 0:1], in1=bs[:, 1:2], op=ALU.subtract), reads=[Bbs], writes=[Bbs])
                P.op('dve', lambda e: e.tensor_copy(out=bs[:, 3:4], in_=bs[:, 1:2]), reads=[Bbs], writes=[Bbs])
                for it_b in range(1, nbis + 1):
                    ck = float(2.0 ** (-it_b))
                    P.op('dve', lambda e, ck=ck: e.scalar_tensor_tensor(out=bs[:, 4:5], in0=bs[:, 2:3], scalar=ck, in1=bs[:, 3:4],
                                                                        op0=ALU.mult, op1=ALU.add), reads=[Bbs], writes=[Bbs])
                    P.op('dve', lambda e, Iv=Iv, nkeys=nkeys: e.tensor_scalar(out=cjunk[:, 0:nkeys], in0=Iv, scalar1=bs[:, 4:5], scalar2=None,
                                                                              op0=ALU.is_ge, op1=ALU.add, accum_out=bs[:, 5:6]),
                         reads=[Bbs, BI], writes=[Bbs])
                    P.op('dve', lambda e: e.tensor_scalar(out=bs[:, 6:7], in0=bs[:, 5:6], scalar1=255.5, scalar2=bs[:, 2:3],
                                                           op0=ALU.is_ge, op1=ALU.mult), reads=[Bbs], writes=[Bbs])
                    P.op('dve', lambda e, ck=ck: e.scalar_tensor_tensor(out=bs[:, 3:4], in0=bs[:, 6:7], scalar=ck, in1=bs[:, 3:4],
                                                                        op0=ALU.mult, op1=ALU.add), reads=[Bbs], writes=[Bbs])
            else:
                P.op('dve', lambda e: e.memset(bs[:, 3:4], -1.0e29), writes=[Bbs])
            P.op('dve', lambda e, Iv=Iv, nkeys=nkeys: e.tensor_scalar(out=Mb[:, 0:nkeys], in0=Iv, scalar1=bs[:, 3:4], scalar2=MASKV,
                                                                      op0=ALU.is_lt, op1=ALU.mult), reads=[BI, Bbs], writes=[BMb])
            items = [(g, c) for g in range(4) for c in range(nk)]

            def emit_S(i):
                g, c = items[i]
                pi = 5 + (i % 2)
                P.op('pe', lambda e, pi=pi, g=g, c=c: e.matmul(X.ps[pi][:, :], lhsT=KT[0:64, g, c * 128:(c + 1) * 128],
                                                               rhs=QT[0:64, g * 4:(g + 1) * 4, :], start=True, stop=False),
                     reads=[BQ], writes=[X.psb[pi]])
                P.op('pe', lambda e, pi=pi, c=c: e.matmul(X.ps[pi][:, :], lhsT=Mb[:, c * 128:(c + 1) * 128], rhs=X.irep,
                                                          start=False, stop=True),
                     reads=[BMb, X.Birep], writes=[X.psb[pi]])
            emit_S(0)
            for i, (g, c) in enumerate(items):
                if i + 1 < len(items):
                    emit_S(i + 1)
                pi = 5 + (i % 2)
                ps_ = i % 2
                ob = 2 if g % 2 == 0 else 7
                P.op('act', lambda e, pi=pi, ps_=ps_: e.activation(out=PT[ps_], in_=X.ps[pi][:, :], func=AF.Exp, scale=0.125),
                     reads=[X.psb[pi]], writes=[BPT[ps_]])
                for r in range(4):
                    P.op('pe', lambda e, r=r, ps_=ps_, c=c, g=g, ob=ob: e.matmul(X.ps[ob][:, r * 65:(r + 1) * 65],
                                                                                 lhsT=PT[ps_][:, r * 128:(r + 1) * 128],
                                                                                 rhs=VA[:, c, g * 65:(g + 1) * 65],
                                                                                 start=(c == 0 and r == 0), stop=(c == nk - 1),
                                                                                 skip_group_check=True),
                         reads=[BPT[ps_]], writes=[X.psb[ob]])
                if c == nk - 1:
                    O3 = X.ps[ob][:, 0:260].rearrange("p (r d) -> p r d", r=4)
                    P.op('dve', lambda e, O3=O3, g=g: e.reciprocal(out=rden[:, g * 4:(g + 1) * 4], in_=O3[:, :, 64]), reads=[X.psb[ob]], writes=[BOn])
                    P.op('dve', lambda e, O3=O3, g=g: e.tensor_tensor(
                        out=On[:, g * 256:(g + 1) * 256].rearrange("p (r d) -> p r d", r=4), in0=O3[:, :, 0:64],
                        in1=rden[:, g * 4:(g + 1) * 4].unsqueeze(2).to_broadcast([128, 4, 64]), op=ALU.mult),
                        reads=[X.psb[ob], BOn], writes=[BOn])
            for k in range(8):
                pi = 0 if k < 4 else 1
                P.op('pe', lambda e, k=k, pi=pi: e.transpose(out=X.ps[pi][:, (k % 4) * 128:(k % 4 + 1) * 128],
                                                             in_=On[:, k * 128:(k + 1) * 128], identity=X.ident),
                     reads=[BOn, X.Bcst], writes=[X.psb[pi]])
            for half in range(2):
                P.op('act', lambda e, half=half: e.copy(out=OnT[:, half * 4:(half + 1) * 4, :],
                                                        in_=X.ps[half][:, :].rearrange("p (k t) -> p k t", k=4)),
                     reads=[X.psb[half]], writes=[BOnT])
            for half in range(2):
                pi = 3 + half
                for k in range(8):
                    P.op('pe', lambda e, k=k, pi=pi, half=half: e.matmul(X.ps[pi][:, :], lhsT=OnT[:, k, :],
                                                                         rhs=Wo[:, k, half * 512:(half + 1) * 512],
                                                                         start=(k == 0), stop=(k == 7)),
                         reads=[BOnT, BWo], writes=[X.psb[pi]])
                P.op('dve', lambda e, pi=pi, half=half, s=s: e.tensor_tensor(out=hm[s][:, half * 512:(half + 1) * 512], in0=X.ps[pi][:, :],
                                                                             in1=gate_bc[:, half * 512:(half + 1) * 512], op=ALU.mult),
                     reads=[X.psb[pi], Bgate], writes=[Bhm[s]])
            P.op('pool', lambda e, s=s: e.tensor_tensor(out=hm[s], in0=hm[s], in1=xt[s], op=ALU.add), reads=[Bhm[s], Bxt[s]], writes=[Bhm[s]])
            P.dma('sp', [(hmid[m * 128:(m + 1) * 128, :], hm[s])], reads=[Bhm[s]], writes=[Buf('o')], sem_buf=Bhm[s])
        P.barrier()


def build_att0(nqb=NQB, nbis=NBIS):
    nc = bass.Bass("TRN2", target_bir_lowering=False)

    def din(name, shape, dt=F32):
        return nc.dram_tensor(name, shape, dt, kind="ExternalInput").ap()
    xk = din("xk", [NKC * 128, 1024])
    posk = din("posk", [1, NKC * 128], I32)
    cst_d = din("cst", [128, NCST])
    cT = din("cT", [128, 8])
    w_ada = din("w_ada", [1024, 3072])
    b_adaT = din("b_adaT", [128, 16])
    b_gate = din("b_gate", [1, 1024])
    gainT = din("gainT", [128, 8])
    w_in = din("w_in", [1024, 2120])
    w_out = din("w_out", [1024, 1024])
    hmid = nc.dram_tensor("hmid", [nqb * 128, 1024], F32, kind="ExternalOutput").ap()

    T = dict(xk=xk, posk=posk, cst_d=cst_d, cT=cT, w_ada=w_ada, b_adaT=b_adaT, b_gate=b_gate, gainT=gainT, w_in=w_in, w_out=w_out, hmid=hmid)
    X = setup_ctx(nc)
    with X.st:
        phase_att0(X, T, nqb, nbis)
        X.P.emit()
    return nc


def att0_inputs(inputs, layer=0):
    x = np.asarray(inputs['x'], np.float32)
    pos = np.asarray(inputs['positions'], np.int32)
    maps = []
    for c in range(8):
        b, j = c // 2, c % 2
        if j == 0:
            xk = np.concatenate([np.zeros((128, 1024), np.float32), x[b, 0:31 * 128]], axis=0)
            pk = np.concatenate([np.zeros((128,), np.int32), pos[b, 0:31 * 128]])
        else:
            xk = x[b]
            pk = pos[b]
        maps.append({
            "xk": np.ascontiguousarray(xk), "posk": np.ascontiguousarray(pk[None, :]),
            "cst": make_consts(j),
            "cT": np.ascontiguousarray(inputs['c'][b].reshape(8, 128).T),
            "w_ada": np.ascontiguousarray(inputs['w_ada'][layer][:, 0:3072]),
            "b_adaT": np.ascontiguousarray(inputs['b_ada'][layer][0:2048].reshape(16, 128).T),
            "b_gate": np.ascontiguousarray(inputs['b_ada'][layer][2048:3072][None, :]),
            "gainT": np.ascontiguousarray(inputs['attn_gain'][layer].reshape(8, 128).T),
            "w_in": np.ascontiguousarray(inputs['a_w_in'][0]),
            "w_out": np.ascontiguousarray(inputs['a_w_out'][0]),
        })
    return maps


def gather_blocks(res, key, nqb=NQB):
    out = np.zeros((4, 4096, 1024), np.float32)
    for c in range(8):
        b, j = c // 2, c % 2
        r = res[c][key]
        for m in range(nqb):
            i = 2 * m + j
            out[b, i * 128:(i + 1) * 128] = r[m * 128:(m + 1) * 128]
    return out


def ffn_phase0(X, cT, w_ada_f, b_adaT, b_gate, gainT):
    P = X.P
    a32 = X.a32
    gate_bc = a32.alloc(1024)
    Bgate = Buf('gate')
    G1 = a32.alloc(8)
    gT = a32.alloc(8)
    cact = a32.alloc(8)
    modT = a32.alloc(16)
    bT = a32.alloc(16)
    m32 = a32.mark()
    wbuf = [a32.alloc(8 * 512), a32.alloc(8 * 512)]
    Bw = [Buf('wa0'), Buf('wa1')]
    modT, Bmod, cact, Bc = mod_vectors(X, cT, w_ada_f[:, 0:2048], b_adaT, 2048, wbuf, Bw, 0, cact, modT, bT)
    bcast_row_vec(X, cact, Bc, w_ada_f[:, 2048:3072], b_gate, gate_bc, Bgate, wbuf, Bw, 1, 2)
    P.dma('sp', [(gT, gainT[:, :])], writes=[Bmod])
    P.op('dve', lambda e: e.scalar_tensor_tensor(out=G1, in0=modT[:, 8:16], scalar=1.0, in1=gT, op0=ALU.add, op1=ALU.mult),
         reads=[Bmod], writes=[Bmod])
    SH = modT[:, 0:8]
    P.barrier()
    a32.reset(m32)
    return G1, SH, Bmod, gate_bc, Bgate


def phase_ffn0(X, T, ntok=2048, dff=2816):
    hin = T['hin']
    cst_d = T['cst_d']
    cT = T['cT']
    w_ada = T['w_ada']
    b_adaT = T['b_adaT']
    b_gate = T['b_gate']
    gainT = T['gainT']
    wg = T['wg']
    wu = T['wu']
    wd = T['wd']
    hout = T['hout']
    NF = dff // 128
    GT = 1024
    ngrp = ntok // GT
    NT = GT // 128
    X.carve(15000)
    P = X.P
    a32, a16 = X.a32, X.a16
    if True:
        load_consts(X, cst_d)
        X.eps_col = a32.alloc(1)
        X.Bcst2 = Buf('cst2')
        P.op('pool', lambda e: e.memset(X.eps_col, 1e-6), writes=[X.Bcst2])
        G1, SH, Bmod, gate_bc, Bgate = ffn_phase0(X, cT, w_ada, b_adaT, b_gate, gainT)

        Wd = a16.alloc(NF * 1024).rearrange("p (f n) -> p f n", f=NF)
        BWd = Buf('Wd')
        wd3 = wd.rearrange("(f p) n -> p f n", p=128)
        nsp = 4
        P.dma('pool', [(Wd[:, (i * NF) // nsp:((i + 1) * NF) // nsp, :], wd3[:, (i * NF) // nsp:((i + 1) * NF) // nsp, :]) for i in range(nsp)],
              writes=[BWd])
        wg3 = wg.rearrange("(k p) n -> p k n", p=128)
        wu3 = wu.rearrange("(k p) n -> p k n", p=128)
        NWB = 3
        Wgb = [a16.alloc(8 * 128).rearrange("p (k n) -> p k n", k=8) for _ in range(NWB)]
        Wub = [a16.alloc(8 * 128).rearrange("p (k n) -> p k n", k=8) for _ in range(NWB)]
        BWg = [Buf('wg%d' % i) for i in range(NWB)]
        u2T = a16.alloc(8 * GT).rearrange("p (k t) -> p k t", k=8)
        Bu2 = Buf('u2T')
        actT = a16.alloc(NF * GT).rearrange("p (f t) -> p f t", f=NF)
        Bact = Buf('actT')
        hres = a32.alloc(NT * 1024).rearrange("p (t n) -> p t n", t=NT)
        Bh = [Buf('h%d' % i) for i in range(NT)]
        xn = a32.alloc(1024)
        Bxn = Buf('xn')
        ss = a32.alloc(4)
        sg = [a32.alloc(512), a32.alloc(512)]
        Bsg = [Buf('sg0'), Buf('sg1')]
        ot = [a32.alloc(1024), a32.alloc(1024)]
        Bot = [Buf('ot0'), Buf('ot1')]

        wi = 0
        for grp in range(ngrp):
            g0 = grp * GT
            for t in range(NT):
                P.dma('sp', [(hres[:, t, :], hin[g0 + t * 128:g0 + (t + 1) * 128, :])], writes=[Bh[t]])
                norm_modT(X, hres[:, t, :], Bh[t], G1, SH, Bmod, lambda k, t=t: u2T[:, k, t * 128:(t + 1) * 128], Bu2,
                          xn, Bxn, ss, 0, 1, xn)
            it = 0
            for f in range(NF):
                wb = wi % NWB
                wi += 1
                P.dma('pool', [(Wgb[wb], wg3[:, :, f * 128:(f + 1) * 128]), (Wub[wb], wu3[:, :, f * 128:(f + 1) * 128])], writes=[BWg[wb]])
                for half in range(GT // 512):
                    pg = 2 + 2 * (it % 2)
                    pu = pg + 1
                    sgi = it % 2
                    it += 1
                    for (W_, pi) in [(Wgb[wb], pg), (Wub[wb], pu)]:
                        for k in range(8):
                            P.op('pe', lambda e, k=k, W_=W_, pi=pi, half=half: e.matmul(X.ps[pi][:, :], lhsT=W_[:, k, :],
                                                                                       rhs=u2T[:, k, half * 512:(half + 1) * 512],
                                                                                       start=(k == 0), stop=(k == 7)),
                                 reads=[BWg[wb], Bu2], writes=[X.psb[pi]])
                    P.op('act', lambda e, pg=pg, sgi=sgi: e.activation(out=sg[sgi], in_=X.ps[pg][:, :], func=AF.Silu),
                         reads=[X.psb[pg]], writes=[Bsg[sgi]])
                    P.op('dve', lambda e, pu=pu, sgi=sgi, f=f, half=half: e.tensor_tensor(out=actT[:, f, half * 512:(half + 1) * 512], in0=X.ps[pu][:, :],
                                                                                        in1=sg[sgi], op=ALU.mult),
                         reads=[X.psb[pu], Bsg[sgi]], writes=[Bact])
            it = 0
            for t in range(NT):
                o = t % 2
                for half in range(2):
                    pi = 6 + (it % 2)
                    it += 1
                    for f in range(NF):
                        P.op('pe', lambda e, f=f, pi=pi, t=t, half=half: e.matmul(X.ps[pi][:, :], lhsT=actT[:, f, t * 128:(t + 1) * 128],
                                                                                 rhs=Wd[:, f, half * 512:(half + 1) * 512],
                                                                                 start=(f == 0), stop=(f == NF - 1)),
                             reads=[Bact, BWd], writes=[X.psb[pi]])
                    P.op('dve', lambda e, pi=pi, o=o, half=half: e.tensor_tensor(out=ot[o][:, half * 512:(half + 1) * 512], in0=X.ps[pi][:, :],
                                                                                in1=gate_bc[:, half * 512:(half + 1) * 512], op=ALU.mult),
                         reads=[X.psb[pi], Bgate], writes=[Bot[o]])
                P.op('pool', lambda e, o=o, t=t: e.tensor_tensor(out=ot[o], in0=ot[o], in1=hres[:, t, :], op=ALU.add),
                     reads=[Bot[o], Bh[t]], writes=[Bot[o]])
                P.dma('sp', [(hout[g0 + t * 128:g0 + (t + 1) * 128, :], ot[o])], reads=[Bot[o]], writes=[Buf('o')], sem_buf=Bot[o])
        P.barrier()


def build_ffn0(ntok=2048, dff=2816):
    nc = bass.Bass("TRN2", target_bir_lowering=False)

    def din(name, shape, dt=F32):
        return nc.dram_tensor(name, shape, dt, kind="ExternalInput").ap()
    hin = din("hin", [ntok, 1024])
    cst_d = din("cst", [128, NCST])
    cT = din("cT", [128, 8])
    w_ada = din("w_ada", [1024, 3072])
    b_adaT = din("b_adaT", [128, 16])
    b_gate = din("b_gate", [1, 1024])
    gainT = din("gainT", [128, 8])
    wg = din("wg", [1024, dff])
    wu = din("wu", [1024, dff])
    wd = din("wd", [dff, 1024])
    hout = nc.dram_tensor("hout", [ntok, 1024], F32, kind="ExternalOutput").ap()
    NF = dff // 128
    GT = 1024
    ngrp = ntok // GT
    NT = GT // 128

    T = dict(hin=hin, cst_d=cst_d, cT=cT, w_ada=w_ada, b_adaT=b_adaT, b_gate=b_gate, gainT=gainT, wg=wg, wu=wu, wd=wd, hout=hout)
    X = setup_ctx(nc)
    with X.st:
        phase_ffn0(X, T, ntok, dff)
        X.P.emit()
    return nc


def ffn0_inputs(inputs, hmid_cores, layer=0):
    maps = []
    for c in range(8):
        b, j = c // 2, c % 2
        maps.append({
            "hin": np.ascontiguousarray(hmid_cores[c]),
            "cst": make_consts(j),
            "cT": np.ascontiguousarray(inputs['c'][b].reshape(8, 128).T),
            "w_ada": np.ascontiguousarray(inputs['w_ada'][layer][:, 3072:6144]),
            "b_adaT": np.ascontiguousarray(inputs['b_ada'][layer][3072:5120].reshape(16, 128).T),
            "b_gate": np.ascontiguousarray(inputs['b_ada'][layer][5120:6144][None, :]),
            "gainT": np.ascontiguousarray(inputs['ffn_gain'][layer].reshape(8, 128).T),
            "wg": np.ascontiguousarray(inputs['ffn_w_gate'][0]),
            "wu": np.ascontiguousarray(inputs['ffn_w_up'][0]),
            "wd": np.ascontiguousarray(inputs['ffn_w_down'][0]),
        })
    return maps


def split_blocks(full, nqb=NQB):
    outs = []
    for c in range(8):
        b, j = c // 2, c % 2
        outs.append(np.concatenate([full[b, (2 * m + j) * 128:(2 * m + j + 1) * 128] for m in range(nqb)], axis=0))
    return outs


NCMP = 255


def phase_kv1(X, T):
    hk = T['hk']
    posk = T['posk']
    cst_d = T['cst_d']
    cT = T['cT']
    w_ada = T['w_ada']
    b_adaT = T['b_adaT']
    gainT = T['gainT']
    w_kv = T['w_kv']
    w1k = T['w1k']
    w1v = T['w1v']
    w2k = T['w2k']
    w2v = T['w2v']
    peTk = T['peTk']
    peTv = T['peTv']
    o_kslcT = T['o_kslcT']
    o_kwinT = T['o_kwinT']
    o_vslc = T['o_vslc']
    o_vwin = T['o_vwin']
    o_kcT = T['o_kcT']
    o_vc = T['o_vc']
    X.carve(14000)
    P = X.P
    a32, a16 = X.a32, X.a16
    if True:
        load_consts(X, cst_d)
        X.eps_col = a32.alloc(1)
        X.Bcst2 = Buf('cst2')
        P.op('pool', lambda e: e.memset(X.eps_col, 1e-6), writes=[X.Bcst2])
        G1 = a32.alloc(8)
        gT = a32.alloc(8)
        cact = a32.alloc(8)
        modT = a32.alloc(16)
        bT = a32.alloc(16)
        m32 = a32.mark()
        wbuf = [a32.alloc(8 * 512), a32.alloc(8 * 512)]
        Bw = [Buf('wa0'), Buf('wa1')]
        modT, Bmod, cact, Bc = mod_vectors(X, cT, w_ada, b_adaT, 2048, wbuf, Bw, 0, cact, modT, bT)
        P.dma('sp', [(gT, gainT[:, :])], writes=[Bmod])
        P.op('dve', lambda e: e.scalar_tensor_tensor(out=G1, in0=modT[:, 8:16], scalar=1.0, in1=gT, op0=ALU.add, op1=ALU.mult),
             reads=[Bmod], writes=[Bmod])
        SH = modT[:, 0:8]
        P.barrier()
        a32.reset(m32)

        W = a16.alloc(8 * 1536).rearrange("p (k n) -> p k n", k=8)
        Wp = a16.alloc(8 * 768).rearrange("p (k n) -> p k n", k=8)
        BW = Buf('W')
        BWp = Buf('Wp')
        load_w_bf16(X, W, w_kv, BW, nsplit=2)
        for (s0, d0) in [(0, 0), (512, 256), (1024, 512)]:
            for k in range(8):
                src = W[:, k, s0:s0 + 256].rearrange("p (h t i) -> p h t i", h=4, t=2)
                dst = Wp[:, k, d0:d0 + 256].rearrange("p (h t i) -> p h t i", h=4, t=2)
                eng = 'pool' if k % 2 == 0 else 'dve'
                P.op(eng, lambda e, src=src, dst=dst: e.tensor_copy(out=dst[:, :, 0, :], in_=src[:, :, 1, :]), reads=[BW], writes=[BWp])
                P.op(eng, lambda e, src=src, dst=dst: e.tensor_copy(out=dst[:, :, 1, :], in_=src[:, :, 0, :]), reads=[BW], writes=[BWp])
        KcT = a16.alloc(4 * NKC * 128).rearrange("p (g t) -> p g t", g=4)
        VcT = a16.alloc(4 * NKC * 128).rearrange("p (g t) -> p g t", g=4)
        Bcmp = Buf('cmpstore')
        xt = [a32.alloc(1024), a32.alloc(1024)]
        Bxt = [Buf('xt0'), Buf('xt1')]
        xn = a32.alloc(1024)
        Bxn = Buf('xn')
        ss = a32.alloc(4)
        uT = a16.alloc(8 * 512).rearrange("p (k t) -> p k t", k=8)
        BuT = Buf('uT')
        Ct = a32.alloc(512)
        St = a32.alloc(512)
        Btab = Buf('tab')
        tmp32 = a32.alloc(3 * 512)
        Btmp = Buf('ttmp')
        r1 = a32.alloc(512)
        r2 = a32.alloc(512)
        Br = Buf('ropetmp')
        kst = [a16.alloc(512), a16.alloc(512)]
        Bkst = [Buf('kst0'), Buf('kst1')]
        vst = [a16.alloc(512), a16.alloc(512)]
        Bvst = [Buf('vst0'), Buf('vst1')]
        ridx_t = None
        if T.get('ridx') is not None:
            ridx_t = a32.alloc(NKC).bitcast(I32)
            Bridx = Buf('ridx')
            P.dma('sp', [(ridx_t, T['ridx'][:, :])], writes=[Bridx])
        ki = 0
        for grp in range(NKC // 4):
            t0 = grp * 512
            rope_tables(X, posk[:, t0:t0 + 512], 512, Ct[0:64, :], St[0:64, :], Btab, tmp32, X.ai, Btmp)
            for cc in range(4):
                ch = grp * 4 + cc
                s = ch % 2
                if ridx_t is None:
                    P.dma('sp', [(xt[s], hk[ch * 128:(ch + 1) * 128, :])], writes=[Bxt[s]])
                else:
                    P.dma('pool', None, reads=[Bridx], writes=[Bxt[s]],
                          fns=[lambda e, s=s, ch=ch: e.indirect_dma_start(out=xt[s], out_offset=None, in_=hk[:, :],
                                                                          in_offset=bass.IndirectOffsetOnAxis(ap=ridx_t[:, ch:ch + 1], axis=0))])
                norm_modT(X, xt[s], Bxt[s], G1, SH, Bmod, lambda k, cc=cc: uT[:, k, cc * 128:(cc + 1) * 128], BuT,
                          xn, Bxn, ss, 0, 1, xn)
                for (vi, c0) in [(0, 768), (1, 1280)]:
                    for k in range(8):
                        P.op('pe', lambda e, k=k, cc=cc, vi=vi, c0=c0: e.matmul(X.ps[2][:, vi * 256:(vi + 1) * 256],
                                                                               lhsT=uT[:, k, cc * 128:(cc + 1) * 128],
                                                                               rhs=W[:, k, c0:c0 + 256], start=(k == 0 and vi == 0), stop=(k == 7),
                                                                               skip_group_check=True),
                             reads=[BuT, BW], writes=[X.psb[2]])
                P.op('act', lambda e, s=s: e.copy(out=vst[s], in_=X.ps[2][:, :]), reads=[X.psb[2]], writes=[Bvst[s]])
                P.dma('sp', [(o_vslc[ch * 128:(ch + 1) * 128, :], vst[s][:, 0:256]), (o_vwin[ch * 128:(ch + 1) * 128, :], vst[s][:, 256:512])],
                      reads=[Bvst[s]], writes=[Buf('o')], sem_buf=Bvst[s])
            for (kind, c0, c0p) in [('kcmp', 0, 0), ('kslc', 512, 256), ('kwin', 1024, 512), ('vcmp', 256, None)]:
                for hh in range(4):
                    srcs = [(W, 3, c0)] if c0p is None else [(W, 3, c0), (Wp, 4, c0p)]
                    for (W_, pi, cb_) in srcs:
                        for k in range(8):
                            P.op('pe', lambda e, k=k, W_=W_, pi=pi, cbase=cb_ + hh * 64: e.matmul(
                                X.ps[pi][0:64, :], lhsT=W_[:, k, cbase:cbase + 64], rhs=uT[:, k, :],
                                start=(k == 0), stop=(k == 7)),
                                reads=[BuT, BW, BWp], writes=[X.psb[pi]])
                    if kind == 'vcmp':
                        P.op('act', lambda e, hh=hh, t0=t0: e.copy(out=VcT[0:64, hh, t0:t0 + 512], in_=X.ps[3][0:64, :]),
                             reads=[X.psb[3]], writes=[Bcmp])
                        continue
                    t1 = r1[0:64, :]
                    t2 = r2[0:64, :]
                    P.op('dve', lambda e, t1=t1: e.tensor_tensor(out=t1, in0=X.ps[3][0:64, :], in1=Ct[0:64, :], op=ALU.mult),
                         reads=[X.psb[3], Btab], writes=[Br])
                    P.op('dve', lambda e, t2=t2: e.tensor_tensor(out=t2, in0=X.ps[4][0:64, :], in1=St[0:64, :], op=ALU.mult),
                         reads=[X.psb[4], Btab], writes=[Br])
                    if kind == 'kcmp':
                        P.op('pool', lambda e, hh=hh, t0=t0, t1=t1, t2=t2: e.tensor_tensor(out=KcT[0:64, hh, t0:t0 + 512], in0=t1, in1=t2, op=ALU.add),
                             reads=[Br], writes=[Bcmp])
                    else:
                        ks = ki % 2
                        ki += 1
                        P.op('pool', lambda e, ks=ks, t1=t1, t2=t2: e.tensor_tensor(out=kst[ks][0:64, :], in0=t1, in1=t2, op=ALU.add),
                             reads=[Br], writes=[Bkst[ks]])
                        od = o_kslcT if kind == 'kslc' else o_kwinT
                        P.dma('sp', [(od[:, hh * NKC * 128 + t0:hh * NKC * 128 + t0 + 512], kst[ks][0:64, :])],
                              reads=[Bkst[ks]], writes=[Buf('o')], sem_buf=Bkst[ks])
        P.barrier()
        w1s = a16.alloc(32 * 256).rearrange("p (l n) -> p l n", l=32)
        w2s = a16.alloc(2 * 64).rearrange("p (c n) -> p c n", c=2)
        peT = a16.alloc(32)
        Bwc = Buf('wc')
        bvec = a32.alloc(2)
        Bbv = Buf('bvec')
        xh = a32.alloc(256)
        x2 = a32.alloc(256)
        Bxh = Buf('xh')
        actT = a16.alloc(2 * 256).rearrange("p (c n) -> p c n", c=2)
        Bat = Buf('actT')
        ost = a16.alloc(1024)
        Bost = Buf('ost')
        ostv = a16.alloc(2 * 256).rearrange("p (c n) -> p c n", c=2)
        Bostv = Buf('ostv')
        P.op('pool', lambda e: e.memset(ost, 0.0), writes=[Bost])
        P.op('pool', lambda e: e.memset(ostv, 0.0), writes=[Bostv])
        for (kv, w1d, w2d, ped, SRC) in [('k', w1k, w2k, peTk, KcT), ('v', w1v, w2v, peTv, VcT)]:
            P.dma('pool', [(w1s[0:64, :, :], w1d.rearrange("(l d) n -> d l n", d=64)), (w2s, w2d.rearrange("(c p) n -> p c n", p=128)),
                           (peT[0:64, :], ped[:, :])], writes=[Bwc])
            for hc in range(2):
                for l in range(32):
                    P.op('pe', lambda e, hc=hc, l=l: e.matmul(X.ps[0][:, hc:hc + 1], lhsT=w1s[0:64, l, hc * 128:(hc + 1) * 128],
                                                              rhs=peT[0:64, l:l + 1], start=(l == 0), stop=(l == 31)),
                         reads=[Bwc], writes=[X.psb[0]])
            P.op('dve', lambda e: e.tensor_copy(out=bvec, in_=X.ps[0][:, 0:2]), reads=[X.psb[0]], writes=[Bbv])
            for g in range(4):
                for hc in range(2):
                    pi = 1 + hc
                    for l in range(32):
                        rhs = SRC[0:64, g, l:l + 16 * (NCMP - 1) + 1:16]
                        P.op('pe', lambda e, hc=hc, l=l, pi=pi, rhs=rhs: e.matmul(X.ps[pi][:, 0:NCMP], lhsT=w1s[0:64, l, hc * 128:(hc + 1) * 128],
                                                                                  rhs=rhs, start=(l == 0), stop=(l == 31)),
                             reads=[Bwc, Bcmp], writes=[X.psb[pi]])
                    xv = xh[:, 0:NCMP]
                    x2v = x2[:, 0:NCMP]
                    P.op('act', lambda e, pi=pi, hc=hc, xv=xv: e.activation(out=xv, in_=X.ps[pi][:, 0:NCMP], func=AF.Identity, bias=bvec[:, hc:hc + 1]),
                         reads=[X.psb[pi], Bbv], writes=[Bxh])
                    P.op('dve', lambda e, xv=xv, x2v=x2v: e.tensor_tensor(out=x2v, in0=xv, in1=xv, op=ALU.mult), reads=[Bxh], writes=[Bxh])
                    P.op('dve', lambda e, x2v=x2v: e.tensor_scalar(out=x2v, in0=x2v, scalar1=0.044715, scalar2=1.0, op0=ALU.mult, op1=ALU.add),
                         reads=[Bxh], writes=[Bxh])
                    P.op('dve', lambda e, xv=xv, x2v=x2v: e.tensor_tensor(out=x2v, in0=x2v, in1=xv, op=ALU.mult), reads=[Bxh], writes=[Bxh])
                    P.op('act', lambda e, x2v=x2v: e.activation(out=x2v, in_=x2v, func=AF.Sigmoid, scale=1.5957691216), reads=[Bxh], writes=[Bxh])
                    P.op('dve', lambda e, hc=hc, xv=xv, x2v=x2v: e.tensor_tensor(out=actT[:, hc, 0:NCMP], in0=x2v, in1=xv, op=ALU.mult),
                         reads=[Bxh], writes=[Bat])
                if kv == 'k':
                    for hc in range(2):
                        P.op('pe', lambda e, hc=hc: e.matmul(X.ps[3][0:64, 0:NCMP], lhsT=w2s[:, hc, :], rhs=actT[:, hc, 0:NCMP],
                                                             start=(hc == 0), stop=(hc == 1)), reads=[Bat, Bwc], writes=[X.psb[3]])
                    P.op('act', lambda e, g=g: e.copy(out=ost[0:64, g * 256:g * 256 + NCMP], in_=X.ps[3][0:64, 0:NCMP]),
                         reads=[X.psb[3]], writes=[Bost])
                else:
                    for nchk in range(2):
                        n0 = nchk * 128
                        nn = min(128, NCMP - n0)
                        for hc in range(2):
                            P.op('pe', lambda e, hc=hc, n0=n0, nn=nn: e.matmul(X.ps[4][0:nn, 0:64], lhsT=actT[:, hc, n0:n0 + nn], rhs=w2s[:, hc, :],
                                                                               start=(hc == 0), stop=(hc == 1)), reads=[Bat, Bwc], writes=[X.psb[4]])
                        P.op('act', lambda e, g=g, nchk=nchk, nn=nn: e.copy(out=ostv[0:nn, nchk, g * 64:(g + 1) * 64], in_=X.ps[4][0:nn, 0:64]),
                             reads=[X.psb[4]], writes=[Bostv])
        P.dma('sp', [(o_kcT[:, :], ost[0:64, :])], reads=[Bost], writes=[Buf('o')], sem_buf=Bost)
        P.dma('sp', [(o_vc.rearrange("(c p) n -> p c n", p=128), ostv)], reads=[Bostv], writes=[Buf('o')], sem_buf=Bostv)
        P.barrier()


def build_kv1():
    nc = bass.Bass("TRN2", target_bir_lowering=False)

    def din(name, shape, dt=F32):
        return nc.dram_tensor(name, shape, dt, kind="ExternalInput").ap()

    def dout(name, shape, dt=BF16):
        return nc.dram_tensor(name, shape, dt, kind="ExternalOutput").ap()
    hk = din("hk", [NKC * 128, 1024])
    posk = din("posk", [1, NKC * 128], I32)
    cst_d = din("cst", [128, NCST])
    cT = din("cT", [128, 8])
    w_ada = din("w_ada", [1024, 2048])
    b_adaT = din("b_adaT", [128, 16])
    gainT = din("gainT", [128, 8])
    w_kv = din("w_kv", [1024, 1536])
    w1k = din("w1k", [2048, 256])
    w1v = din("w1v", [2048, 256])
    w2k = din("w2k", [256, 64])
    w2v = din("w2v", [256, 64])
    peTk = din("peTk", [64, 32])
    peTv = din("peTv", [64, 32])
    o_kslcT = dout("kslcT", [64, 4 * NKC * 128])
    o_kwinT = dout("kwinT", [64, 4 * NKC * 128])
    o_vslc = dout("vslc", [NKC * 128, 256])
    o_vwin = dout("vwin", [NKC * 128, 256])
    o_kcT = dout("kcT", [64, 4 * 256])
    o_vc = dout("vc", [256, 256])

    T = dict(hk=hk, posk=posk, cst_d=cst_d, cT=cT, w_ada=w_ada, b_adaT=b_adaT, gainT=gainT, w_kv=w_kv, w1k=w1k, w1v=w1v, w2k=w2k, w2v=w2v, peTk=peTk, peTv=peTv, o_kslcT=o_kslcT, o_kwinT=o_kwinT, o_vslc=o_vslc, o_vwin=o_vwin, o_kcT=o_kcT, o_vc=o_vc)
    X = setup_ctx(nc)
    with X.st:
        phase_kv1(X, T)
        X.P.emit()
    return nc


def storage_order(full_b, j):
    if j == 0:
        pad = np.zeros((128,) + full_b.shape[1:], full_b.dtype)
        return np.ascontiguousarray(np.concatenate([pad, full_b[0:31 * 128]], axis=0))
    return np.ascontiguousarray(full_b)


def kv1_inputs(inputs, h1_full):
    maps = []
    for c in range(8):
        b, j = c // 2, c % 2
        maps.append({
            "hk": storage_order(h1_full[b], j),
            "posk": np.ascontiguousarray(storage_order(np.asarray(inputs['positions'][b], np.int32), j)[None, :]),
            "cst": make_consts(j),
            "cT": np.ascontiguousarray(inputs['c'][b].reshape(8, 128).T),
            "w_ada": np.ascontiguousarray(inputs['w_kv_ada']),
            "b_adaT": np.ascontiguousarray(inputs['b_kv_ada'].reshape(16, 128).T),
            "gainT": np.ascontiguousarray(inputs['kv_gain'].reshape(8, 128).T),
            "w_kv": np.ascontiguousarray(inputs['w_kv']),
            "w1k": np.ascontiguousarray(inputs['cmp_w1_k']), "w1v": np.ascontiguousarray(inputs['cmp_w1_v']),
            "w2k": np.ascontiguousarray(inputs['cmp_w2_k']), "w2v": np.ascontiguousarray(inputs['cmp_w2_v']),
            "peTk": np.ascontiguousarray(inputs['cmp_pe_k'].T), "peTv": np.ascontiguousarray(inputs['cmp_pe_v'].T),
        })
    return maps


C_TRIU = NCST
NCST1 = NCST + 128


def make_consts1(j):
    c = np.zeros((128, NCST1), np.float32)
    c[:, 0:NCST] = make_consts(j)
    q = np.arange(128)[:, None]
    k = np.arange(128)[None, :]
    c[:, C_TRIU:C_TRIU + 128] = np.where(k > q, 0.0, NEG)
    return c


def make_tables1(j, nqb=NQB):
    import ml_dtypes
    cmpMb = np.zeros((nqb, 128, 256), np.float32)
    vm = np.zeros((nqb, 128, 64), np.float32)
    va = np.zeros((nqb, 128, 64), np.float32)
    shift = 128 * (1 - j)
    n = np.arange(256)[None, :]
    jb_st = np.arange(64)[None, :]
    for m in range(nqb):
        t_st = (2 * m + 1) * 128 + np.arange(128)[:, None]
        valid = (n <= 254) & (n >= 8 * (1 - j)) & (16 * n + 31 <= t_st)
        cmpMb[m] = np.where(valid, 0.0, MASKV)
        t_g = t_st - shift
        jb = jb_st - 2 * (1 - j)
        jt = t_g // 64
        valid_b = (jb >= 0) & (jb * 64 <= t_g)
        f0 = (jb == 0)
        f1 = (jb == jt)
        f2 = (jb == jt - 1)
        forced = f0 | f1 | f2
        vm[m] = np.where(valid_b & ~forced, 1.0, 0.0)
        a = np.where(f0, 1.0e30, np.where(f1, 0.9e30, np.where(f2, 0.8e30, 0.0)))
        va[m] = np.where(valid_b, a, NEG)
    cs = np.arange(256)[:, None] * 16
    ss_ = np.arange(64)[None, :] * 64
    ov = np.minimum(cs + 32, ss_ + 64) - np.maximum(cs, ss_)
    agg = (np.clip(ov, 0, None) / 32.0).astype(np.float32)
    agg[255] = 0.0
    return {"cmpMb": cmpMb.astype(ml_dtypes.bfloat16), "selvm": vm, "selva": va, "agg": agg.astype(ml_dtypes.bfloat16)}


def phase_att1(X, T, nqb=NQB):
    hq = T['hq']
    posq = T['posq']
    cst_d = T['cst_d']
    cT = T['cT']
    w_ada = T['w_ada']
    b_adaT = T['b_adaT']
    b_gate = T['b_gate']
    gainT = T['gainT']
    w_q = T['w_q']
    w_out = T['w_out']
    kslcT_d = T['kslcT_d']
    kwinT_d = T['kwinT_d']
    vslc_d = T['vslc_d']
    vwin_d = T['vwin_d']
    kcT_d = T['kcT_d']
    vc_d = T['vc_d']
    cmpMb_d = T['cmpMb_d']
    selvm_d = T['selvm_d']
    selva_d = T['selva_d']
    agg_d = T['agg_d']
    hmid = T['hmid']
    X.carve(15000)
    P = X.P
    a32, a16 = X.a32, X.a16
    if True:
        X.cst = a32.alloc(NCST1)
        X.Bcst = Buf('cst')
        P.dma('sp', [(X.cst, cst_d[:, :])], writes=[X.Bcst])
        X.ident = X.cst[:, C_IDENT:C_IDENT + 128]
        X.irep = a16.alloc(512)
        X.Birep = Buf('irep')
        for r in range(4):
            P.op('dve', lambda e, r=r: e.tensor_copy(out=X.irep[:, r * 128:(r + 1) * 128], in_=X.ident), reads=[X.Bcst], writes=[X.Birep])
        trib = a16.alloc(128)
        triub = a16.alloc(128)
        P.op('dve', lambda e: e.tensor_copy(out=trib, in_=X.cst[:, C_TRI:C_TRI + 128]), reads=[X.Bcst], writes=[X.Birep])
        P.op('dve', lambda e: e.tensor_copy(out=triub, in_=X.cst[:, C_TRIU:C_TRIU + 128]), reads=[X.Bcst], writes=[X.Birep])
        X.eps_col = a32.alloc(1)
        tiny = a32.alloc(1)
        X.Bcst2 = Buf('cst2')
        P.op('pool', lambda e: e.memset(X.eps_col, 1e-6), writes=[X.Bcst2])
        P.op('pool', lambda e: e.memset(tiny, 1e-30), writes=[X.Bcst2])
        G1, SH, Bmod, gate_bc, Bgate = ffn_phase0(X, cT, w_ada, b_adaT, b_gate, gainT)

        KsT = a16.alloc(4 * NKC * 128).rearrange("p (g t) -> p g t", g=4)
        VsAf = a16.alloc(NKC * 260)
        VsA = VsAf.rearrange("p (c n) -> p c n", c=NKC)
        kcT = a16.alloc(1024).rearrange("p (g n) -> p g n", g=4)
        vcxf = a16.alloc(2 * 260)
        vcx = vcxf.rearrange("p (c n) -> p c n", c=2)
        agg = a16.alloc(128).rearrange("p (c n) -> p c n", c=2)
        BKV = Buf('kv')
        P.op('pool', lambda e: e.memset(VsAf, 1.0), writes=[BKV])
        P.op('pool', lambda e: e.memset(vcxf, 1.0), writes=[BKV])
        ks3 = kslcT_d.rearrange("p (g t) -> p g t", g=4)
        P.dma('sp', [(KsT[0:64, g, :], ks3[:, g, :]) for g in range(4)], writes=[BKV])
        vs4 = vslc_d.rearrange("(c p) (g d) -> p c g d", p=128, g=4)
        VsA4 = VsAf.rearrange("p (c g d) -> p c g d", c=NKC, g=4)
        P.dma('sp', [(VsA4[:, :, g, 0:64], vs4[:, :, g, :]) for g in range(4)], writes=[BKV])
        P.dma('sp', [(kcT[0:64, :, :], kcT_d.rearrange("p (g n) -> p g n", g=4))], writes=[BKV])
        vcx4 = vcxf.rearrange("p (c g d) -> p c g d", c=2, g=4)
        vc4 = vc_d.rearrange("(c p) (g d) -> p c g d", p=128, g=4)
        P.dma('sp', [(vcx4[:, :, g, 0:64], vc4[:, :, g, :]) for g in range(4)], writes=[BKV])
        P.dma('sp', [(agg, agg_d.rearrange("(c p) n -> p c n", p=128))], writes=[BKV])
        WB = a16.alloc(8 * 1072).rearrange("p (k n) -> p k n", k=8)
        WBp = a16.alloc(8 * 1024).rearrange("p (k n) -> p k n", k=8)
        BW = Buf('WB')
        BWp = Buf('WBp')
        load_w_bf16(X, WB, w_q, BW, nsplit=2)
        for k in range(8):
            src = WB[:, k, 0:1024].rearrange("p (h t i) -> p h t i", h=16, t=2)
            dst = WBp[:, k, 0:1024].rearrange("p (h t i) -> p h t i", h=16, t=2)
            eng = 'pool' if k % 2 == 0 else 'dve'
            P.op(eng, lambda e, src=src, dst=dst: e.tensor_copy(out=dst[:, :, 0, :], in_=src[:, :, 1, :]), reads=[BW], writes=[BWp])
            P.op(eng, lambda e, src=src, dst=dst: e.tensor_copy(out=dst[:, :, 1, :], in_=src[:, :, 0, :]), reads=[BW], writes=[BWp])
        Wo = a16.alloc(8 * 1024).rearrange("p (k n) -> p k n", k=8)
        BWo = Buf('Wout')
        load_w_bf16(X, Wo, w_out, BWo, nsplit=2)
        xt = [a32.alloc(1024), a32.alloc(1024)]
        Bxt = [Buf('xt0'), Buf('xt1')]
        xn = a32.alloc(1024)
        Bxn = Buf('xn')
        ss = a32.alloc(4)
        Ct = a32.alloc(128)
        St = a32.alloc(128)
        Btab = Buf('tab')
        tmp32 = a32.alloc(3 * 128)
        Btmp = Buf('ttmp')
        r1 = a32.alloc(512)
        r2 = a32.alloc(512)
        Br = Buf('ropetmp')
        uq = a16.alloc(8 * 128).rearrange("p (k t) -> p k t", k=8)
        Buq = Buf('uq')
        QT = a16.alloc(16 * 128).rearrange("p (h t) -> p h t", h=16)
        BQ = Buf('QT')
        gts = a32.alloc(48)
        Bgts = Buf('gts')
        gts3 = gts.rearrange("p (h b) -> p h b", b=3)
        KwT = [a16.alloc(4 * 640).rearrange("p (g t) -> p g t", g=4) for _ in range(2)]
        VwAf = [a16.alloc(5 * 260) for _ in range(2)]
        BKw = [Buf('kw0'), Buf('kw1')]
        for i in range(2):
            P.op('pool', lambda e, i=i: e.memset(VwAf[i], 1.0), writes=[BKw[i]])
        cmb = [a16.alloc(256), a16.alloc(256)]
        svm = [a32.alloc(64), a32.alloc(64)]
        sva = [a32.alloc(64), a32.alloc(64)]
        Btb = [Buf('tb0'), Buf('tb1')]
        PT = [a16.alloc(512), a16.alloc(512)]
        BPT = [Buf('pt0'), Buf('pt1')]
        rden = a32.alloc(8)
        coef = a32.alloc(8)
        Brd = Buf('rden')
        imp = a32.alloc(64)
        imp2 = a32.alloc(64)
        mx = a32.alloc(16)
        Bimp = Buf('imp')
        selb = a16.alloc(64)
        Bselb = Buf('selb')
        Mbs = [a16.alloc(NKC * 128), a16.alloc(NKC * 128)]
        BMbs = [Buf('mbs0'), Buf('mbs1')]
        On = a32.alloc(1024)
        BOn = Buf('On')
        otmp = a32.alloc(256)
        Botmp = Buf('otmp')
        OnT = a16.alloc(8 * 128).rearrange("p (k t) -> p k t", k=8)
        BOnT = Buf('OnT')
        hm = a32.alloc(1024)
        Bhm = Buf('hm')
        kw3 = kwinT_d.rearrange("p (g t) -> p g t", g=4)

        state = {'it': 0, 'ob': 0}

        def attend(g, chunks, out_slot_fn):
            ob = 2 if state['ob'] % 2 == 0 else 7
            state['ob'] += 1
            n = len(chunks)
            base = state['it']
            state['it'] += n

            def emit_S(ci):
                kl, ml, bias, vr = chunks[ci]
                pi = 5 + ((base + ci) % 2)
                P.op('pe', lambda e, pi=pi, kl=kl, ml=ml, g=g: e.matmul(X.ps[pi][:, :], lhsT=kl, rhs=QT[0:64, g * 4:(g + 1) * 4, :],
                                                                       start=True, stop=(ml is None)),
                     reads=[BQ, BKw[0], BKw[1], BKV], writes=[X.psb[pi]])
                if ml is not None:
                    P.op('pe', lambda e, pi=pi, ml=ml: e.matmul(X.ps[pi][:, :], lhsT=ml, rhs=X.irep, start=False, stop=True),
                         reads=[X.Birep, BMbs[0], BMbs[1], Btb[0], Btb[1]], writes=[X.psb[pi]])
            emit_S(0)
            for ci, (kl, ml, bias, vr) in enumerate(chunks):
                if ci + 1 < n:
                    emit_S(ci + 1)
                pi = 5 + ((base + ci) % 2)
                ps_ = (base + ci) % 2
                if bias is None:
                    P.op('act', lambda e, pi=pi, ps_=ps_: e.activation(out=PT[ps_], in_=X.ps[pi][:, :], func=AF.Exp, scale=0.125),
                         reads=[X.psb[pi]], writes=[BPT[ps_]])
                else:
                    P.op('act', lambda e, pi=pi, ps_=ps_, bias=bias: e.activation(out=PT[ps_], in_=X.ps[pi][:, :], func=AF.Exp, scale=0.125, bias=bias),
                         reads=[X.psb[pi], X.Bcst], writes=[BPT[ps_]])
                for r in range(4):
                    P.op('pe', lambda e, r=r, ps_=ps_, vr=vr, ob=ob, ci=ci, n=n: e.matmul(X.ps[ob][:, r * 65:(r + 1) * 65],
                                                                                       lhsT=PT[ps_][:, r * 128:(r + 1) * 128], rhs=vr,
                                                                                       start=(ci == 0 and r == 0), stop=(ci == n - 1),
                                                                                       skip_group_check=True),
                         reads=[BPT[ps_], BKw[0], BKw[1]], writes=[X.psb[ob]])
                out_slot_fn(ci, ps_)
            return ob

        for m in range(nqb):
            sc = 2 * m + 1
            nk = sc + 1
            nkeys = nk * 128
            s = m % 2
            P.dma('sp', [(xt[s], hq[m * 128:(m + 1) * 128, :])], writes=[Bxt[s]])
            P.dma('sp', [(cmb[s], cmpMb_d[m, :, :]), (svm[s], selvm_d[m, :, :]), (sva[s], selva_d[m, :, :])], writes=[Btb[s]])
            c_lo = max(0, sc - 4)
            nwc = sc - c_lo + 1
            VwA = VwAf[s].rearrange("p (c n) -> p c n", c=5)
            VwA4 = VwAf[s].rearrange("p (c g d) -> p c g d", c=5, g=4)
            P.dma('sp', [(KwT[s][0:64, g, 0:nwc * 128], kw3[:, g, c_lo * 128:(sc + 1) * 128]) for g in range(4)] +
                  [(VwA4[:, 0:nwc, g, 0:64], vwin_d[c_lo * 128:(sc + 1) * 128, :].rearrange("(c p) (g d) -> p c g d", p=128, g=4)[:, :, g, :]) for g in range(4)],
                  writes=[BKw[s]])
            rope_tables(X, posq[:, m * 128:(m + 1) * 128], 128, Ct[0:64, :], St[0:64, :], Btab, tmp32, X.ai, Btmp)
            norm_modT(X, xt[s], Bxt[s], G1, SH, Bmod, lambda k: uq[:, k, :], Buq, xn, Bxn, ss, 0, 1, xn)
            for b4 in range(4):
                for (W_, pi) in [(WB, 3), (WBp, 4)]:
                    for hh in range(4):
                        cbase = (b4 * 4 + hh) * 64
                        for k in range(8):
                            P.op('pe', lambda e, k=k, W_=W_, pi=pi, cbase=cbase, hh=hh: e.matmul(
                                X.ps[pi][0:64, hh * 128:(hh + 1) * 128], lhsT=W_[:, k, cbase:cbase + 64], rhs=uq[:, k, :],
                                start=(k == 0), stop=(k == 7)),
                                reads=[Buq, BW, BWp], writes=[X.psb[pi]])
                A = X.ps[3][0:64, :].rearrange("p (h t) -> p h t", h=4)
                B = X.ps[4][0:64, :].rearrange("p (h t) -> p h t", h=4)
                c_b = Ct[0:64, :].unsqueeze(1).to_broadcast([64, 4, 128])
                s_b = St[0:64, :].unsqueeze(1).to_broadcast([64, 4, 128])
                t1 = r1[0:64, :].rearrange("p (h t) -> p h t", h=4)
                t2 = r2[0:64, :].rearrange("p (h t) -> p h t", h=4)
                P.op('dve', lambda e, A=A, c_b=c_b, t1=t1: e.tensor_tensor(out=t1, in0=A, in1=c_b, op=ALU.mult), reads=[X.psb[3], Btab], writes=[Br])
                P.op('dve', lambda e, B=B, s_b=s_b, t2=t2: e.tensor_tensor(out=t2, in0=B, in1=s_b, op=ALU.mult), reads=[X.psb[4], Btab], writes=[Br])
                P.op('pool', lambda e, b4=b4, t1=t1, t2=t2: e.tensor_tensor(out=QT[0:64, b4 * 4:(b4 + 1) * 4, :], in0=t1, in1=t2, op=ALU.add),
                     reads=[Br], writes=[BQ])
            for k in range(8):
                P.op('pe', lambda e, k=k: e.matmul(X.ps[2][:, 0:48], lhsT=uq[:, k, :], rhs=WB[:, k, 1024:1072],
                                                   start=(k == 0), stop=(k == 7)), reads=[Buq, BW], writes=[X.psb[2]])
            P.op('act', lambda e: e.activation(out=gts, in_=X.ps[2][:, 0:48], func=AF.Sigmoid), reads=[X.psb[2]], writes=[Bgts])

            for g in range(4):
                ms = g % 2
                def cmp_extra(ci, ps_, g=g):
                    for r in range(4):
                        P.op('pe', lambda e, r=r, ps_=ps_, ci=ci: e.matmul(X.ps[3][:, r * 64:(r + 1) * 64], lhsT=PT[ps_][:, r * 128:(r + 1) * 128],
                                                                          rhs=agg[:, ci, :], start=(ci == 0 and r == 0), stop=(ci == 1),
                                                                          skip_group_check=True),
                             reads=[BPT[ps_], BKV], writes=[X.psb[3]])
                chunks = [(kcT[0:64, g, ci * 128:(ci + 1) * 128], cmb[s][:, ci * 128:(ci + 1) * 128], None, vcx[:, ci, g * 65:(g + 1) * 65])
                          for ci in range(2)]
                ob = attend(g, chunks, cmp_extra)
                O3 = X.ps[ob][:, 0:260].rearrange("p (r d) -> p r d", r=4)
                P.op('dve', lambda e, O3=O3: e.tensor_scalar(out=rden[:, 0:4], in0=O3[:, :, 64], scalar1=tiny[:, 0:1], scalar2=None, op0=ALU.add),
                     reads=[X.psb[ob], X.Bcst2], writes=[Brd])
                P.op('dve', lambda e: e.reciprocal(out=rden[:, 0:4], in_=rden[:, 0:4]), reads=[Brd], writes=[Brd])
                P.op('dve', lambda e, g=g: e.tensor_tensor(out=coef[:, 0:4], in0=rden[:, 0:4], in1=gts3[:, g * 4:(g + 1) * 4, 0], op=ALU.mult),
                     reads=[Brd, Bgts], writes=[Brd])
                P.op('dve', lambda e, O3=O3, g=g: e.tensor_tensor(
                    out=On[:, g * 256:(g + 1) * 256].rearrange("p (r d) -> p r d", r=4), in0=O3[:, :, 0:64],
                    in1=coef[:, 0:4].unsqueeze(2).to_broadcast([128, 4, 64]), op=ALU.mult),
                    reads=[X.psb[ob], Brd], writes=[BOn])
                for r in range(4):
                    if r == 0:
                        P.op('dve', lambda e: e.tensor_scalar(out=imp, in0=X.ps[3][:, 0:64], scalar1=rden[:, 0:1], scalar2=None, op0=ALU.mult),
                             reads=[X.psb[3], Brd], writes=[Bimp])
                    else:
                        P.op('dve', lambda e, r=r: e.scalar_tensor_tensor(out=imp, in0=X.ps[3][:, r * 64:(r + 1) * 64], scalar=rden[:, r:r + 1],
                                                                          in1=imp, op0=ALU.mult, op1=ALU.add),
                             reads=[X.psb[3], Brd, Bimp], writes=[Bimp])
                P.op('dve', lambda e, s=s: e.tensor_tensor(out=imp, in0=imp, in1=svm[s], op=ALU.mult), reads=[Bimp, Btb[s]], writes=[Bimp])
                P.op('dve', lambda e, s=s: e.tensor_tensor(out=imp, in0=imp, in1=sva[s], op=ALU.add), reads=[Bimp, Btb[s]], writes=[Bimp])
                P.op('dve', lambda e: e.max(out=mx[:, 0:8], in_=imp), reads=[Bimp], writes=[Bimp])
                P.op('dve', lambda e: e.match_replace(out=imp2, in_to_replace=mx[:, 0:8], in_values=imp, imm_value=-3.0e38), reads=[Bimp], writes=[Bimp])
                P.op('dve', lambda e: e.max(out=mx[:, 8:16], in_=imp2), reads=[Bimp], writes=[Bimp])
                P.op('dve', lambda e: e.tensor_reduce(out=mx[:, 0:1], in_=mx[:, 8:16], axis=AX.X, op=ALU.min), reads=[Bimp], writes=[Bimp])
                P.op('dve', lambda e: e.tensor_scalar(out=selb, in0=imp, scalar1=mx[:, 0:1], scalar2=MASKV, op0=ALU.is_lt, op1=ALU.mult),
                     reads=[Bimp], writes=[Bselb])
                nblk = 2 * nk
                P.op('pool', lambda e, ms=ms, nblk=nblk, nkeys=nkeys: e.tensor_copy(
                    out=Mbs[ms][:, 0:nkeys].rearrange("p (b l) -> p b l", l=64),
                    in_=selb[:, 0:nblk].unsqueeze(2).to_broadcast([128, nblk, 64])), reads=[Bselb], writes=[BMbs[ms]])
                P.op('pool', lambda e, ms=ms, sc=sc: e.tensor_tensor(out=Mbs[ms][:, sc * 128:(sc + 1) * 128], in0=Mbs[ms][:, sc * 128:(sc + 1) * 128],
                                                                    in1=trib, op=ALU.add), reads=[BMbs[ms], X.Birep], writes=[BMbs[ms]])
                chunks = [(KsT[0:64, g, c * 128:(c + 1) * 128], Mbs[ms][:, c * 128:(c + 1) * 128],
                           (X.cst[:, C_PAD:C_PAD + 1] if c == 0 else None), VsA[:, c, g * 65:(g + 1) * 65]) for c in range(nk)]
                ob = attend(g, chunks, lambda ci, ps_: None)

                def accum_branch(ob, br, g=g):
                    O3 = X.ps[ob][:, 0:260].rearrange("p (r d) -> p r d", r=4)
                    P.op('dve', lambda e, O3=O3: e.reciprocal(out=rden[:, 4:8], in_=O3[:, :, 64]), reads=[X.psb[ob]], writes=[Brd])
                    P.op('dve', lambda e: e.tensor_tensor(out=coef[:, 4:8], in0=rden[:, 4:8], in1=gts3[:, g * 4:(g + 1) * 4, br], op=ALU.mult),
                         reads=[Brd, Bgts], writes=[Brd])
                    P.op('dve', lambda e, O3=O3: e.tensor_tensor(out=otmp.rearrange("p (r d) -> p r d", r=4), in0=O3[:, :, 0:64],
                                                                in1=coef[:, 4:8].unsqueeze(2).to_broadcast([128, 4, 64]), op=ALU.mult),
                         reads=[X.psb[ob], Brd], writes=[Botmp])
                    P.op('pool', lambda e: e.tensor_tensor(out=On[:, g * 256:(g + 1) * 256], in0=On[:, g * 256:(g + 1) * 256], in1=otmp, op=ALU.add),
                         reads=[Botmp, BOn], writes=[BOn])
                accum_branch(ob, 1)
                chunks = []
                for wi_, c in enumerate(range(c_lo, sc + 1)):
                    if c == sc:
                        ml = trib
                    elif c == sc - 4:
                        ml = triub
                    else:
                        ml = None
                    chunks.append((KwT[s][0:64, g, wi_ * 128:(wi_ + 1) * 128], ml,
                                   (X.cst[:, C_PAD:C_PAD + 1] if c == 0 else None), VwA[:, wi_, g * 65:(g + 1) * 65]))
                ob = attend(g, chunks, lambda ci, ps_: None)
                accum_branch(ob, 2)
            for k in range(8):
                pi = 0 if k < 4 else 1
                P.op('pe', lambda e, k=k, pi=pi: e.transpose(out=X.ps[pi][:, (k % 4) * 128:(k % 4 + 1) * 128],
                                                             in_=On[:, k * 128:(k + 1) * 128], identity=X.ident),
                     reads=[BOn, X.Bcst], writes=[X.psb[pi]])
            for half in range(2):
                P.op('act', lambda e, half=half: e.copy(out=OnT[:, half * 4:(half + 1) * 4, :],
                                                        in_=X.ps[half][:, :].rearrange("p (k t) -> p k t", k=4)),
                     reads=[X.psb[half]], writes=[BOnT])
            for half in range(2):
                pi = 3 + half
                for k in range(8):
                    P.op('pe', lambda e, k=k, pi=pi, half=half: e.matmul(X.ps[pi][:, :], lhsT=OnT[:, k, :],
                                                                         rhs=Wo[:, k, half * 512:(half + 1) * 512],
                                                                         start=(k == 0), stop=(k == 7)),
                         reads=[BOnT, BWo], writes=[X.psb[pi]])
                P.op('dve', lambda e, pi=pi, half=half: e.tensor_tensor(out=hm[:, half * 512:(half + 1) * 512], in0=X.ps[pi][:, :],
                                                                        in1=gate_bc[:, half * 512:(half + 1) * 512], op=ALU.mult),
                     reads=[X.psb[pi], Bgate], writes=[Bhm])
            P.op('pool', lambda e, s=s: e.tensor_tensor(out=hm, in0=hm, in1=xt[s], op=ALU.add), reads=[Bhm, Bxt[s]], writes=[Bhm])
            P.dma('sp', [(hmid[m * 128:(m + 1) * 128, :], hm)], reads=[Bhm], writes=[Buf('o')], sem_buf=Bhm)
        P.barrier()


def build_att1(nqb=NQB):
    nc = bass.Bass("TRN2", target_bir_lowering=False)

    def din(name, shape, dt=F32):
        return nc.dram_tensor(name, shape, dt, kind="ExternalInput").ap()
    hq = din("hq", [nqb * 128, 1024])
    posq = din("posq", [1, nqb * 128], I32)
    cst_d = din("cst", [128, NCST1])
    cT = din("cT", [128, 8])
    w_ada = din("w_ada", [1024, 3072])
    b_adaT = din("b_adaT", [128, 16])
    b_gate = din("b_gate", [1, 1024])
    gainT = din("gainT", [128, 8])
    w_q = din("w_q", [1024, 1072])
    w_out = din("w_out", [1024, 1024])
    kslcT_d = din("kslcT", [64, 4 * NKC * 128], BF16)
    kwinT_d = din("kwinT", [64, 4 * NKC * 128], BF16)
    vslc_d = din("vslc", [NKC * 128, 256], BF16)
    vwin_d = din("vwin", [NKC * 128, 256], BF16)
    kcT_d = din("kcT", [64, 1024], BF16)
    vc_d = din("vc", [256, 256], BF16)
    cmpMb_d = din("cmpMb", [nqb, 128, 256], BF16)
    selvm_d = din("selvm", [nqb, 128, 64])
    selva_d = din("selva", [nqb, 128, 64])
    agg_d = din("agg", [256, 64], BF16)
    hmid = nc.dram_tensor("hmid", [nqb * 128, 1024], F32, kind="ExternalOutput").ap()

    T = dict(hq=hq, posq=posq, cst_d=cst_d, cT=cT, w_ada=w_ada, b_adaT=b_adaT, b_gate=b_gate, gainT=gainT, w_q=w_q, w_out=w_out, kslcT_d=kslcT_d, kwinT_d=kwinT_d, vslc_d=vslc_d, vwin_d=vwin_d, kcT_d=kcT_d, vc_d=vc_d, cmpMb_d=cmpMb_d, selvm_d=selvm_d, selva_d=selva_d, agg_d=agg_d, hmid=hmid)
    X = setup_ctx(nc)
    with X.st:
        phase_att1(X, T, nqb)
        X.P.emit()
    return nc


def att1_inputs(inputs, h1_cores, kv_res, nqb=NQB):
    maps = []
    for c in range(8):
        b, j = c // 2, c % 2
        pos = np.asarray(inputs['positions'][b], np.int32)
        posq = np.concatenate([pos[(2 * m + j) * 128:(2 * m + j + 1) * 128] for m in range(nqb)])
        mp = {
            "hq": np.ascontiguousarray(h1_cores[c][0:nqb * 128]),
            "posq": np.ascontiguousarray(posq[None, :]),
            "cst": make_consts1(j),
            "cT": np.ascontiguousarray(inputs['c'][b].reshape(8, 128).T),
            "w_ada": np.ascontiguousarray(inputs['w_ada'][1][:, 0:3072]),
            "b_adaT": np.ascontiguousarray(inputs['b_ada'][1][0:2048].reshape(16, 128).T),
            "b_gate": np.ascontiguousarray(inputs['b_ada'][1][2048:3072][None, :]),
            "gainT": np.ascontiguousarray(inputs['attn_gain'][1].reshape(8, 128).T),
            "w_q": np.ascontiguousarray(inputs['b_w_q'][0]),
            "w_out": np.ascontiguousarray(inputs['b_w_out'][0]),
        }
        for k in ["kslcT", "kwinT", "vslc", "vwin", "kcT", "vc"]:
            mp[k] = kv_res[c][k]
        mp.update(make_tables1(j, nqb))
        maps.append(mp)
    return maps


def phase_ffn1(X, T, ntok=2048, dff=3584, nexp=8):
    hin = T['hin']
    cst_d = T['cst_d']
    cT = T['cT']
    w_ada = T['w_ada']
    b_adaT = T['b_adaT']
    b_gate = T['b_gate']
    gainT = T['gainT']
    fgain = T['fgain']
    wr = T['wr']
    wg = T['wg']
    wu = T['wu']
    wd = T['wd']
    hout = T['hout']
    NF = dff // 128
    FB = 4
    GT = 1024
    ngrp = ntok // GT
    NT = GT // 128
    DQ = 256
    X.carve(16300)
    P = X.P
    a32, a16 = X.a32, X.a16
    if True:
        load_consts(X, cst_d)
        X.eps_col = a32.alloc(1)
        X.Bcst2 = Buf('cst2')
        P.op('pool', lambda e: e.memset(X.eps_col, 1e-6), writes=[X.Bcst2])
        G1, SH, Bmod, gate_bc, Bgate = ffn_phase0(X, cT, w_ada, b_adaT, b_gate, gainT)
        fg_bc = a32.alloc(1024)
        P.dma('sp', [(fg_bc, fgain.partition_broadcast(128))], writes=[Bgate])
        Wr = a32.alloc(8 * nexp).rearrange("p (k n) -> p k n", k=8)
        BWr = Buf('Wr')
        P.dma('sp', [(Wr, wr.rearrange("(k p) n -> p k n", p=128))], writes=[BWr])

        NWB = 2
        Wgb = [a16.alloc(8 * 128 * FB).rearrange("p (k n) -> p k n", k=8) for _ in range(NWB)]
        Wub = [a16.alloc(8 * 128 * FB).rearrange("p (k n) -> p k n", k=8) for _ in range(NWB)]
        BWg = [Buf('wg%d' % i) for i in range(NWB)]
        Wdb = [a16.alloc(NF * DQ).rearrange("p (f n) -> p f n", f=NF) for _ in range(2)]
        BWd = [Buf('wd0'), Buf('wd1')]
        u2T = a16.alloc(8 * GT).rearrange("p (k t) -> p k t", k=8)
        Bu2 = Buf('u2T')
        actT = a16.alloc(NF * GT).rearrange("p (f t) -> p f t", f=NF)
        Bact = Buf('actT')
        yacc = a32.alloc(NT * 1024).rearrange("p (t n) -> p t n", t=NT)
        By = [Buf('y%d' % i) for i in range(NT)]
        xt = [a32.alloc(1024), a32.alloc(1024)]
        Bxt = [Buf('xt0'), Buf('xt1')]
        xn = a32.alloc(1024)
        Bxn = Buf('xn')
        ss = a32.alloc(4)
        u32 = a32.alloc(8 * 128).rearrange("p (k t) -> p k t", k=8)
        Bu32 = Buf('u32')
        gall = a32.alloc(NT * nexp).rearrange("p (t n) -> p t n", t=NT)
        Bgall = Buf('gall')
        rt = a32.alloc(64)
        Brt = Buf('rt')
        sg = [a32.alloc(512), a32.alloc(512)]
        Bsg = [Buf('sg0'), Buf('sg1')]

        wi = 0
        di = 0
        for grp in range(ngrp):
            g0 = grp * GT
            for t in range(NT):
                s = t % 2
                P.dma('sp', [(xt[s], hin[g0 + t * 128:g0 + (t + 1) * 128, :])], writes=[Bxt[s]])
                norm_modT(X, xt[s], Bxt[s], G1, SH, Bmod, lambda k, t=t: u2T[:, k, t * 128:(t + 1) * 128], Bu2,
                          xn, Bxn, ss, 0, 1, xn, u32_dst=lambda k: u32[:, k, :], Bu32=Bu32)
                for k in range(8):
                    P.op('pe', lambda e, k=k: e.matmul(X.ps[2][:, 0:nexp], lhsT=u32[:, k, :], rhs=Wr[:, k, :], start=(k == 0), stop=(k == 7)),
                         reads=[Bu32, BWr], writes=[X.psb[2]])
                lg = rt[:, 0:8]
                e1 = rt[:, 8:16]
                lg2 = rt[:, 16:24]
                e2 = rt[:, 24:32]
                m1 = rt[:, 32:33]
                m2 = rt[:, 33:34]
                dl = rt[:, 34:35]
                w1_ = rt[:, 35:36]
                w2_ = rt[:, 36:37]
                gt_ = gall[:, t, :]
                P.op('dve', lambda e, lg=lg: e.tensor_copy(out=lg, in_=X.ps[2][:, 0:nexp]), reads=[X.psb[2]], writes=[Brt])
                P.op('dve', lambda e, lg=lg, m1=m1: e.tensor_reduce(out=m1, in_=lg, axis=AX.X, op=ALU.max), reads=[Brt], writes=[Brt])
                P.op('dve', lambda e, lg=lg, m1=m1, e1=e1: e.tensor_scalar(out=e1, in0=lg, scalar1=m1, scalar2=None, op0=ALU.is_equal), reads=[Brt], writes=[Brt])
                P.op('dve', lambda e, lg=lg, e1=e1, lg2=lg2: e.scalar_tensor_tensor(out=lg2, in0=e1, scalar=NEG, in1=lg, op0=ALU.mult, op1=ALU.add),
                     reads=[Brt], writes=[Brt])
                P.op('dve', lambda e, lg2=lg2, m2=m2: e.tensor_reduce(out=m2, in_=lg2, axis=AX.X, op=ALU.max), reads=[Brt], writes=[Brt])
                P.op('dve', lambda e, lg2=lg2, m2=m2, e2=e2: e.tensor_scalar(out=e2, in0=lg2, scalar1=m2, scalar2=None, op0=ALU.is_equal), reads=[Brt], writes=[Brt])
                P.op('dve', lambda e, m1=m1, m2=m2, dl=dl: e.tensor_tensor(out=dl, in0=m1, in1=m2, op=ALU.subtract), reads=[Brt], writes=[Brt])
                P.op('act', lambda e, dl=dl, w1_=w1_: e.activation(out=w1_, in_=dl, func=AF.Sigmoid), reads=[Brt], writes=[Brt])
                P.op('act', lambda e, dl=dl, w2_=w2_: e.activation(out=w2_, in_=dl, func=AF.Sigmoid, scale=-1.0), reads=[Brt], writes=[Brt])
                P.op('dve', lambda e, e1=e1, w1_=w1_, gt_=gt_: e.tensor_scalar(out=gt_, in0=e1, scalar1=w1_, scalar2=None, op0=ALU.mult),
                     reads=[Brt], writes=[Bgall])
                P.op('dve', lambda e, e2=e2, w2_=w2_, gt_=gt_: e.scalar_tensor_tensor(out=gt_, in0=e2, scalar=w2_, in1=gt_, op0=ALU.mult, op1=ALU.add),
                     reads=[Brt, Bgall], writes=[Bgall])
            for ex in range(nexp):
                wg3 = wg[ex].rearrange("(k p) n -> p k n", p=128)
                wu3 = wu[ex].rearrange("(k p) n -> p k n", p=128)
                wd3 = wd[ex].rearrange("(f p) n -> p f n", p=128)
                it = 0
                for fb in range(NF // FB):
                    wb = wi % NWB
                    wi += 1
                    f0 = fb * FB
                    P.dma('pool', [(Wgb[wb], wg3[:, :, f0 * 128:(f0 + FB) * 128]), (Wub[wb], wu3[:, :, f0 * 128:(f0 + FB) * 128])], writes=[BWg[wb]])
                    for fi in range(FB):
                        f = f0 + fi
                        for half in range(GT // 512):
                            pg = 2 + 2 * (it % 2)
                            pu = pg + 1
                            sgi = it % 2
                            it += 1
                            for (W_, pi) in [(Wgb[wb], pg), (Wub[wb], pu)]:
                                for k in range(8):
                                    P.op('pe', lambda e, k=k, W_=W_, pi=pi, half=half, fi=fi: e.matmul(
                                        X.ps[pi][:, :], lhsT=W_[:, k, fi * 128:(fi + 1) * 128], rhs=u2T[:, k, half * 512:(half + 1) * 512],
                                        start=(k == 0), stop=(k == 7)), reads=[BWg[wb], Bu2], writes=[X.psb[pi]])
                            P.op('act', lambda e, pg=pg, sgi=sgi: e.activation(out=sg[sgi], in_=X.ps[pg][:, :], func=AF.Silu),
                                 reads=[X.psb[pg]], writes=[Bsg[sgi]])
                            P.op('dve', lambda e, pu=pu, sgi=sgi, f=f, half=half: e.tensor_tensor(
                                out=actT[:, f, half * 512:(half + 1) * 512], in0=X.ps[pu][:, :], in1=sg[sgi], op=ALU.mult),
                                reads=[X.psb[pu], Bsg[sgi]], writes=[Bact])
                it = 0
                for q in range(1024 // DQ):
                    db = di % 2
                    di += 1
                    P.dma('pool', [(Wdb[db][:, 0:NF // 2, :], wd3[:, 0:NF // 2, q * DQ:(q + 1) * DQ]),
                                   (Wdb[db][:, NF // 2:NF, :], wd3[:, NF // 2:NF, q * DQ:(q + 1) * DQ])], writes=[BWd[db]])
                    for t in range(NT):
                        pi = 6 + (it % 2)
                        it += 1
                        for f in range(NF):
                            P.op('pe', lambda e, f=f, pi=pi, t=t, db=db: e.matmul(X.ps[pi][:, 0:DQ], lhsT=actT[:, f, t * 128:(t + 1) * 128],
                                                                                 rhs=Wdb[db][:, f, :], start=(f == 0), stop=(f == NF - 1)),
                                 reads=[Bact, BWd[db]], writes=[X.psb[pi]])
                        if ex == 0:
                            P.op('dve', lambda e, pi=pi, t=t, q=q, ex=ex: e.tensor_scalar(out=yacc[:, t, q * DQ:(q + 1) * DQ], in0=X.ps[pi][:, 0:DQ],
                                                                                         scalar1=gall[:, t, ex:ex + 1], scalar2=None, op0=ALU.mult),
                                 reads=[X.psb[pi], Bgall], writes=[By[t]])
                        else:
                            P.op('dve', lambda e, pi=pi, t=t, q=q, ex=ex: e.scalar_tensor_tensor(
                                out=yacc[:, t, q * DQ:(q + 1) * DQ], in0=X.ps[pi][:, 0:DQ], scalar=gall[:, t, ex:ex + 1],
                                in1=yacc[:, t, q * DQ:(q + 1) * DQ], op0=ALU.mult, op1=ALU.add),
                                reads=[X.psb[pi], Bgall, By[t]], writes=[By[t]])
            for t in range(NT):
                s = t % 2
                P.dma('sp', [(xt[s], hin[g0 + t * 128:g0 + (t + 1) * 128, :])], writes=[Bxt[s]])
                P.op('pool', lambda e, t=t: e.tensor_tensor(out=yacc[:, t, :], in0=yacc[:, t, :], in1=gate_bc, op=ALU.mult),
                     reads=[By[t], Bgate], writes=[By[t]])
                P.op('pool', lambda e, t=t, s=s: e.tensor_tensor(out=yacc[:, t, :], in0=yacc[:, t, :], in1=xt[s], op=ALU.add),
                     reads=[By[t], Bxt[s]], writes=[By[t]])
                P.op('act', lambda e, t=t: e.activation(out=xn, in_=yacc[:, t, :], func=AF.Square, accum_out=ss[:, 0:1]), reads=[By[t]], writes=[Bxn])
                P.op('act', lambda e: e.activation(out=ss[:, 1:2], in_=ss[:, 0:1], func=AF.Sqrt, scale=1.0 / 1024.0, bias=X.eps_col),
                     reads=[Bxn, X.Bcst2], writes=[Bxn])
                P.op('dve', lambda e: e.reciprocal(out=ss[:, 2:3], in_=ss[:, 1:2]), reads=[Bxn], writes=[Bxn])
                P.op('dve', lambda e, t=t: e.scalar_tensor_tensor(out=yacc[:, t, :], in0=yacc[:, t, :], scalar=ss[:, 2:3], in1=fg_bc,
                                                                  op0=ALU.mult, op1=ALU.mult), reads=[By[t], Bxn, Bgate], writes=[By[t]])
                P.dma('sp', [(hout[g0 + t * 128:g0 + (t + 1) * 128, :], yacc[:, t, :])], reads=[By[t]], writes=[Buf('o')], sem_buf=By[t])
        P.barrier()


def build_ffn1(ntok=2048, dff=3584, nexp=8):
    nc = bass.Bass("TRN2", target_bir_lowering=False)

    def din(name, shape, dt=F32):
        return nc.dram_tensor(name, shape, dt, kind="ExternalInput").ap()
    hin = din("hin", [ntok, 1024])
    cst_d = din("cst", [128, NCST])
    cT = din("cT", [128, 8])
    w_ada = din("w_ada", [1024, 3072])
    b_adaT = din("b_adaT", [128, 16])
    b_gate = din("b_gate", [1, 1024])
    gainT = din("gainT", [128, 8])
    fgain = din("fgain", [1, 1024])
    wr = din("wr", [1024, nexp])
    wg = din("wg", [nexp, 1024, dff])
    wu = din("wu", [nexp, 1024, dff])
    wd = din("wd", [nexp, dff, 1024])
    hout = nc.dram_tensor("hout", [ntok, 1024], F32, kind="ExternalOutput").ap()
    NF = dff // 128
    FB = 4
    GT = 1024
    ngrp = ntok // GT
    NT = GT // 128
    DQ = 256

    T = dict(hin=hin, cst_d=cst_d, cT=cT, w_ada=w_ada, b_adaT=b_adaT, b_gate=b_gate, gainT=gainT, fgain=fgain, wr=wr, wg=wg, wu=wu, wd=wd, hout=hout)
    X = setup_ctx(nc)
    with X.st:
        phase_ffn1(X, T, ntok, dff, nexp)
        X.P.emit()
    return nc


def ffn1_inputs(inputs, hmid_cores):
    maps = []
    for c in range(8):
        b, j = c // 2, c % 2
        maps.append({
            "hin": np.ascontiguousarray(hmid_cores[c]),
            "cst": make_consts(j),
            "cT": np.ascontiguousarray(inputs['c'][b].reshape(8, 128).T),
            "w_ada": np.ascontiguousarray(inputs['w_ada'][1][:, 3072:6144]),
            "b_adaT": np.ascontiguousarray(inputs['b_ada'][1][3072:5120].reshape(16, 128).T),
            "b_gate": np.ascontiguousarray(inputs['b_ada'][1][5120:6144][None, :]),
            "gainT": np.ascontiguousarray(inputs['ffn_gain'][1].reshape(8, 128).T),
            "fgain": np.ascontiguousarray(inputs['final_gain'][None, :]),
            "wr": np.ascontiguousarray(inputs['moe_w_router'][0]),
            "wg": np.ascontiguousarray(inputs['moe_w_gate'][0]),
            "wu": np.ascontiguousarray(inputs['moe_w_up'][0]),
            "wd": np.ascontiguousarray(inputs['moe_w_down'][0]),
        })
    return maps


def _run(nc, maps):
    res = run_bass_kernel_spmd(nc, maps, core_ids=list(range(8)))
    return res.results


def _kernel_unfused_impl(**inputs):
    inputs = {k: np.asarray(v) for k, v in inputs.items()}
    r0 = _run(build_att0(), att0_inputs(inputs))
    hmid0 = [np.asarray(r0[c]["hmid"]) for c in range(8)]
    r1 = _run(build_ffn0(), ffn0_inputs(inputs, hmid0))
    h1c = [np.asarray(r1[c]["hout"]) for c in range(8)]
    h1_full = gather_blocks(r1, "hout")
    r2 = _run(build_kv1(), kv1_inputs(inputs, h1_full))
    kv_res = [{k: np.asarray(r2[c][k]) for k in ["kslcT", "kwinT", "vslc", "vwin", "kcT", "vc"]} for c in range(8)]
    r3 = _run(build_att1(), att1_inputs(inputs, h1c, kv_res))
    hmid1 = [np.asarray(r3[c]["hmid"]) for c in range(8)]
    r4 = _run(build_ffn1(), ffn1_inputs(inputs, hmid1))
    return gather_blocks(r4, "hout").astype(np.float32)


def build_fused(stop_after=None):
    nc = bass.Bass("TRN2", target_bir_lowering=False)

    def din(name, shape, dt=F32):
        return nc.dram_tensor(name, shape, dt, kind="ExternalInput").ap()

    def dint(name, shape, dt=F32):
        return nc.dram_tensor(name, shape, dt, kind="Internal").ap()
    xk = din("xk", [NKC * 128, 1024])
    posk = din("posk", [1, NKC * 128], I32)
    posq = din("posq", [1, NQB * 128], I32)
    cst = din("cst", [128, NCST1])
    cT = din("cT", [128, 8])
    w_ada = din("w_ada", [2, 1024, 6144])
    b_adaT = din("b_adaT", [2, 128, 48])
    b_row = din("b_row", [2, 1, 6144])
    agT = din("agT", [2, 128, 8])
    fgT = din("fgT", [2, 128, 8])
    kvgT = din("kvgT", [128, 8])
    w_kv_ada = din("w_kv_ada", [1024, 2048])
    b_kvT = din("b_kvT", [128, 16])
    a_w_in = din("a_w_in", [1024, 2120])
    a_w_out = din("a_w_out", [1024, 1024])
    b_w_q = din("b_w_q", [1024, 1072])
    b_w_out = din("b_w_out", [1024, 1024])
    w_kv = din("w_kv", [1024, 1536])
    w1k = din("w1k", [2048, 256])
    w1v = din("w1v", [2048, 256])
    w2k = din("w2k", [256, 64])
    w2v = din("w2v", [256, 64])
    peTk = din("peTk", [64, 32])
    peTv = din("peTv", [64, 32])
    fwg = din("fwg", [1024, 2816])
    fwu = din("fwu", [1024, 2816])
    fwd = din("fwd", [2816, 1024])
    mwr = din("mwr", [1024, 8])
    if stop_after is None:
        mwg = din("mwg", [8, 1024, 3584])
        mwu = din("mwu", [8, 1024, 3584])
        mwd = din("mwd", [8, 3584, 1024])
    fgain = din("fgain", [1, 1024])
    cmpMb = din("cmpMb", [NQB, 128, 256], BF16)
    selvm = din("selvm", [NQB, 128, 64])
    selva = din("selva", [NQB, 128, 64])
    agg = din("agg", [256, 64], BF16)
    ridx = din("ridx", [128, NKC], I32)
    out = nc.dram_tensor("out", [NQB * 128, 1024], F32, kind="ExternalOutput").ap()
    hmid0 = dint("hmid0", [NQB * 128, 1024])
    h1own = dint("h1own", [NQB * 128, 1024])
    h1pair = dint("h1pair", [2 * NQB * 128, 1024])
    hmid1 = dint("hmid1", [NQB * 128, 1024])
    i_kslcT = dint("i_kslcT", [64, 4 * NKC * 128], BF16)
    i_kwinT = dint("i_kwinT", [64, 4 * NKC * 128], BF16)
    i_vslc = dint("i_vslc", [NKC * 128, 256], BF16)
    i_vwin = dint("i_vwin", [NKC * 128, 256], BF16)
    i_kcT = dint("i_kcT", [64, 1024], BF16)
    i_vc = dint("i_vc", [256, 256], BF16)

    X = setup_ctx(nc)
    P = X.P
    with X.st:
        def dbg_out(src, rows):
            dbg = nc.dram_tensor("dbg", [rows, 1024], F32, kind="ExternalOutput").ap()
            X.carve(15000)
            tl = X.a32.alloc(1024)
            for r in range(rows // 128):
                Bt = Buf('dbg')
                P.dma('sp', [(tl, src[r * 128:(r + 1) * 128, :])], writes=[Bt])
                P.dma('sp', [(dbg[r * 128:(r + 1) * 128, :], tl)], reads=[Bt], writes=[Buf('o')], sem_buf=Bt)
                P.barrier()
            P.emit()
            return nc
        phase_att0(X, dict(xk=xk, posk=posk, cst_d=cst, cT=cT, w_ada=w_ada[0][:, 0:3072], b_adaT=b_adaT[0][:, 0:16],
                           b_gate=b_row[0][:, 2048:3072], gainT=agT[0], w_in=a_w_in, w_out=a_w_out, hmid=hmid0))
        P.emit()
        if stop_after == 'att0':
            return dbg_out(hmid0, NQB * 128)
        if stop_after in ('att0q1', 'att0q2', 'att0q3'):
            X.carve(20000)
            tq = X.a32.alloc(4096)
            if stop_after == 'att0q1':
                P.dma('sp', [(tq[:, 0:1024], b_row[0][:, 5120:6144].partition_broadcast(128))], writes=[Buf('q')])
            elif stop_after == 'att0q2':
                P.dma('sp', [(tq.rearrange("p (k n) -> p k n", k=8), w_ada[0][:, 3072:3584].rearrange("(k p) n -> p k n", p=128))], writes=[Buf('q')])
            else:
                P.dma('sp', [(tq[:, 0:8], cT[:, :]), (tq[:, 8:24], b_adaT[0][:, 24:40]), (tq[:, 24:32], fgT[0][:, :])], writes=[Buf('q')])
            P.barrier()
            stop_after = 'att0r'
        if stop_after == 'att0m':
            for i in range(0, NBIG, 2000):
                P.op('pool', lambda e, i=i: e.memset(X.big[:, i:min(i + 2000, NBIG)], 12345.0), writes=[Buf('z')])
            P.barrier()
            stop_after = 'att0r'
        if stop_after == 'att0p':
            dbgA = nc.dram_tensor("dbgA", [NQB * 128, 1024], F32, kind="ExternalOutput").ap()
            X.carve(52000)
            X.a32.alloc(34000)
            hrA = X.a32.alloc(16 * 1024).rearrange("p (t n) -> p t n", t=16)
            BsA = [Buf('rA%d' % t) for t in range(16)]
            for t in range(16):
                P.dma('sp', [(hrA[:, t, :], hmid0[t * 128:(t + 1) * 128, :])], writes=[BsA[t]])
            for t in range(16):
                P.dma('sp', [(dbgA[t * 128:(t + 1) * 128, :], hrA[:, t, :])], reads=[BsA[t]], writes=[Buf('o')], sem_buf=BsA[t])
            P.barrier()
            X.carve(15000)
            load_consts(X, cst)
            X.eps_col = X.a32.alloc(1)
            X.Bcst2 = Buf('cst2')
            P.op('pool', lambda e: e.memset(X.eps_col, 1e-6), writes=[X.Bcst2])
            ffn_phase0(X, cT, w_ada[0][:, 3072:6144], b_adaT[0][:, 24:40], b_row[0][:, 5120:6144], fgT[0])
            P.barrier()
            stop_after = 'att0r'
        if stop_after == 'att0r':
            dbg = nc.dram_tensor("dbg", [NQB * 128, 1024], F32, kind="ExternalOutput").ap()
            X.carve(52000)
            X.a32.alloc(34000)
            hr = X.a32.alloc(16 * 1024).rearrange("p (t n) -> p t n", t=16)
            Bs = [Buf('r%d' % t) for t in range(16)]
            for t in range(16):
                P.dma('sp', [(hr[:, t, :], hmid0[t * 128:(t + 1) * 128, :])], writes=[Bs[t]])
            for t in range(16):
                P.dma('sp', [(dbg[t * 128:(t + 1) * 128, :], hr[:, t, :])], reads=[Bs[t]], writes=[Buf('o')], sem_buf=Bs[t])
            P.barrier()
            P.emit()
            return nc
        if stop_after == 'att0s':
            X.carve(15000)
            Wt = X.a16.alloc(22 * 1024).rearrange("p (f n) -> p f n", f=22)
            P.dma('pool', [(Wt, fwd.rearrange("(f p) n -> p f n", p=128))], writes=[Buf('wt')])
            P.barrier()
            return dbg_out(hmid0, NQB * 128)
        phase_ffn0(X, dict(hin=(hmid0 if stop_after != 'ffn0y' else din('hin_dbg', [NQB * 128, 1024])), cst_d=cst, cT=cT, w_ada=w_ada[0][:, 3072:6144], b_adaT=b_adaT[0][:, 24:40],
                           b_gate=b_row[0][:, 5120:6144], gainT=fgT[0], wg=fwg, wu=fwu, wd=fwd,
                           hout=(h1own if stop_after not in ('ffn0x', 'ffn0y') else nc.dram_tensor("dbg", [NQB * 128, 1024], F32, kind="ExternalOutput").ap())))
        P.emit()
        if stop_after in ('ffn0x', 'ffn0y'):
            return nc
        if stop_after == 'ffn0':
            return dbg_out(h1own, NQB * 128)
        Bcc = Buf('cc')
        P.dma('pool', None, writes=[Bcc], inc=1,
              fns=[lambda e, q=q: e.collective_compute("AllGather", ALU.bypass, replica_groups=[[0, 1], [2, 3], [4, 5], [6, 7]],
                                                       ins=[h1own[q * 512:(q + 1) * 512, :]], outs=[h1pair[q * 1024:(q + 1) * 1024, :]])
                   for q in range(4)])
        P.barrier()
        P.emit()
        if stop_after == 'cc':
            return dbg_out(h1pair, 2 * NQB * 128)
        phase_kv1(X, dict(hk=h1pair, posk=posk, cst_d=cst, cT=cT, w_ada=w_kv_ada, b_adaT=b_kvT, gainT=kvgT, w_kv=w_kv,
                          w1k=w1k, w1v=w1v, w2k=w2k, w2v=w2v, peTk=peTk, peTv=peTv, ridx=ridx,
                          o_kslcT=i_kslcT, o_kwinT=i_kwinT, o_vslc=i_vslc, o_vwin=i_vwin, o_kcT=i_kcT, o_vc=i_vc))
        P.emit()
        phase_att1(X, dict(hq=h1own, posq=posq, cst_d=cst, cT=cT, w_ada=w_ada[1][:, 0:3072], b_adaT=b_adaT[1][:, 0:16],
                           b_gate=b_row[1][:, 2048:3072], gainT=agT[1], w_q=b_w_q, w_out=b_w_out,
                           kslcT_d=i_kslcT, kwinT_d=i_kwinT, vslc_d=i_vslc, vwin_d=i_vwin, kcT_d=i_kcT, vc_d=i_vc,
                           cmpMb_d=cmpMb, selvm_d=selvm, selva_d=selva, agg_d=agg, hmid=hmid1))
        P.emit()
        phase_ffn1(X, dict(hin=hmid1, cst_d=cst, cT=cT, w_ada=w_ada[1][:, 3072:6144], b_adaT=b_adaT[1][:, 24:40],
                           b_gate=b_row[1][:, 5120:6144], gainT=fgT[1], fgain=fgain, wr=mwr, wg=mwg, wu=mwu, wd=mwd, hout=out))
        P.emit()
    return nc


def fused_inputs(inputs):
    A = lambda a: np.ascontiguousarray(a)
    maps = []
    shared = {
        "w_ada": A(inputs['w_ada']),
        "b_adaT": A(inputs['b_ada'].reshape(2, 48, 128).transpose(0, 2, 1)),
        "b_row": A(inputs['b_ada'][:, None, :]),
        "agT": A(inputs['attn_gain'].reshape(2, 8, 128).transpose(0, 2, 1)),
        "fgT": A(inputs['ffn_gain'].reshape(2, 8, 128).transpose(0, 2, 1)),
        "kvgT": A(inputs['kv_gain'].reshape(8, 128).T),
        "w_kv_ada": A(inputs['w_kv_ada']),
        "b_kvT": A(inputs['b_kv_ada'].reshape(16, 128).T),
        "a_w_in": A(inputs['a_w_in'][0]), "a_w_out": A(inputs['a_w_out'][0]),
        "b_w_q": A(inputs['b_w_q'][0]), "b_w_out": A(inputs['b_w_out'][0]),
        "w_kv": A(inputs['w_kv']),
        "w1k": A(inputs['cmp_w1_k']), "w1v": A(inputs['cmp_w1_v']), "w2k": A(inputs['cmp_w2_k']), "w2v": A(inputs['cmp_w2_v']),
        "peTk": A(inputs['cmp_pe_k'].T), "peTv": A(inputs['cmp_pe_v'].T),
        "fwg": A(inputs['ffn_w_gate'][0]), "fwu": A(inputs['ffn_w_up'][0]), "fwd": A(inputs['ffn_w_down'][0]),
        "mwr": A(inputs['moe_w_router'][0]), "mwg": A(inputs['moe_w_gate'][0]), "mwu": A(inputs['moe_w_up'][0]),
        "mwd": A(inputs['moe_w_down'][0]),
        "fgain": A(inputs['final_gain'][None, :]),
    }
    tabs = [make_tables1(0), make_tables1(1)]
    for c in range(8):
        b, j = c // 2, c % 2
        pos = np.asarray(inputs['positions'][b], np.int32)
        posq = np.concatenate([pos[(2 * m + j) * 128:(2 * m + j + 1) * 128] for m in range(NQB)])
        ridx = np.zeros((128, NKC), np.int32)
        for sc in range(NKC):
            gc = max(0, sc - (1 - j))
            o_ = (gc // 2) * 128 + np.arange(128)
            ridx[:, sc] = (o_ // 512) * 1024 + (gc % 2) * 512 + (o_ % 512)
        mp = dict(shared)
        mp.update({
            "xk": storage_order(np.asarray(inputs['x'][b], np.float32), j),
            "posk": A(storage_order(pos, j)[None, :]),
            "posq": A(posq[None, :]),
            "cst": make_consts1(j),
            "cT": A(inputs['c'][b].reshape(8, 128).T),
            "ridx": ridx,
        })
        mp.update(tabs[j])
        maps.append(mp)
    return maps


def kernel_unfused(**inputs):
    return _kernel_unfused_impl(**inputs)


def kernel(**inputs):
    inputs = {k: np.asarray(v) for k, v in inputs.items()}
    res = _run(build_fused(), fused_inputs(inputs))
    return gather_blocks(res, "out").astype(np.float32)
```

```python
import contextlib
import numpy as np
import concourse.bass as bass
import concourse.mybir as mybir
from concourse.bass_utils import run_bass_kernel_spmd

F32 = mybir.dt.float32
BF16 = mybir.dt.bfloat16
I32 = mybir.dt.int32
AF = mybir.ActivationFunctionType
ALU = mybir.AluOpType
AX = mybir.AxisListType

ENGS = ['pe', 'act', 'dve', 'pool', 'sp']
NEG = -1.0e30
MASKV = -30000.0
TWO_PI = 6.283185


class Buf:
    __slots__ = ('w', 'r', 'name', 'dsem')

    def __init__(self, name=''):
        self.name = name
        self.w = None
        self.r = []
        self.dsem = None


class Prog:
    def __init__(self, nc, same_engine_sync=True):
        self.nc = nc
        self.ops = {e: [] for e in ENGS}
        self.cnt = {e: 0 for e in ENGS}
        self.known = {e: {} for e in ENGS}
        self.ndsem = 0
        self.dsem_val = {}
        self.same_engine_sync = same_engine_sync

    def _deps(self, eng, reads, writes):
        toks = []
        for b in reads:
            if b.w is not None:
                toks.append(b.w)
        for b in writes:
            if b.w is not None:
                toks.append(b.w)
            toks.extend(b.r)
        need = {}
        for (k, v) in toks:
            if k == eng and (eng == 'pe' or not self.same_engine_sync):
                continue
            if self.known[eng].get(k, 0) >= v:
                continue
            if need.get(k, 0) < v:
                need[k] = v
        for k, v in need.items():
            self.known[eng][k] = v
        return list(need.items())

    def _commit(self, tok, reads, writes):
        for b in reads:
            b.r.append(tok)
        for b in writes:
            b.w = tok
            b.r = []

    def op(self, eng, fn, reads=(), writes=()):
        waits = self._deps(eng, reads, writes)
        self.cnt[eng] += 1
        tok = (eng, self.cnt[eng])
        self.ops[eng].append((waits, fn, (eng, 1)))
        self._commit(tok, reads, writes)
        return tok

    def dma(self, q, items, reads=(), writes=(), sem_buf=None, fns=None, inc=16, **kw):
        sb = sem_buf if sem_buf is not None else writes[0]
        if sb.dsem is None:
            sb.dsem = ('d', self.ndsem)
            self.dsem_val[sb.dsem] = 0
            self.ndsem += 1
        key = sb.dsem
        waits = self._deps(q, reads, writes)
        if fns is None:
            fns = []
            for (o, a) in items:
                def fn(e, o=o, a=a):
                    return e.dma_start(out=o, in_=a, **kw)
                fns.append(fn)
        for i, fn in enumerate(fns):
            self.dsem_val[key] += inc
            self.ops[q].append((waits if i == 0 else [], fn, (key, inc)))
        tok = (key, self.dsem_val[key])
        self._commit(tok, reads, writes)
        return tok

    def barrier(self):
        for e in ENGS:
            waits = []
            for k in ENGS:
                if k != e and self.cnt[k] > self.known[e].get(k, 0):
                    waits.append((k, self.cnt[k]))
                    self.known[e][k] = self.cnt[k]
            for k, v in self.dsem_val.items():
                if v > self.known[e].get(k, 0):
                    waits.append((k, v))
                    self.known[e][k] = v
            if e != 'pe' and self.cnt[e] > self.known[e].get(e, 0):
                waits.append((e, self.cnt[e]))
                self.known[e][e] = self.cnt[e]
            self.ops[e].append((waits, None, None))

    def emit(self):
        nc = self.nc
        st = self.st
        if not hasattr(self, 'sems'):
            self.sems = {}
        sems = self.sems
        for e in ENGS:
            if e not in sems:
                sems[e] = st.enter_context(nc.semaphore('s_' + e))
        for i in range(self.ndsem):
            if ('d', i) not in sems:
                sems[('d', i)] = st.enter_context(nc.semaphore('d_%d' % i))
        with nc.Block() as block:
            def replay(ename):
                def run(e):
                    for (waits, fn, inc) in self.ops[ename]:
                        for (k, v) in waits:
                            e.wait_ge(sems[k], v)
                        if fn is not None:
                            ins = fn(e)
                            ins.then_inc(sems[inc[0]], inc[1])
                return run
            block.tensor(replay('pe'))
            block.scalar(replay('act'))
            block.vector(replay('dve'))
            block.gpsimd(replay('pool'))
            block.sync(replay('sp'))
        self.ops = {e: [] for e in ENGS}


class Arena:
    def __init__(self, t, n):
        self.t = t
        self.n = n
        self.off = 0

    def alloc(self, ncols):
        o = self.off
        self.off += ncols
        assert self.off <= self.n, (self.off, self.n)
        return self.t[:, o:o + ncols]

    def mark(self):
        return self.off

    def reset(self, m=0):
        self.off = m


class Ctx:
    pass


C_IDENT = 0
C_TRI = 128
C_PAD = 256
C_INV = 384
C_SSC = 385
C_CSC = 386
C_ONES = 387
NCST = 392


def make_consts(j):
    c = np.zeros((128, NCST), np.float32)
    c[:, C_IDENT:C_IDENT + 128] = np.eye(128, dtype=np.float32)
    q = np.arange(128)[:, None]
    k = np.arange(128)[None, :]
    c[:, C_TRI:C_TRI + 128] = np.where(k <= q, 0.0, NEG)
    c[:, C_PAD:C_PAD + 128] = NEG if j == 0 else 0.0
    inv = 1.0 / (10000.0 ** (np.arange(0, 64, 2, dtype=np.float32) / np.float32(64)))
    inv = inv.astype(np.float32)
    c[0:64, C_INV] = np.concatenate([inv, inv])
    c[0:32, C_SSC] = -TWO_PI
    c[32:64, C_SSC] = TWO_PI
    c[:, C_CSC] = TWO_PI
    c[:, C_ONES] = 1.0
    return c


NBIG = 52600


def setup_ctx(nc):
    X = Ctx()
    X.nc = nc
    X.st = contextlib.ExitStack()
    X.big = X.st.enter_context(nc.sbuf_tensor("big", [128, NBIG], F32))

    def carve(n32):
        X.a32 = Arena(X.big[:, 0:n32], n32)
        X.a16 = Arena(X.big[:, n32:NBIG].bitcast(BF16), 2 * (NBIG - n32))
    X.carve = carve
    X.ai = X.st.enter_context(nc.sbuf_tensor("ai32", [128, 512], I32))
    X.ps = [X.st.enter_context(nc.psum_tensor("ps%d" % i, [128, 512], F32)) for i in range(8)]
    X.psb = [Buf('ps%d' % i) for i in range(8)]
    X.P = Prog(nc)
    X.P.st = X.st
    return X


def load_consts(X, cst_dram):
    P = X.P
    X.cst = X.a32.alloc(cst_dram.shape[1])
    X.Bcst = Buf('cst')
    P.dma('sp', [(X.cst, cst_dram[:, :])], writes=[X.Bcst])
    X.ident = X.cst[:, C_IDENT:C_IDENT + 128]
    X.irep = X.a16.alloc(512)
    X.Birep = Buf('irep')
    for r in range(4):
        P.op('dve', lambda e, r=r: e.tensor_copy(out=X.irep[:, r * 128:(r + 1) * 128], in_=X.ident),
             reads=[X.Bcst], writes=[X.Birep])


def rope_tables(X, pos_i_dram_row, n, Ct, St, Bt, tmp32, tmpi, Btmp):
    P = X.P
    cst = X.cst
    pi_ = tmpi[0:64, 0:n]
    y = tmp32[0:64, 0:n]
    f = tmp32[0:64, n:2 * n]
    g = tmp32[0:64, 2 * n:3 * n]
    P.dma('sp', [(pi_, pos_i_dram_row.partition_broadcast(64))], writes=[Btmp])
    P.op('dve', lambda e: e.tensor_copy(out=y, in_=pi_), reads=[Btmp], writes=[Btmp])
    P.op('dve', lambda e: e.tensor_scalar(out=y, in0=y, scalar1=cst[0:64, C_INV:C_INV + 1], scalar2=float(1.0 / (2 * np.pi)),
                                           op0=ALU.mult, op1=ALU.mult), reads=[Btmp, X.Bcst], writes=[Btmp])

    def frac_to(dst, src, addc):
        if addc != 0.0:
            P.op('dve', lambda e: e.tensor_scalar_add(out=dst, in0=src, scalar1=addc), reads=[Btmp], writes=[Btmp])
            s2 = dst
        else:
            s2 = src
        P.op('dve', lambda e: e.tensor_copy(out=pi_, in_=s2), reads=[Btmp], writes=[Btmp])
        P.op('dve', lambda e: e.tensor_copy(out=g, in_=pi_), reads=[Btmp], writes=[Btmp])
        P.op('dve', lambda e: e.tensor_tensor(out=dst, in0=s2, in1=g, op=ALU.subtract), reads=[Btmp], writes=[Btmp])
        P.op('dve', lambda e: e.tensor_single_scalar(out=g, in_=dst, scalar=0.5, op=ALU.is_gt), reads=[Btmp], writes=[Btmp])
        P.op('dve', lambda e: e.tensor_tensor(out=dst, in0=dst, in1=g, op=ALU.subtract), reads=[Btmp], writes=[Btmp])
        P.op('dve', lambda e: e.tensor_single_scalar(out=g, in_=dst, scalar=-0.5, op=ALU.is_lt), reads=[Btmp], writes=[Btmp])
        P.op('dve', lambda e: e.tensor_tensor(out=dst, in0=dst, in1=g, op=ALU.add), reads=[Btmp], writes=[Btmp])

    frac_to(f, y, 0.0)
    P.op('act', lambda e: e.activation(out=St, in_=f, func=AF.Sin, scale=cst[0:64, C_SSC:C_SSC + 1]),
         reads=[Btmp, X.Bcst], writes=[Bt])
    frac_to(f, y, 0.25)
    P.op('act', lambda e: e.activation(out=Ct, in_=f, func=AF.Sin, scale=cst[0:64, C_CSC:C_CSC + 1]),
         reads=[Btmp, X.Bcst], writes=[Bt])


def mod_vectors(X, cT_dram, w_ada_dram, b_adaT_dram, ncols, wbuf, Bw, psum_idx, cact, modT, bT):
    P = X.P
    nj = ncols // 128
    Bc = Buf('cact')
    P.dma('sp', [(cact, cT_dram[:, :])], writes=[Bc])
    P.op('act', lambda e: e.activation(out=cact, in_=cact, func=AF.Silu), reads=[Bc], writes=[Bc])
    Bm = Buf('modT')
    P.dma('sp', [(bT, b_adaT_dram[:, :])], writes=[Bm])
    ps = X.ps[psum_idx]
    Bps = X.psb[psum_idx]
    ngrp = ncols // 512
    for jg in range(ngrp):
        s = jg % 2
        w3 = wbuf[s].rearrange("p (k n) -> p k n", k=8)
        P.dma('sp', [(w3, w_ada_dram[:, jg * 512:(jg + 1) * 512].rearrange("(k p) n -> p k n", p=128))], writes=[Bw[s]])
        for jc in range(4):
            J = jg * 4 + jc
            for k in range(8):
                P.op('pe', lambda e, J=J, k=k, jc=jc, w3=w3: e.matmul(ps[:, J:J + 1], lhsT=w3[:, k, jc * 128:(jc + 1) * 128],
                                                                      rhs=cact[:, k:k + 1], start=(k == 0), stop=(k == 7)),
                     reads=[Bw[s], Bc], writes=[Bps])
    P.op('dve', lambda e: e.tensor_tensor(out=modT, in0=ps[:, 0:nj], in1=bT, op=ALU.add), reads=[Bps, Bm], writes=[Bm])
    return modT, Bm, cact, Bc


def bcast_row_vec(X, cact, Bc, w_dram_cols, b_dram_row, out_bc, Bout, wbuf, Bw, psA, psB):
    P = X.P
    crep = X.a32.alloc(8 * 128)
    Bcr = Buf('crep')
    crep3 = crep.rearrange("p (k n) -> p k n", k=8)
    for k in range(8):
        P.op('dve', lambda e, k=k: e.tensor_copy(out=crep3[:, k, :], in_=cact[:, k:k + 1].to_broadcast([128, 128])),
             reads=[Bc], writes=[Bcr])
    P.dma('sp', [(out_bc, b_dram_row.partition_broadcast(128))], writes=[Bout])
    for half in range(2):
        s = half % 2
        w3 = wbuf[s].rearrange("p (k n) -> p k n", k=8)
        P.dma('sp', [(w3, w_dram_cols[:, half * 512:(half + 1) * 512].rearrange("(k p) n -> p k n", p=128))], writes=[Bw[s]])
        pi = psA if half == 0 else psB
        for k in range(8):
            P.op('pe', lambda e, k=k, w3=w3, pi=pi: e.matmul(X.ps[pi][:, :], lhsT=crep3[:, k, :], rhs=w3[:, k, :],
                                                            start=(k == 0), stop=(k == 7)),
                 reads=[Bw[s], Bcr], writes=[X.psb[pi]])
        P.op('dve', lambda e, half=half, pi=pi: e.tensor_tensor(out=out_bc[:, half * 512:(half + 1) * 512], in0=X.ps[pi][:, :],
                                                                in1=out_bc[:, half * 512:(half + 1) * 512], op=ALU.add),
             reads=[X.psb[pi], Bout], writes=[Bout])


def norm_modT(X, xt, Bx, G1, SH, Bmod, uT_dst, Buo, xn, Bxn, ss, psA, psB, junk, u32_dst=None, Bu32=None):
    P = X.P
    P.op('act', lambda e: e.activation(out=junk, in_=xt, func=AF.Square, accum_out=ss[:, 0:1]), reads=[Bx], writes=[Bxn])
    P.op('act', lambda e: e.activation(out=ss[:, 1:2], in_=ss[:, 0:1], func=AF.Sqrt, scale=1.0 / 1024.0, bias=X.eps_col),
         reads=[Bxn, X.Bcst2], writes=[Bxn])
    P.op('dve', lambda e: e.reciprocal(out=ss[:, 2:3], in_=ss[:, 1:2]), reads=[Bxn], writes=[Bxn])
    P.op('dve', lambda e: e.tensor_scalar(out=xn, in0=xt, scalar1=ss[:, 2:3], scalar2=None, op0=ALU.mult),
         reads=[Bx, Bxn], writes=[Bxn])
    for k in range(8):
        pi = psA if k < 4 else psB
        P.op('pe', lambda e, k=k, pi=pi: e.transpose(out=X.ps[pi][:, (k % 4) * 128:(k % 4 + 1) * 128],
                                                     in_=xn[:, k * 128:(k + 1) * 128], identity=X.ident),
             reads=[Bxn, X.Bcst], writes=[X.psb[pi]])
    for k in range(8):
        pi = psA if k < 4 else psB
        P.op('act', lambda e, k=k, pi=pi: e.activation(out=uT_dst(k), in_=X.ps[pi][:, (k % 4) * 128:(k % 4 + 1) * 128],
                                                       func=AF.Identity, scale=G1[:, k:k + 1], bias=SH[:, k:k + 1]),
             reads=[X.psb[pi], Bmod], writes=[Buo])
        if u32_dst is not None:
            P.op('act', lambda e, k=k, pi=pi: e.activation(out=u32_dst(k), in_=X.ps[pi][:, (k % 4) * 128:(k % 4 + 1) * 128],
                                                           func=AF.Identity, scale=G1[:, k:k + 1], bias=SH[:, k:k + 1]),
                 reads=[X.psb[pi], Bmod], writes=[Bu32])


def load_w_bf16(X, dst3, w_dram_cols, Bw, nsplit=1):
    src = w_dram_cols.rearrange("(k p) n -> p k n", p=128)
    items = []
    for s in range(nsplit):
        k0 = s * 8 // nsplit
        k1 = (s + 1) * 8 // nsplit
        items.append((dst3[:, k0:k1, :], src[:, k0:k1, :]))
    X.P.dma('pool', items, writes=[Bw])


NQB = 16
NKC = 32
NBIS = 26


def phase_att0(X, T, nqb=NQB, nbis=NBIS):
    xk = T['xk']
    posk = T['posk']
    cst_d = T['cst_d']
    cT = T['cT']
    w_ada = T['w_ada']
    b_adaT = T['b_adaT']
    b_gate = T['b_gate']
    gainT = T['gainT']
    w_in = T['w_in']
    w_out = T['w_out']
    hmid = T['hmid']
    X.carve(12700)
    P = X.P
    a32, a16 = X.a32, X.a16
    if True:
        load_consts(X, cst_d)
        X.eps_col = a32.alloc(1)
        X.Bcst2 = Buf('cst2')
        P.op('pool', lambda e: e.memset(X.eps_col, 1e-6), writes=[X.Bcst2])

        gate_bc = a32.alloc(1024)
        Bgate = Buf('gate')
        G1 = a32.alloc(8)
        gT = a32.alloc(8)
        cact = a32.alloc(8)
        modT = a32.alloc(16)
        bT = a32.alloc(16)
        m32 = a32.mark()
        wbuf = [a32.alloc(8 * 512), a32.alloc(8 * 512)]
        Bw = [Buf('wa0'), Buf('wa1')]
        modT, Bmod, cact, Bc = mod_vectors(X, cT, w_ada[:, 0:2048], b_adaT, 2048, wbuf, Bw, 0, cact, modT, bT)
        bcast_row_vec(X, cact, Bc, w_ada[:, 2048:3072], b_gate, gate_bc, Bgate, wbuf, Bw, 1, 2)
        P.dma('sp', [(gT, gainT[:, :])], writes=[Bmod])
        P.op('dve', lambda e: e.scalar_tensor_tensor(out=G1, in0=modT[:, 8:16], scalar=1.0, in1=gT, op0=ALU.add, op1=ALU.mult),
             reads=[Bmod], writes=[Bmod])
        SH = modT[:, 0:8]
        P.barrier()
        a32.reset(m32)

        KT = a16.alloc(4 * NKC * 128).rearrange("p (g t) -> p g t", g=4)
        IKT = a16.alloc(NKC * 128)
        VAf = a16.alloc(NKC * 260)
        VA = VAf.rearrange("p (c n) -> p c n", c=NKC)
        BKV = Buf('kv')
        P.op('pool', lambda e: e.memset(VAf, 1.0), writes=[BKV])
        m16 = a16.mark()

        WA = a16.alloc(8 * 576).rearrange("p (k n) -> p k n", k=8)
        WAp = a16.alloc(8 * 320).rearrange("p (k n) -> p k n", k=8)
        BW = Buf('WA')
        BWp = Buf('WAp')
        w_in3 = w_in.rearrange("(k p) n -> p k n", p=128)
        P.dma('pool', [(WA[:, :, 0:512], w_in3[:, :, 1024:1536]), (WA[:, :, 512:576], w_in3[:, :, 2048:2112])], writes=[BW])

        def perm_copy(Wsrc, Wdst, pairs, Bs, Bd):
            for (s0, d0, n) in pairs:
                nh = n // 64
                for k in range(8):
                    src = Wsrc[:, k, s0:s0 + n].rearrange("p (h t i) -> p h t i", h=nh, t=2)
                    dst = Wdst[:, k, d0:d0 + n].rearrange("p (h t i) -> p h t i", h=nh, t=2)
                    eng = 'pool' if k % 2 == 0 else 'dve'
                    P.op(eng, lambda e, src=src, dst=dst: e.tensor_copy(out=dst[:, :, 0, :], in_=src[:, :, 1, :]), reads=[Bs], writes=[Bd])
                    P.op(eng, lambda e, src=src, dst=dst: e.tensor_copy(out=dst[:, :, 1, :], in_=src[:, :, 0, :]), reads=[Bs], writes=[Bd])
        perm_copy(WA, WAp, [(0, 0, 256), (512, 256, 64)], BW, BWp)
        uT = a16.alloc(8 * 512).rearrange("p (k t) -> p k t", k=8)
        BuT = Buf('uT')

        xt = [a32.alloc(1024), a32.alloc(1024)]
        Bxt = [Buf('xt0'), Buf('xt1')]
        xn = a32.alloc(1024)
        Bxn = Buf('xn')
        ss = a32.alloc(4)
        junk = xn
        Ct = a16.alloc(1024).bitcast(F32)
        St = a16.alloc(1024).bitcast(F32)
        Btab = Buf('tab')
        tmp32 = a16.alloc(3 * 1024).bitcast(F32)
        Btmp = Buf('ttmp')
        r1 = a32.alloc(512)
        r2 = a32.alloc(512)
        Br = Buf('ropetmp')

        def rope_combine(psA_i, psB_i, n, nh, dst):
            A = X.ps[psA_i][0:64, 0:nh * n].rearrange("p (h t) -> p h t", h=nh)
            B = X.ps[psB_i][0:64, 0:nh * n].rearrange("p (h t) -> p h t", h=nh)
            c_b = Ct[0:64, 0:n].unsqueeze(1).to_broadcast([64, nh, n])
            s_b = St[0:64, 0:n].unsqueeze(1).to_broadcast([64, nh, n])
            t1 = r1[0:64, 0:nh * n].rearrange("p (h t) -> p h t", h=nh)
            t2 = r2[0:64, 0:nh * n].rearrange("p (h t) -> p h t", h=nh)
            P.op('dve', lambda e: e.tensor_tensor(out=t1, in0=A, in1=c_b, op=ALU.mult), reads=[X.psb[psA_i], Btab], writes=[Br])
            P.op('dve', lambda e: e.tensor_tensor(out=t2, in0=B, in1=s_b, op=ALU.mult), reads=[X.psb[psB_i], Btab], writes=[Br])
            return t1, t2

        for grp in range(NKC // 4):
            t0 = grp * 512
            rope_tables(X, posk[:, t0:t0 + 512], 512, Ct[0:64, :], St[0:64, :], Btab, tmp32, X.ai, Btmp)
            for cc in range(4):
                ch = grp * 4 + cc
                s = ch % 2
                P.dma('sp', [(xt[s], xk[ch * 128:(ch + 1) * 128, :])], writes=[Bxt[s]])
                norm_modT(X, xt[s], Bxt[s], G1, SH, Bmod, lambda k, cc=cc: uT[:, k, cc * 128:(cc + 1) * 128], BuT,
                          xn, Bxn, ss, 0, 1, junk)
                for k in range(8):
                    P.op('pe', lambda e, k=k, cc=cc: e.matmul(X.ps[2][:, 0:256], lhsT=uT[:, k, cc * 128:(cc + 1) * 128],
                                                              rhs=WA[:, k, 256:512], start=(k == 0), stop=(k == 7)),
                         reads=[BuT, BW], writes=[X.psb[2]])
                P.op('act', lambda e, ch=ch: e.copy(out=VA[:, ch, :].rearrange("p (g d) -> p g d", g=4)[:, :, 0:64],
                                                    in_=X.ps[2][:, 0:256].rearrange("p (g d) -> p g d", g=4)),
                     reads=[X.psb[2]], writes=[BKV])
            for (kind, c0, c0p, nh) in [('k', 0, 0, 4), ('ik', 512, 256, 1)]:
                for hh in range(nh):
                    for (W_, pi, cb_) in [(WA, 3, c0), (WAp, 4, c0p)]:
                        for k in range(8):
                            P.op('pe', lambda e, k=k, W_=W_, pi=pi, cbase=cb_ + hh * 64: e.matmul(
                                X.ps[pi][0:64, :], lhsT=W_[:, k, cbase:cbase + 64], rhs=uT[:, k, :],
                                start=(k == 0), stop=(k == 7)),
                                reads=[BuT, BW, BWp], writes=[X.psb[pi]])
                    t1, t2 = rope_combine(3, 4, 512, 1, None)
                    if kind == 'k':
                        dst = KT[0:64, hh, t0:t0 + 512]
                    else:
                        dst = IKT[0:64, t0:t0 + 512]
                    P.op('pool', lambda e, dst=dst, t1=t1, t2=t2: e.tensor_tensor(out=dst, in0=t1[:, 0, :], in1=t2[:, 0, :], op=ALU.add),
                         reads=[Br], writes=[BKV])
        P.barrier()

        a16.reset(m16)
        WB = a16.alloc(8 * 1544).rearrange("p (k n) -> p k n", k=8)
        WBp = a16.alloc(8 * 1536).rearrange("p (k n) -> p k n", k=8)
        BW = Buf('WB')
        BWp = Buf('WBp')
        P.dma('pool', [(WB[:, 0:4, 0:1024], w_in3[:, 0:4, 0:1024]), (WB[:, 4:8, 0:1024], w_in3[:, 4:8, 0:1024]),
                       (WB[:, :, 1024:1536], w_in3[:, :, 1536:2048]), (WB[:, :, 1536:1544], w_in3[:, :, 2112:2120])], writes=[BW])
        perm_copy(WB, WBp, [(0, 0, 1024), (1024, 1024, 512)], BW, BWp)
        Wo = a16.alloc(8 * 1024).rearrange("p (k n) -> p k n", k=8)
        BWo = Buf('Wout')
        load_w_bf16(X, Wo, w_out, BWo, nsplit=2)
        QT2 = [a16.alloc(16 * 128).rearrange("p (h t) -> p h t", h=16) for _ in range(2)]
        BQ2 = [Buf('QT0'), Buf('QT1')]
        IQT = a16.alloc(8 * 128).rearrange("p (h t) -> p h t", h=8)
        BIQ = Buf('IQT')
        Ct = a32.alloc(128)
        St = a32.alloc(128)
        tmp32 = a32.alloc(3 * 128)
        iw = a32.alloc(24)
        Biw = Buf('iw')
        Isc = a32.alloc(NKC * 128)
        BI = Buf('I')
        rl = [a32.alloc(512), a32.alloc(512)]
        Brl = [Buf('rl0'), Buf('rl1')]
        bs = a32.alloc(16)
        Bbs = Buf('bs')
        Mb2 = [a16.alloc(NKC * 128) for _ in range(2)]
        BMb2 = [Buf('Mb0'), Buf('Mb1')]
        PT = [a16.alloc(512), a16.alloc(512)]
        BPT = [Buf('pt0'), Buf('pt1')]
        On = a32.alloc(1024)
        BOn = Buf('On')
        rden = a32.alloc(16)
        OnT = a16.alloc(8 * 128).rearrange("p (k t) -> p k t", k=8)
        BOnT = Buf('OnT')
        uq = a16.alloc(8 * 128).rearrange("p (k t) -> p k t", k=8)
        Buq = Buf('uq')

        def geom(m):
            sc = 2 * m + 1
            nk = sc + 1
            return sc, nk, nk * 128

        def frontA(m):
            sc, nk, nkeys = geom(m)
            s = m % 2
            QT = QT2[s]
            P.dma('sp', [(xt[s], xk[sc * 128:(sc + 1) * 128, :])], writes=[Bxt[s]])
            rope_tables(X, posk[:, sc * 128:(sc + 1) * 128], 128, Ct[0:64, 0:128], St[0:64, 0:128], Btab, tmp32, X.ai, Btmp)
            norm_modT(X, xt[s], Bxt[s], G1, SH, Bmod, lambda k: uq[:, k, :], Buq, xn, Bxn, ss, 0, 1, junk)
            for (c0, nb, dstT, Bd) in [(0, 4, QT, BQ2[s]), (1024, 2, IQT, BIQ)]:
                for b4 in range(nb):
                    for (W_, pi) in [(WB, 3), (WBp, 4)]:
                        for hh in range(4):
                            cbase = c0 + (b4 * 4 + hh) * 64
                            for k in range(8):
                                P.op('pe', lambda e, k=k, W_=W_, pi=pi, cbase=cbase, hh=hh: e.matmul(
                                    X.ps[pi][0:64, hh * 128:(hh + 1) * 128], lhsT=W_[:, k, cbase:cbase + 64], rhs=uq[:, k, :],
                                    start=(k == 0), stop=(k == 7)),
                                    reads=[Buq, BW, BWp], writes=[X.psb[pi]])
                    t1, t2 = rope_combine(3, 4, 128, 4, None)
                    P.op('pool', lambda e, dstT=dstT, b4=b4, t1=t1, t2=t2: e.tensor_tensor(
                        out=dstT[0:64, b4 * 4:(b4 + 1) * 4, :], in0=t1, in1=t2, op=ALU.add), reads=[Br], writes=[Bd])
            for k in range(8):
                P.op('pe', lambda e, k=k: e.matmul(X.ps[2][:, 0:8], lhsT=uq[:, k, :], rhs=WB[:, k, 1536:1544],
                                                   start=(k == 0), stop=(k == 7)), reads=[Buq, BW], writes=[X.psb[2]])
            P.op('act', lambda e: e.activation(out=iw[:, 0:8], in_=X.ps[2][:, 0:8], func=AF.Abs),
                 reads=[X.psb[2]], writes=[Biw])
            P.op('dve', lambda e: e.tensor_scalar(out=iw[:, 8:16], in0=X.ps[2][:, 0:8], scalar1=0.0, scalar2=0.5,
                                                   op0=ALU.is_ge, op1=ALU.subtract), reads=[X.psb[2]], writes=[Biw])
            ngr = (nkeys + 511) // 512
            it = 0
            for kg in range(ngr):
                k0 = kg * 512
                wdt = min(512, nkeys - k0)
                for h in range(8):
                    pi = 5 + (it % 2)
                    rs = it % 2
                    it += 1
                    P.op('pe', lambda e, pi=pi, h=h, k0=k0, wdt=wdt: e.matmul(X.ps[pi][:, 0:wdt], lhsT=IQT[0:64, h, :],
                                                                              rhs=IKT[0:64, k0:k0 + wdt], start=True, stop=True),
                         reads=[BIQ], writes=[X.psb[pi]])
                    P.op('act', lambda e, pi=pi, h=h, rs=rs, wdt=wdt: e.activation(out=rl[rs][:, 0:wdt], in_=X.ps[pi][:, 0:wdt],
                                                                                   func=AF.Relu, scale=iw[:, h:h + 1]),
                         reads=[X.psb[pi], Biw], writes=[Brl[rs]])
                    if h == 0:
                        P.op('dve', lambda e, rs=rs, k0=k0, wdt=wdt: e.tensor_scalar(
                            out=Isc[:, k0:k0 + wdt], in0=rl[rs][:, 0:wdt], scalar1=iw[:, 8:9], scalar2=None, op0=ALU.mult),
                            reads=[Brl[rs], Biw], writes=[BI])
                    else:
                        P.op('dve', lambda e, rs=rs, k0=k0, wdt=wdt, h=h: e.scalar_tensor_tensor(
                            out=Isc[:, k0:k0 + wdt], in0=rl[rs][:, 0:wdt], scalar=iw[:, 8 + h:9 + h], in1=Isc[:, k0:k0 + wdt],
                            op0=ALU.mult, op1=ALU.add), reads=[Brl[rs], Biw, BI], writes=[BI])
            Iv = Isc[:, 0:nkeys]
            if m >= 1:
                P.op('dve', lambda e, Iv=Iv: e.tensor_reduce(out=bs[:, 0:1], in_=Iv, axis=AX.X, op=ALU.max), reads=[BI], writes=[Bbs])
                P.op('dve', lambda e, Iv=Iv: e.tensor_reduce(out=bs[:, 1:2], in_=Iv, axis=AX.X, op=ALU.min), reads=[BI], writes=[Bbs])
            P.op('dve', lambda e, sc=sc: e.tensor_tensor(out=Isc[:, sc * 128:(sc + 1) * 128], in0=Isc[:, sc * 128:(sc + 1) * 128],
                                                         in1=X.cst[:, C_TRI:C_TRI + 128], op=ALU.add), reads=[BI, X.Bcst], writes=[BI])
            P.op('dve', lambda e: e.tensor_tensor(out=Isc[:, 0:128], in0=Isc[:, 0:128], in1=X.cst[:, C_PAD:C_PAD + 128], op=ALU.add),
                 reads=[BI, X.Bcst], writes=[BI])
            if m >= 1:
                P.op('dve', lambda e: e.tensor_tensor(out=bs[:, 2:3], in0=bs[:, 0:1], in1=bs[:, 1:2], op=ALU.subtract), reads=[Bbs], writes=[Bbs])
                P.op('dve', lambda e: e.tensor_copy(out=bs[:, 3:4], in_=bs[:, 1:2]), reads=[Bbs], writes=[Bbs])
            else:
                P.op('dve', lambda e: e.memset(bs[:, 3:4], -1.0e29), writes=[Bbs])

        def bis_iter(m, it_b):
            sc, nk, nkeys = geom(m)
            Iv = Isc[:, 0:nkeys]
            cjunk = Mb2[m % 2]
            ck = float(2.0 ** (-it_b))
            P.op('dve', lambda e, ck=ck: e.scalar_tensor_tensor(out=bs[:, 4:5], in0=bs[:, 2:3], scalar=ck, in1=bs[:, 3:4],
                                                                op0=ALU.mult, op1=ALU.add), reads=[Bbs], writes=[Bbs])
            P.op('dve', lambda e, Iv=Iv, nkeys=nkeys, cjunk=cjunk: e.tensor_scalar(out=cjunk[:, 0:nkeys], in0=Iv, scalar1=bs[:, 4:5], scalar2=None,
                                                                                  op0=ALU.is_ge, op1=ALU.add, accum_out=bs[:, 5:6]),
                 reads=[Bbs, BI], writes=[Bbs, BMb2[m % 2]])
            P.op('dve', lambda e: e.tensor_scalar(out=bs[:, 6:7], in0=bs[:, 5:6], scalar1=255.5, scalar2=bs[:, 2:3],
                                                   op0=ALU.is_ge, op1=ALU.mult), reads=[Bbs], writes=[Bbs])
            P.op('dve', lambda e, ck=ck: e.scalar_tensor_tensor(out=bs[:, 3:4], in0=bs[:, 6:7], scalar=ck, in1=bs[:, 3:4],
                                                                op0=ALU.mult, op1=ALU.add), reads=[Bbs], writes=[Bbs])

        def bis_fin(m):
            sc, nk, nkeys = geom(m)
            Iv = Isc[:, 0:nkeys]
            Mb = Mb2[m % 2]
            P.op('dve', lambda e, Iv=Iv, nkeys=nkeys, Mb=Mb: e.tensor_scalar(out=Mb[:, 0:nkeys], in0=Iv, scalar1=bs[:, 3:4], scalar2=MASKV,
                                                                             op0=ALU.is_lt, op1=ALU.mult), reads=[BI, Bbs], writes=[BMb2[m % 2]])

        def back(m, hook):
            sc, nk, nkeys = geom(m)
            s = m % 2
            QT = QT2[s]
            Mb = Mb2[s]
            items = [(g, c) for g in range(4) for c in range(nk)]

            def emit_S(i):
                g, c = items[i]
                pi = 5 + (i % 2)
                P.op('pe', lambda e, pi=pi, g=g, c=c: e.matmul(X.ps[pi][:, :], lhsT=KT[0:64, g, c * 128:(c + 1) * 128],
                                                               rhs=QT[0:64, g * 4:(g + 1) * 4, :], start=True, stop=False),
                     reads=[BQ2[s]], writes=[X.psb[pi]])
                P.op('pe', lambda e, pi=pi, c=c: e.matmul(X.ps[pi][:, :], lhsT=Mb[:, c * 128:(c + 1) * 128], rhs=X.irep,
                                                          start=False, stop=True),
                     reads=[BMb2[s], X.Birep], writes=[X.psb[pi]])
            emit_S(0)
            for i, (g, c) in enumerate(items):
                if i + 1 < len(items):
                    emit_S(i + 1)
                pi = 5 + (i % 2)
                ps_ = i % 2
                ob = 2 if g % 2 == 0 else 7
                P.op('act', lambda e, pi=pi, ps_=ps_: e.activation(out=PT[ps_], in_=X.ps[pi][:, :], func=AF.Exp, scale=0.125),
                     reads=[X.psb[pi]], writes=[BPT[ps_]])
                for r in range(4):
                    P.op('pe', lambda e, r=r, ps_=ps_, c=c, g=g, ob=ob: e.matmul(X.ps[ob][:, r * 65:(r + 1) * 65],
                                                                                 lhsT=PT[ps_][:, r * 128:(r + 1) * 128],
                                                                                 rhs=VA[:, c, g * 65:(g + 1) * 65],
                                                                                 start=(c == 0 and r == 0), stop=(c == nk - 1),
                                                                                 skip_group_check=True),
                         reads=[BPT[ps_]], writes=[X.psb[ob]])
                if c == nk - 1:
                    O3 = X.ps[ob][:, 0:260].rearrange("p (r d) -> p r d", r=4)
                    P.op('dve', lambda e, O3=O3, g=g: e.reciprocal(out=rden[:, g * 4:(g + 1) * 4], in_=O3[:, :, 64]), reads=[X.psb[ob]], writes=[BOn])
                    P.op('dve', lambda e, O3=O3, g=g: e.tensor_tensor(
                        out=On[:, g * 256:(g + 1) * 256].rearrange("p (r d) -> p r d", r=4), in0=O3[:, :, 0:64],
                        in1=rden[:, g * 4:(g + 1) * 4].unsqueeze(2).to_broadcast([128, 4, 64]), op=ALU.mult),
                        reads=[X.psb[ob], BOn], writes=[BOn])
                hook(i, len(items))
            for k in range(8):
                pi = 0 if k < 4 else 1
                P.op('pe', lambda e, k=k, pi=pi: e.transpose(out=X.ps[pi][:, (k % 4) * 128:(k % 4 + 1) * 128],
                                                             in_=On[:, k * 128:(k + 1) * 128], identity=X.ident),
                     reads=[BOn, X.Bcst], writes=[X.psb[pi]])
            for half in range(2):
                P.op('act', lambda e, half=half: e.copy(out=OnT[:, half * 4:(half + 1) * 4, :],
                                                        in_=X.ps[half][:, :].rearrange("p (k t) -> p k t", k=4)),
                     reads=[X.psb[half]], writes=[BOnT])
            for half in range(2):
                pi = 3 + half
                for k in range(8):
                    P.op('pe', lambda e, k=k, pi=pi, half=half: e.matmul(X.ps[pi][:, :], lhsT=OnT[:, k, :],
                                                                         rhs=Wo[:, k, half * 512:(half + 1) * 512],
                                                                         start=(k == 0), stop=(k == 7)),
                         reads=[BOnT, BWo], writes=[X.psb[pi]])
                P.op('dve', lambda e, pi=pi, half=half: e.tensor_tensor(out=On[:, half * 512:(half + 1) * 512], in0=X.ps[pi][:, :],
                                                                        in1=gate_bc[:, half * 512:(half + 1) * 512], op=ALU.mult),
                     reads=[X.psb[pi], Bgate], writes=[BOn])
            P.op('pool', lambda e, s=s: e.tensor_tensor(out=On, in0=On, in1=xt[s], op=ALU.add), reads=[BOn, Bxt[s]], writes=[BOn])
            P.dma('sp', [(hmid[m * 128:(m + 1) * 128, :], On)], reads=[BOn], writes=[Buf('o')], sem_buf=BOn)

        frontA(0)
        bis_fin(0)
        for m in range(nqb):
            nxt = m + 1 if m + 1 < nqb else None
            st_ = {'done': 0}
            if nxt is not None:
                frontA(nxt)

            def hook(i, total, nxt=nxt, st_=st_):
                if nxt is None:
                    return
                target = (nbis * (i + 1) + total - 1) // total
                while st_['done'] < min(target, nbis):
                    st_['done'] += 1
                    bis_iter(nxt, st_['done'])
            back(m, hook)
            if nxt is not None:
                while st_['done'] < nbis:
                    st_['done'] += 1
                    bis_iter(nxt, st_['done'])
                bis_fin(nxt)
        print("att0 arena usage a32", a32.off, "/", a32.n, "a16", a16.off, "/", a16.n)
        P.barrier()


def build_att0(nqb=NQB, nbis=NBIS):
    nc = bass.Bass("TRN2", target_bir_lowering=False)

    def din(name, shape, dt=F32):
        return nc.dram_tensor(name, shape, dt, kind="ExternalInput").ap()
    xk = din("xk", [NKC * 128, 1024])
    posk = din("posk", [1, NKC * 128], I32)
    cst_d = din("cst", [128, NCST])
    cT = din("cT", [128, 8])
    w_ada = din("w_ada", [1024, 3072])
    b_adaT = din("b_adaT", [128, 16])
    b_gate = din("b_gate", [1, 1024])
    gainT = din("gainT", [128, 8])
    w_in = din("w_in", [1024, 2120])
    w_out = din("w_out", [1024, 1024])
    hmid = nc.dram_tensor("hmid", [nqb * 128, 1024], F32, kind="ExternalOutput").ap()

    T = dict(xk=xk, posk=posk, cst_d=cst_d, cT=cT, w_ada=w_ada, b_adaT=b_adaT, b_gate=b_gate, gainT=gainT, w_in=w_in, w_out=w_out, hmid=hmid)
    X = setup_ctx(nc)
    with X.st:
        phase_att0(X, T, nqb, nbis)
        X.P.emit()
    return nc


def att0_inputs(inputs, layer=0):
    x = np.asarray(inputs['x'], np.float32)
    pos = np.asarray(inputs['positions'], np.int32)
    maps = []
    for c in range(8):
        b, j = c // 2, c % 2
        if j == 0:
            xk = np.concatenate([np.zeros((128, 1024), np.float32), x[b, 0:31 * 128]], axis=0)
            pk = np.concatenate([np.zeros((128,), np.int32), pos[b, 0:31 * 128]])
        else:
            xk = x[b]
            pk = pos[b]
        maps.append({
            "xk": np.ascontiguousarray(xk), "posk": np.ascontiguousarray(pk[None, :]),
            "cst": make_consts(j),
            "cT": np.ascontiguousarray(inputs['c'][b].reshape(8, 128).T),
            "w_ada": np.ascontiguousarray(inputs['w_ada'][layer][:, 0:3072]),
            "b_adaT": np.ascontiguousarray(inputs['b_ada'][layer][0:2048].reshape(16, 128).T),
            "b_gate": np.ascontiguousarray(inputs['b_ada'][layer][2048:3072][None, :]),
            "gainT": np.ascontiguousarray(inputs['attn_gain'][layer].reshape(8, 128).T),
            "w_in": np.ascontiguousarray(inputs['a_w_in'][0]),
            "w_out": np.ascontiguousarray(inputs['a_w_out'][0]),
        })
    return maps


def gather_blocks(res, key, nqb=NQB):
    out = np.zeros((4, 4096, 1024), np.float32)
    for c in range(8):
        b, j = c // 2, c % 2
        r = res[c][key]
        for m in range(nqb):
            i = 2 * m + j
            out[b, i * 128:(i + 1) * 128] = r[m * 128:(m + 1) * 128]
    return out


def ffn_phase0(X, cT, w_ada_f, b_adaT, b_gate, gainT):
    P = X.P
    a32 = X.a32
    gate_bc = a32.alloc(1024)
    Bgate = Buf('gate')
    G1 = a32.alloc(8)
    gT = a32.alloc(8)
    cact = a32.alloc(8)
    modT = a32.alloc(16)
    bT = a32.alloc(16)
    m32 = a32.mark()
    wbuf = [a32.alloc(8 * 512), a32.alloc(8 * 512)]
    Bw = [Buf('wa0'), Buf('wa1')]
    modT, Bmod, cact, Bc = mod_vectors(X, cT, w_ada_f[:, 0:2048], b_adaT, 2048, wbuf, Bw, 0, cact, modT, bT)
    bcast_row_vec(X, cact, Bc, w_ada_f[:, 2048:3072], b_gate, gate_bc, Bgate, wbuf, Bw, 1, 2)
    P.dma('sp', [(gT, gainT[:, :])], writes=[Bmod])
    P.op('dve', lambda e: e.scalar_tensor_tensor(out=G1, in0=modT[:, 8:16], scalar=1.0, in1=gT, op0=ALU.add, op1=ALU.mult),
         reads=[Bmod], writes=[Bmod])
    SH = modT[:, 0:8]
    P.barrier()
    a32.reset(m32)
    return G1, SH, Bmod, gate_bc, Bgate


def phase_ffn0(X, T, ntok=2048, dff=2816):
    hin = T['hin']
    cst_d = T['cst_d']
    cT = T['cT']
    w_ada = T['w_ada']
    b_adaT = T['b_adaT']
    b_gate = T['b_gate']
    gainT = T['gainT']
    wg = T['wg']
    wu = T['wu']
    wd = T['wd']
    hout = T['hout']
    NF = dff // 128
    GT = 1024
    ngrp = ntok // GT
    NT = GT // 128
    X.carve(15000)
    P = X.P
    a32, a16 = X.a32, X.a16
    if True:
        load_consts(X, cst_d)
        X.eps_col = a32.alloc(1)
        X.Bcst2 = Buf('cst2')
        P.op('pool', lambda e: e.memset(X.eps_col, 1e-6), writes=[X.Bcst2])
        G1, SH, Bmod, gate_bc, Bgate = ffn_phase0(X, cT, w_ada, b_adaT, b_gate, gainT)

        Wd = a16.alloc(NF * 1024).rearrange("p (f n) -> p f n", f=NF)
        BWd = Buf('Wd')
        wd3 = wd.rearrange("(f p) n -> p f n", p=128)
        nsp = 4
        P.dma('pool', [(Wd[:, (i * NF) // nsp:((i + 1) * NF) // nsp, :], wd3[:, (i * NF) // nsp:((i + 1) * NF) // nsp, :]) for i in range(nsp)],
              writes=[BWd])
        wg3 = wg.rearrange("(k p) n -> p k n", p=128)
        wu3 = wu.rearrange("(k p) n -> p k n", p=128)
        NWB = 3
        Wgb = [a16.alloc(8 * 128).rearrange("p (k n) -> p k n", k=8) for _ in range(NWB)]
        Wub = [a16.alloc(8 * 128).rearrange("p (k n) -> p k n", k=8) for _ in range(NWB)]
        BWg = [Buf('wg%d' % i) for i in range(NWB)]
        u2T = a16.alloc(8 * GT).rearrange("p (k t) -> p k t", k=8)
        Bu2 = Buf('u2T')
        actT = a16.alloc(NF * GT).rearrange("p (f t) -> p f t", f=NF)
        Bact = Buf('actT')
        hres = a32.alloc(NT * 1024).rearrange("p (t n) -> p t n", t=NT)
        Bh = [Buf('h%d' % i) for i in range(NT)]
        xn = a32.alloc(1024)
        Bxn = Buf('xn')
        ss = a32.alloc(4)
        sg = [a32.alloc(512), a32.alloc(512)]
        Bsg = [Buf('sg0'), Buf('sg1')]
        ot = [a32.alloc(1024), a32.alloc(1024)]
        Bot = [Buf('ot0'), Buf('ot1')]

        wi = 0
        for grp in range(ngrp):
            g0 = grp * GT
            for t in range(NT):
                P.dma('sp', [(hres[:, t, :], hin[g0 + t * 128:g0 + (t + 1) * 128, :])], writes=[Bh[t]])
                norm_modT(X, hres[:, t, :], Bh[t], G1, SH, Bmod, lambda k, t=t: u2T[:, k, t * 128:(t + 1) * 128], Bu2,
                          xn, Bxn, ss, 0, 1, xn)
            it = 0
            for f in range(NF):
                wb = wi % NWB
                wi += 1
                P.dma('pool', [(Wgb[wb], wg3[:, :, f * 128:(f + 1) * 128]), (Wub[wb], wu3[:, :, f * 128:(f + 1) * 128])], writes=[BWg[wb]])
                for half in range(GT // 512):
                    pg = 2 + 2 * (it % 2)
                    pu = pg + 1
                    sgi = it % 2
                    it += 1
                    for (W_, pi) in [(Wgb[wb], pg), (Wub[wb], pu)]:
                        for k in range(8):
                            P.op('pe', lambda e, k=k, W_=W_, pi=pi, half=half: e.matmul(X.ps[pi][:, :], lhsT=W_[:, k, :],
                                                                                       rhs=u2T[:, k, half * 512:(half + 1) * 512],
                                                                                       start=(k == 0), stop=(k == 7)),
                                 reads=[BWg[wb], Bu2], writes=[X.psb[pi]])
                    P.op('act', lambda e, pg=pg, sgi=sgi: e.activation(out=sg[sgi], in_=X.ps[pg][:, :], func=AF.Silu),
                         reads=[X.psb[pg]], writes=[Bsg[sgi]])
                    P.op('dve', lambda e, pu=pu, sgi=sgi, f=f, half=half: e.tensor_tensor(out=actT[:, f, half * 512:(half + 1) * 512], in0=X.ps[pu][:, :],
                                                                                        in1=sg[sgi], op=ALU.mult),
                         reads=[X.psb[pu], Bsg[sgi]], writes=[Bact])
            it = 0
            for t in range(NT):
                o = t % 2
                for half in range(2):
                    pi = 6 + (it % 2)
                    it += 1
                    for f in range(NF):
                        P.op('pe', lambda e, f=f, pi=pi, t=t, half=half: e.matmul(X.ps[pi][:, :], lhsT=actT[:, f, t * 128:(t + 1) * 128],
                                                                                 rhs=Wd[:, f, half * 512:(half + 1) * 512],
                                                                                 start=(f == 0), stop=(f == NF - 1)),
                             reads=[Bact, BWd], writes=[X.psb[pi]])
                    P.op('dve', lambda e, pi=pi, o=o, half=half: e.tensor_tensor(out=ot[o][:, half * 512:(half + 1) * 512], in0=X.ps[pi][:, :],
                                                                                in1=gate_bc[:, half * 512:(half + 1) * 512], op=ALU.mult),
                         reads=[X.psb[pi], Bgate], writes=[Bot[o]])
                P.op('pool', lambda e, o=o, t=t: e.tensor_tensor(out=ot[o], in0=ot[o], in1=hres[:, t, :], op=ALU.add),
                     reads=[Bot[o], Bh[t]], writes=[Bot[o]])
                P.dma('sp', [(hout[g0 + t * 128:g0 + (t + 1) * 128, :], ot[o])], reads=[Bot[o]], writes=[Buf('o')], sem_buf=Bot[o])
        P.barrier()


def build_ffn0(ntok=2048, dff=2816):
    nc = bass.Bass("TRN2", target_bir_lowering=False)

    def din(name, shape, dt=F32):
        return nc.dram_tensor(name, shape, dt, kind="ExternalInput").ap()
    hin = din("hin", [ntok, 1024])
    cst_d = din("cst", [128, NCST])
    cT = din("cT", [128, 8])
    w_ada = din("w_ada", [1024, 3072])
    b_adaT = din("b_adaT", [128, 16])
    b_gate = din("b_gate", [1, 1024])
    gainT = din("gainT", [128, 8])
    wg = din("wg", [1024, dff])
    wu = din("wu", [1024, dff])
    wd = din("wd", [dff, 1024])
    hout = nc.dram_tensor("hout", [ntok, 1024], F32, kind="ExternalOutput").ap()
    NF = dff // 128
    GT = 1024
    ngrp = ntok // GT
    NT = GT // 128

    T = dict(hin=hin, cst_d=cst_d, cT=cT, w_ada=w_ada, b_adaT=b_adaT, b_gate=b_gate, gainT=gainT, wg=wg, wu=wu, wd=wd, hout=hout)
    X = setup_ctx(nc)
    with X.st:
        phase_ffn0(X, T, ntok, dff)
        X.P.emit()
    return nc


def ffn0_inputs(inputs, hmid_cores, layer=0):
    maps = []
    for c in range(8):
        b, j = c // 2, c % 2
        maps.append({
            "hin": np.ascontiguousarray(hmid_cores[c]),
            "cst": make_consts(j),
            "cT": np.ascontiguousarray(inputs['c'][b].reshape(8, 128).T),
            "w_ada": np.ascontiguousarray(inputs['w_ada'][layer][:, 3072:6144]),
            "b_adaT": np.ascontiguousarray(inputs['b_ada'][layer][3072:5120].reshape(16, 128).T),
            "b_gate": np.ascontiguousarray(inputs['b_ada'][layer][5120:6144][None, :]),
            "gainT": np.ascontiguousarray(inputs['ffn_gain'][layer].reshape(8, 128).T),
            "wg": np.ascontiguousarray(inputs['ffn_w_gate'][0]),
            "wu": np.ascontiguousarray(inputs['ffn_w_up'][0]),
            "wd": np.ascontiguousarray(inputs['ffn_w_down'][0]),
        })
    return maps


def split_blocks(full, nqb=NQB):
    outs = []
    for c in range(8):
        b, j = c // 2, c % 2
        outs.append(np.concatenate([full[b, (2 * m + j) * 128:(2 * m + j + 1) * 128] for m in range(nqb)], axis=0))
    return outs


NCMP = 255


def phase_kv1(X, T):
    hk = T['hk']
    posk = T['posk']
    cst_d = T['cst_d']
    cT = T['cT']
    w_ada = T['w_ada']
    b_adaT = T['b_adaT']
    gainT = T['gainT']
    w_kv = T['w_kv']
    w1k = T['w1k']
    w1v = T['w1v']
    w2k = T['w2k']
    w2v = T['w2v']
    peTk = T['peTk']
    peTv = T['peTv']
    o_kslcT = T['o_kslcT']
    o_kwinT = T['o_kwinT']
    o_vslc = T['o_vslc']
    o_vwin = T['o_vwin']
    o_kcT = T['o_kcT']
    o_vc = T['o_vc']
    X.carve(14000)
    P = X.P
    a32, a16 = X.a32, X.a16
    if True:
        load_consts(X, cst_d)
        X.eps_col = a32.alloc(1)
        X.Bcst2 = Buf('cst2')
        P.op('pool', lambda e: e.memset(X.eps_col, 1e-6), writes=[X.Bcst2])
        G1 = a32.alloc(8)
        gT = a32.alloc(8)
        cact = a32.alloc(8)
        modT = a32.alloc(16)
        bT = a32.alloc(16)
        m32 = a32.mark()
        wbuf = [a32.alloc(8 * 512), a32.alloc(8 * 512)]
        Bw = [Buf('wa0'), Buf('wa1')]
        modT, Bmod, cact, Bc = mod_vectors(X, cT, w_ada, b_adaT, 2048, wbuf, Bw, 0, cact, modT, bT)
        P.dma('sp', [(gT, gainT[:, :])], writes=[Bmod])
        P.op('dve', lambda e: e.scalar_tensor_tensor(out=G1, in0=modT[:, 8:16], scalar=1.0, in1=gT, op0=ALU.add, op1=ALU.mult),
             reads=[Bmod], writes=[Bmod])
        SH = modT[:, 0:8]
        P.barrier()
        a32.reset(m32)

        W = a16.alloc(8 * 1536).rearrange("p (k n) -> p k n", k=8)
        Wp = a16.alloc(8 * 768).rearrange("p (k n) -> p k n", k=8)
        BW = Buf('W')
        BWp = Buf('Wp')
        load_w_bf16(X, W, w_kv, BW, nsplit=2)
        for (s0, d0) in [(0, 0), (512, 256), (1024, 512)]:
            for k in range(8):
                src = W[:, k, s0:s0 + 256].rearrange("p (h t i) -> p h t i", h=4, t=2)
                dst = Wp[:, k, d0:d0 + 256].rearrange("p (h t i) -> p h t i", h=4, t=2)
                eng = 'pool' if k % 2 == 0 else 'dve'
                P.op(eng, lambda e, src=src, dst=dst: e.tensor_copy(out=dst[:, :, 0, :], in_=src[:, :, 1, :]), reads=[BW], writes=[BWp])
                P.op(eng, lambda e, src=src, dst=dst: e.tensor_copy(out=dst[:, :, 1, :], in_=src[:, :, 0, :]), reads=[BW], writes=[BWp])
        KcT = a16.alloc(4 * NKC * 128).rearrange("p (g t) -> p g t", g=4)
        VcT = a16.alloc(4 * NKC * 128).rearrange("p (g t) -> p g t", g=4)
        Bcmp = Buf('cmpstore')
        xt = [a32.alloc(1024), a32.alloc(1024)]
        Bxt = [Buf('xt0'), Buf('xt1')]
        xn = a32.alloc(1024)
        Bxn = Buf('xn')
        ss = a32.alloc(4)
        uT = a16.alloc(8 * 512).rearrange("p (k t) -> p k t", k=8)
        BuT = Buf('uT')
        Ct = a32.alloc(512)
        St = a32.alloc(512)
        Btab = Buf('tab')
        tmp32 = a32.alloc(3 * 512)
        Btmp = Buf('ttmp')
        r1 = a32.alloc(512)
        r2 = a32.alloc(512)
        Br = Buf('ropetmp')
        kst = [a16.alloc(512), a16.alloc(512)]
        Bkst = [Buf('kst0'), Buf('kst1')]
        vst = [a16.alloc(512), a16.alloc(512)]
        Bvst = [Buf('vst0'), Buf('vst1')]
        ridx_t = None
        if T.get('ridx') is not None:
            ridx_t = a32.alloc(NKC).bitcast(I32)
            Bridx = Buf('ridx')
            P.dma('sp', [(ridx_t, T['ridx'][:, :])], writes=[Bridx])
        ki = 0
        for grp in range(NKC // 4):
            t0 = grp * 512
            rope_tables(X, posk[:, t0:t0 + 512], 512, Ct[0:64, :], St[0:64, :], Btab, tmp32, X.ai, Btmp)
            for cc in range(4):
                ch = grp * 4 + cc
                s = ch % 2
                if ridx_t is None:
                    P.dma('sp', [(xt[s], hk[ch * 128:(ch + 1) * 128, :])], writes=[Bxt[s]])
                else:
                    P.dma('pool', None, reads=[Bridx], writes=[Bxt[s]],
                          fns=[lambda e, s=s, ch=ch: e.indirect_dma_start(out=xt[s], out_offset=None, in_=hk[:, :],
                                                                          in_offset=bass.IndirectOffsetOnAxis(ap=ridx_t[:, ch:ch + 1], axis=0))])
                norm_modT(X, xt[s], Bxt[s], G1, SH, Bmod, lambda k, cc=cc: uT[:, k, cc * 128:(cc + 1) * 128], BuT,
                          xn, Bxn, ss, 0, 1, xn)
                for (vi, c0) in [(0, 768), (1, 1280)]:
                    for k in range(8):
                        P.op('pe', lambda e, k=k, cc=cc, vi=vi, c0=c0: e.matmul(X.ps[2][:, vi * 256:(vi + 1) * 256],
                                                                               lhsT=uT[:, k, cc * 128:(cc + 1) * 128],
                                                                               rhs=W[:, k, c0:c0 + 256], start=(k == 0 and vi == 0), stop=(k == 7),
                                                                               skip_group_check=True),
                             reads=[BuT, BW], writes=[X.psb[2]])
                P.op('act', lambda e, s=s: e.copy(out=vst[s], in_=X.ps[2][:, :]), reads=[X.psb[2]], writes=[Bvst[s]])
                P.dma('sp', [(o_vslc[ch * 128:(ch + 1) * 128, :], vst[s][:, 0:256]), (o_vwin[ch * 128:(ch + 1) * 128, :], vst[s][:, 256:512])],
                      reads=[Bvst[s]], writes=[Buf('o')], sem_buf=Bvst[s])
            for (kind, c0, c0p) in [('kcmp', 0, 0), ('kslc', 512, 256), ('kwin', 1024, 512), ('vcmp', 256, None)]:
                for hh in range(4):
                    srcs = [(W, 3, c0)] if c0p is None else [(W, 3, c0), (Wp, 4, c0p)]
                    for (W_, pi, cb_) in srcs:
                        for k in range(8):
                            P.op('pe', lambda e, k=k, W_=W_, pi=pi, cbase=cb_ + hh * 64: e.matmul(
                                X.ps[pi][0:64, :], lhsT=W_[:, k, cbase:cbase + 64], rhs=uT[:, k, :],
                                start=(k == 0), stop=(k == 7)),
                                reads=[BuT, BW, BWp], writes=[X.psb[pi]])
                    if kind == 'vcmp':
                        P.op('act', lambda e, hh=hh, t0=t0: e.copy(out=VcT[0:64, hh, t0:t0 + 512], in_=X.ps[3][0:64, :]),
                             reads=[X.psb[3]], writes=[Bcmp])
                        continue
                    t1 = r1[0:64, :]
                    t2 = r2[0:64, :]
                    P.op('dve', lambda e, t1=t1: e.tensor_tensor(out=t1, in0=X.ps[3][0:64, :], in1=Ct[0:64, :], op=ALU.mult),
                         reads=[X.psb[3], Btab], writes=[Br])
                    P.op('dve', lambda e, t2=t2: e.tensor_tensor(out=t2, in0=X.ps[4][0:64, :], in1=St[0:64, :], op=ALU.mult),
                         reads=[X.psb[4], Btab], writes=[Br])
                    if kind == 'kcmp':
                        P.op('pool', lambda e, hh=hh, t0=t0, t1=t1, t2=t2: e.tensor_tensor(out=KcT[0:64, hh, t0:t0 + 512], in0=t1, in1=t2, op=ALU.add),
                             reads=[Br], writes=[Bcmp])
                    else:
                        ks = ki % 2
                        ki += 1
                        P.op('pool', lambda e, ks=ks, t1=t1, t2=t2: e.tensor_tensor(out=kst[ks][0:64, :], in0=t1, in1=t2, op=ALU.add),
                             reads=[Br], writes=[Bkst[ks]])
                        od = o_kslcT if kind == 'kslc' else o_kwinT
                        P.dma('sp', [(od[:, hh * NKC * 128 + t0:hh * NKC * 128 + t0 + 512], kst[ks][0:64, :])],
                              reads=[Bkst[ks]], writes=[Buf('o')], sem_buf=Bkst[ks])
        P.barrier()
        w1s = a16.alloc(32 * 256).rearrange("p (l n) -> p l n", l=32)
        w2s = a16.alloc(2 * 64).rearrange("p (c n) -> p c n", c=2)
        peT = a16.alloc(32)
        Bwc = Buf('wc')
        bvec = a32.alloc(2)
        Bbv = Buf('bvec')
        xh = a32.alloc(256)
        x2 = a32.alloc(256)
        Bxh = Buf('xh')
        actT = a16.alloc(2 * 256).rearrange("p (c n) -> p c n", c=2)
        Bat = Buf('actT')
        ost = a16.alloc(1024)
        Bost = Buf('ost')
        ostv = a16.alloc(2 * 256).rearrange("p (c n) -> p c n", c=2)
        Bostv = Buf('ostv')
        P.op('pool', lambda e: e.memset(ost, 0.0), writes=[Bost])
        P.op('pool', lambda e: e.memset(ostv, 0.0), writes=[Bostv])
        for (kv, w1d, w2d, ped, SRC) in [('k', w1k, w2k, peTk, KcT), ('v', w1v, w2v, peTv, VcT)]:
            P.dma('pool', [(w1s[0:64, :, :], w1d.rearrange("(l d) n -> d l n", d=64)), (w2s, w2d.rearrange("(c p) n -> p c n", p=128)),
                           (peT[0:64, :], ped[:, :])], writes=[Bwc])
            for hc in range(2):
                for l in range(32):
                    P.op('pe', lambda e, hc=hc, l=l: e.matmul(X.ps[0][:, hc:hc + 1], lhsT=w1s[0:64, l, hc * 128:(hc + 1) * 128],
                                                              rhs=peT[0:64, l:l + 1], start=(l == 0), stop=(l == 31)),
                         reads=[Bwc], writes=[X.psb[0]])
            P.op('dve', lambda e: e.tensor_copy(out=bvec, in_=X.ps[0][:, 0:2]), reads=[X.psb[0]], writes=[Bbv])
            for g in range(4):
                for hc in range(2):
                    pi = 1 + hc
                    for l in range(32):
                        rhs = SRC[0:64, g, l:l + 16 * (NCMP - 1) + 1:16]
                        P.op('pe', lambda e, hc=hc, l=l, pi=pi, rhs=rhs: e.matmul(X.ps[pi][:, 0:NCMP], lhsT=w1s[0:64, l, hc * 128:(hc + 1) * 128],
                                                                                  rhs=rhs, start=(l == 0), stop=(l == 31)),
                             reads=[Bwc, Bcmp], writes=[X.psb[pi]])
                    xv = xh[:, 0:NCMP]
                    x2v = x2[:, 0:NCMP]
                    P.op('act', lambda e, pi=pi, hc=hc, xv=xv: e.activation(out=xv, in_=X.ps[pi][:, 0:NCMP], func=AF.Identity, bias=bvec[:, hc:hc + 1]),
                         reads=[X.psb[pi], Bbv], writes=[Bxh])
                    P.op('dve', lambda e, xv=xv, x2v=x2v: e.tensor_tensor(out=x2v, in0=xv, in1=xv, op=ALU.mult), reads=[Bxh], writes=[Bxh])
                    P.op('dve', lambda e, x2v=x2v: e.tensor_scalar(out=x2v, in0=x2v, scalar1=0.044715, scalar2=1.0, op0=ALU.mult, op1=ALU.add),
                         reads=[Bxh], writes=[Bxh])
                    P.op('dve', lambda e, xv=xv, x2v=x2v: e.tensor_tensor(out=x2v, in0=x2v, in1=xv, op=ALU.mult), reads=[Bxh], writes=[Bxh])
                    P.op('act', lambda e, x2v=x2v: e.activation(out=x2v, in_=x2v, func=AF.Sigmoid, scale=1.5957691216), reads=[Bxh], writes=[Bxh])
                    P.op('dve', lambda e, hc=hc, xv=xv, x2v=x2v: e.tensor_tensor(out=actT[:, hc, 0:NCMP], in0=x2v, in1=xv, op=ALU.mult),
                         reads=[Bxh], writes=[Bat])
                if kv == 'k':
                    for hc in range(2):
                        P.op('pe', lambda e, hc=hc: e.matmul(X.ps[3][0:64, 0:NCMP], lhsT=w2s[:, hc, :], rhs=actT[:, hc, 0:NCMP],
                                                             start=(hc == 0), stop=(hc == 1)), reads=[Bat, Bwc], writes=[X.psb[3]])
                    P.op('act', lambda e, g=g: e.copy(out=ost[0:64, g * 256:g * 256 + NCMP], in_=X.ps[3][0:64, 0:NCMP]),
                         reads=[X.psb[3]], writes=[Bost])
                else:
                    for nchk in range(2):
                        n0 = nchk * 128
                        nn = min(128, NCMP - n0)
                        for hc in range(2):
                            P.op('pe', lambda e, hc=hc, n0=n0, nn=nn: e.matmul(X.ps[4][0:nn, 0:64], lhsT=actT[:, hc, n0:n0 + nn], rhs=w2s[:, hc, :],
                                                                               start=(hc == 0), stop=(hc == 1)), reads=[Bat, Bwc], writes=[X.psb[4]])
                        P.op('act', lambda e, g=g, nchk=nchk, nn=nn: e.copy(out=ostv[0:nn, nchk, g * 64:(g + 1) * 64], in_=X.ps[4][0:nn, 0:64]),
                             reads=[X.psb[4]], writes=[Bostv])
        P.dma('sp', [(o_kcT[:, :], ost[0:64, :])], reads=[Bost], writes=[Buf('o')], sem_buf=Bost)
        P.dma('sp', [(o_vc.rearrange("(c p) n -> p c n", p=128), ostv)], reads=[Bostv], writes=[Buf('o')], sem_buf=Bostv)
        P.barrier()


def build_kv1():
    nc = bass.Bass("TRN2", target_bir_lowering=False)

    def din(name, shape, dt=F32):
        return nc.dram_tensor(name, shape, dt, kind="ExternalInput").ap()

    def dout(name, shape, dt=BF16):
        return nc.dram_tensor(name, shape, dt, kind="ExternalOutput").ap()
    hk = din("hk", [NKC * 128, 1024])
    posk = din("posk", [1, NKC * 128], I32)
    cst_d = din("cst", [128, NCST])
    cT = din("cT", [128, 8])
    w_ada = din("w_ada", [1024, 2048])
    b_adaT = din("b_adaT", [128, 16])
    gainT = din("gainT", [128, 8])
    w_kv = din("w_kv", [1024, 1536])
    w1k = din("w1k", [2048, 256])
    w1v = din("w1v", [2048, 256])
    w2k = din("w2k", [256, 64])
    w2v = din("w2v", [256, 64])
    peTk = din("peTk", [64, 32])
    peTv = din("peTv", [64, 32])
    o_kslcT = dout("kslcT", [64, 4 * NKC * 128])
    o_kwinT = dout("kwinT", [64, 4 * NKC * 128])
    o_vslc = dout("vslc", [NKC * 128, 256])
    o_vwin = dout("vwin", [NKC * 128, 256])
    o_kcT = dout("kcT", [64, 4 * 256])
    o_vc = dout("vc", [256, 256])

    T = dict(hk=hk, posk=posk, cst_d=cst_d, cT=cT, w_ada=w_ada, b_adaT=b_adaT, gainT=gainT, w_kv=w_kv, w1k=w1k, w1v=w1v, w2k=w2k, w2v=w2v, peTk=peTk, peTv=peTv, o_kslcT=o_kslcT, o_kwinT=o_kwinT, o_vslc=o_vslc, o_vwin=o_vwin, o_kcT=o_kcT, o_vc=o_vc)
    X = setup_ctx(nc)
    with X.st:
        phase_kv1(X, T)
        X.P.emit()
    return nc


def storage_order(full_b, j):
    if j == 0:
        pad = np.zeros((128,) + full_b.shape[1:], full_b.dtype)
        return np.ascontiguousarray(np.concatenate([pad, full_b[0:31 * 128]], axis=0))
    return np.ascontiguousarray(full_b)


def kv1_inputs(inputs, h1_full):
    maps = []
    for c in range(8):
        b, j = c // 2, c % 2
        maps.append({
            "hk": storage_order(h1_full[b], j),
            "posk": np.ascontiguousarray(storage_order(np.asarray(inputs['positions'][b], np.int32), j)[None, :]),
            "cst": make_consts(j),
            "cT": np.ascontiguousarray(inputs['c'][b].reshape(8, 128).T),
            "w_ada": np.ascontiguousarray(inputs['w_kv_ada']),
            "b_adaT": np.ascontiguousarray(inputs['b_kv_ada'].reshape(16, 128).T),
            "gainT": np.ascontiguousarray(inputs['kv_gain'].reshape(8, 128).T),
            "w_kv": np.ascontiguousarray(inputs['w_kv']),
            "w1k": np.ascontiguousarray(inputs['cmp_w1_k']), "w1v": np.ascontiguousarray(inputs['cmp_w1_v']),
            "w2k": np.ascontiguousarray(inputs['cmp_w2_k']), "w2v": np.ascontiguousarray(inputs['cmp_w2_v']),
            "peTk": np.ascontiguousarray(inputs['cmp_pe_k'].T), "peTv": np.ascontiguousarray(inputs['cmp_pe_v'].T),
        })
    return maps


C_TRIU = NCST
NCST1 = NCST + 128


def make_consts1(j):
    c = np.zeros((128, NCST1), np.float32)
    c[:, 0:NCST] = make_consts(j)
    q = np.arange(128)[:, None]
    k = np.arange(128)[None, :]
    c[:, C_TRIU:C_TRIU + 128] = np.where(k > q, 0.0, NEG)
    return c


def make_tables1(j, nqb=NQB):
    import ml_dtypes
    cmpMb = np.zeros((nqb, 128, 256), np.float32)
    vm = np.zeros((nqb, 128, 64), np.float32)
    va = np.zeros((nqb, 128, 64), np.float32)
    shift = 128 * (1 - j)
    n = np.arange(256)[None, :]
    jb_st = np.arange(64)[None, :]
    for m in range(nqb):
        t_st = (2 * m + 1) * 128 + np.arange(128)[:, None]
        valid = (n <= 254) & (n >= 8 * (1 - j)) & (16 * n + 31 <= t_st)
        cmpMb[m] = np.where(valid, 0.0, MASKV)
        t_g = t_st - shift
        jb = jb_st - 2 * (1 - j)
        jt = t_g // 64
        valid_b = (jb >= 0) & (jb * 64 <= t_g)
        f0 = (jb == 0)
        f1 = (jb == jt)
        f2 = (jb == jt - 1)
        forced = f0 | f1 | f2
        vm[m] = np.where(valid_b & ~forced, 1.0, 0.0)
        a = np.where(f0, 1.0e30, np.where(f1, 0.9e30, np.where(f2, 0.8e30, 0.0)))
        va[m] = np.where(valid_b, a, NEG)
    cs = np.arange(256)[:, None] * 16
    ss_ = np.arange(64)[None, :] * 64
    ov = np.minimum(cs + 32, ss_ + 64) - np.maximum(cs, ss_)
    agg = (np.clip(ov, 0, None) / 32.0).astype(np.float32)
    agg[255] = 0.0
    return {"cmpMb": cmpMb.astype(ml_dtypes.bfloat16), "selvm": vm, "selva": va, "agg": agg.astype(ml_dtypes.bfloat16)}


def phase_att1(X, T, nqb=NQB):
    hq = T['hq']
    posq = T['posq']
    cst_d = T['cst_d']
    cT = T['cT']
    w_ada = T['w_ada']
    b_adaT = T['b_adaT']
    b_gate = T['b_gate']
    gainT = T['gainT']
    w_q = T['w_q']
    w_out = T['w_out']
    kslcT_d = T['kslcT_d']
    kwinT_d = T['kwinT_d']
    vslc_d = T['vslc_d']
    vwin_d = T['vwin_d']
    kcT_d = T['kcT_d']
    vc_d = T['vc_d']
    cmpMb_d = T['cmpMb_d']
    selvm_d = T['selvm_d']
    selva_d = T['selva_d']
    agg_d = T['agg_d']
    hmid = T['hmid']
    X.carve(15000)
    P = X.P
    a32, a16 = X.a32, X.a16
    if True:
        X.cst = a32.alloc(NCST1)
        X.Bcst = Buf('cst')
        P.dma('sp', [(X.cst, cst_d[:, :])], writes=[X.Bcst])
        X.ident = X.cst[:, C_IDENT:C_IDENT + 128]
        X.irep = a16.alloc(512)
        X.Birep = Buf('irep')
        for r in range(4):
            P.op('dve', lambda e, r=r: e.tensor_copy(out=X.irep[:, r * 128:(r + 1) * 128], in_=X.ident), reads=[X.Bcst], writes=[X.Birep])
        trib = a16.alloc(128)
        triub = a16.alloc(128)
        P.op('dve', lambda e: e.tensor_copy(out=trib, in_=X.cst[:, C_TRI:C_TRI + 128]), reads=[X.Bcst], writes=[X.Birep])
        P.op('dve', lambda e: e.tensor_copy(out=triub, in_=X.cst[:, C_TRIU:C_TRIU + 128]), reads=[X.Bcst], writes=[X.Birep])
        X.eps_col = a32.alloc(1)
        tiny = a32.alloc(1)
        X.Bcst2 = Buf('cst2')
        P.op('pool', lambda e: e.memset(X.eps_col, 1e-6), writes=[X.Bcst2])
        P.op('pool', lambda e: e.memset(tiny, 1e-30), writes=[X.Bcst2])
        G1, SH, Bmod, gate_bc, Bgate = ffn_phase0(X, cT, w_ada, b_adaT, b_gate, gainT)

        KsT = a16.alloc(4 * NKC * 128).rearrange("p (g t) -> p g t", g=4)
        VsAf = a16.alloc(NKC * 260)
        VsA = VsAf.rearrange("p (c n) -> p c n", c=NKC)
        kcT = a16.alloc(1024).rearrange("p (g n) -> p g n", g=4)
        vcxf = a16.alloc(2 * 260)
        vcx = vcxf.rearrange("p (c n) -> p c n", c=2)
        agg = a16.alloc(128).rearrange("p (c n) -> p c n", c=2)
        BKV = Buf('kv')
        P.op('pool', lambda e: e.memset(VsAf, 1.0), writes=[BKV])
        P.op('pool', lambda e: e.memset(vcxf, 1.0), writes=[BKV])
        ks3 = kslcT_d.rearrange("p (g t) -> p g t", g=4)
        P.dma('sp', [(KsT[0:64, g, :], ks3[:, g, :]) for g in range(4)], writes=[BKV])
        vs4 = vslc_d.rearrange("(c p) (g d) -> p c g d", p=128, g=4)
        VsA4 = VsAf.rearrange("p (c g d) -> p c g d", c=NKC, g=4)
        P.dma('sp', [(VsA4[:, :, g, 0:64], vs4[:, :, g, :]) for g in range(4)], writes=[BKV])
        P.dma('sp', [(kcT[0:64, :, :], kcT_d.rearrange("p (g n) -> p g n", g=4))], writes=[BKV])
        vcx4 = vcxf.rearrange("p (c g d) -> p c g d", c=2, g=4)
        vc4 = vc_d.rearrange("(c p) (g d) -> p c g d", p=128, g=4)
        P.dma('sp', [(vcx4[:, :, g, 0:64], vc4[:, :, g, :]) for g in range(4)], writes=[BKV])
        P.dma('sp', [(agg, agg_d.rearrange("(c p) n -> p c n", p=128))], writes=[BKV])
        WB = a16.alloc(8 * 1072).rearrange("p (k n) -> p k n", k=8)
        WBp = a16.alloc(8 * 1024).rearrange("p (k n) -> p k n", k=8)
        BW = Buf('WB')
        BWp = Buf('WBp')
        load_w_bf16(X, WB, w_q, BW, nsplit=2)
        for k in range(8):
            src = WB[:, k, 0:1024].rearrange("p (h t i) -> p h t i", h=16, t=2)
            dst = WBp[:, k, 0:1024].rearrange("p (h t i) -> p h t i", h=16, t=2)
            eng = 'pool' if k % 2 == 0 else 'dve'
            P.op(eng, lambda e, src=src, dst=dst: e.tensor_copy(out=dst[:, :, 0, :], in_=src[:, :, 1, :]), reads=[BW], writes=[BWp])
            P.op(eng, lambda e, src=src, dst=dst: e.tensor_copy(out=dst[:, :, 1, :], in_=src[:, :, 0, :]), reads=[BW], writes=[BWp])
        Wo = a16.alloc(8 * 1024).rearrange("p (k n) -> p k n", k=8)
        BWo = Buf('Wout')
        load_w_bf16(X, Wo, w_out, BWo, nsplit=2)
        xt = [a32.alloc(1024), a32.alloc(1024)]
        Bxt = [Buf('xt0'), Buf('xt1')]
        xn = a32.alloc(1024)
        Bxn = Buf('xn')
        ss = a32.alloc(4)
        Ct = a32.alloc(128)
        St = a32.alloc(128)
        Btab = Buf('tab')
        tmp32 = a32.alloc(3 * 128)
        Btmp = Buf('ttmp')
        r1 = a32.alloc(512)
        r2 = a32.alloc(512)
        Br = Buf('ropetmp')
        uq = a16.alloc(8 * 128).rearrange("p (k t) -> p k t", k=8)
        Buq = Buf('uq')
        QT = a16.alloc(16 * 128).rearrange("p (h t) -> p h t", h=16)
        BQ = Buf('QT')
        gts = a32.alloc(48)
        Bgts = Buf('gts')
        gts3 = gts.rearrange("p (h b) -> p h b", b=3)
        KwT = [a16.alloc(4 * 640).rearrange("p (g t) -> p g t", g=4) for _ in range(2)]
        VwAf = [a16.alloc(5 * 260) for _ in range(2)]
        BKw = [Buf('kw0'), Buf('kw1')]
        for i in range(2):
            P.op('pool', lambda e, i=i: e.memset(VwAf[i], 1.0), writes=[BKw[i]])
        cmb = [a16.alloc(256), a16.alloc(256)]
        svm = [a32.alloc(64), a32.alloc(64)]
        sva = [a32.alloc(64), a32.alloc(64)]
        Btb = [Buf('tb0'), Buf('tb1')]
        PT = [a16.alloc(512) for _ in range(4)]
        BPT = [Buf('pt%d' % i) for i in range(4)]
        SB = [5, 6, 0, 1]
        LA = 3
        rden = a32.alloc(8)
        coef = a32.alloc(8)
        Brd = Buf('rden')
        imp = a32.alloc(64)
        imp2 = a32.alloc(64)
        mx = a32.alloc(16)
        Bimp = Buf('imp')
        selb = a16.alloc(64)
        Bselb = Buf('selb')
        Mbs = [a16.alloc(NKC * 128), a16.alloc(NKC * 128)]
        BMbs = [Buf('mbs0'), Buf('mbs1')]
        On = a32.alloc(1024)
        BOn = Buf('On')
        otmp = a32.alloc(256)
        Botmp = Buf('otmp')
        OnT = a16.alloc(8 * 128).rearrange("p (k t) -> p k t", k=8)
        BOnT = Buf('OnT')
        hm = a32.alloc(1024)
        Bhm = Buf('hm')
        kw3 = kwinT_d.rearrange("p (g t) -> p g t", g=4)

        state = {'it': 0, 'ob': 0}

        def attend(g, chunks, out_slot_fn):
            ob = 2 if state['ob'] % 2 == 0 else 7
            state['ob'] += 1
            n = len(chunks)
            base = state['it']
            state['it'] += n

            def emit_S(ci):
                kl, ml, bias, vr = chunks[ci]
                pi = SB[(base + ci) % 4]
                P.op('pe', lambda e, pi=pi, kl=kl, ml=ml, g=g: e.matmul(X.ps[pi][:, :], lhsT=kl, rhs=QT[0:64, g * 4:(g + 1) * 4, :],
                                                                       start=True, stop=(ml is None)),
                     reads=[BQ, BKw[0], BKw[1], BKV], writes=[X.psb[pi]])
                if ml is not None:
                    P.op('pe', lambda e, pi=pi, ml=ml: e.matmul(X.ps[pi][:, :], lhsT=ml, rhs=X.irep, start=False, stop=True),
                         reads=[X.Birep, BMbs[0], BMbs[1], Btb[0], Btb[1]], writes=[X.psb[pi]])
            for ci in range(min(LA, n)):
                emit_S(ci)
            for ci, (kl, ml, bias, vr) in enumerate(chunks):
                if ci + LA < n:
                    emit_S(ci + LA)
                pi = SB[(base + ci) % 4]
                ps_ = (base + ci) % 4
                if bias is None:
                    P.op('act', lambda e, pi=pi, ps_=ps_: e.activation(out=PT[ps_], in_=X.ps[pi][:, :], func=AF.Exp, scale=0.125),
                         reads=[X.psb[pi]], writes=[BPT[ps_]])
                else:
                    P.op('act', lambda e, pi=pi, ps_=ps_, bias=bias: e.activation(out=PT[ps_], in_=X.ps[pi][:, :], func=AF.Exp, scale=0.125, bias=bias),
                         reads=[X.psb[pi], X.Bcst], writes=[BPT[ps_]])
                for r in range(4):
                    P.op('pe', lambda e, r=r, ps_=ps_, vr=vr, ob=ob, ci=ci, n=n: e.matmul(X.ps[ob][:, r * 65:(r + 1) * 65],
                                                                                       lhsT=PT[ps_][:, r * 128:(r + 1) * 128], rhs=vr,
                                                                                       start=(ci == 0 and r == 0), stop=(ci == n - 1),
                                                                                       skip_group_check=True),
                         reads=[BPT[ps_], BKw[0], BKw[1]], writes=[X.psb[ob]])
                out_slot_fn(ci, ps_)
            return ob

        for m in range(nqb):
            sc = 2 * m + 1
            nk = sc + 1
            nkeys = nk * 128
            s = m % 2
            P.dma('sp', [(xt[s], hq[m * 128:(m + 1) * 128, :])], writes=[Bxt[s]])
            P.dma('sp', [(cmb[s], cmpMb_d[m, :, :]), (svm[s], selvm_d[m, :, :]), (sva[s], selva_d[m, :, :])], writes=[Btb[s]])
            c_lo = max(0, sc - 4)
            nwc = sc - c_lo + 1
            VwA = VwAf[s].rearrange("p (c n) -> p c n", c=5)
            VwA4 = VwAf[s].rearrange("p (c g d) -> p c g d", c=5, g=4)
            P.dma('sp', [(KwT[s][0:64, g, 0:nwc * 128], kw3[:, g, c_lo * 128:(sc + 1) * 128]) for g in range(4)] +
                  [(VwA4[:, 0:nwc, g, 0:64], vwin_d[c_lo * 128:(sc + 1) * 128, :].rearrange("(c p) (g d) -> p c g d", p=128, g=4)[:, :, g, :]) for g in range(4)],
                  writes=[BKw[s]])
            rope_tables(X, posq[:, m * 128:(m + 1) * 128], 128, Ct[0:64, :], St[0:64, :], Btab, tmp32, X.ai, Btmp)
            norm_modT(X, xt[s], Bxt[s], G1, SH, Bmod, lambda k: uq[:, k, :], Buq, xn, Bxn, ss, 0, 1, xn)
            for b4 in range(4):
                for (W_, pi) in [(WB, 3), (WBp, 4)]:
                    for hh in range(4):
                        cbase = (b4 * 4 + hh) * 64
                        for k in range(8):
                            P.op('pe', lambda e, k=k, W_=W_, pi=pi, cbase=cbase, hh=hh: e.matmul(
                                X.ps[pi][0:64, hh * 128:(hh + 1) * 128], lhsT=W_[:, k, cbase:cbase + 64], rhs=uq[:, k, :],
                                start=(k == 0), stop=(k == 7)),
                                reads=[Buq, BW, BWp], writes=[X.psb[pi]])
                A = X.ps[3][0:64, :].rearrange("p (h t) -> p h t", h=4)
                B = X.ps[4][0:64, :].rearrange("p (h t) -> p h t", h=4)
                c_b = Ct[0:64, :].unsqueeze(1).to_broadcast([64, 4, 128])
                s_b = St[0:64, :].unsqueeze(1).to_broadcast([64, 4, 128])
                t1 = r1[0:64, :].rearrange("p (h t) -> p h t", h=4)
                t2 = r2[0:64, :].rearrange("p (h t) -> p h t", h=4)
                P.op('dve', lambda e, A=A, c_b=c_b, t1=t1: e.tensor_tensor(out=t1, in0=A, in1=c_b, op=ALU.mult), reads=[X.psb[3], Btab], writes=[Br])
                P.op('dve', lambda e, B=B, s_b=s_b, t2=t2: e.tensor_tensor(out=t2, in0=B, in1=s_b, op=ALU.mult), reads=[X.psb[4], Btab], writes=[Br])
                P.op('pool', lambda e, b4=b4, t1=t1, t2=t2: e.tensor_tensor(out=QT[0:64, b4 * 4:(b4 + 1) * 4, :], in0=t1, in1=t2, op=ALU.add),
                     reads=[Br], writes=[BQ])
            for k in range(8):
                P.op('pe', lambda e, k=k: e.matmul(X.ps[2][:, 0:48], lhsT=uq[:, k, :], rhs=WB[:, k, 1024:1072],
                                                   start=(k == 0), stop=(k == 7)), reads=[Buq, BW], writes=[X.psb[2]])
            P.op('act', lambda e: e.activation(out=gts, in_=X.ps[2][:, 0:48], func=AF.Sigmoid), reads=[X.psb[2]], writes=[Bgts])

            for g in range(4):
                ms = g % 2
                def cmp_extra(ci, ps_, g=g):
                    for r in range(4):
                        P.op('pe', lambda e, r=r, ps_=ps_, ci=ci: e.matmul(X.ps[3][:, r * 64:(r + 1) * 64], lhsT=PT[ps_][:, r * 128:(r + 1) * 128],
                                                                          rhs=agg[:, ci, :], start=(ci == 0 and r == 0), stop=(ci == 1),
                                                                          skip_group_check=True),
                             reads=[BPT[ps_], BKV], writes=[X.psb[3]])
                chunks = [(kcT[0:64, g, ci * 128:(ci + 1) * 128], cmb[s][:, ci * 128:(ci + 1) * 128], None, vcx[:, ci, g * 65:(g + 1) * 65])
                          for ci in range(2)]
                ob = attend(g, chunks, cmp_extra)
                O3 = X.ps[ob][:, 0:260].rearrange("p (r d) -> p r d", r=4)
                P.op('dve', lambda e, O3=O3: e.tensor_scalar(out=rden[:, 0:4], in0=O3[:, :, 64], scalar1=tiny[:, 0:1], scalar2=None, op0=ALU.add),
                     reads=[X.psb[ob], X.Bcst2], writes=[Brd])
                P.op('dve', lambda e: e.reciprocal(out=rden[:, 0:4], in_=rden[:, 0:4]), reads=[Brd], writes=[Brd])
                P.op('dve', lambda e, g=g: e.tensor_tensor(out=coef[:, 0:4], in0=rden[:, 0:4], in1=gts3[:, g * 4:(g + 1) * 4, 0], op=ALU.mult),
                     reads=[Brd, Bgts], writes=[Brd])
                P.op('dve', lambda e, O3=O3, g=g: e.tensor_tensor(
                    out=On[:, g * 256:(g + 1) * 256].rearrange("p (r d) -> p r d", r=4), in0=O3[:, :, 0:64],
                    in1=coef[:, 0:4].unsqueeze(2).to_broadcast([128, 4, 64]), op=ALU.mult),
                    reads=[X.psb[ob], Brd], writes=[BOn])
                for r in range(4):
                    if r == 0:
                        P.op('dve', lambda e: e.tensor_scalar(out=imp, in0=X.ps[3][:, 0:64], scalar1=rden[:, 0:1], scalar2=None, op0=ALU.mult),
                             reads=[X.psb[3], Brd], writes=[Bimp])
                    else:
                        P.op('dve', lambda e, r=r: e.scalar_tensor_tensor(out=imp, in0=X.ps[3][:, r * 64:(r + 1) * 64], scalar=rden[:, r:r + 1],
                                                                          in1=imp, op0=ALU.mult, op1=ALU.add),
                             reads=[X.psb[3], Brd, Bimp], writes=[Bimp])
                P.op('dve', lambda e, s=s: e.tensor_tensor(out=imp, in0=imp, in1=svm[s], op=ALU.mult), reads=[Bimp, Btb[s]], writes=[Bimp])
                P.op('dve', lambda e, s=s: e.tensor_tensor(out=imp, in0=imp, in1=sva[s], op=ALU.add), reads=[Bimp, Btb[s]], writes=[Bimp])
                P.op('dve', lambda e: e.max(out=mx[:, 0:8], in_=imp), reads=[Bimp], writes=[Bimp])
                P.op('dve', lambda e: e.match_replace(out=imp2, in_to_replace=mx[:, 0:8], in_values=imp, imm_value=-3.0e38), reads=[Bimp], writes=[Bimp])
                P.op('dve', lambda e: e.max(out=mx[:, 8:16], in_=imp2), reads=[Bimp], writes=[Bimp])
                P.op('dve', lambda e: e.tensor_reduce(out=mx[:, 0:1], in_=mx[:, 8:16], axis=AX.X, op=ALU.min), reads=[Bimp], writes=[Bimp])
                P.op('dve', lambda e: e.tensor_scalar(out=selb, in0=imp, scalar1=mx[:, 0:1], scalar2=MASKV, op0=ALU.is_lt, op1=ALU.mult),
                     reads=[Bimp], writes=[Bselb])
                nblk = 2 * nk
                P.op('pool', lambda e, ms=ms, nblk=nblk, nkeys=nkeys: e.tensor_copy(
                    out=Mbs[ms][:, 0:nkeys].rearrange("p (b l) -> p b l", l=64),
                    in_=selb[:, 0:nblk].unsqueeze(2).to_broadcast([128, nblk, 64])), reads=[Bselb], writes=[BMbs[ms]])
                P.op('pool', lambda e, ms=ms, sc=sc: e.tensor_tensor(out=Mbs[ms][:, sc * 128:(sc + 1) * 128], in0=Mbs[ms][:, sc * 128:(sc + 1) * 128],
                                                                    in1=trib, op=ALU.add), reads=[BMbs[ms], X.Birep], writes=[BMbs[ms]])
                chunks = [(KsT[0:64, g, c * 128:(c + 1) * 128], Mbs[ms][:, c * 128:(c + 1) * 128],
                           (X.cst[:, C_PAD:C_PAD + 1] if c == 0 else None), VsA[:, c, g * 65:(g + 1) * 65]) for c in range(nk)]
                ob = attend(g, chunks, lambda ci, ps_: None)

                def accum_branch(ob, br, g=g):
                    O3 = X.ps[ob][:, 0:260].rearrange("p (r d) -> p r d", r=4)
                    P.op('dve', lambda e, O3=O3: e.reciprocal(out=rden[:, 4:8], in_=O3[:, :, 64]), reads=[X.psb[ob]], writes=[Brd])
                    P.op('dve', lambda e: e.tensor_tensor(out=coef[:, 4:8], in0=rden[:, 4:8], in1=gts3[:, g * 4:(g + 1) * 4, br], op=ALU.mult),
                         reads=[Brd, Bgts], writes=[Brd])
                    P.op('dve', lambda e, O3=O3: e.tensor_tensor(out=otmp.rearrange("p (r d) -> p r d", r=4), in0=O3[:, :, 0:64],
                                                                in1=coef[:, 4:8].unsqueeze(2).to_broadcast([128, 4, 64]), op=ALU.mult),
                         reads=[X.psb[ob], Brd], writes=[Botmp])
                    P.op('pool', lambda e: e.tensor_tensor(out=On[:, g * 256:(g + 1) * 256], in0=On[:, g * 256:(g + 1) * 256], in1=otmp, op=ALU.add),
                         reads=[Botmp, BOn], writes=[BOn])
                accum_branch(ob, 1)
                chunks = []
                for wi_, c in enumerate(range(c_lo, sc + 1)):
                    if c == sc:
                        ml = trib
                    elif c == sc - 4:
                        ml = triub
                    else:
                        ml = None
                    chunks.append((KwT[s][0:64, g, wi_ * 128:(wi_ + 1) * 128], ml,
                                   (X.cst[:, C_PAD:C_PAD + 1] if c == 0 else None), VwA[:, wi_, g * 65:(g + 1) * 65]))
                ob = attend(g, chunks, lambda ci, ps_: None)
                accum_branch(ob, 2)
            for k in range(8):
                pi = 0 if k < 4 else 1
                P.op('pe', lambda e, k=k, pi=pi: e.transpose(out=X.ps[pi][:, (k % 4) * 128:(k % 4 + 1) * 128],
                                                             in_=On[:, k * 128:(k + 1) * 128], identity=X.ident),
                     reads=[BOn, X.Bcst], writes=[X.psb[pi]])
            for half in range(2):
                P.op('act', lambda e, half=half: e.copy(out=OnT[:, half * 4:(half + 1) * 4, :],
                                                        in_=X.ps[half][:, :].rearrange("p (k t) -> p k t", k=4)),
                     reads=[X.psb[half]], writes=[BOnT])
            for half in range(2):
                pi = 3 + half
                for k in range(8):
                    P.op('pe', lambda e, k=k, pi=pi, half=half: e.matmul(X.ps[pi][:, :], lhsT=OnT[:, k, :],
                                                                         rhs=Wo[:, k, half * 512:(half + 1) * 512],
                                                                         start=(k == 0), stop=(k == 7)),
                         reads=[BOnT, BWo], writes=[X.psb[pi]])
                P.op('dve', lambda e, pi=pi, half=half: e.tensor_tensor(out=hm[:, half * 512:(half + 1) * 512], in0=X.ps[pi][:, :],
                                                                        in1=gate_bc[:, half * 512:(half + 1) * 512], op=ALU.mult),
                     reads=[X.psb[pi], Bgate], writes=[Bhm])
            P.op('pool', lambda e, s=s: e.tensor_tensor(out=hm, in0=hm, in1=xt[s], op=ALU.add), reads=[Bhm, Bxt[s]], writes=[Bhm])
            P.dma('sp', [(hmid[m * 128:(m + 1) * 128, :], hm)], reads=[Bhm], writes=[Buf('o')], sem_buf=Bhm)
        P.barrier()


def build_att1(nqb=NQB):
    nc = bass.Bass("TRN2", target_bir_lowering=False)

    def din(name, shape, dt=F32):
        return nc.dram_tensor(name, shape, dt, kind="ExternalInput").ap()
    hq = din("hq", [nqb * 128, 1024])
    posq = din("posq", [1, nqb * 128], I32)
    cst_d = din("cst", [128, NCST1])
    cT = din("cT", [128, 8])
    w_ada = din("w_ada", [1024, 3072])
    b_adaT = din("b_adaT", [128, 16])
    b_gate = din("b_gate", [1, 1024])
    gainT = din("gainT", [128, 8])
    w_q = din("w_q", [1024, 1072])
    w_out = din("w_out", [1024, 1024])
    kslcT_d = din("kslcT", [64, 4 * NKC * 128], BF16)
    kwinT_d = din("kwinT", [64, 4 * NKC * 128], BF16)
    vslc_d = din("vslc", [NKC * 128, 256], BF16)
    vwin_d = din("vwin", [NKC * 128, 256], BF16)
    kcT_d = din("kcT", [64, 1024], BF16)
    vc_d = din("vc", [256, 256], BF16)
    cmpMb_d = din("cmpMb", [nqb, 128, 256], BF16)
    selvm_d = din("selvm", [nqb, 128, 64])
    selva_d = din("selva", [nqb, 128, 64])
    agg_d = din("agg", [256, 64], BF16)
    hmid = nc.dram_tensor("hmid", [nqb * 128, 1024], F32, kind="ExternalOutput").ap()

    T = dict(hq=hq, posq=posq, cst_d=cst_d, cT=cT, w_ada=w_ada, b_adaT=b_adaT, b_gate=b_gate, gainT=gainT, w_q=w_q, w_out=w_out, kslcT_d=kslcT_d, kwinT_d=kwinT_d, vslc_d=vslc_d, vwin_d=vwin_d, kcT_d=kcT_d, vc_d=vc_d, cmpMb_d=cmpMb_d, selvm_d=selvm_d, selva_d=selva_d, agg_d=agg_d, hmid=hmid)
    X = setup_ctx(nc)
    with X.st:
        phase_att1(X, T, nqb)
        X.P.emit()
    return nc


def att1_inputs(inputs, h1_cores, kv_res, nqb=NQB):
    maps = []
    for c in range(8):
        b, j = c // 2, c % 2
        pos = np.asarray(inputs['positions'][b], np.int32)
        posq = np.concatenate([pos[(2 * m + j) * 128:(2 * m + j + 1) * 128] for m in range(nqb)])
        mp = {
            "hq": np.ascontiguousarray(h1_cores[c][0:nqb * 128]),
            "posq": np.ascontiguousarray(posq[None, :]),
            "cst": make_consts1(j),
            "cT": np.ascontiguousarray(inputs['c'][b].reshape(8, 128).T),
            "w_ada": np.ascontiguousarray(inputs['w_ada'][1][:, 0:3072]),
            "b_adaT": np.ascontiguousarray(inputs['b_ada'][1][0:2048].reshape(16, 128).T),
            "b_gate": np.ascontiguousarray(inputs['b_ada'][1][2048:3072][None, :]),
            "gainT": np.ascontiguousarray(inputs['attn_gain'][1].reshape(8, 128).T),
            "w_q": np.ascontiguousarray(inputs['b_w_q'][0]),
            "w_out": np.ascontiguousarray(inputs['b_w_out'][0]),
        }
        for k in ["kslcT", "kwinT", "vslc", "vwin", "kcT", "vc"]:
            mp[k] = kv_res[c][k]
        mp.update(make_tables1(j, nqb))
        maps.append(mp)
    return maps


def phase_ffn1(X, T, ntok=2048, dff=3584, nexp=8):
    hin = T['hin']
    cst_d = T['cst_d']
    cT = T['cT']
    w_ada = T['w_ada']
    b_adaT = T['b_adaT']
    b_gate = T['b_gate']
    gainT = T['gainT']
    fgain = T['fgain']
    wr = T['wr']
    wg = T['wg']
    wu = T['wu']
    wd = T['wd']
    hout = T['hout']
    NF = dff // 128
    FB = 4
    GT = 1024
    ngrp = ntok // GT
    NT = GT // 128
    DQ = 256
    X.carve(16300)
    P = X.P
    a32, a16 = X.a32, X.a16
    if True:
        load_consts(X, cst_d)
        X.eps_col = a32.alloc(1)
        X.Bcst2 = Buf('cst2')
        P.op('pool', lambda e: e.memset(X.eps_col, 1e-6), writes=[X.Bcst2])
        G1, SH, Bmod, gate_bc, Bgate = ffn_phase0(X, cT, w_ada, b_adaT, b_gate, gainT)
        fg_bc = a32.alloc(1024)
        P.dma('sp', [(fg_bc, fgain.partition_broadcast(128))], writes=[Bgate])
        Wr = a32.alloc(8 * nexp).rearrange("p (k n) -> p k n", k=8)
        BWr = Buf('Wr')
        P.dma('sp', [(Wr, wr.rearrange("(k p) n -> p k n", p=128))], writes=[BWr])

        NWB = 2
        Wgb = [a16.alloc(8 * 128 * FB).rearrange("p (k n) -> p k n", k=8) for _ in range(NWB)]
        Wub = [a16.alloc(8 * 128 * FB).rearrange("p (k n) -> p k n", k=8) for _ in range(NWB)]
        BWg = [Buf('wg%d' % i) for i in range(NWB)]
        Wdb = [a16.alloc(NF * DQ).rearrange("p (f n) -> p f n", f=NF) for _ in range(2)]
        BWd = [Buf('wd0'), Buf('wd1')]
        u2T = a16.alloc(8 * GT).rearrange("p (k t) -> p k t", k=8)
        Bu2 = Buf('u2T')
        actT = a16.alloc(NF * GT).rearrange("p (f t) -> p f t", f=NF)
        Bact = Buf('actT')
        yacc = a32.alloc(NT * 1024).rearrange("p (t n) -> p t n", t=NT)
        By = [Buf('y%d' % i) for i in range(NT)]
        xt = [a32.alloc(1024), a32.alloc(1024)]
        Bxt = [Buf('xt0'), Buf('xt1')]
        xn = a32.alloc(1024)
        Bxn = Buf('xn')
        ss = a32.alloc(4)
        u32 = a32.alloc(8 * 128).rearrange("p (k t) -> p k t", k=8)
        Bu32 = Buf('u32')
        gall = a32.alloc(NT * nexp).rearrange("p (t n) -> p t n", t=NT)
        Bgall = Buf('gall')
        rt = a32.alloc(64)
        Brt = Buf('rt')
        sg = [a32.alloc(512), a32.alloc(512)]
        Bsg = [Buf('sg0'), Buf('sg1')]

        wi = 0
        di = 0
        for grp in range(ngrp):
            g0 = grp * GT
            for t in range(NT):
                s = t % 2
                P.dma('sp', [(xt[s], hin[g0 + t * 128:g0 + (t + 1) * 128, :])], writes=[Bxt[s]])
                norm_modT(X, xt[s], Bxt[s], G1, SH, Bmod, lambda k, t=t: u2T[:, k, t * 128:(t + 1) * 128], Bu2,
                          xn, Bxn, ss, 0, 1, xn, u32_dst=lambda k: u32[:, k, :], Bu32=Bu32)
                for k in range(8):
                    P.op('pe', lambda e, k=k: e.matmul(X.ps[2][:, 0:nexp], lhsT=u32[:, k, :], rhs=Wr[:, k, :], start=(k == 0), stop=(k == 7)),
                         reads=[Bu32, BWr], writes=[X.psb[2]])
                lg = rt[:, 0:8]
                e1 = rt[:, 8:16]
                lg2 = rt[:, 16:24]
                e2 = rt[:, 24:32]
                m1 = rt[:, 32:33]
                m2 = rt[:, 33:34]
                dl = rt[:, 34:35]
                w1_ = rt[:, 35:36]
                w2_ = rt[:, 36:37]
                gt_ = gall[:, t, :]
                P.op('dve', lambda e, lg=lg: e.tensor_copy(out=lg, in_=X.ps[2][:, 0:nexp]), reads=[X.psb[2]], writes=[Brt])
                P.op('dve', lambda e, lg=lg, m1=m1: e.tensor_reduce(out=m1, in_=lg, axis=AX.X, op=ALU.max), reads=[Brt], writes=[Brt])
                P.op('dve', lambda e, lg=lg, m1=m1, e1=e1: e.tensor_scalar(out=e1, in0=lg, scalar1=m1, scalar2=None, op0=ALU.is_equal), reads=[Brt], writes=[Brt])
                P.op('dve', lambda e, lg=lg, e1=e1, lg2=lg2: e.scalar_tensor_tensor(out=lg2, in0=e1, scalar=NEG, in1=lg, op0=ALU.mult, op1=ALU.add),
                     reads=[Brt], writes=[Brt])
                P.op('dve', lambda e, lg2=lg2, m2=m2: e.tensor_reduce(out=m2, in_=lg2, axis=AX.X, op=ALU.max), reads=[Brt], writes=[Brt])
                P.op('dve', lambda e, lg2=lg2, m2=m2, e2=e2: e.tensor_scalar(out=e2, in0=lg2, scalar1=m2, scalar2=None, op0=ALU.is_equal), reads=[Brt], writes=[Brt])
                P.op('dve', lambda e, m1=m1, m2=m2, dl=dl: e.tensor_tensor(out=dl, in0=m1, in1=m2, op=ALU.subtract), reads=[Brt], writes=[Brt])
                P.op('act', lambda e, dl=dl, w1_=w1_: e.activation(out=w1_, in_=dl, func=AF.Sigmoid), reads=[Brt], writes=[Brt])
                P.op('act', lambda e, dl=dl, w2_=w2_: e.activation(out=w2_, in_=dl, func=AF.Sigmoid, scale=-1.0), reads=[Brt], writes=[Brt])
                P.op('dve', lambda e, e1=e1, w1_=w1_, gt_=gt_: e.tensor_scalar(out=gt_, in0=e1, scalar1=w1_, scalar2=None, op0=ALU.mult),
                     reads=[Brt], writes=[Bgall])
                P.op('dve', lambda e, e2=e2, w2_=w2_, gt_=gt_: e.scalar_tensor_tensor(out=gt_, in0=e2, scalar=w2_, in1=gt_, op0=ALU.mult, op1=ALU.add),
                     reads=[Brt, Bgall], writes=[Bgall])
            for ex in range(nexp):
                wg3 = wg[ex].rearrange("(k p) n -> p k n", p=128)
                wu3 = wu[ex].rearrange("(k p) n -> p k n", p=128)
                wd3 = wd[ex].rearrange("(f p) n -> p f n", p=128)
                it = 0
                for fb in range(NF // FB):
                    wb = wi % NWB
                    wi += 1
                    f0 = fb * FB
                    P.dma('pool', [(Wgb[wb], wg3[:, :, f0 * 128:(f0 + FB) * 128]), (Wub[wb], wu3[:, :, f0 * 128:(f0 + FB) * 128])], writes=[BWg[wb]])
                    for fi in range(FB):
                        f = f0 + fi
                        for half in range(GT // 512):
                            pg = 2 + 2 * (it % 2)
                            pu = pg + 1
                            sgi = it % 2
                            it += 1
                            for (W_, pi) in [(Wgb[wb], pg), (Wub[wb], pu)]:
                                for k in range(8):
                                    P.op('pe', lambda e, k=k, W_=W_, pi=pi, half=half, fi=fi: e.matmul(
                                        X.ps[pi][:, :], lhsT=W_[:, k, fi * 128:(fi + 1) * 128], rhs=u2T[:, k, half * 512:(half + 1) * 512],
                                        start=(k == 0), stop=(k == 7)), reads=[BWg[wb], Bu2], writes=[X.psb[pi]])
                            P.op('act', lambda e, pg=pg, sgi=sgi: e.activation(out=sg[sgi], in_=X.ps[pg][:, :], func=AF.Silu),
                                 reads=[X.psb[pg]], writes=[Bsg[sgi]])
                            P.op('dve', lambda e, pu=pu, sgi=sgi, f=f, half=half: e.tensor_tensor(
                                out=actT[:, f, half * 512:(half + 1) * 512], in0=X.ps[pu][:, :], in1=sg[sgi], op=ALU.mult),
                                reads=[X.psb[pu], Bsg[sgi]], writes=[Bact])
                it = 0
                for q in range(1024 // DQ):
                    db = di % 2
                    di += 1
                    P.dma('pool', [(Wdb[db][:, 0:NF // 2, :], wd3[:, 0:NF // 2, q * DQ:(q + 1) * DQ]),
                                   (Wdb[db][:, NF // 2:NF, :], wd3[:, NF // 2:NF, q * DQ:(q + 1) * DQ])], writes=[BWd[db]])
                    for t in range(NT):
                        pi = 6 + (it % 2)
                        it += 1
                        for f in range(NF):
                            P.op('pe', lambda e, f=f, pi=pi, t=t, db=db: e.matmul(X.ps[pi][:, 0:DQ], lhsT=actT[:, f, t * 128:(t + 1) * 128],
                                                                                 rhs=Wdb[db][:, f, :], start=(f == 0), stop=(f == NF - 1)),
                                 reads=[Bact, BWd[db]], writes=[X.psb[pi]])
                        if ex == 0:
                            P.op('dve', lambda e, pi=pi, t=t, q=q, ex=ex: e.tensor_scalar(out=yacc[:, t, q * DQ:(q + 1) * DQ], in0=X.ps[pi][:, 0:DQ],
                                                                                         scalar1=gall[:, t, ex:ex + 1], scalar2=None, op0=ALU.mult),
                                 reads=[X.psb[pi], Bgall], writes=[By[t]])
                        else:
                            P.op('dve', lambda e, pi=pi, t=t, q=q, ex=ex: e.scalar_tensor_tensor(
                                out=yacc[:, t, q * DQ:(q + 1) * DQ], in0=X.ps[pi][:, 0:DQ], scalar=gall[:, t, ex:ex + 1],
                                in1=yacc[:, t, q * DQ:(q + 1) * DQ], op0=ALU.mult, op1=ALU.add),
                                reads=[X.psb[pi], Bgall, By[t]], writes=[By[t]])
            for t in range(NT):
                s = t % 2
                P.dma('sp', [(xt[s], hin[g0 + t * 128:g0 + (t + 1) * 128, :])], writes=[Bxt[s]])
                P.op('pool', lambda e, t=t: e.tensor_tensor(out=yacc[:, t, :], in0=yacc[:, t, :], in1=gate_bc, op=ALU.mult),
                     reads=[By[t], Bgate], writes=[By[t]])
                P.op('pool', lambda e, t=t, s=s: e.tensor_tensor(out=yacc[:, t, :], in0=yacc[:, t, :], in1=xt[s], op=ALU.add),
                     reads=[By[t], Bxt[s]], writes=[By[t]])
                P.op('act', lambda e, t=t: e.activation(out=xn, in_=yacc[:, t, :], func=AF.Square, accum_out=ss[:, 0:1]), reads=[By[t]], writes=[Bxn])
                P.op('act', lambda e: e.activation(out=ss[:, 1:2], in_=ss[:, 0:1], func=AF.Sqrt, scale=1.0 / 1024.0, bias=X.eps_col),
                     reads=[Bxn, X.Bcst2], writes=[Bxn])
                P.op('dve', lambda e: e.reciprocal(out=ss[:, 2:3], in_=ss[:, 1:2]), reads=[Bxn], writes=[Bxn])
                P.op('dve', lambda e, t=t: e.scalar_tensor_tensor(out=yacc[:, t, :], in0=yacc[:, t, :], scalar=ss[:, 2:3], in1=fg_bc,
                                                                  op0=ALU.mult, op1=ALU.mult), reads=[By[t], Bxn, Bgate], writes=[By[t]])
                P.dma('sp', [(hout[g0 + t * 128:g0 + (t + 1) * 128, :], yacc[:, t, :])], reads=[By[t]], writes=[Buf('o')], sem_buf=By[t])
        P.barrier()


def build_ffn1(ntok=2048, dff=3584, nexp=8):
    nc = bass.Bass("TRN2", target_bir_lowering=False)

    def din(name, shape, dt=F32):
        return nc.dram_tensor(name, shape, dt, kind="ExternalInput").ap()
    hin = din("hin", [ntok, 1024])
    cst_d = din("cst", [128, NCST])
    cT = din("cT", [128, 8])
    w_ada = din("w_ada", [1024, 3072])
    b_adaT = din("b_adaT", [128, 16])
    b_gate = din("b_gate", [1, 1024])
    gainT = din("gainT", [128, 8])
    fgain = din("fgain", [1, 1024])
    wr = din("wr", [1024, nexp])
    wg = din("wg", [nexp, 1024, dff])
    wu = din("wu", [nexp, 1024, dff])
    wd = din("wd", [nexp, dff, 1024])
    hout = nc.dram_tensor("hout", [ntok, 1024], F32, kind="ExternalOutput").ap()
    NF = dff // 128
    FB = 4
    GT = 1024
    ngrp = ntok // GT
    NT = GT // 128
    DQ = 256

    T = dict(hin=hin, cst_d=cst_d, cT=cT, w_ada=w_ada, b_adaT=b_adaT, b_gate=b_gate, gainT=gainT, fgain=fgain, wr=wr, wg=wg, wu=wu, wd=wd, hout=hout)
    X = setup_ctx(nc)
    with X.st:
        phase_ffn1(X, T, ntok, dff, nexp)
        X.P.emit()
    return nc


def ffn1_inputs(inputs, hmid_cores):
    maps = []
    for c in range(8):
        b, j = c // 2, c % 2
        maps.append({
            "hin": np.ascontiguousarray(hmid_cores[c]),
            "cst": make_consts(j),
            "cT": np.ascontiguousarray(inputs['c'][b].reshape(8, 128).T),
            "w_ada": np.ascontiguousarray(inputs['w_ada'][1][:, 3072:6144]),
            "b_adaT": np.ascontiguousarray(inputs['b_ada'][1][3072:5120].reshape(16, 128).T),
            "b_gate": np.ascontiguousarray(inputs['b_ada'][1][5120:6144][None, :]),
            "gainT": np.ascontiguousarray(inputs['ffn_gain'][1].reshape(8, 128).T),
            "fgain": np.ascontiguousarray(inputs['final_gain'][None, :]),
            "wr": np.ascontiguousarray(inputs['moe_w_router'][0]),
            "wg": np.ascontiguousarray(inputs['moe_w_gate'][0]),
            "wu": np.ascontiguousarray(inputs['moe_w_up'][0]),
            "wd": np.ascontiguousarray(inputs['moe_w_down'][0]),
        })
    return maps


def _run(nc, maps):
    res = run_bass_kernel_spmd(nc, maps, core_ids=list(range(8)))
    return res.results


def _kernel_unfused_impl(**inputs):
    inputs = {k: np.asarray(v) for k, v in inputs.items()}
    r0 = _run(build_att0(), att0_inputs(inputs))
    hmid0 = [np.asarray(r0[c]["hmid"]) for c in range(8)]
    r1 = _run(build_ffn0(), ffn0_inputs(inputs, hmid0))
    h1c = [np.asarray(r1[c]["hout"]) for c in range(8)]
    h1_full = gather_blocks(r1, "hout")
    r2 = _run(build_kv1(), kv1_inputs(inputs, h1_full))
    kv_res = [{k: np.asarray(r2[c][k]) for k in ["kslcT", "kwinT", "vslc", "vwin", "kcT", "vc"]} for c in range(8)]
    r3 = _run(build_att1(), att1_inputs(inputs, h1c, kv_res))
    hmid1 = [np.asarray(r3[c]["hmid"]) for c in range(8)]
    r4 = _run(build_ffn1(), ffn1_inputs(inputs, hmid1))
    return gather_blocks(r4, "hout").astype(np.float32)


def build_fused(stop_after=None):
    nc = bass.Bass("TRN2", target_bir_lowering=False)

    def din(name, shape, dt=F32):
        return nc.dram_tensor(name, shape, dt, kind="ExternalInput").ap()

    def dint(name, shape, dt=F32):
        return nc.dram_tensor(name, shape, dt, kind="Internal").ap()
    xk = din("xk", [NKC * 128, 1024])
    posk = din("posk", [1, NKC * 128], I32)
    posq = din("posq", [1, NQB * 128], I32)
    cst = din("cst", [128, NCST1])
    cT = din("cT", [128, 8])
    w_ada = din("w_ada", [2, 1024, 6144])
    b_adaT = din("b_adaT", [2, 128, 48])
    b_row = din("b_row", [2, 1, 6144])
    agT = din("agT", [2, 128, 8])
    fgT = din("fgT", [2, 128, 8])
    kvgT = din("kvgT", [128, 8])
    w_kv_ada = din("w_kv_ada", [1024, 2048])
    b_kvT = din("b_kvT", [128, 16])
    a_w_in = din("a_w_in", [1024, 2120])
    a_w_out = din("a_w_out", [1024, 1024])
    b_w_q = din("b_w_q", [1024, 1072])
    b_w_out = din("b_w_out", [1024, 1024])
    w_kv = din("w_kv", [1024, 1536])
    w1k = din("w1k", [2048, 256])
    w1v = din("w1v", [2048, 256])
    w2k = din("w2k", [256, 64])
    w2v = din("w2v", [256, 64])
    peTk = din("peTk", [64, 32])
    peTv = din("peTv", [64, 32])
    fwg = din("fwg", [1024, 2816])
    fwu = din("fwu", [1024, 2816])
    fwd = din("fwd", [2816, 1024])
    mwr = din("mwr", [1024, 8])
    if stop_after is None:
        mwg = din("mwg", [8, 1024, 3584])
        mwu = din("mwu", [8, 1024, 3584])
        mwd = din("mwd", [8, 3584, 1024])
    fgain = din("fgain", [1, 1024])
    cmpMb = din("cmpMb", [NQB, 128, 256], BF16)
    selvm = din("selvm", [NQB, 128, 64])
    selva = din("selva", [NQB, 128, 64])
    agg = din("agg", [256, 64], BF16)
    ridx = din("ridx", [128, NKC], I32)
    out = nc.dram_tensor("out", [NQB * 128, 1024], F32, kind="ExternalOutput").ap()
    hmid0 = dint("hmid0", [NQB * 128, 1024])
    h1own = dint("h1own", [NQB * 128, 1024])
    h1pair = dint("h1pair", [2 * NQB * 128, 1024])
    hmid1 = dint("hmid1", [NQB * 128, 1024])
    i_kslcT = dint("i_kslcT", [64, 4 * NKC * 128], BF16)
    i_kwinT = dint("i_kwinT", [64, 4 * NKC * 128], BF16)
    i_vslc = dint("i_vslc", [NKC * 128, 256], BF16)
    i_vwin = dint("i_vwin", [NKC * 128, 256], BF16)
    i_kcT = dint("i_kcT", [64, 1024], BF16)
    i_vc = dint("i_vc", [256, 256], BF16)

    X = setup_ctx(nc)
    P = X.P
    with X.st:
        def dbg_out(src, rows):
            dbg = nc.dram_tensor("dbg", [rows, 1024], F32, kind="ExternalOutput").ap()
            X.carve(15000)
            tl = X.a32.alloc(1024)
            for r in range(rows // 128):
                Bt = Buf('dbg')
                P.dma('sp', [(tl, src[r * 128:(r + 1) * 128, :])], writes=[Bt])
                P.dma('sp', [(dbg[r * 128:(r + 1) * 128, :], tl)], reads=[Bt], writes=[Buf('o')], sem_buf=Bt)
                P.barrier()
            P.emit()
            return nc
        phase_att0(X, dict(xk=xk, posk=posk, cst_d=cst, cT=cT, w_ada=w_ada[0][:, 0:3072], b_adaT=b_adaT[0][:, 0:16],
                           b_gate=b_row[0][:, 2048:3072], gainT=agT[0], w_in=a_w_in, w_out=a_w_out, hmid=hmid0))
        P.emit()
        if stop_after == 'att0':
            return dbg_out(hmid0, NQB * 128)
        if stop_after in ('att0q1', 'att0q2', 'att0q3'):
            X.carve(20000)
            tq = X.a32.alloc(4096)
            if stop_after == 'att0q1':
                P.dma('sp', [(tq[:, 0:1024], b_row[0][:, 5120:6144].partition_broadcast(128))], writes=[Buf('q')])
            elif stop_after == 'att0q2':
                P.dma('sp', [(tq.rearrange("p (k n) -> p k n", k=8), w_ada[0][:, 3072:3584].rearrange("(k p) n -> p k n", p=128))], writes=[Buf('q')])
            else:
                P.dma('sp', [(tq[:, 0:8], cT[:, :]), (tq[:, 8:24], b_adaT[0][:, 24:40]), (tq[:, 24:32], fgT[0][:, :])], writes=[Buf('q')])
            P.barrier()
            stop_after = 'att0r'
        if stop_after == 'att0m':
            for i in range(0, NBIG, 2000):
                P.op('pool', lambda e, i=i: e.memset(X.big[:, i:min(i + 2000, NBIG)], 12345.0), writes=[Buf('z')])
            P.barrier()
            stop_after = 'att0r'
        if stop_after == 'att0p':
            dbgA = nc.dram_tensor("dbgA", [NQB * 128, 1024], F32, kind="ExternalOutput").ap()
            X.carve(52000)
            X.a32.alloc(34000)
            hrA = X.a32.alloc(16 * 1024).rearrange("p (t n) -> p t n", t=16)
            BsA = [Buf('rA%d' % t) for t in range(16)]
            for t in range(16):
                P.dma('sp', [(hrA[:, t, :], hmid0[t * 128:(t + 1) * 128, :])], writes=[BsA[t]])
            for t in range(16):
                P.dma('sp', [(dbgA[t * 128:(t + 1) * 128, :], hrA[:, t, :])], reads=[BsA[t]], writes=[Buf('o')], sem_buf=BsA[t])
            P.barrier()
            X.carve(15000)
            load_consts(X, cst)
            X.eps_col = X.a32.alloc(1)
            X.Bcst2 = Buf('cst2')
            P.op('pool', lambda e: e.memset(X.eps_col, 1e-6), writes=[X.Bcst2])
            ffn_phase0(X, cT, w_ada[0][:, 3072:6144], b_adaT[0][:, 24:40], b_row[0][:, 5120:6144], fgT[0])
            P.barrier()
            stop_after = 'att0r'
        if stop_after == 'att0r':
            dbg = nc.dram_tensor("dbg", [NQB * 128, 1024], F32, kind="ExternalOutput").ap()
            X.carve(52000)
            X.a32.alloc(34000)
            hr = X.a32.alloc(16 * 1024).rearrange("p (t n) -> p t n", t=16)
            Bs = [Buf('r%d' % t) for t in range(16)]
            for t in range(16):
                P.dma('sp', [(hr[:, t, :], hmid0[t * 128:(t + 1) * 128, :])], writes=[Bs[t]])
            for t in range(16):
                P.dma('sp', [(dbg[t * 128:(t + 1) * 128, :], hr[:, t, :])], reads=[Bs[t]], writes=[Buf('o')], sem_buf=Bs[t])
            P.barrier()
            P.emit()
            return nc
        if stop_after == 'att0s':
            X.carve(15000)
            Wt = X.a16.alloc(22 * 1024).rearrange("p (f n) -> p f n", f=22)
            P.dma('pool', [(Wt, fwd.rearrange("(f p) n -> p f n", p=128))], writes=[Buf('wt')])
            P.barrier()
            return dbg_out(hmid0, NQB * 128)
        phase_ffn0(X, dict(hin=(hmid0 if stop_after != 'ffn0y' else din('hin_dbg', [NQB * 128, 1024])), cst_d=cst, cT=cT, w_ada=w_ada[0][:, 3072:6144], b_adaT=b_adaT[0][:, 24:40],
                           b_gate=b_row[0][:, 5120:6144], gainT=fgT[0], wg=fwg, wu=fwu, wd=fwd,
                           hout=(h1own if stop_after not in ('ffn0x', 'ffn0y') else nc.dram_tensor("dbg", [NQB * 128, 1024], F32, kind="ExternalOutput").ap())))
        P.emit()
        if stop_after in ('ffn0x', 'ffn0y'):
            return nc
        if stop_after == 'ffn0':
            return dbg_out(h1own, NQB * 128)
        Bcc = Buf('cc')
        P.dma('pool', None, writes=[Bcc], inc=1,
              fns=[lambda e, q=q: e.collective_compute("AllGather", ALU.bypass, replica_groups=[[0, 1], [2, 3], [4, 5], [6, 7]],
                                                       ins=[h1own[q * 512:(q + 1) * 512, :]], outs=[h1pair[q * 1024:(q + 1) * 1024, :]])
                   for q in range(4)])
        P.barrier()
        P.emit()
        if stop_after == 'cc':
            return dbg_out(h1pair, 2 * NQB * 128)
        phase_kv1(X, dict(hk=h1pair, posk=posk, cst_d=cst, cT=cT, w_ada=w_kv_ada, b_adaT=b_kvT, gainT=kvgT, w_kv=w_kv,
                          w1k=w1k, w1v=w1v, w2k=w2k, w2v=w2v, peTk=peTk, peTv=peTv, ridx=ridx,
                          o_kslcT=i_kslcT, o_kwinT=i_kwinT, o_vslc=i_vslc, o_vwin=i_vwin, o_kcT=i_kcT, o_vc=i_vc))
        P.emit()
        phase_att1(X, dict(hq=h1own, posq=posq, cst_d=cst, cT=cT, w_ada=w_ada[1][:, 0:3072], b_adaT=b_adaT[1][:, 0:16],
                           b_gate=b_row[1][:, 2048:3072], gainT=agT[1], w_q=b_w_q, w_out=b_w_out,
                           kslcT_d=i_kslcT, kwinT_d=i_kwinT, vslc_d=i_vslc, vwin_d=i_vwin, kcT_d=i_kcT, vc_d=i_vc,
                           cmpMb_d=cmpMb, selvm_d=selvm, selva_d=selva, agg_d=agg, hmid=hmid1))
        P.emit()
        phase_ffn1(X, dict(hin=hmid1, cst_d=cst, cT=cT, w_ada=w_ada[1][:, 3072:6144], b_adaT=b_adaT[1][:, 24:40],
                           b_gate=b_row[1][:, 5120:6144], gainT=fgT[1], fgain=fgain, wr=mwr, wg=mwg, wu=mwu, wd=mwd, hout=out))
        P.emit()
    return nc


def fused_inputs(inputs):
    A = lambda a: np.ascontiguousarray(a)
    maps = []
    shared = {
        "w_ada": A(inputs['w_ada']),
        "b_adaT": A(inputs['b_ada'].reshape(2, 48, 128).transpose(0, 2, 1)),
        "b_row": A(inputs['b_ada'][:, None, :]),
        "agT": A(inputs['attn_gain'].reshape(2, 8, 128).transpose(0, 2, 1)),
        "fgT": A(inputs['ffn_gain'].reshape(2, 8, 128).transpose(0, 2, 1)),
        "kvgT": A(inputs['kv_gain'].reshape(8, 128).T),
        "w_kv_ada": A(inputs['w_kv_ada']),
        "b_kvT": A(inputs['b_kv_ada'].reshape(16, 128).T),
        "a_w_in": A(inputs['a_w_in'][0]), "a_w_out": A(inputs['a_w_out'][0]),
        "b_w_q": A(inputs['b_w_q'][0]), "b_w_out": A(inputs['b_w_out'][0]),
        "w_kv": A(inputs['w_kv']),
        "w1k": A(inputs['cmp_w1_k']), "w1v": A(inputs['cmp_w1_v']), "w2k": A(inputs['cmp_w2_k']), "w2v": A(inputs['cmp_w2_v']),
        "peTk": A(inputs['cmp_pe_k'].T), "peTv": A(inputs['cmp_pe_v'].T),
        "fwg": A(inputs['ffn_w_gate'][0]), "fwu": A(inputs['ffn_w_up'][0]), "fwd": A(inputs['ffn_w_down'][0]),
        "mwr": A(inputs['moe_w_router'][0]), "mwg": A(inputs['moe_w_gate'][0]), "mwu": A(inputs['moe_w_up'][0]),
        "mwd": A(inputs['moe_w_down'][0]),
        "fgain": A(inputs['final_gain'][None, :]),
    }
    tabs = [make_tables1(0), make_tables1(1)]
    for c in range(8):
        b, j = c // 2, c % 2
        pos = np.asarray(inputs['positions'][b], np.int32)
        posq = np.concatenate([pos[(2 * m + j) * 128:(2 * m + j + 1) * 128] for m in range(NQB)])
        ridx = np.zeros((128, NKC), np.int32)
        for sc in range(NKC):
            gc = max(0, sc - (1 - j))
            o_ = (gc // 2) * 128 + np.arange(128)
            ridx[:, sc] = (o_ // 512) * 1024 + (gc % 2) * 512 + (o_ % 512)
        mp = dict(shared)
        mp.update({
            "xk": storage_order(np.asarray(inputs['x'][b], np.float32), j),
            "posk": A(storage_order(pos, j)[None, :]),
            "posq": A(posq[None, :]),
            "cst": make_consts1(j),
            "cT": A(inputs['c'][b].reshape(8, 128).T),
            "ridx": ridx,
        })
        mp.update(tabs[j])
        maps.append(mp)
    return maps


def kernel_unfused(**inputs):
    return _kernel_unfused_impl(**inputs)


def kernel(**inputs):
    inputs = {k: np.asarray(v) for k, v in inputs.items()}
    res = _run(build_fused(), fused_inputs(inputs))
    return gather_blocks(res, "out").astype(np.float32)
```

```python
import contextlib
import numpy as np
import concourse.bass as bass
import concourse.mybir as mybir
from concourse.bass_utils import run_bass_kernel_spmd

F32 = mybir.dt.float32
BF16 = mybir.dt.bfloat16
I32 = mybir.dt.int32
AF = mybir.ActivationFunctionType
ALU = mybir.AluOpType
AX = mybir.AxisListType

ENGS = ['pe', 'act', 'dve', 'pool', 'sp']
NEG = -1.0e30
MASKV = -30000.0
TWO_PI = 6.283185


class Buf:
    __slots__ = ('w', 'r', 'name', 'dsem')

    def __init__(self, name=''):
        self.name = name
        self.w = None
        self.r = []
        self.dsem = None


class Prog:
    def __init__(self, nc, same_engine_sync=True):
        self.nc = nc
        self.ops = {e: [] for e in ENGS}
        self.cnt = {e: 0 for e in ENGS}
        self.known = {e: {} for e in ENGS}
        self.ndsem = 0
        self.dsem_val = {}
        self.same_engine_sync = same_engine_sync

    def _deps(self, eng, reads, writes):
        toks = []
        for b in reads:
            if b.w is not None:
                toks.append(b.w)
        for b in writes:
            if b.w is not None:
                toks.append(b.w)
            toks.extend(b.r)
        need = {}
        for (k, v) in toks:
            if k == eng and (eng == 'pe' or not self.same_engine_sync):
                continue
            if self.known[eng].get(k, 0) >= v:
                continue
            if need.get(k, 0) < v:
                need[k] = v
        for k, v in need.items():
            self.known[eng][k] = v
        return list(need.items())

    def _commit(self, tok, reads, writes):
        for b in reads:
            b.r.append(tok)
        for b in writes:
            b.w = tok
            b.r = []

    def op(self, eng, fn, reads=(), writes=()):
        waits = self._deps(eng, reads, writes)
        self.cnt[eng] += 1
        tok = (eng, self.cnt[eng])
        self.ops[eng].append((waits, fn, (eng, 1)))
        self._commit(tok, reads, writes)
        return tok

    def dma(self, q, items, reads=(), writes=(), sem_buf=None, fns=None, inc=16, **kw):
        sb = sem_buf if sem_buf is not None else writes[0]
        if sb.dsem is None:
            sb.dsem = ('d', self.ndsem)
            self.dsem_val[sb.dsem] = 0
            self.ndsem += 1
        key = sb.dsem
        waits = self._deps(q, reads, writes)
        if fns is None:
            fns = []
            for (o, a) in items:
                def fn(e, o=o, a=a):
                    return e.dma_start(out=o, in_=a, **kw)
                fns.append(fn)
        for i, fn in enumerate(fns):
            self.dsem_val[key] += inc
            self.ops[q].append((waits if i == 0 else [], fn, (key, inc)))
        tok = (key, self.dsem_val[key])
        self._commit(tok, reads, writes)
        return tok

    def barrier(self):
        for e in ENGS:
            waits = []
            for k in ENGS:
                if k != e and self.cnt[k] > self.known[e].get(k, 0):
                    waits.append((k, self.cnt[k]))
                    self.known[e][k] = self.cnt[k]
            for k, v in self.dsem_val.items():
                if v > self.known[e].get(k, 0):
                    waits.append((k, v))
                    self.known[e][k] = v
            if e != 'pe' and self.cnt[e] > self.known[e].get(e, 0):
                waits.append((e, self.cnt[e]))
                self.known[e][e] = self.cnt[e]
            self.ops[e].append((waits, None, None))

    def emit(self):
        nc = self.nc
        st = self.st
        if not hasattr(self, 'sems'):
            self.sems = {}
        sems = self.sems
        for e in ENGS:
            if e not in sems:
                sems[e] = st.enter_context(nc.semaphore('s_' + e))
        for i in range(self.ndsem):
            if ('d', i) not in sems:
                sems[('d', i)] = st.enter_context(nc.semaphore('d_%d' % i))
        with nc.Block() as block:
            def replay(ename):
                def run(e):
                    for (waits, fn, inc) in self.ops[ename]:
                        for (k, v) in waits:
                            e.wait_ge(sems[k], v)
                        if fn is not None:
                            ins = fn(e)
                            ins.then_inc(sems[inc[0]], inc[1])
                return run
            block.tensor(replay('pe'))
            block.scalar(replay('act'))
            block.vector(replay('dve'))
            block.gpsimd(replay('pool'))
            block.sync(replay('sp'))
        self.ops = {e: [] for e in ENGS}


class Arena:
    def __init__(self, t, n):
        self.t = t
        self.n = n
        self.off = 0

    def alloc(self, ncols):
        o = self.off
        self.off += ncols
        assert self.off <= self.n, (self.off, self.n)
        return self.t[:, o:o + ncols]

    def mark(self):
        return self.off

    def reset(self, m=0):
        self.off = m


class Ctx:
    pass


C_IDENT = 0
C_TRI = 128
C_PAD = 256
C_INV = 384
C_SSC = 385
C_CSC = 386
C_ONES = 387
NCST = 392


def make_consts(j):
    c = np.zeros((128, NCST), np.float32)
    c[:, C_IDENT:C_IDENT + 128] = np.eye(128, dtype=np.float32)
    q = np.arange(128)[:, None]
    k = np.arange(128)[None, :]
    c[:, C_TRI:C_TRI + 128] = np.where(k <= q, 0.0, NEG)
    c[:, C_PAD:C_PAD + 128] = NEG if j == 0 else 0.0
    inv = 1.0 / (10000.0 ** (np.arange(0, 64, 2, dtype=np.float32) / np.float32(64)))
    inv = inv.astype(np.float32)
    c[0:64, C_INV] = np.concatenate([inv, inv])
    c[0:32, C_SSC] = -TWO_PI
    c[32:64, C_SSC] = TWO_PI
    c[:, C_CSC] = TWO_PI
    c[:, C_ONES] = 1.0
    return c


NBIG = 52600


def setup_ctx(nc):
    X = Ctx()
    X.nc = nc
    X.st = contextlib.ExitStack()
    X.big = X.st.enter_context(nc.sbuf_tensor("big", [128, NBIG], F32))

    def carve(n32):
        X.a32 = Arena(X.big[:, 0:n32], n32)
        X.a16 = Arena(X.big[:, n32:NBIG].bitcast(BF16), 2 * (NBIG - n32))
    X.carve = carve
    X.ai = X.st.enter_context(nc.sbuf_tensor("ai32", [128, 512], I32))
    X.ps = [X.st.enter_context(nc.psum_tensor("ps%d" % i, [128, 512], F32)) for i in range(8)]
    X.psb = [Buf('ps%d' % i) for i in range(8)]
    X.P = Prog(nc)
    X.P.st = X.st
    return X


def load_consts(X, cst_dram):
    P = X.P
    X.cst = X.a32.alloc(cst_dram.shape[1])
    X.Bcst = Buf('cst')
    P.dma('sp', [(X.cst, cst_dram[:, :])], writes=[X.Bcst])
    X.ident = X.cst[:, C_IDENT:C_IDENT + 128]
    X.irep = X.a16.alloc(512)
    X.Birep = Buf('irep')
    for r in range(4):
        P.op('dve', lambda e, r=r: e.tensor_copy(out=X.irep[:, r * 128:(r + 1) * 128], in_=X.ident),
             reads=[X.Bcst], writes=[X.Birep])


def rope_tables(X, pos_i_dram_row, n, Ct, St, Bt, tmp32, tmpi, Btmp):
    P = X.P
    cst = X.cst
    pi_ = tmpi[0:64, 0:n]
    y = tmp32[0:64, 0:n]
    f = tmp32[0:64, n:2 * n]
    g = tmp32[0:64, 2 * n:3 * n]
    P.dma('sp', [(pi_, pos_i_dram_row.partition_broadcast(64))], writes=[Btmp])
    P.op('dve', lambda e: e.tensor_copy(out=y, in_=pi_), reads=[Btmp], writes=[Btmp])
    P.op('dve', lambda e: e.tensor_scalar(out=y, in0=y, scalar1=cst[0:64, C_INV:C_INV + 1], scalar2=float(1.0 / (2 * np.pi)),
                                           op0=ALU.mult, op1=ALU.mult), reads=[Btmp, X.Bcst], writes=[Btmp])

    def frac_to(dst, src, addc):
        if addc != 0.0:
            P.op('dve', lambda e: e.tensor_scalar_add(out=dst, in0=src, scalar1=addc), reads=[Btmp], writes=[Btmp])
            s2 = dst
        else:
            s2 = src
        P.op('dve', lambda e: e.tensor_copy(out=pi_, in_=s2), reads=[Btmp], writes=[Btmp])
        P.op('dve', lambda e: e.tensor_copy(out=g, in_=pi_), reads=[Btmp], writes=[Btmp])
        P.op('dve', lambda e: e.tensor_tensor(out=dst, in0=s2, in1=g, op=ALU.subtract), reads=[Btmp], writes=[Btmp])
        P.op('dve', lambda e: e.tensor_single_scalar(out=g, in_=dst, scalar=0.5, op=ALU.is_gt), reads=[Btmp], writes=[Btmp])
        P.op('dve', lambda e: e.tensor_tensor(out=dst, in0=dst, in1=g, op=ALU.subtract), reads=[Btmp], writes=[Btmp])
        P.op('dve', lambda e: e.tensor_single_scalar(out=g, in_=dst, scalar=-0.5, op=ALU.is_lt), reads=[Btmp], writes=[Btmp])
        P.op('dve', lambda e: e.tensor_tensor(out=dst, in0=dst, in1=g, op=ALU.add), reads=[Btmp], writes=[Btmp])

    frac_to(f, y, 0.0)
    P.op('act', lambda e: e.activation(out=St, in_=f, func=AF.Sin, scale=cst[0:64, C_SSC:C_SSC + 1]),
         reads=[Btmp, X.Bcst], writes=[Bt])
    frac_to(f, y, 0.25)
    P.op('act', lambda e: e.activation(out=Ct, in_=f, func=AF.Sin, scale=cst[0:64, C_CSC:C_CSC + 1]),
         reads=[Btmp, X.Bcst], writes=[Bt])


def mod_vectors(X, cT_dram, w_ada_dram, b_adaT_dram, ncols, wbuf, Bw, psum_idx, cact, modT, bT):
    P = X.P
    nj = ncols // 128
    Bc = Buf('cact')
    P.dma('sp', [(cact, cT_dram[:, :])], writes=[Bc])
    P.op('act', lambda e: e.activation(out=cact, in_=cact, func=AF.Silu), reads=[Bc], writes=[Bc])
    Bm = Buf('modT')
    P.dma('sp', [(bT, b_adaT_dram[:, :])], writes=[Bm])
    ps = X.ps[psum_idx]
    Bps = X.psb[psum_idx]
    ngrp = ncols // 512
    for jg in range(ngrp):
        s = jg % 2
        w3 = wbuf[s].rearrange("p (k n) -> p k n", k=8)
        P.dma('sp', [(w3, w_ada_dram[:, jg * 512:(jg + 1) * 512].rearrange("(k p) n -> p k n", p=128))], writes=[Bw[s]])
        for jc in range(4):
            J = jg * 4 + jc
            for k in range(8):
                P.op('pe', lambda e, J=J, k=k, jc=jc, w3=w3: e.matmul(ps[:, J:J + 1], lhsT=w3[:, k, jc * 128:(jc + 1) * 128],
                                                                      rhs=cact[:, k:k + 1], start=(k == 0), stop=(k == 7)),
                     reads=[Bw[s], Bc], writes=[Bps])
    P.op('dve', lambda e: e.tensor_tensor(out=modT, in0=ps[:, 0:nj], in1=bT, op=ALU.add), reads=[Bps, Bm], writes=[Bm])
    return modT, Bm, cact, Bc


def bcast_row_vec(X, cact, Bc, w_dram_cols, b_dram_row, out_bc, Bout, wbuf, Bw, psA, psB):
    P = X.P
    crep = X.a32.alloc(8 * 128)
    Bcr = Buf('crep')
    crep3 = crep.rearrange("p (k n) -> p k n", k=8)
    for k in range(8):
        P.op('dve', lambda e, k=k: e.tensor_copy(out=crep3[:, k, :], in_=cact[:, k:k + 1].to_broadcast([128, 128])),
             reads=[Bc], writes=[Bcr])
    P.dma('sp', [(out_bc, b_dram_row.partition_broadcast(128))], writes=[Bout])
    for half in range(2):
        s = half % 2
        w3 = wbuf[s].rearrange("p (k n) -> p k n", k=8)
        P.dma('sp', [(w3, w_dram_cols[:, half * 512:(half + 1) * 512].rearrange("(k p) n -> p k n", p=128))], writes=[Bw[s]])
        pi = psA if half == 0 else psB
        for k in range(8):
            P.op('pe', lambda e, k=k, w3=w3, pi=pi: e.matmul(X.ps[pi][:, :], lhsT=crep3[:, k, :], rhs=w3[:, k, :],
                                                            start=(k == 0), stop=(k == 7)),
                 reads=[Bw[s], Bcr], writes=[X.psb[pi]])
        P.op('dve', lambda e, half=half, pi=pi: e.tensor_tensor(out=out_bc[:, half * 512:(half + 1) * 512], in0=X.ps[pi][:, :],
                                                                in1=out_bc[:, half * 512:(half + 1) * 512], op=ALU.add),
             reads=[X.psb[pi], Bout], writes=[Bout])


def norm_modT(X, xt, Bx, G1, SH, Bmod, uT_dst, Buo, xn, Bxn, ss, psA, psB, junk, u32_dst=None, Bu32=None):
    P = X.P
    P.op('act', lambda e: e.activation(out=junk, in_=xt, func=AF.Square, accum_out=ss[:, 0:1]), reads=[Bx], writes=[Bxn])
    P.op('act', lambda e: e.activation(out=ss[:, 1:2], in_=ss[:, 0:1], func=AF.Sqrt, scale=1.0 / 1024.0, bias=X.eps_col),
         reads=[Bxn, X.Bcst2], writes=[Bxn])
    P.op('dve', lambda e: e.reciprocal(out=ss[:, 2:3], in_=ss[:, 1:2]), reads=[Bxn], writes=[Bxn])
    P.op('dve', lambda e: e.tensor_scalar(out=xn, in0=xt, scalar1=ss[:, 2:3], scalar2=None, op0=ALU.mult),
         reads=[Bx, Bxn], writes=[Bxn])
    for k in range(8):
        pi = psA if k < 4 else psB
        P.op('pe', lambda e, k=k, pi=pi: e.transpose(out=X.ps[pi][:, (k % 4) * 128:(k % 4 + 1) * 128],
                                                     in_=xn[:, k * 128:(k + 1) * 128], identity=X.ident),
             reads=[Bxn, X.Bcst], writes=[X.psb[pi]])
    for k in range(8):
        pi = psA if k < 4 else psB
        P.op('act', lambda e, k=k, pi=pi: e.activation(out=uT_dst(k), in_=X.ps[pi][:, (k % 4) * 128:(k % 4 + 1) * 128],
                                                       func=AF.Identity, scale=G1[:, k:k + 1], bias=SH[:, k:k + 1]),
             reads=[X.psb[pi], Bmod], writes=[Buo])
        if u32_dst is not None:
            P.op('act', lambda e, k=k, pi=pi: e.activation(out=u32_dst(k), in_=X.ps[pi][:, (k % 4) * 128:(k % 4 + 1) * 128],
                                                           func=AF.Identity, scale=G1[:, k:k + 1], bias=SH[:, k:k + 1]),
                 reads=[X.psb[pi], Bmod], writes=[Bu32])


def load_w_bf16(X, dst3, w_dram_cols, Bw, nsplit=1):
    src = w_dram_cols.rearrange("(k p) n -> p k n", p=128)
    items = []
    for s in range(nsplit):
        k0 = s * 8 // nsplit
        k1 = (s + 1) * 8 // nsplit
        items.append((dst3[:, k0:k1, :], src[:, k0:k1, :]))
    X.P.dma('pool', items, writes=[Bw])


NQB = 16
NKC = 32
NBIS = 26


def phase_att0(X, T, nqb=NQB, nbis=NBIS):
    xk = T['xk']
    posk = T['posk']
    cst_d = T['cst_d']
    cT = T['cT']
    w_ada = T['w_ada']
    b_adaT = T['b_adaT']
    b_gate = T['b_gate']
    gainT = T['gainT']
    w_in = T['w_in']
    w_out = T['w_out']
    hmid = T['hmid']
    X.carve(12700)
    P = X.P
    a32, a16 = X.a32, X.a16
    if True:
        load_consts(X, cst_d)
        X.eps_col = a32.alloc(1)
        X.Bcst2 = Buf('cst2')
        P.op('pool', lambda e: e.memset(X.eps_col, 1e-6), writes=[X.Bcst2])

        gate_bc = a32.alloc(1024)
        Bgate = Buf('gate')
        G1 = a32.alloc(8)
        gT = a32.alloc(8)
        cact = a32.alloc(8)
        modT = a32.alloc(16)
        bT = a32.alloc(16)
        m32 = a32.mark()
        wbuf = [a32.alloc(8 * 512), a32.alloc(8 * 512)]
        Bw = [Buf('wa0'), Buf('wa1')]
        modT, Bmod, cact, Bc = mod_vectors(X, cT, w_ada[:, 0:2048], b_adaT, 2048, wbuf, Bw, 0, cact, modT, bT)
        bcast_row_vec(X, cact, Bc, w_ada[:, 2048:3072], b_gate, gate_bc, Bgate, wbuf, Bw, 1, 2)
        P.dma('sp', [(gT, gainT[:, :])], writes=[Bmod])
        P.op('dve', lambda e: e.scalar_tensor_tensor(out=G1, in0=modT[:, 8:16], scalar=1.0, in1=gT, op0=ALU.add, op1=ALU.mult),
             reads=[Bmod], writes=[Bmod])
        SH = modT[:, 0:8]
        P.barrier()
        a32.reset(m32)

        KT = a16.alloc(4 * NKC * 128).rearrange("p (g t) -> p g t", g=4)
        IKT = a16.alloc(NKC * 128)
        VAf = a16.alloc(NKC * 260)
        VA = VAf.rearrange("p (c n) -> p c n", c=NKC)
        BKV = Buf('kv')
        P.op('pool', lambda e: e.memset(VAf, 1.0), writes=[BKV])
        m16 = a16.mark()

        WA = a16.alloc(8 * 576).rearrange("p (k n) -> p k n", k=8)
        WAp = a16.alloc(8 * 320).rearrange("p (k n) -> p k n", k=8)
        BW = Buf('WA')
        BWp = Buf('WAp')
        w_in3 = w_in.rearrange("(k p) n -> p k n", p=128)
        P.dma('pool', [(WA[:, :, 0:512], w_in3[:, :, 1024:1536]), (WA[:, :, 512:576], w_in3[:, :, 2048:2112])], writes=[BW])

        def perm_copy(Wsrc, Wdst, pairs, Bs, Bd):
            for (s0, d0, n) in pairs:
                nh = n // 64
                for k in range(8):
                    src = Wsrc[:, k, s0:s0 + n].rearrange("p (h t i) -> p h t i", h=nh, t=2)
                    dst = Wdst[:, k, d0:d0 + n].rearrange("p (h t i) -> p h t i", h=nh, t=2)
                    eng = 'pool' if k % 2 == 0 else 'dve'
                    P.op(eng, lambda e, src=src, dst=dst: e.tensor_copy(out=dst[:, :, 0, :], in_=src[:, :, 1, :]), reads=[Bs], writes=[Bd])
                    P.op(eng, lambda e, src=src, dst=dst: e.tensor_copy(out=dst[:, :, 1, :], in_=src[:, :, 0, :]), reads=[Bs], writes=[Bd])
        perm_copy(WA, WAp, [(0, 0, 256), (512, 256, 64)], BW, BWp)
        uT = a16.alloc(8 * 512).rearrange("p (k t) -> p k t", k=8)
        BuT = Buf('uT')

        xt = [a32.alloc(1024), a32.alloc(1024)]
        Bxt = [Buf('xt0'), Buf('xt1')]
        xn = a32.alloc(1024)
        Bxn = Buf('xn')
        ss = a32.alloc(4)
        junk = xn
        Ct = a16.alloc(1024).bitcast(F32)
        St = a16.alloc(1024).bitcast(F32)
        Btab = Buf('tab')
        tmp32 = a16.alloc(3 * 1024).bitcast(F32)
        Btmp = Buf('ttmp')
        r1 = a32.alloc(512)
        r2 = a32.alloc(512)
        Br = Buf('ropetmp')

        def rope_combine(psA_i, psB_i, n, nh, dst):
            A = X.ps[psA_i][0:64, 0:nh * n].rearrange("p (h t) -> p h t", h=nh)
            B = X.ps[psB_i][0:64, 0:nh * n].rearrange("p (h t) -> p h t", h=nh)
            c_b = Ct[0:64, 0:n].unsqueeze(1).to_broadcast([64, nh, n])
            s_b = St[0:64, 0:n].unsqueeze(1).to_broadcast([64, nh, n])
            t1 = r1[0:64, 0:nh * n].rearrange("p (h t) -> p h t", h=nh)
            t2 = r2[0:64, 0:nh * n].rearrange("p (h t) -> p h t", h=nh)
            P.op('dve', lambda e: e.tensor_tensor(out=t1, in0=A, in1=c_b, op=ALU.mult), reads=[X.psb[psA_i], Btab], writes=[Br])
            P.op('dve', lambda e: e.tensor_tensor(out=t2, in0=B, in1=s_b, op=ALU.mult), reads=[X.psb[psB_i], Btab], writes=[Br])
            return t1, t2

        for grp in range(NKC // 4):
            t0 = grp * 512
            rope_tables(X, posk[:, t0:t0 + 512], 512, Ct[0:64, :], St[0:64, :], Btab, tmp32, X.ai, Btmp)
            for cc in range(4):
                ch = grp * 4 + cc
                s = ch % 2
                P.dma('sp', [(xt[s], xk[ch * 128:(ch + 1) * 128, :])], writes=[Bxt[s]])
                norm_modT(X, xt[s], Bxt[s], G1, SH, Bmod, lambda k, cc=cc: uT[:, k, cc * 128:(cc + 1) * 128], BuT,
                          xn, Bxn, ss, 0, 1, junk)
                for k in range(8):
                    P.op('pe', lambda e, k=k, cc=cc: e.matmul(X.ps[2][:, 0:256], lhsT=uT[:, k, cc * 128:(cc + 1) * 128],
                                                              rhs=WA[:, k, 256:512], start=(k == 0), stop=(k == 7)),
                         reads=[BuT, BW], writes=[X.psb[2]])
                P.op('act', lambda e, ch=ch: e.copy(out=VA[:, ch, :].rearrange("p (g d) -> p g d", g=4)[:, :, 0:64],
                                                    in_=X.ps[2][:, 0:256].rearrange("p (g d) -> p g d", g=4)),
                     reads=[X.psb[2]], writes=[BKV])
            for (kind, c0, c0p, nh) in [('k', 0, 0, 4), ('ik', 512, 256, 1)]:
                for hh in range(nh):
                    for (W_, pi, cb_) in [(WA, 3, c0), (WAp, 4, c0p)]:
                        for k in range(8):
                            P.op('pe', lambda e, k=k, W_=W_, pi=pi, cbase=cb_ + hh * 64: e.matmul(
                                X.ps[pi][0:64, :], lhsT=W_[:, k, cbase:cbase + 64], rhs=uT[:, k, :],
                                start=(k == 0), stop=(k == 7)),
                                reads=[BuT, BW, BWp], writes=[X.psb[pi]])
                    t1, t2 = rope_combine(3, 4, 512, 1, None)
                    if kind == 'k':
                        dst = KT[0:64, hh, t0:t0 + 512]
                    else:
                        dst = IKT[0:64, t0:t0 + 512]
                    P.op('pool', lambda e, dst=dst, t1=t1, t2=t2: e.tensor_tensor(out=dst, in0=t1[:, 0, :], in1=t2[:, 0, :], op=ALU.add),
                         reads=[Br], writes=[BKV])
        P.barrier()

        a16.reset(m16)
        WB = a16.alloc(8 * 1544).rearrange("p (k n) -> p k n", k=8)
        WBp = a16.alloc(8 * 1536).rearrange("p (k n) -> p k n", k=8)
        BW = Buf('WB')
        BWp = Buf('WBp')
        P.dma('pool', [(WB[:, 0:4, 0:1024], w_in3[:, 0:4, 0:1024]), (WB[:, 4:8, 0:1024], w_in3[:, 4:8, 0:1024]),
                       (WB[:, :, 1024:1536], w_in3[:, :, 1536:2048]), (WB[:, :, 1536:1544], w_in3[:, :, 2112:2120])], writes=[BW])
        perm_copy(WB, WBp, [(0, 0, 1024), (1024, 1024, 512)], BW, BWp)
        Wo = a16.alloc(8 * 1024).rearrange("p (k n) -> p k n", k=8)
        BWo = Buf('Wout')
        load_w_bf16(X, Wo, w_out, BWo, nsplit=2)
        QT2 = [a16.alloc(16 * 128).rearrange("p (h t) -> p h t", h=16) for _ in range(2)]
        BQ2 = [Buf('QT0'), Buf('QT1')]
        IQT = a16.alloc(8 * 128).rearrange("p (h t) -> p h t", h=8)
        BIQ = Buf('IQT')
        Ct = a32.alloc(128)
        St = a32.alloc(128)
        tmp32 = a32.alloc(3 * 128)
        iw = a32.alloc(24)
        Biw = Buf('iw')
        Isc = a32.alloc(NKC * 128)
        BI = Buf('I')
        rl = [a32.alloc(512), a32.alloc(512)]
        Brl = [Buf('rl0'), Buf('rl1')]
        bs = a32.alloc(16)
        Bbs = Buf('bs')
        Mb2 = [a16.alloc(NKC * 128) for _ in range(2)]
        BMb2 = [Buf('Mb0'), Buf('Mb1')]
        PT = [a16.alloc(512) for _ in range(4)]
        BPT = [Buf('pt%d' % i) for i in range(4)]
        SB = [5, 6, 0, 1]
        LA = 3
        On = a32.alloc(1024)
        BOn = Buf('On')
        rden = a32.alloc(16)
        OnT = a16.alloc(8 * 128).rearrange("p (k t) -> p k t", k=8)
        BOnT = Buf('OnT')
        uq = a16.alloc(8 * 128).rearrange("p (k t) -> p k t", k=8)
        Buq = Buf('uq')

        def geom(m):
            sc = 2 * m + 1
            nk = sc + 1
            return sc, nk, nk * 128

        def frontA(m):
            sc, nk, nkeys = geom(m)
            s = m % 2
            QT = QT2[s]
            P.dma('sp', [(xt[s], xk[sc * 128:(sc + 1) * 128, :])], writes=[Bxt[s]])
            rope_tables(X, posk[:, sc * 128:(sc + 1) * 128], 128, Ct[0:64, 0:128], St[0:64, 0:128], Btab, tmp32, X.ai, Btmp)
            norm_modT(X, xt[s], Bxt[s], G1, SH, Bmod, lambda k: uq[:, k, :], Buq, xn, Bxn, ss, 0, 1, junk)
            for (c0, nb, dstT, Bd) in [(0, 4, QT, BQ2[s]), (1024, 2, IQT, BIQ)]:
                for b4 in range(nb):
                    for (W_, pi) in [(WB, 3), (WBp, 4)]:
                        for hh in range(4):
                            cbase = c0 + (b4 * 4 + hh) * 64
                            for k in range(8):
                                P.op('pe', lambda e, k=k, W_=W_, pi=pi, cbase=cbase, hh=hh: e.matmul(
                                    X.ps[pi][0:64, hh * 128:(hh + 1) * 128], lhsT=W_[:, k, cbase:cbase + 64], rhs=uq[:, k, :],
                                    start=(k == 0), stop=(k == 7)),
                                    reads=[Buq, BW, BWp], writes=[X.psb[pi]])
                    t1, t2 = rope_combine(3, 4, 128, 4, None)
                    P.op('pool', lambda e, dstT=dstT, b4=b4, t1=t1, t2=t2: e.tensor_tensor(
                        out=dstT[0:64, b4 * 4:(b4 + 1) * 4, :], in0=t1, in1=t2, op=ALU.add), reads=[Br], writes=[Bd])
            for k in range(8):
                P.op('pe', lambda e, k=k: e.matmul(X.ps[2][:, 0:8], lhsT=uq[:, k, :], rhs=WB[:, k, 1536:1544],
                                                   start=(k == 0), stop=(k == 7)), reads=[Buq, BW], writes=[X.psb[2]])
            P.op('act', lambda e: e.activation(out=iw[:, 0:8], in_=X.ps[2][:, 0:8], func=AF.Abs),
                 reads=[X.psb[2]], writes=[Biw])
            P.op('dve', lambda e: e.tensor_scalar(out=iw[:, 8:16], in0=X.ps[2][:, 0:8], scalar1=0.0, scalar2=0.5,
                                                   op0=ALU.is_ge, op1=ALU.subtract), reads=[X.psb[2]], writes=[Biw])
            ngr = (nkeys + 511) // 512
            it = 0
            for kg in range(ngr):
                k0 = kg * 512
                wdt = min(512, nkeys - k0)
                for h in range(8):
                    pi = 5 + (it % 2)
                    rs = it % 2
                    it += 1
                    P.op('pe', lambda e, pi=pi, h=h, k0=k0, wdt=wdt: e.matmul(X.ps[pi][:, 0:wdt], lhsT=IQT[0:64, h, :],
                                                                              rhs=IKT[0:64, k0:k0 + wdt], start=True, stop=True),
                         reads=[BIQ], writes=[X.psb[pi]])
                    P.op('act', lambda e, pi=pi, h=h, rs=rs, wdt=wdt: e.activation(out=rl[rs][:, 0:wdt], in_=X.ps[pi][:, 0:wdt],
                                                                                   func=AF.Relu, scale=iw[:, h:h + 1]),
                         reads=[X.psb[pi], Biw], writes=[Brl[rs]])
                    if h == 0:
                        P.op('dve', lambda e, rs=rs, k0=k0, wdt=wdt: e.tensor_scalar(
                            out=Isc[:, k0:k0 + wdt], in0=rl[rs][:, 0:wdt], scalar1=iw[:, 8:9], scalar2=None, op0=ALU.mult),
                            reads=[Brl[rs], Biw], writes=[BI])
                    else:
                        P.op('dve', lambda e, rs=rs, k0=k0, wdt=wdt, h=h: e.scalar_tensor_tensor(
                            out=Isc[:, k0:k0 + wdt], in0=rl[rs][:, 0:wdt], scalar=iw[:, 8 + h:9 + h], in1=Isc[:, k0:k0 + wdt],
                            op0=ALU.mult, op1=ALU.add), reads=[Brl[rs], Biw, BI], writes=[BI])
            Iv = Isc[:, 0:nkeys]
            if m >= 1:
                P.op('dve', lambda e, Iv=Iv: e.tensor_reduce(out=bs[:, 0:1], in_=Iv, axis=AX.X, op=ALU.max), reads=[BI], writes=[Bbs])
                P.op('dve', lambda e, Iv=Iv: e.tensor_reduce(out=bs[:, 1:2], in_=Iv, axis=AX.X, op=ALU.min), reads=[BI], writes=[Bbs])
            P.op('dve', lambda e, sc=sc: e.tensor_tensor(out=Isc[:, sc * 128:(sc + 1) * 128], in0=Isc[:, sc * 128:(sc + 1) * 128],
                                                         in1=X.cst[:, C_TRI:C_TRI + 128], op=ALU.add), reads=[BI, X.Bcst], writes=[BI])
            P.op('dve', lambda e: e.tensor_tensor(out=Isc[:, 0:128], in0=Isc[:, 0:128], in1=X.cst[:, C_PAD:C_PAD + 128], op=ALU.add),
                 reads=[BI, X.Bcst], writes=[BI])
            if m >= 1:
                P.op('dve', lambda e: e.tensor_tensor(out=bs[:, 2:3], in0=bs[:, 0:1], in1=bs[:, 1:2], op=ALU.subtract), reads=[Bbs], writes=[Bbs])
                P.op('dve', lambda e: e.tensor_copy(out=bs[:, 3:4], in_=bs[:, 1:2]), reads=[Bbs], writes=[Bbs])
            else:
                P.op('dve', lambda e: e.memset(bs[:, 3:4], -1.0e29), writes=[Bbs])

        def bis_iter(m, it_b):
            sc, nk, nkeys = geom(m)
            Iv = Isc[:, 0:nkeys]
            cjunk = Mb2[m % 2]
            ck = float(2.0 ** (-it_b))
            P.op('dve', lambda e, ck=ck: e.scalar_tensor_tensor(out=bs[:, 4:5], in0=bs[:, 2:3], scalar=ck, in1=bs[:, 3:4],
                                                                op0=ALU.mult, op1=ALU.add), reads=[Bbs], writes=[Bbs])
            P.op('dve', lambda e, Iv=Iv, nkeys=nkeys, cjunk=cjunk: e.tensor_scalar(out=cjunk[:, 0:nkeys], in0=Iv, scalar1=bs[:, 4:5], scalar2=None,
                                                                                  op0=ALU.is_ge, op1=ALU.add, accum_out=bs[:, 5:6]),
                 reads=[Bbs, BI], writes=[Bbs, BMb2[m % 2]])
            P.op('dve', lambda e: e.tensor_scalar(out=bs[:, 6:7], in0=bs[:, 5:6], scalar1=255.5, scalar2=bs[:, 2:3],
                                                   op0=ALU.is_ge, op1=ALU.mult), reads=[Bbs], writes=[Bbs])
            P.op('dve', lambda e, ck=ck: e.scalar_tensor_tensor(out=bs[:, 3:4], in0=bs[:, 6:7], scalar=ck, in1=bs[:, 3:4],
                                                                op0=ALU.mult, op1=ALU.add), reads=[Bbs], writes=[Bbs])

        def bis_fin(m):
            sc, nk, nkeys = geom(m)
            Iv = Isc[:, 0:nkeys]
            Mb = Mb2[m % 2]
            P.op('dve', lambda e, Iv=Iv, nkeys=nkeys, Mb=Mb: e.tensor_scalar(out=Mb[:, 0:nkeys], in0=Iv, scalar1=bs[:, 3:4], scalar2=MASKV,
                                                                             op0=ALU.is_lt, op1=ALU.mult), reads=[BI, Bbs], writes=[BMb2[m % 2]])

        def back(m, hook):
            sc, nk, nkeys = geom(m)
            s = m % 2
            QT = QT2[s]
            Mb = Mb2[s]
            items = [(g, c) for g in range(4) for c in range(nk)]

            def emit_S(i):
                g, c = items[i]
                pi = SB[i % 4]
                P.op('pe', lambda e, pi=pi, g=g, c=c: e.matmul(X.ps[pi][:, :], lhsT=KT[0:64, g, c * 128:(c + 1) * 128],
                                                               rhs=QT[0:64, g * 4:(g + 1) * 4, :], start=True, stop=False),
                     reads=[BQ2[s]], writes=[X.psb[pi]])
                P.op('pe', lambda e, pi=pi, c=c: e.matmul(X.ps[pi][:, :], lhsT=Mb[:, c * 128:(c + 1) * 128], rhs=X.irep,
                                                          start=False, stop=True),
                     reads=[BMb2[s], X.Birep], writes=[X.psb[pi]])
            for i0_ in range(min(LA, len(items))):
                emit_S(i0_)
            for i, (g, c) in enumerate(items):
                if i + LA < len(items):
                    emit_S(i + LA)
                pi = SB[i % 4]
                ps_ = i % 4
                ob = 2 if g % 2 == 0 else 7
                P.op('act', lambda e, pi=pi, ps_=ps_: e.activation(out=PT[ps_], in_=X.ps[pi][:, :], func=AF.Exp, scale=0.125),
                     reads=[X.psb[pi]], writes=[BPT[ps_]])
                for r in range(4):
                    P.op('pe', lambda e, r=r, ps_=ps_, c=c, g=g, ob=ob: e.matmul(X.ps[ob][:, r * 65:(r + 1) * 65],
                                                                                 lhsT=PT[ps_][:, r * 128:(r + 1) * 128],
                                                                                 rhs=VA[:, c, g * 65:(g + 1) * 65],
                                                                                 start=(c == 0 and r == 0), stop=(c == nk - 1),
                                                                                 skip_group_check=True),
                         reads=[BPT[ps_]], writes=[X.psb[ob]])
                if c == nk - 1:
                    O3 = X.ps[ob][:, 0:260].rearrange("p (r d) -> p r d", r=4)
                    P.op('dve', lambda e, O3=O3, g=g: e.reciprocal(out=rden[:, g * 4:(g + 1) * 4], in_=O3[:, :, 64]), reads=[X.psb[ob]], writes=[BOn])
                    P.op('dve', lambda e, O3=O3, g=g: e.tensor_tensor(
                        out=On[:, g * 256:(g + 1) * 256].rearrange("p (r d) -> p r d", r=4), in0=O3[:, :, 0:64],
                        in1=rden[:, g * 4:(g + 1) * 4].unsqueeze(2).to_broadcast([128, 4, 64]), op=ALU.mult),
                        reads=[X.psb[ob], BOn], writes=[BOn])
                hook(i, len(items))
            for k in range(8):
                pi = 0 if k < 4 else 1
                P.op('pe', lambda e, k=k, pi=pi: e.transpose(out=X.ps[pi][:, (k % 4) * 128:(k % 4 + 1) * 128],
                                                             in_=On[:, k * 128:(k + 1) * 128], identity=X.ident),
                     reads=[BOn, X.Bcst], writes=[X.psb[pi]])
            for half in range(2):
                P.op('act', lambda e, half=half: e.copy(out=OnT[:, half * 4:(half + 1) * 4, :],
                                                        in_=X.ps[half][:, :].rearrange("p (k t) -> p k t", k=4)),
                     reads=[X.psb[half]], writes=[BOnT])
            for half in range(2):
                pi = 3 + half
                for k in range(8):
                    P.op('pe', lambda e, k=k, pi=pi, half=half: e.matmul(X.ps[pi][:, :], lhsT=OnT[:, k, :],
                                                                         rhs=Wo[:, k, half * 512:(half + 1) * 512],
                                                                         start=(k == 0), stop=(k == 7)),
                         reads=[BOnT, BWo], writes=[X.psb[pi]])
                P.op('dve', lambda e, pi=pi, half=half: e.tensor_tensor(out=On[:, half * 512:(half + 1) * 512], in0=X.ps[pi][:, :],
                                                                        in1=gate_bc[:, half * 512:(half + 1) * 512], op=ALU.mult),
                     reads=[X.psb[pi], Bgate], writes=[BOn])
            P.op('pool', lambda e, s=s: e.tensor_tensor(out=On, in0=On, in1=xt[s], op=ALU.add), reads=[BOn, Bxt[s]], writes=[BOn])
            P.dma('sp', [(hmid[m * 128:(m + 1) * 128, :], On)], reads=[BOn], writes=[Buf('o')], sem_buf=BOn)

        frontA(0)
        bis_fin(0)
        for m in range(nqb):
            nxt = m + 1 if m + 1 < nqb else None
            st_ = {'done': 0}
            if nxt is not None:
                frontA(nxt)

            def hook(i, total, nxt=nxt, st_=st_):
                if nxt is None:
                    return
                target = (nbis * (i + 1) + total - 1) // total
                while st_['done'] < min(target, nbis):
                    st_['done'] += 1
                    bis_iter(nxt, st_['done'])
            back(m, hook)
            if nxt is not None:
                while st_['done'] < nbis:
                    st_['done'] += 1
                    bis_iter(nxt, st_['done'])
                bis_fin(nxt)
        print("att0 arena usage a32", a32.off, "/", a32.n, "a16", a16.off, "/", a16.n)
        P.barrier()


def build_att0(nqb=NQB, nbis=NBIS):
    nc = bass.Bass("TRN2", target_bir_lowering=False)

    def din(name, shape, dt=F32):
        return nc.dram_tensor(name, shape, dt, kind="ExternalInput").ap()
    xk = din("xk", [NKC * 128, 1024])
    posk = din("posk", [1, NKC * 128], I32)
    cst_d = din("cst", [128, NCST])
    cT = din("cT", [128, 8])
    w_ada = din("w_ada", [1024, 3072])
    b_adaT = din("b_adaT", [128, 16])
    b_gate = din("b_gate", [1, 1024])
    gainT = din("gainT", [128, 8])
    w_in = din("w_in", [1024, 2120])
    w_out = din("w_out", [1024, 1024])
    hmid = nc.dram_tensor("hmid", [nqb * 128, 1024], F32, kind="ExternalOutput").ap()

    T = dict(xk=xk, posk=posk, cst_d=cst_d, cT=cT, w_ada=w_ada, b_adaT=b_adaT, b_gate=b_gate, gainT=gainT, w_in=w_in, w_out=w_out, hmid=hmid)
    X = setup_ctx(nc)
    with X.st:
        phase_att0(X, T, nqb, nbis)
        X.P.emit()
    return nc


def att0_inputs(inputs, layer=0):
    x = np.asarray(inputs['x'], np.float32)
    pos = np.asarray(inputs['positions'], np.int32)
    maps = []
    for c in range(8):
        b, j = c // 2, c % 2
        if j == 0:
            xk = np.concatenate([np.zeros((128, 1024), np.float32), x[b, 0:31 * 128]], axis=0)
            pk = np.concatenate([np.zeros((128,), np.int32), pos[b, 0:31 * 128]])
        else:
            xk = x[b]
            pk = pos[b]
        maps.append({
            "xk": np.ascontiguousarray(xk), "posk": np.ascontiguousarray(pk[None, :]),
            "cst": make_consts(j),
            "cT": np.ascontiguousarray(inputs['c'][b].reshape(8, 128).T),
            "w_ada": np.ascontiguousarray(inputs['w_ada'][layer][:, 0:3072]),
            "b_adaT": np.ascontiguousarray(inputs['b_ada'][layer][0:2048].reshape(16, 128).T),
            "b_gate": np.ascontiguousarray(inputs['b_ada'][layer][2048:3072][None, :]),
            "gainT": np.ascontiguousarray(inputs['attn_gain'][layer].reshape(8, 128).T),
            "w_in": np.ascontiguousarray(inputs['a_w_in'][0]),
            "w_out": np.ascontiguousarray(inputs['a_w_out'][0]),
        })
    return maps


def gather_blocks(res, key, nqb=NQB):
    out = np.zeros((4, 4096, 1024), np.float32)
    for c in range(8):
        b, j = c // 2, c % 2
        r = res[c][key]
        for m in range(nqb):
            i = 2 * m + j
            out[b, i * 128:(i + 1) * 128] = r[m * 128:(m + 1) * 128]
    return out


def ffn_phase0(X, cT, w_ada_f, b_adaT, b_gate, gainT):
    P = X.P
    a32 = X.a32
    gate_bc = a32.alloc(1024)
    Bgate = Buf('gate')
    G1 = a32.alloc(8)
    gT = a32.alloc(8)
    cact = a32.alloc(8)
    modT = a32.alloc(16)
    bT = a32.alloc(16)
    m32 = a32.mark()
    wbuf = [a32.alloc(8 * 512), a32.alloc(8 * 512)]
    Bw = [Buf('wa0'), Buf('wa1')]
    modT, Bmod, cact, Bc = mod_vectors(X, cT, w_ada_f[:, 0:2048], b_adaT, 2048, wbuf, Bw, 0, cact, modT, bT)
    bcast_row_vec(X, cact, Bc, w_ada_f[:, 2048:3072], b_gate, gate_bc, Bgate, wbuf, Bw, 1, 2)
    P.dma('sp', [(gT, gainT[:, :])], writes=[Bmod])
    P.op('dve', lambda e: e.scalar_tensor_tensor(out=G1, in0=modT[:, 8:16], scalar=1.0, in1=gT, op0=ALU.add, op1=ALU.mult),
         reads=[Bmod], writes=[Bmod])
    SH = modT[:, 0:8]
    P.barrier()
    a32.reset(m32)
    return G1, SH, Bmod, gate_bc, Bgate


def phase_ffn0(X, T, ntok=2048, dff=2816):
    hin = T['hin']
    cst_d = T['cst_d']
    cT = T['cT']
    w_ada = T['w_ada']
    b_adaT = T['b_adaT']
    b_gate = T['b_gate']
    gainT = T['gainT']
    wg = T['wg']
    wu = T['wu']
    wd = T['wd']
    hout = T['hout']
    NF = dff // 128
    GT = 1024
    ngrp = ntok // GT
    NT = GT // 128
    X.carve(15000)
    P = X.P
    a32, a16 = X.a32, X.a16
    if True:
        load_consts(X, cst_d)
        X.eps_col = a32.alloc(1)
        X.Bcst2 = Buf('cst2')
        P.op('pool', lambda e: e.memset(X.eps_col, 1e-6), writes=[X.Bcst2])
        G1, SH, Bmod, gate_bc, Bgate = ffn_phase0(X, cT, w_ada, b_adaT, b_gate, gainT)

        Wd = a16.alloc(NF * 1024).rearrange("p (f n) -> p f n", f=NF)
        BWd = Buf('Wd')
        wd3 = wd.rearrange("(f p) n -> p f n", p=128)
        nsp = 4
        P.dma('pool', [(Wd[:, (i * NF) // nsp:((i + 1) * NF) // nsp, :], wd3[:, (i * NF) // nsp:((i + 1) * NF) // nsp, :]) for i in range(nsp)],
              writes=[BWd])
        wg3 = wg.rearrange("(k p) n -> p k n", p=128)
        wu3 = wu.rearrange("(k p) n -> p k n", p=128)
        NWB = 3
        Wgb = [a16.alloc(8 * 128).rearrange("p (k n) -> p k n", k=8) for _ in range(NWB)]
        Wub = [a16.alloc(8 * 128).rearrange("p (k n) -> p k n", k=8) for _ in range(NWB)]
        BWg = [Buf('wg%d' % i) for i in range(NWB)]
        u2T = a16.alloc(8 * GT).rearrange("p (k t) -> p k t", k=8)
        Bu2 = Buf('u2T')
        actT = a16.alloc(NF * GT).rearrange("p (f t) -> p f t", f=NF)
        Bact = Buf('actT')
        hres = a32.alloc(NT * 1024).rearrange("p (t n) -> p t n", t=NT)
        Bh = [Buf('h%d' % i) for i in range(NT)]
        xn = a32.alloc(1024)
        Bxn = Buf('xn')
        ss = a32.alloc(4)
        sg = [a32.alloc(512), a32.alloc(512)]
        Bsg = [Buf('sg0'), Buf('sg1')]
        ot = [a32.alloc(1024), a32.alloc(1024)]
        Bot = [Buf('ot0'), Buf('ot1')]

        wi = 0
        for grp in range(ngrp):
            g0 = grp * GT
            for t in range(NT):
                P.dma('sp', [(hres[:, t, :], hin[g0 + t * 128:g0 + (t + 1) * 128, :])], writes=[Bh[t]])
                norm_modT(X, hres[:, t, :], Bh[t], G1, SH, Bmod, lambda k, t=t: u2T[:, k, t * 128:(t + 1) * 128], Bu2,
                          xn, Bxn, ss, 0, 1, xn)
            it = 0
            for f in range(NF):
                wb = wi % NWB
                wi += 1
                P.dma('pool', [(Wgb[wb], wg3[:, :, f * 128:(f + 1) * 128]), (Wub[wb], wu3[:, :, f * 128:(f + 1) * 128])], writes=[BWg[wb]])
                for half in range(GT // 512):
                    pg = 2 + 2 * (it % 2)
                    pu = pg + 1
                    sgi = it % 2
                    it += 1
                    for (W_, pi) in [(Wgb[wb], pg), (Wub[wb], pu)]:
                        for k in range(8):
                            P.op('pe', lambda e, k=k, W_=W_, pi=pi, half=half: e.matmul(X.ps[pi][:, :], lhsT=W_[:, k, :],
                                                                                       rhs=u2T[:, k, half * 512:(half + 1) * 512],
                                                                                       start=(k == 0), stop=(k == 7)),
                                 reads=[BWg[wb], Bu2], writes=[X.psb[pi]])
                    P.op('act', lambda e, pg=pg, sgi=sgi: e.activation(out=sg[sgi], in_=X.ps[pg][:, :], func=AF.Silu),
                         reads=[X.psb[pg]], writes=[Bsg[sgi]])
                    P.op('dve', lambda e, pu=pu, sgi=sgi, f=f, half=half: e.tensor_tensor(out=actT[:, f, half * 512:(half + 1) * 512], in0=X.ps[pu][:, :],
                                                                                        in1=sg[sgi], op=ALU.mult),
                         reads=[X.psb[pu], Bsg[sgi]], writes=[Bact])
            it = 0
            for t in range(NT):
                o = t % 2
                for half in range(2):
                    pi = 6 + (it % 2)
                    it += 1
                    for f in range(NF):
                        P.op('pe', lambda e, f=f, pi=pi, t=t, half=half: e.matmul(X.ps[pi][:, :], lhsT=actT[:, f, t * 128:(t + 1) * 128],
                                                                                 rhs=Wd[:, f, half * 512:(half + 1) * 512],
                                                                                 start=(f == 0), stop=(f == NF - 1)),
                             reads=[Bact, BWd], writes=[X.psb[pi]])
                    P.op('dve', lambda e, pi=pi, o=o, half=half: e.tensor_tensor(out=ot[o][:, half * 512:(half + 1) * 512], in0=X.ps[pi][:, :],
                                                                                in1=gate_bc[:, half * 512:(half + 1) * 512], op=ALU.mult),
                         reads=[X.psb[pi], Bgate], writes=[Bot[o]])
                P.op('pool', lambda e, o=o, t=t: e.tensor_tensor(out=ot[o], in0=ot[o], in1=hres[:, t, :], op=ALU.add),
                     reads=[Bot[o], Bh[t]], writes=[Bot[o]])
                P.dma('sp', [(hout[g0 + t * 128:g0 + (t + 1) * 128, :], ot[o])], reads=[Bot[o]], writes=[Buf('o')], sem_buf=Bot[o])
        P.barrier()


def build_ffn0(ntok=2048, dff=2816):
    nc = bass.Bass("TRN2", target_bir_lowering=False)

    def din(name, shape, dt=F32):
        return nc.dram_tensor(name, shape, dt, kind="ExternalInput").ap()
    hin = din("hin", [ntok, 1024])
    cst_d = din("cst", [128, NCST])
    cT = din("cT", [128, 8])
    w_ada = din("w_ada", [1024, 3072])
    b_adaT = din("b_adaT", [128, 16])
    b_gate = din("b_gate", [1, 1024])
    gainT = din("gainT", [128, 8])
    wg = din("wg", [1024, dff])
    wu = din("wu", [1024, dff])
    wd = din("wd", [dff, 1024])
    hout = nc.dram_tensor("hout", [ntok, 1024], F32, kind="ExternalOutput").ap()
    NF = dff // 128
    GT = 1024
    ngrp = ntok // GT
    NT = GT // 128

    T = dict(hin=hin, cst_d=cst_d, cT=cT, w_ada=w_ada, b_adaT=b_adaT, b_gate=b_gate, gainT=gainT, wg=wg, wu=wu, wd=wd, hout=hout)
    X = setup_ctx(nc)
    with X.st:
        phase_ffn0(X, T, ntok, dff)
        X.P.emit()
    return nc


def ffn0_inputs(inputs, hmid_cores, layer=0):
    maps = []
    for c in range(8):
        b, j = c // 2, c % 2
        maps.append({
            "hin": np.ascontiguousarray(hmid_cores[c]),
            "cst": make_consts(j),
            "cT": np.ascontiguousarray(inputs['c'][b].reshape(8, 128).T),
            "w_ada": np.ascontiguousarray(inputs['w_ada'][layer][:, 3072:6144]),
            "b_adaT": np.ascontiguousarray(inputs['b_ada'][layer][3072:5120].reshape(16, 128).T),
            "b_gate": np.ascontiguousarray(inputs['b_ada'][layer][5120:6144][None, :]),
            "gainT": np.ascontiguousarray(inputs['ffn_gain'][layer].reshape(8, 128).T),
            "wg": np.ascontiguousarray(inputs['ffn_w_gate'][0]),
            "wu": np.ascontiguousarray(inputs['ffn_w_up'][0]),
            "wd": np.ascontiguousarray(inputs['ffn_w_down'][0]),
        })
    return maps


def split_blocks(full, nqb=NQB):
    outs = []
    for c in range(8):
        b, j = c // 2, c % 2
        outs.append(np.concatenate([full[b, (2 * m + j) * 128:(2 * m + j + 1) * 128] for m in range(nqb)], axis=0))
    return outs


NCMP = 255


def phase_kv1(X, T):
    hk = T['hk']
    posk = T['posk']
    cst_d = T['cst_d']
    cT = T['cT']
    w_ada = T['w_ada']
    b_adaT = T['b_adaT']
    gainT = T['gainT']
    w_kv = T['w_kv']
    w1k = T['w1k']
    w1v = T['w1v']
    w2k = T['w2k']
    w2v = T['w2v']
    peTk = T['peTk']
    peTv = T['peTv']
    o_kslcT = T['o_kslcT']
    o_kwinT = T['o_kwinT']
    o_vslc = T['o_vslc']
    o_vwin = T['o_vwin']
    o_kcT = T['o_kcT']
    o_vc = T['o_vc']
    X.carve(14000)
    P = X.P
    a32, a16 = X.a32, X.a16
    if True:
        load_consts(X, cst_d)
        X.eps_col = a32.alloc(1)
        X.Bcst2 = Buf('cst2')
        P.op('pool', lambda e: e.memset(X.eps_col, 1e-6), writes=[X.Bcst2])
        G1 = a32.alloc(8)
        gT = a32.alloc(8)
        cact = a32.alloc(8)
        modT = a32.alloc(16)
        bT = a32.alloc(16)
        m32 = a32.mark()
        wbuf = [a32.alloc(8 * 512), a32.alloc(8 * 512)]
        Bw = [Buf('wa0'), Buf('wa1')]
        modT, Bmod, cact, Bc = mod_vectors(X, cT, w_ada, b_adaT, 2048, wbuf, Bw, 0, cact, modT, bT)
        P.dma('sp', [(gT, gainT[:, :])], writes=[Bmod])
        P.op('dve', lambda e: e.scalar_tensor_tensor(out=G1, in0=modT[:, 8:16], scalar=1.0, in1=gT, op0=ALU.add, op1=ALU.mult),
             reads=[Bmod], writes=[Bmod])
        SH = modT[:, 0:8]
        P.barrier()
        a32.reset(m32)

        W = a16.alloc(8 * 1536).rearrange("p (k n) -> p k n", k=8)
        Wp = a16.alloc(8 * 768).rearrange("p (k n) -> p k n", k=8)
        BW = Buf('W')
        BWp = Buf('Wp')
        load_w_bf16(X, W, w_kv, BW, nsplit=2)
        for (s0, d0) in [(0, 0), (512, 256), (1024, 512)]:
            for k in range(8):
                src = W[:, k, s0:s0 + 256].rearrange("p (h t i) -> p h t i", h=4, t=2)
                dst = Wp[:, k, d0:d0 + 256].rearrange("p (h t i) -> p h t i", h=4, t=2)
                eng = 'pool' if k % 2 == 0 else 'dve'
                P.op(eng, lambda e, src=src, dst=dst: e.tensor_copy(out=dst[:, :, 0, :], in_=src[:, :, 1, :]), reads=[BW], writes=[BWp])
                P.op(eng, lambda e, src=src, dst=dst: e.tensor_copy(out=dst[:, :, 1, :], in_=src[:, :, 0, :]), reads=[BW], writes=[BWp])
        KcT = a16.alloc(4 * NKC * 128).rearrange("p (g t) -> p g t", g=4)
        VcT = a16.alloc(4 * NKC * 128).rearrange("p (g t) -> p g t", g=4)
        Bcmp = Buf('cmpstore')
        xt = [a32.alloc(1024), a32.alloc(1024)]
        Bxt = [Buf('xt0'), Buf('xt1')]
        xn = a32.alloc(1024)
        Bxn = Buf('xn')
        ss = a32.alloc(4)
        uT = a16.alloc(8 * 512).rearrange("p (k t) -> p k t", k=8)
        BuT = Buf('uT')
        Ct = a32.alloc(512)
        St = a32.alloc(512)
        Btab = Buf('tab')
        tmp32 = a32.alloc(3 * 512)
        Btmp = Buf('ttmp')
        r1 = a32.alloc(512)
        r2 = a32.alloc(512)
        Br = Buf('ropetmp')
        kst = [a16.alloc(512), a16.alloc(512)]
        Bkst = [Buf('kst0'), Buf('kst1')]
        vst = [a16.alloc(512), a16.alloc(512)]
        Bvst = [Buf('vst0'), Buf('vst1')]
        ridx_t = None
        if T.get('ridx') is not None:
            ridx_t = a32.alloc(NKC).bitcast(I32)
            Bridx = Buf('ridx')
            P.dma('sp', [(ridx_t, T['ridx'][:, :])], writes=[Bridx])
        ki = 0
        for grp in range(NKC // 4):
            t0 = grp * 512
            rope_tables(X, posk[:, t0:t0 + 512], 512, Ct[0:64, :], St[0:64, :], Btab, tmp32, X.ai, Btmp)
            for cc in range(4):
                ch = grp * 4 + cc
                s = ch % 2
                if ridx_t is None:
                    P.dma('sp', [(xt[s], hk[ch * 128:(ch + 1) * 128, :])], writes=[Bxt[s]])
                else:
                    P.dma('pool', None, reads=[Bridx], writes=[Bxt[s]],
                          fns=[lambda e, s=s, ch=ch: e.indirect_dma_start(out=xt[s], out_offset=None, in_=hk[:, :],
                                                                          in_offset=bass.IndirectOffsetOnAxis(ap=ridx_t[:, ch:ch + 1], axis=0))])
                norm_modT(X, xt[s], Bxt[s], G1, SH, Bmod, lambda k, cc=cc: uT[:, k, cc * 128:(cc + 1) * 128], BuT,
                          xn, Bxn, ss, 0, 1, xn)
                for (vi, c0) in [(0, 768), (1, 1280)]:
                    for k in range(8):
                        P.op('pe', lambda e, k=k, cc=cc, vi=vi, c0=c0: e.matmul(X.ps[2][:, vi * 256:(vi + 1) * 256],
                                                                               lhsT=uT[:, k, cc * 128:(cc + 1) * 128],
                                                                               rhs=W[:, k, c0:c0 + 256], start=(k == 0 and vi == 0), stop=(k == 7),
                                                                               skip_group_check=True),
                             reads=[BuT, BW], writes=[X.psb[2]])
                P.op('act', lambda e, s=s: e.copy(out=vst[s], in_=X.ps[2][:, :]), reads=[X.psb[2]], writes=[Bvst[s]])
                P.dma('sp', [(o_vslc[ch * 128:(ch + 1) * 128, :], vst[s][:, 0:256]), (o_vwin[ch * 128:(ch + 1) * 128, :], vst[s][:, 256:512])],
                      reads=[Bvst[s]], writes=[Buf('o')], sem_buf=Bvst[s])
            for (kind, c0, c0p) in [('kcmp', 0, 0), ('kslc', 512, 256), ('kwin', 1024, 512), ('vcmp', 256, None)]:
                for hh in range(4):
                    srcs = [(W, 3, c0)] if c0p is None else [(W, 3, c0), (Wp, 4, c0p)]
                    for (W_, pi, cb_) in srcs:
                        for k in range(8):
                            P.op('pe', lambda e, k=k, W_=W_, pi=pi, cbase=cb_ + hh * 64: e.matmul(
                                X.ps[pi][0:64, :], lhsT=W_[:, k, cbase:cbase + 64], rhs=uT[:, k, :],
                                start=(k == 0), stop=(k == 7)),
                                reads=[BuT, BW, BWp], writes=[X.psb[pi]])
                    if kind == 'vcmp':
                        P.op('act', lambda e, hh=hh, t0=t0: e.copy(out=VcT[0:64, hh, t0:t0 + 512], in_=X.ps[3][0:64, :]),
                             reads=[X.psb[3]], writes=[Bcmp])
                        continue
                    t1 = r1[0:64, :]
                    t2 = r2[0:64, :]
                    P.op('dve', lambda e, t1=t1: e.tensor_tensor(out=t1, in0=X.ps[3][0:64, :], in1=Ct[0:64, :], op=ALU.mult),
                         reads=[X.psb[3], Btab], writes=[Br])
                    P.op('dve', lambda e, t2=t2: e.tensor_tensor(out=t2, in0=X.ps[4][0:64, :], in1=St[0:64, :], op=ALU.mult),
                         reads=[X.psb[4], Btab], writes=[Br])
                    if kind == 'kcmp':
                        P.op('pool', lambda e, hh=hh, t0=t0, t1=t1, t2=t2: e.tensor_tensor(out=KcT[0:64, hh, t0:t0 + 512], in0=t1, in1=t2, op=ALU.add),
                             reads=[Br], writes=[Bcmp])
                    else:
                        ks = ki % 2
                        ki += 1
                        P.op('pool', lambda e, ks=ks, t1=t1, t2=t2: e.tensor_tensor(out=kst[ks][0:64, :], in0=t1, in1=t2, op=ALU.add),
                             reads=[Br], writes=[Bkst[ks]])
                        od = o_kslcT if kind == 'kslc' else o_kwinT
                        P.dma('sp', [(od[:, hh * NKC * 128 + t0:hh * NKC * 128 + t0 + 512], kst[ks][0:64, :])],
                              reads=[Bkst[ks]], writes=[Buf('o')], sem_buf=Bkst[ks])
        P.barrier()
        w1s = a16.alloc(32 * 256).rearrange("p (l n) -> p l n", l=32)
        w2s = a16.alloc(2 * 64).rearrange("p (c n) -> p c n", c=2)
        peT = a16.alloc(32)
        Bwc = Buf('wc')
        bvec = a32.alloc(2)
        Bbv = Buf('bvec')
        xh = a32.alloc(256)
        x2 = a32.alloc(256)
        Bxh = Buf('xh')
        actT = a16.alloc(2 * 256).rearrange("p (c n) -> p c n", c=2)
        Bat = Buf('actT')
        ost = a16.alloc(1024)
        Bost = Buf('ost')
        ostv = a16.alloc(2 * 256).rearrange("p (c n) -> p c n", c=2)
        Bostv = Buf('ostv')
        P.op('pool', lambda e: e.memset(ost, 0.0), writes=[Bost])
        P.op('pool', lambda e: e.memset(ostv, 0.0), writes=[Bostv])
        for (kv, w1d, w2d, ped, SRC) in [('k', w1k, w2k, peTk, KcT), ('v', w1v, w2v, peTv, VcT)]:
            P.dma('pool', [(w1s[0:64, :, :], w1d.rearrange("(l d) n -> d l n", d=64)), (w2s, w2d.rearrange("(c p) n -> p c n", p=128)),
                           (peT[0:64, :], ped[:, :])], writes=[Bwc])
            for hc in range(2):
                for l in range(32):
                    P.op('pe', lambda e, hc=hc, l=l: e.matmul(X.ps[0][:, hc:hc + 1], lhsT=w1s[0:64, l, hc * 128:(hc + 1) * 128],
                                                              rhs=peT[0:64, l:l + 1], start=(l == 0), stop=(l == 31)),
                         reads=[Bwc], writes=[X.psb[0]])
            P.op('dve', lambda e: e.tensor_copy(out=bvec, in_=X.ps[0][:, 0:2]), reads=[X.psb[0]], writes=[Bbv])
            for g in range(4):
                for hc in range(2):
                    pi = 1 + hc
                    for l in range(32):
                        rhs = SRC[0:64, g, l:l + 16 * (NCMP - 1) + 1:16]
                        P.op('pe', lambda e, hc=hc, l=l, pi=pi, rhs=rhs: e.matmul(X.ps[pi][:, 0:NCMP], lhsT=w1s[0:64, l, hc * 128:(hc + 1) * 128],
                                                                                  rhs=rhs, start=(l == 0), stop=(l == 31)),
                             reads=[Bwc, Bcmp], writes=[X.psb[pi]])
                    xv = xh[:, 0:NCMP]
                    x2v = x2[:, 0:NCMP]
                    P.op('act', lambda e, pi=pi, hc=hc, xv=xv: e.activation(out=xv, in_=X.ps[pi][:, 0:NCMP], func=AF.Identity, bias=bvec[:, hc:hc + 1]),
                         reads=[X.psb[pi], Bbv], writes=[Bxh])
                    P.op('dve', lambda e, xv=xv, x2v=x2v: e.tensor_tensor(out=x2v, in0=xv, in1=xv, op=ALU.mult), reads=[Bxh], writes=[Bxh])
                    P.op('dve', lambda e, x2v=x2v: e.tensor_scalar(out=x2v, in0=x2v, scalar1=0.044715, scalar2=1.0, op0=ALU.mult, op1=ALU.add),
                         reads=[Bxh], writes=[Bxh])
                    P.op('dve', lambda e, xv=xv, x2v=x2v: e.tensor_tensor(out=x2v, in0=x2v, in1=xv, op=ALU.mult), reads=[Bxh], writes=[Bxh])
                    P.op('act', lambda e, x2v=x2v: e.activation(out=x2v, in_=x2v, func=AF.Sigmoid, scale=1.5957691216), reads=[Bxh], writes=[Bxh])
                    P.op('dve', lambda e, hc=hc, xv=xv, x2v=x2v: e.tensor_tensor(out=actT[:, hc, 0:NCMP], in0=x2v, in1=xv, op=ALU.mult),
                         reads=[Bxh], writes=[Bat])
                if kv == 'k':
                    for hc in range(2):
                        P.op('pe', lambda e, hc=hc: e.matmul(X.ps[3][0:64, 0:NCMP], lhsT=w2s[:, hc, :], rhs=actT[:, hc, 0:NCMP],
                                                             start=(hc == 0), stop=(hc == 1)), reads=[Bat, Bwc], writes=[X.psb[3]])
                    P.op('act', lambda e, g=g: e.copy(out=ost[0:64, g * 256:g * 256 + NCMP], in_=X.ps[3][0:64, 0:NCMP]),
                         reads=[X.psb[3]], writes=[Bost])
                else:
                    for nchk in range(2):
                        n0 = nchk * 128
                        nn = min(128, NCMP - n0)
                        for hc in range(2):
                            P.op('pe', lambda e, hc=hc, n0=n0, nn=nn: e.matmul(X.ps[4][0:nn, 0:64], lhsT=actT[:, hc, n0:n0 + nn], rhs=w2s[:, hc, :],
                                                                               start=(hc == 0), stop=(hc == 1)), reads=[Bat, Bwc], writes=[X.psb[4]])
                        P.op('act', lambda e, g=g, nchk=nchk, nn=nn: e.copy(out=ostv[0:nn, nchk, g * 64:(g + 1) * 64], in_=X.ps[4][0:nn, 0:64]),
                             reads=[X.psb[4]], writes=[Bostv])
        P.dma('sp', [(o_kcT[:, :], ost[0:64, :])], reads=[Bost], writes=[Buf('o')], sem_buf=Bost)
        P.dma('sp', [(o_vc.rearrange("(c p) n -> p c n", p=128), ostv)], reads=[Bostv], writes=[Buf('o')], sem_buf=Bostv)
        P.barrier()


def build_kv1():
    nc = bass.Bass("TRN2", target_bir_lowering=False)

    def din(name, shape, dt=F32):
        return nc.dram_tensor(name, shape, dt, kind="ExternalInput").ap()

    def dout(name, shape, dt=BF16):
        return nc.dram_tensor(name, shape, dt, kind="ExternalOutput").ap()
    hk = din("hk", [NKC * 128, 1024])
    posk = din("posk", [1, NKC * 128], I32)
    cst_d = din("cst", [128, NCST])
    cT = din("cT", [128, 8])
    w_ada = din("w_ada", [1024, 2048])
    b_adaT = din("b_adaT", [128, 16])
    gainT = din("gainT", [128, 8])
    w_kv = din("w_kv", [1024, 1536])
    w1k = din("w1k", [2048, 256])
    w1v = din("w1v", [2048, 256])
    w2k = din("w2k", [256, 64])
    w2v = din("w2v", [256, 64])
    peTk = din("peTk", [64, 32])
    peTv = din("peTv", [64, 32])
    o_kslcT = dout("kslcT", [64, 4 * NKC * 128])
    o_kwinT = dout("kwinT", [64, 4 * NKC * 128])
    o_vslc = dout("vslc", [NKC * 128, 256])
    o_vwin = dout("vwin", [NKC * 128, 256])
    o_kcT = dout("kcT", [64, 4 * 256])
    o_vc = dout("vc", [256, 256])

    T = dict(hk=hk, posk=posk, cst_d=cst_d, cT=cT, w_ada=w_ada, b_adaT=b_adaT, gainT=gainT, w_kv=w_kv, w1k=w1k, w1v=w1v, w2k=w2k, w2v=w2v, peTk=peTk, peTv=peTv, o_kslcT=o_kslcT, o_kwinT=o_kwinT, o_vslc=o_vslc, o_vwin=o_vwin, o_kcT=o_kcT, o_vc=o_vc)
    X = setup_ctx(nc)
    with X.st:
        phase_kv1(X, T)
        X.P.emit()
    return nc


def storage_order(full_b, j):
    if j == 0:
        pad = np.zeros((128,) + full_b.shape[1:], full_b.dtype)
        return np.ascontiguousarray(np.concatenate([pad, full_b[0:31 * 128]], axis=0))
    return np.ascontiguousarray(full_b)


def kv1_inputs(inputs, h1_full):
    maps = []
    for c in range(8):
        b, j = c // 2, c % 2
        maps.append({
            "hk": storage_order(h1_full[b], j),
            "posk": np.ascontiguousarray(storage_order(np.asarray(inputs['positions'][b], np.int32), j)[None, :]),
            "cst": make_consts(j),
            "cT": np.ascontiguousarray(inputs['c'][b].reshape(8, 128).T),
            "w_ada": np.ascontiguousarray(inputs['w_kv_ada']),
            "b_adaT": np.ascontiguousarray(inputs['b_kv_ada'].reshape(16, 128).T),
            "gainT": np.ascontiguousarray(inputs['kv_gain'].reshape(8, 128).T),
            "w_kv": np.ascontiguousarray(inputs['w_kv']),
            "w1k": np.ascontiguousarray(inputs['cmp_w1_k']), "w1v": np.ascontiguousarray(inputs['cmp_w1_v']),
            "w2k": np.ascontiguousarray(inputs['cmp_w2_k']), "w2v": np.ascontiguousarray(inputs['cmp_w2_v']),
            "peTk": np.ascontiguousarray(inputs['cmp_pe_k'].T), "peTv": np.ascontiguousarray(inputs['cmp_pe_v'].T),
        })
    return maps


C_TRIU = NCST
NCST1 = NCST + 128


def make_consts1(j):
    c = np.zeros((128, NCST1), np.float32)
    c[:, 0:NCST] = make_consts(j)
    q = np.arange(128)[:, None]
    k = np.arange(128)[None, :]
    c[:, C_TRIU:C_TRIU + 128] = np.where(k > q, 0.0, NEG)
    return c


def make_tables1(j, nqb=NQB):
    import ml_dtypes
    cmpMb = np.zeros((nqb, 128, 256), np.float32)
    vm = np.zeros((nqb, 128, 64), np.float32)
    va = np.zeros((nqb, 128, 64), np.float32)
    shift = 128 * (1 - j)
    n = np.arange(256)[None, :]
    jb_st = np.arange(64)[None, :]
    for m in range(nqb):
        t_st = (2 * m + 1) * 128 + np.arange(128)[:, None]
        valid = (n <= 254) & (n >= 8 * (1 - j)) & (16 * n + 31 <= t_st)
        cmpMb[m] = np.where(valid, 0.0, MASKV)
        t_g = t_st - shift
        jb = jb_st - 2 * (1 - j)
        jt = t_g // 64
        valid_b = (jb >= 0) & (jb * 64 <= t_g)
        f0 = (jb == 0)
        f1 = (jb == jt)
        f2 = (jb == jt - 1)
        forced = f0 | f1 | f2
        vm[m] = np.where(valid_b & ~forced, 1.0, 0.0)
        a = np.where(f0, 1.0e30, np.where(f1, 0.9e30, np.where(f2, 0.8e30, 0.0)))
        va[m] = np.where(valid_b, a, NEG)
    cs = np.arange(256)[:, None] * 16
    ss_ = np.arange(64)[None, :] * 64
    ov = np.minimum(cs + 32, ss_ + 64) - np.maximum(cs, ss_)
    agg = (np.clip(ov, 0, None) / 32.0).astype(np.float32)
    agg[255] = 0.0
    return {"cmpMb": cmpMb.astype(ml_dtypes.bfloat16), "selvm": vm, "selva": va, "agg": agg.astype(ml_dtypes.bfloat16)}


def phase_att1(X, T, nqb=NQB):
    hq = T['hq']
    posq = T['posq']
    cst_d = T['cst_d']
    cT = T['cT']
    w_ada = T['w_ada']
    b_adaT = T['b_adaT']
    b_gate = T['b_gate']
    gainT = T['gainT']
    w_q = T['w_q']
    w_out = T['w_out']
    kslcT_d = T['kslcT_d']
    kwinT_d = T['kwinT_d']
    vslc_d = T['vslc_d']
    vwin_d = T['vwin_d']
    kcT_d = T['kcT_d']
    vc_d = T['vc_d']
    cmpMb_d = T['cmpMb_d']
    selvm_d = T['selvm_d']
    selva_d = T['selva_d']
    agg_d = T['agg_d']
    hmid = T['hmid']
    X.carve(15000)
    P = X.P
    a32, a16 = X.a32, X.a16
    if True:
        X.cst = a32.alloc(NCST1)
        X.Bcst = Buf('cst')
        P.dma('sp', [(X.cst, cst_d[:, :])], writes=[X.Bcst])
        X.ident = X.cst[:, C_IDENT:C_IDENT + 128]
        X.irep = a16.alloc(512)
        X.Birep = Buf('irep')
        for r in range(4):
            P.op('dve', lambda e, r=r: e.tensor_copy(out=X.irep[:, r * 128:(r + 1) * 128], in_=X.ident), reads=[X.Bcst], writes=[X.Birep])
        trib = a16.alloc(128)
        triub = a16.alloc(128)
        P.op('dve', lambda e: e.tensor_copy(out=trib, in_=X.cst[:, C_TRI:C_TRI + 128]), reads=[X.Bcst], writes=[X.Birep])
        P.op('dve', lambda e: e.tensor_copy(out=triub, in_=X.cst[:, C_TRIU:C_TRIU + 128]), reads=[X.Bcst], writes=[X.Birep])
        X.eps_col = a32.alloc(1)
        tiny = a32.alloc(1)
        X.Bcst2 = Buf('cst2')
        P.op('pool', lambda e: e.memset(X.eps_col, 1e-6), writes=[X.Bcst2])
        P.op('pool', lambda e: e.memset(tiny, 1e-30), writes=[X.Bcst2])
        G1, SH, Bmod, gate_bc, Bgate = ffn_phase0(X, cT, w_ada, b_adaT, b_gate, gainT)

        KsT = a16.alloc(4 * NKC * 128).rearrange("p (g t) -> p g t", g=4)
        VsAf = a16.alloc(NKC * 260)
        VsA = VsAf.rearrange("p (c n) -> p c n", c=NKC)
        kcT = a16.alloc(1024).rearrange("p (g n) -> p g n", g=4)
        vcxf = a16.alloc(2 * 260)
        vcx = vcxf.rearrange("p (c n) -> p c n", c=2)
        agg = a16.alloc(128).rearrange("p (c n) -> p c n", c=2)
        BKV = Buf('kv')
        P.op('pool', lambda e: e.memset(VsAf, 1.0), writes=[BKV])
        P.op('pool', lambda e: e.memset(vcxf, 1.0), writes=[BKV])
        ks3 = kslcT_d.rearrange("p (g t) -> p g t", g=4)
        P.dma('sp', [(KsT[0:64, g, :], ks3[:, g, :]) for g in range(4)], writes=[BKV])
        vs4 = vslc_d.rearrange("(c p) (g d) -> p c g d", p=128, g=4)
        VsA4 = VsAf.rearrange("p (c g d) -> p c g d", c=NKC, g=4)
        P.dma('sp', [(VsA4[:, :, g, 0:64], vs4[:, :, g, :]) for g in range(4)], writes=[BKV])
        P.dma('sp', [(kcT[0:64, :, :], kcT_d.rearrange("p (g n) -> p g n", g=4))], writes=[BKV])
        vcx4 = vcxf.rearrange("p (c g d) -> p c g d", c=2, g=4)
        vc4 = vc_d.rearrange("(c p) (g d) -> p c g d", p=128, g=4)
        P.dma('sp', [(vcx4[:, :, g, 0:64], vc4[:, :, g, :]) for g in range(4)], writes=[BKV])
        P.dma('sp', [(agg, agg_d.rearrange("(c p) n -> p c n", p=128))], writes=[BKV])
        WB = a16.alloc(8 * 1072).rearrange("p (k n) -> p k n", k=8)
        WBp = a16.alloc(8 * 1024).rearrange("p (k n) -> p k n", k=8)
        BW = Buf('WB')
        BWp = Buf('WBp')
        load_w_bf16(X, WB, w_q, BW, nsplit=2)
        for k in range(8):
            src = WB[:, k, 0:1024].rearrange("p (h t i) -> p h t i", h=16, t=2)
            dst = WBp[:, k, 0:1024].rearrange("p (h t i) -> p h t i", h=16, t=2)
            eng = 'pool' if k % 2 == 0 else 'dve'
            P.op(eng, lambda e, src=src, dst=dst: e.tensor_copy(out=dst[:, :, 0, :], in_=src[:, :, 1, :]), reads=[BW], writes=[BWp])
            P.op(eng, lambda e, src=src, dst=dst: e.tensor_copy(out=dst[:, :, 1, :], in_=src[:, :, 0, :]), reads=[BW], writes=[BWp])
        Wo = a16.alloc(8 * 1024).rearrange("p (k n) -> p k n", k=8)
        BWo = Buf('Wout')
        load_w_bf16(X, Wo, w_out, BWo, nsplit=2)
        xt = [a32.alloc(1024), a32.alloc(1024)]
        Bxt = [Buf('xt0'), Buf('xt1')]
        xn = a32.alloc(1024)
        Bxn = Buf('xn')
        ss = a32.alloc(4)
        Ct = a32.alloc(128)
        St = a32.alloc(128)
        Btab = Buf('tab')
        tmp32 = a32.alloc(3 * 128)
        Btmp = Buf('ttmp')
        r1 = a32.alloc(512)
        r2 = a32.alloc(512)
        Br = Buf('ropetmp')
        uq = a16.alloc(8 * 128).rearrange("p (k t) -> p k t", k=8)
        Buq = Buf('uq')
        QT = a16.alloc(16 * 128).rearrange("p (h t) -> p h t", h=16)
        BQ = Buf('QT')
        gts = a32.alloc(48)
        Bgts = Buf('gts')
        gts3 = gts.rearrange("p (h b) -> p h b", b=3)
        KwT = [a16.alloc(4 * 640).rearrange("p (g t) -> p g t", g=4) for _ in range(2)]
        VwAf = [a16.alloc(5 * 260) for _ in range(2)]
        BKw = [Buf('kw0'), Buf('kw1')]
        for i in range(2):
            P.op('pool', lambda e, i=i: e.memset(VwAf[i], 1.0), writes=[BKw[i]])
        cmb = [a16.alloc(256), a16.alloc(256)]
        svm = [a32.alloc(64), a32.alloc(64)]
        sva = [a32.alloc(64), a32.alloc(64)]
        Btb = [Buf('tb0'), Buf('tb1')]
        PT = [a16.alloc(512) for _ in range(4)]
        BPT = [Buf('pt%d' % i) for i in range(4)]
        SB = [5, 6, 0, 1]
        LA = 3
        rden = a32.alloc(8)
        coef = a32.alloc(8)
        Brd = Buf('rden')
        imp = a32.alloc(64)
        imp2 = a32.alloc(64)
        mx = a32.alloc(16)
        Bimp = Buf('imp')
        selb = a16.alloc(64)
        Bselb = Buf('selb')
        Mbs = [a16.alloc(NKC * 128), a16.alloc(NKC * 128)]
        BMbs = [Buf('mbs0'), Buf('mbs1')]
        On = a32.alloc(1024)
        BOn = Buf('On')
        otmp = a32.alloc(256)
        Botmp = Buf('otmp')
        OnT = a16.alloc(8 * 128).rearrange("p (k t) -> p k t", k=8)
        BOnT = Buf('OnT')
        hm = a32.alloc(1024)
        Bhm = Buf('hm')
        kw3 = kwinT_d.rearrange("p (g t) -> p g t", g=4)

        state = {'it': 0, 'ob': 0}

        def attend(g, chunks, out_slot_fn):
            ob = 2 if state['ob'] % 2 == 0 else 7
            state['ob'] += 1
            n = len(chunks)
            base = state['it']
            state['it'] += n

            def emit_S(ci):
                kl, ml, bias, vr = chunks[ci]
                pi = SB[(base + ci) % 4]
                P.op('pe', lambda e, pi=pi, kl=kl, ml=ml, g=g: e.matmul(X.ps[pi][:, :], lhsT=kl, rhs=QT[0:64, g * 4:(g + 1) * 4, :],
                                                                       start=True, stop=(ml is None)),
                     reads=[BQ, BKw[0], BKw[1], BKV], writes=[X.psb[pi]])
                if ml is not None:
                    P.op('pe', lambda e, pi=pi, ml=ml: e.matmul(X.ps[pi][:, :], lhsT=ml, rhs=X.irep, start=False, stop=True),
                         reads=[X.Birep, BMbs[0], BMbs[1], Btb[0], Btb[1]], writes=[X.psb[pi]])
            for ci in range(min(LA, n)):
                emit_S(ci)
            for ci, (kl, ml, bias, vr) in enumerate(chunks):
                if ci + LA < n:
                    emit_S(ci + LA)
                pi = SB[(base + ci) % 4]
                ps_ = (base + ci) % 4
                if bias is None:
                    P.op('act', lambda e, pi=pi, ps_=ps_: e.activation(out=PT[ps_], in_=X.ps[pi][:, :], func=AF.Exp, scale=0.125),
                         reads=[X.psb[pi]], writes=[BPT[ps_]])
                else:
                    P.op('act', lambda e, pi=pi, ps_=ps_, bias=bias: e.activation(out=PT[ps_], in_=X.ps[pi][:, :], func=AF.Exp, scale=0.125, bias=bias),
                         reads=[X.psb[pi], X.Bcst], writes=[BPT[ps_]])
                for r in range(4):
                    P.op('pe', lambda e, r=r, ps_=ps_, vr=vr, ob=ob, ci=ci, n=n: e.matmul(X.ps[ob][:, r * 65:(r + 1) * 65],
                                                                                       lhsT=PT[ps_][:, r * 128:(r + 1) * 128], rhs=vr,
                                                                                       start=(ci == 0 and r == 0), stop=(ci == n - 1),
                                                                                       skip_group_check=True),
                         reads=[BPT[ps_], BKw[0], BKw[1]], writes=[X.psb[ob]])
                out_slot_fn(ci, ps_)
            return ob

        for m in range(nqb):
            sc = 2 * m + 1
            nk = sc + 1
            nkeys = nk * 128
            s = m % 2
            P.dma('sp', [(xt[s], hq[m * 128:(m + 1) * 128, :])], writes=[Bxt[s]])
            P.dma('sp', [(cmb[s], cmpMb_d[m, :, :]), (svm[s], selvm_d[m, :, :]), (sva[s], selva_d[m, :, :])], writes=[Btb[s]])
            c_lo = max(0, sc - 4)
            nwc = sc - c_lo + 1
            VwA = VwAf[s].rearrange("p (c n) -> p c n", c=5)
            VwA4 = VwAf[s].rearrange("p (c g d) -> p c g d", c=5, g=4)
            P.dma('sp', [(KwT[s][0:64, g, 0:nwc * 128], kw3[:, g, c_lo * 128:(sc + 1) * 128]) for g in range(4)] +
                  [(VwA4[:, 0:nwc, g, 0:64], vwin_d[c_lo * 128:(sc + 1) * 128, :].rearrange("(c p) (g d) -> p c g d", p=128, g=4)[:, :, g, :]) for g in range(4)],
                  writes=[BKw[s]])
            rope_tables(X, posq[:, m * 128:(m + 1) * 128], 128, Ct[0:64, :], St[0:64, :], Btab, tmp32, X.ai, Btmp)
            norm_modT(X, xt[s], Bxt[s], G1, SH, Bmod, lambda k: uq[:, k, :], Buq, xn, Bxn, ss, 0, 1, xn)
            for b4 in range(4):
                for (W_, pi) in [(WB, 3), (WBp, 4)]:
                    for hh in range(4):
                        cbase = (b4 * 4 + hh) * 64
                        for k in range(8):
                            P.op('pe', lambda e, k=k, W_=W_, pi=pi, cbase=cbase, hh=hh: e.matmul(
                                X.ps[pi][0:64, hh * 128:(hh + 1) * 128], lhsT=W_[:, k, cbase:cbase + 64], rhs=uq[:, k, :],
                                start=(k == 0), stop=(k == 7)),
                                reads=[Buq, BW, BWp], writes=[X.psb[pi]])
                A = X.ps[3][0:64, :].rearrange("p (h t) -> p h t", h=4)
                B = X.ps[4][0:64, :].rearrange("p (h t) -> p h t", h=4)
                c_b = Ct[0:64, :].unsqueeze(1).to_broadcast([64, 4, 128])
                s_b = St[0:64, :].unsqueeze(1).to_broadcast([64, 4, 128])
                t1 = r1[0:64, :].rearrange("p (h t) -> p h t", h=4)
                t2 = r2[0:64, :].rearrange("p (h t) -> p h t", h=4)
                P.op('dve', lambda e, A=A, c_b=c_b, t1=t1: e.tensor_tensor(out=t1, in0=A, in1=c_b, op=ALU.mult), reads=[X.psb[3], Btab], writes=[Br])
                P.op('dve', lambda e, B=B, s_b=s_b, t2=t2: e.tensor_tensor(out=t2, in0=B, in1=s_b, op=ALU.mult), reads=[X.psb[4], Btab], writes=[Br])
                P.op('pool', lambda e, b4=b4, t1=t1, t2=t2: e.tensor_tensor(out=QT[0:64, b4 * 4:(b4 + 1) * 4, :], in0=t1, in1=t2, op=ALU.add),
                     reads=[Br], writes=[BQ])
            for k in range(8):
                P.op('pe', lambda e, k=k: e.matmul(X.ps[2][:, 0:48], lhsT=uq[:, k, :], rhs=WB[:, k, 1024:1072],
                                                   start=(k == 0), stop=(k == 7)), reads=[Buq, BW], writes=[X.psb[2]])
            P.op('act', lambda e: e.activation(out=gts, in_=X.ps[2][:, 0:48], func=AF.Sigmoid), reads=[X.psb[2]], writes=[Bgts])

            for g in range(4):
                ms = g % 2
                def cmp_extra(ci, ps_, g=g):
                    for r in range(4):
                        P.op('pe', lambda e, r=r, ps_=ps_, ci=ci: e.matmul(X.ps[3][:, r * 64:(r + 1) * 64], lhsT=PT[ps_][:, r * 128:(r + 1) * 128],
                                                                          rhs=agg[:, ci, :], start=(ci == 0 and r == 0), stop=(ci == 1),
                                                                          skip_group_check=True),
                             reads=[BPT[ps_], BKV], writes=[X.psb[3]])
                chunks = [(kcT[0:64, g, ci * 128:(ci + 1) * 128], cmb[s][:, ci * 128:(ci + 1) * 128], None, vcx[:, ci, g * 65:(g + 1) * 65])
                          for ci in range(2)]
                ob = attend(g, chunks, cmp_extra)
                O3 = X.ps[ob][:, 0:260].rearrange("p (r d) -> p r d", r=4)
                P.op('dve', lambda e, O3=O3: e.tensor_scalar(out=rden[:, 0:4], in0=O3[:, :, 64], scalar1=tiny[:, 0:1], scalar2=None, op0=ALU.add),
                     reads=[X.psb[ob], X.Bcst2], writes=[Brd])
                P.op('dve', lambda e: e.reciprocal(out=rden[:, 0:4], in_=rden[:, 0:4]), reads=[Brd], writes=[Brd])
                P.op('dve', lambda e, g=g: e.tensor_tensor(out=coef[:, 0:4], in0=rden[:, 0:4], in1=gts3[:, g * 4:(g + 1) * 4, 0], op=ALU.mult),
                     reads=[Brd, Bgts], writes=[Brd])
                P.op('dve', lambda e, O3=O3, g=g: e.tensor_tensor(
                    out=On[:, g * 256:(g + 1) * 256].rearrange("p (r d) -> p r d", r=4), in0=O3[:, :, 0:64],
                    in1=coef[:, 0:4].unsqueeze(2).to_broadcast([128, 4, 64]), op=ALU.mult),
                    reads=[X.psb[ob], Brd], writes=[BOn])
                for r in range(4):
                    if r == 0:
                        P.op('dve', lambda e: e.tensor_scalar(out=imp, in0=X.ps[3][:, 0:64], scalar1=rden[:, 0:1], scalar2=None, op0=ALU.mult),
                             reads=[X.psb[3], Brd], writes=[Bimp])
                    else:
                        P.op('dve', lambda e, r=r: e.scalar_tensor_tensor(out=imp, in0=X.ps[3][:, r * 64:(r + 1) * 64], scalar=rden[:, r:r + 1],
                                                                          in1=imp, op0=ALU.mult, op1=ALU.add),
                             reads=[X.psb[3], Brd, Bimp], writes=[Bimp])
                P.op('dve', lambda e, s=s: e.tensor_tensor(out=imp, in0=imp, in1=svm[s], op=ALU.mult), reads=[Bimp, Btb[s]], writes=[Bimp])
                P.op('dve', lambda e, s=s: e.tensor_tensor(out=imp, in0=imp, in1=sva[s], op=ALU.add), reads=[Bimp, Btb[s]], writes=[Bimp])
                P.op('dve', lambda e: e.max(out=mx[:, 0:8], in_=imp), reads=[Bimp], writes=[Bimp])
                P.op('dve', lambda e: e.match_replace(out=imp2, in_to_replace=mx[:, 0:8], in_values=imp, imm_value=-3.0e38), reads=[Bimp], writes=[Bimp])
                P.op('dve', lambda e: e.max(out=mx[:, 8:16], in_=imp2), reads=[Bimp], writes=[Bimp])
                P.op('dve', lambda e: e.tensor_reduce(out=mx[:, 0:1], in_=mx[:, 8:16], axis=AX.X, op=ALU.min), reads=[Bimp], writes=[Bimp])
                P.op('dve', lambda e: e.tensor_scalar(out=selb, in0=imp, scalar1=mx[:, 0:1], scalar2=MASKV, op0=ALU.is_lt, op1=ALU.mult),
                     reads=[Bimp], writes=[Bselb])
                nblk = 2 * nk
                P.op('pool', lambda e, ms=ms, nblk=nblk, nkeys=nkeys: e.tensor_copy(
                    out=Mbs[ms][:, 0:nkeys].rearrange("p (b l) -> p b l", l=64),
                    in_=selb[:, 0:nblk].unsqueeze(2).to_broadcast([128, nblk, 64])), reads=[Bselb], writes=[BMbs[ms]])
                P.op('pool', lambda e, ms=ms, sc=sc: e.tensor_tensor(out=Mbs[ms][:, sc * 128:(sc + 1) * 128], in0=Mbs[ms][:, sc * 128:(sc + 1) * 128],
                                                                    in1=trib, op=ALU.add), reads=[BMbs[ms], X.Birep], writes=[BMbs[ms]])
                chunks = [(KsT[0:64, g, c * 128:(c + 1) * 128], Mbs[ms][:, c * 128:(c + 1) * 128],
                           (X.cst[:, C_PAD:C_PAD + 1] if c == 0 else None), VsA[:, c, g * 65:(g + 1) * 65]) for c in range(nk)]
                ob = attend(g, chunks, lambda ci, ps_: None)

                def accum_branch(ob, br, g=g):
                    O3 = X.ps[ob][:, 0:260].rearrange("p (r d) -> p r d", r=4)
                    P.op('dve', lambda e, O3=O3: e.reciprocal(out=rden[:, 4:8], in_=O3[:, :, 64]), reads=[X.psb[ob]], writes=[Brd])
                    P.op('dve', lambda e: e.tensor_tensor(out=coef[:, 4:8], in0=rden[:, 4:8], in1=gts3[:, g * 4:(g + 1) * 4, br], op=ALU.mult),
                         reads=[Brd, Bgts], writes=[Brd])
                    P.op('dve', lambda e, O3=O3: e.tensor_tensor(out=otmp.rearrange("p (r d) -> p r d", r=4), in0=O3[:, :, 0:64],
                                                                in1=coef[:, 4:8].unsqueeze(2).to_broadcast([128, 4, 64]), op=ALU.mult),
                         reads=[X.psb[ob], Brd], writes=[Botmp])
                    P.op('pool', lambda e: e.tensor_tensor(out=On[:, g * 256:(g + 1) * 256], in0=On[:, g * 256:(g + 1) * 256], in1=otmp, op=ALU.add),
                         reads=[Botmp, BOn], writes=[BOn])
                accum_branch(ob, 1)
                chunks = []
                for wi_, c in enumerate(range(c_lo, sc + 1)):
                    if c == sc:
                        ml = trib
                    elif c == sc - 4:
                        ml = triub
                    else:
                        ml = None
                    chunks.append((KwT[s][0:64, g, wi_ * 128:(wi_ + 1) * 128], ml,
                                   (X.cst[:, C_PAD:C_PAD + 1] if c == 0 else None), VwA[:, wi_, g * 65:(g + 1) * 65]))
                ob = attend(g, chunks, lambda ci, ps_: None)
                accum_branch(ob, 2)
            for k in range(8):
                pi = 0 if k < 4 else 1
                P.op('pe', lambda e, k=k, pi=pi: e.transpose(out=X.ps[pi][:, (k % 4) * 128:(k % 4 + 1) * 128],
                                                             in_=On[:, k * 128:(k + 1) * 128], identity=X.ident),
                     reads=[BOn, X.Bcst], writes=[X.psb[pi]])
            for half in range(2):
                P.op('act', lambda e, half=half: e.copy(out=OnT[:, half * 4:(half + 1) * 4, :],
                                                        in_=X.ps[half][:, :].rearrange("p (k t) -> p k t", k=4)),
                     reads=[X.psb[half]], writes=[BOnT])
            for half in range(2):
                pi = 3 + half
                for k in range(8):
                    P.op('pe', lambda e, k=k, pi=pi, half=half: e.matmul(X.ps[pi][:, :], lhsT=OnT[:, k, :],
                                                                         rhs=Wo[:, k, half * 512:(half + 1) * 512],
                                                                         start=(k == 0), stop=(k == 7)),
                         reads=[BOnT, BWo], writes=[X.psb[pi]])
                P.op('dve', lambda e, pi=pi, half=half: e.tensor_tensor(out=hm[:, half * 512:(half + 1) * 512], in0=X.ps[pi][:, :],
                                                                        in1=gate_bc[:, half * 512:(half + 1) * 512], op=ALU.mult),
                     reads=[X.psb[pi], Bgate], writes=[Bhm])
            P.op('pool', lambda e, s=s: e.tensor_tensor(out=hm, in0=hm, in1=xt[s], op=ALU.add), reads=[Bhm, Bxt[s]], writes=[Bhm])
            P.dma('sp', [(hmid[m * 128:(m + 1) * 128, :], hm)], reads=[Bhm], writes=[Buf('o')], sem_buf=Bhm)
        P.barrier()


def build_att1(nqb=NQB):
    nc = bass.Bass("TRN2", target_bir_lowering=False)

    def din(name, shape, dt=F32):
        return nc.dram_tensor(name, shape, dt, kind="ExternalInput").ap()
    hq = din("hq", [nqb * 128, 1024])
    posq = din("posq", [1, nqb * 128], I32)
    cst_d = din("cst", [128, NCST1])
    cT = din("cT", [128, 8])
    w_ada = din("w_ada", [1024, 3072])
    b_adaT = din("b_adaT", [128, 16])
    b_gate = din("b_gate", [1, 1024])
    gainT = din("gainT", [128, 8])
    w_q = din("w_q", [1024, 1072])
    w_out = din("w_out", [1024, 1024])
    kslcT_d = din("kslcT", [64, 4 * NKC * 128], BF16)
    kwinT_d = din("kwinT", [64, 4 * NKC * 128], BF16)
    vslc_d = din("vslc", [NKC * 128, 256], BF16)
    vwin_d = din("vwin", [NKC * 128, 256], BF16)
    kcT_d = din("kcT", [64, 1024], BF16)
    vc_d = din("vc", [256, 256], BF16)
    cmpMb_d = din("cmpMb", [nqb, 128, 256], BF16)
    selvm_d = din("selvm", [nqb, 128, 64])
    selva_d = din("selva", [nqb, 128, 64])
    agg_d = din("agg", [256, 64], BF16)
    hmid = nc.dram_tensor("hmid", [nqb * 128, 1024], F32, kind="ExternalOutput").ap()

    T = dict(hq=hq, posq=posq, cst_d=cst_d, cT=cT, w_ada=w_ada, b_adaT=b_adaT, b_gate=b_gate, gainT=gainT, w_q=w_q, w_out=w_out, kslcT_d=kslcT_d, kwinT_d=kwinT_d, vslc_d=vslc_d, vwin_d=vwin_d, kcT_d=kcT_d, vc_d=vc_d, cmpMb_d=cmpMb_d, selvm_d=selvm_d, selva_d=selva_d, agg_d=agg_d, hmid=hmid)
    X = setup_ctx(nc)
    with X.st:
        phase_att1(X, T, nqb)
        X.P.emit()
    return nc


def att1_inputs(inputs, h1_cores, kv_res, nqb=NQB):
    maps = []
    for c in range(8):
        b, j = c // 2, c % 2
        pos = np.asarray(inputs['positions'][b], np.int32)
        posq = np.concatenate([pos[(2 * m + j) * 128:(2 * m + j + 1) * 128] for m in range(nqb)])
        mp = {
            "hq": np.ascontiguousarray(h1_cores[c][0:nqb * 128]),
            "posq": np.ascontiguousarray(posq[None, :]),
            "cst": make_consts1(j),
            "cT": np.ascontiguousarray(inputs['c'][b].reshape(8, 128).T),
            "w_ada": np.ascontiguousarray(inputs['w_ada'][1][:, 0:3072]),
            "b_adaT": np.ascontiguousarray(inputs['b_ada'][1][0:2048].reshape(16, 128).T),
            "b_gate": np.ascontiguousarray(inputs['b_ada'][1][2048:3072][None, :]),
            "gainT": np.ascontiguousarray(inputs['attn_gain'][1].reshape(8, 128).T),
            "w_q": np.ascontiguousarray(inputs['b_w_q'][0]),
            "w_out": np.ascontiguousarray(inputs['b_w_out'][0]),
        }
        for k in ["kslcT", "kwinT", "vslc", "vwin", "kcT", "vc"]:
            mp[k] = kv_res[c][k]
        mp.update(make_tables1(j, nqb))
        maps.append(mp)
    return maps


def phase_ffn1(X, T, ntok=2048, dff=3584, nexp=8):
    hin = T['hin']
    cst_d = T['cst_d']
    cT = T['cT']
    w_ada = T['w_ada']
    b_adaT = T['b_adaT']
    b_gate = T['b_gate']
    gainT = T['gainT']
    fgain = T['fgain']
    wr = T['wr']
    wg = T['wg']
    wu = T['wu']
    wd = T['wd']
    hout = T['hout']
    NF = dff // 128
    FB = 4
    GT = 1024
    ngrp = ntok // GT
    NT = GT // 128
    DQ = 256
    X.carve(16300)
    P = X.P
    a32, a16 = X.a32, X.a16
    if True:
        load_consts(X, cst_d)
        X.eps_col = a32.alloc(1)
        X.Bcst2 = Buf('cst2')
        P.op('pool', lambda e: e.memset(X.eps_col, 1e-6), writes=[X.Bcst2])
        G1, SH, Bmod, gate_bc, Bgate = ffn_phase0(X, cT, w_ada, b_adaT, b_gate, gainT)
        fg_bc = a32.alloc(1024)
        P.dma('sp', [(fg_bc, fgain.partition_broadcast(128))], writes=[Bgate])
        Wr = a32.alloc(8 * nexp).rearrange("p (k n) -> p k n", k=8)
        BWr = Buf('Wr')
        P.dma('sp', [(Wr, wr.rearrange("(k p) n -> p k n", p=128))], writes=[BWr])

        NWB = 2
        Wgb = [a16.alloc(8 * 128 * FB).rearrange("p (k n) -> p k n", k=8) for _ in range(NWB)]
        Wub = [a16.alloc(8 * 128 * FB).rearrange("p (k n) -> p k n", k=8) for _ in range(NWB)]
        BWg = [Buf('wg%d' % i) for i in range(NWB)]
        Wdb = [a16.alloc(NF * DQ).rearrange("p (f n) -> p f n", f=NF) for _ in range(2)]
        BWd = [Buf('wd0'), Buf('wd1')]
        u2T = a16.alloc(8 * GT).rearrange("p (k t) -> p k t", k=8)
        Bu2 = Buf('u2T')
        actT = a16.alloc(NF * GT).rearrange("p (f t) -> p f t", f=NF)
        Bact = Buf('actT')
        yacc = a32.alloc(NT * 1024).rearrange("p (t n) -> p t n", t=NT)
        By = [Buf('y%d' % i) for i in range(NT)]
        xt = [a32.alloc(1024), a32.alloc(1024)]
        Bxt = [Buf('xt0'), Buf('xt1')]
        xn = a32.alloc(1024)
        Bxn = Buf('xn')
        ss = a32.alloc(4)
        u32 = a32.alloc(8 * 128).rearrange("p (k t) -> p k t", k=8)
        Bu32 = Buf('u32')
        gall = a32.alloc(NT * nexp).rearrange("p (t n) -> p t n", t=NT)
        Bgall = Buf('gall')
        rt = a32.alloc(64)
        Brt = Buf('rt')
        sg = [a32.alloc(512), a32.alloc(512)]
        Bsg = [Buf('sg0'), Buf('sg1')]

        wi = 0
        di = 0
        for grp in range(ngrp):
            g0 = grp * GT
            for t in range(NT):
                s = t % 2
                P.dma('sp', [(xt[s], hin[g0 + t * 128:g0 + (t + 1) * 128, :])], writes=[Bxt[s]])
                norm_modT(X, xt[s], Bxt[s], G1, SH, Bmod, lambda k, t=t: u2T[:, k, t * 128:(t + 1) * 128], Bu2,
                          xn, Bxn, ss, 0, 1, xn, u32_dst=lambda k: u32[:, k, :], Bu32=Bu32)
                for k in range(8):
                    P.op('pe', lambda e, k=k: e.matmul(X.ps[2][:, 0:nexp], lhsT=u32[:, k, :], rhs=Wr[:, k, :], start=(k == 0), stop=(k == 7)),
                         reads=[Bu32, BWr], writes=[X.psb[2]])
                lg = rt[:, 0:8]
                e1 = rt[:, 8:16]
                lg2 = rt[:, 16:24]
                e2 = rt[:, 24:32]
                m1 = rt[:, 32:33]
                m2 = rt[:, 33:34]
                dl = rt[:, 34:35]
                w1_ = rt[:, 35:36]
                w2_ = rt[:, 36:37]
                gt_ = gall[:, t, :]
                P.op('dve', lambda e, lg=lg: e.tensor_copy(out=lg, in_=X.ps[2][:, 0:nexp]), reads=[X.psb[2]], writes=[Brt])
                P.op('dve', lambda e, lg=lg, m1=m1: e.tensor_reduce(out=m1, in_=lg, axis=AX.X, op=ALU.max), reads=[Brt], writes=[Brt])
                P.op('dve', lambda e, lg=lg, m1=m1, e1=e1: e.tensor_scalar(out=e1, in0=lg, scalar1=m1, scalar2=None, op0=ALU.is_equal), reads=[Brt], writes=[Brt])
                P.op('dve', lambda e, lg=lg, e1=e1, lg2=lg2: e.scalar_tensor_tensor(out=lg2, in0=e1, scalar=NEG, in1=lg, op0=ALU.mult, op1=ALU.add),
                     reads=[Brt], writes=[Brt])
                P.op('dve', lambda e, lg2=lg2, m2=m2: e.tensor_reduce(out=m2, in_=lg2, axis=AX.X, op=ALU.max), reads=[Brt], writes=[Brt])
                P.op('dve', lambda e, lg2=lg2, m2=m2, e2=e2: e.tensor_scalar(out=e2, in0=lg2, scalar1=m2, scalar2=None, op0=ALU.is_equal), reads=[Brt], writes=[Brt])
                P.op('dve', lambda e, m1=m1, m2=m2, dl=dl: e.tensor_tensor(out=dl, in0=m1, in1=m2, op=ALU.subtract), reads=[Brt], writes=[Brt])
                P.op('act', lambda e, dl=dl, w1_=w1_: e.activation(out=w1_, in_=dl, func=AF.Sigmoid), reads=[Brt], writes=[Brt])
                P.op('act', lambda e, dl=dl, w2_=w2_: e.activation(out=w2_, in_=dl, func=AF.Sigmoid, scale=-1.0), reads=[Brt], writes=[Brt])
                P.op('dve', lambda e, e1=e1, w1_=w1_, gt_=gt_: e.tensor_scalar(out=gt_, in0=e1, scalar1=w1_, scalar2=None, op0=ALU.mult),
                     reads=[Brt], writes=[Bgall])
                P.op('dve', lambda e, e2=e2, w2_=w2_, gt_=gt_: e.scalar_tensor_tensor(out=gt_, in0=e2, scalar=w2_, in1=gt_, op0=ALU.mult, op1=ALU.add),
                     reads=[Brt, Bgall], writes=[Bgall])
            for ex in range(nexp):
                wg3 = wg[ex].rearrange("(k p) n -> p k n", p=128)
                wu3 = wu[ex].rearrange("(k p) n -> p k n", p=128)
                wd3 = wd[ex].rearrange("(f p) n -> p f n", p=128)
                it = 0
                for fb in range(NF // FB):
                    wb = wi % NWB
                    wi += 1
                    f0 = fb * FB
                    P.dma('pool', [(Wgb[wb], wg3[:, :, f0 * 128:(f0 + FB) * 128]), (Wub[wb], wu3[:, :, f0 * 128:(f0 + FB) * 128])], writes=[BWg[wb]])
                    for fi in range(FB):
                        f = f0 + fi
                        for half in range(GT // 512):
                            pg = 2 + 2 * (it % 2)
                            pu = pg + 1
                            sgi = it % 2
                            it += 1
                            for (W_, pi) in [(Wgb[wb], pg), (Wub[wb], pu)]:
                                for k in range(8):
                                    P.op('pe', lambda e, k=k, W_=W_, pi=pi, half=half, fi=fi: e.matmul(
                                        X.ps[pi][:, :], lhsT=W_[:, k, fi * 128:(fi + 1) * 128], rhs=u2T[:, k, half * 512:(half + 1) * 512],
                                        start=(k == 0), stop=(k == 7)), reads=[BWg[wb], Bu2], writes=[X.psb[pi]])
                            P.op('act', lambda e, pg=pg, sgi=sgi: e.activation(out=sg[sgi], in_=X.ps[pg][:, :], func=AF.Silu),
                                 reads=[X.psb[pg]], writes=[Bsg[sgi]])
                            P.op('dve', lambda e, pu=pu, sgi=sgi, f=f, half=half: e.tensor_tensor(
                                out=actT[:, f, half * 512:(half + 1) * 512], in0=X.ps[pu][:, :], in1=sg[sgi], op=ALU.mult),
                                reads=[X.psb[pu], Bsg[sgi]], writes=[Bact])
                it = 0
                for q in range(1024 // DQ):
                    db = di % 2
                    di += 1
                    P.dma('pool', [(Wdb[db][:, 0:NF // 2, :], wd3[:, 0:NF // 2, q * DQ:(q + 1) * DQ]),
                                   (Wdb[db][:, NF // 2:NF, :], wd3[:, NF // 2:NF, q * DQ:(q + 1) * DQ])], writes=[BWd[db]])
                    for t in range(NT):
                        pi = 6 + (it % 2)
                        it += 1
                        for f in range(NF):
                            P.op('pe', lambda e, f=f, pi=pi, t=t, db=db: e.matmul(X.ps[pi][:, 0:DQ], lhsT=actT[:, f, t * 128:(t + 1) * 128],
                                                                                 rhs=Wdb[db][:, f, :], start=(f == 0), stop=(f == NF - 1)),
                                 reads=[Bact, BWd[db]], writes=[X.psb[pi]])
                        if ex == 0:
                            P.op('dve', lambda e, pi=pi, t=t, q=q, ex=ex: e.tensor_scalar(out=yacc[:, t, q * DQ:(q + 1) * DQ], in0=X.ps[pi][:, 0:DQ],
                                                                                         scalar1=gall[:, t, ex:ex + 1], scalar2=None, op0=ALU.mult),
                                 reads=[X.psb[pi], Bgall], writes=[By[t]])
                        else:
                            P.op('dve', lambda e, pi=pi, t=t, q=q, ex=ex: e.scalar_tensor_tensor(
                                out=yacc[:, t, q * DQ:(q + 1) * DQ], in0=X.ps[pi][:, 0:DQ], scalar=gall[:, t, ex:ex + 1],
                                in1=yacc[:, t, q * DQ:(q + 1) * DQ], op0=ALU.mult, op1=ALU.add),
                                reads=[X.psb[pi], Bgall, By[t]], writes=[By[t]])
            for t in range(NT):
                s = t % 2
                P.dma('sp', [(xt[s], hin[g0 + t * 128:g0 + (t + 1) * 128, :])], writes=[Bxt[s]])
                P.op('pool', lambda e, t=t: e.tensor_tensor(out=yacc[:, t, :], in0=yacc[:, t, :], in1=gate_bc, op=ALU.mult),
                     reads=[By[t], Bgate], writes=[By[t]])
                P.op('pool', lambda e, t=t, s=s: e.tensor_tensor(out=yacc[:, t, :], in0=yacc[:, t, :], in1=xt[s], op=ALU.add),
                     reads=[By[t], Bxt[s]], writes=[By[t]])
                P.op('act', lambda e, t=t: e.activation(out=xn, in_=yacc[:, t, :], func=AF.Square, accum_out=ss[:, 0:1]), reads=[By[t]], writes=[Bxn])
                P.op('act', lambda e: e.activation(out=ss[:, 1:2], in_=ss[:, 0:1], func=AF.Sqrt, scale=1.0 / 1024.0, bias=X.eps_col),
                     reads=[Bxn, X.Bcst2], writes=[Bxn])
                P.op('dve', lambda e: e.reciprocal(out=ss[:, 2:3], in_=ss[:, 1:2]), reads=[Bxn], writes=[Bxn])
                P.op('dve', lambda e, t=t: e.scalar_tensor_tensor(out=yacc[:, t, :], in0=yacc[:, t, :], scalar=ss[:, 2:3], in1=fg_bc,
                                                                  op0=ALU.mult, op1=ALU.mult), reads=[By[t], Bxn, Bgate], writes=[By[t]])
                P.dma('sp', [(hout[g0 + t * 128:g0 + (t + 1) * 128, :], yacc[:, t, :])], reads=[By[t]], writes=[Buf('o')], sem_buf=By[t])
        P.barrier()


def build_ffn1(ntok=2048, dff=3584, nexp=8):
    nc = bass.Bass("TRN2", target_bir_lowering=False)

    def din(name, shape, dt=F32):
        return nc.dram_tensor(name, shape, dt, kind="ExternalInput").ap()
    hin = din("hin", [ntok, 1024])
    cst_d = din("cst", [128, NCST])
    cT = din("cT", [128, 8])
    w_ada = din("w_ada", [1024, 3072])
    b_adaT = din("b_adaT", [128, 16])
    b_gate = din("b_gate", [1, 1024])
    gainT = din("gainT", [128, 8])
    fgain = din("fgain", [1, 1024])
    wr = din("wr", [1024, nexp])
    wg = din("wg", [nexp, 1024, dff])
    wu = din("wu", [nexp, 1024, dff])
    wd = din("wd", [nexp, dff, 1024])
    hout = nc.dram_tensor("hout", [ntok, 1024], F32, kind="ExternalOutput").ap()
    NF = dff // 128
    FB = 4
    GT = 1024
    ngrp = ntok // GT
    NT = GT // 128
    DQ = 256

    T = dict(hin=hin, cst_d=cst_d, cT=cT, w_ada=w_ada, b_adaT=b_adaT, b_gate=b_gate, gainT=gainT, fgain=fgain, wr=wr, wg=wg, wu=wu, wd=wd, hout=hout)
    X = setup_ctx(nc)
    with X.st:
        phase_ffn1(X, T, ntok, dff, nexp)
        X.P.emit()
    return nc


def ffn1_inputs(inputs, hmid_cores):
    maps = []
    for c in range(8):
        b, j = c // 2, c % 2
        maps.append({
            "hin": np.ascontiguousarray(hmid_cores[c]),
            "cst": make_consts(j),
            "cT": np.ascontiguousarray(inputs['c'][b].reshape(8, 128).T),
            "w_ada": np.ascontiguousarray(inputs['w_ada'][1][:, 3072:6144]),
            "b_adaT": np.ascontiguousarray(inputs['b_ada'][1][3072:5120].reshape(16, 128).T),
            "b_gate": np.ascontiguousarray(inputs['b_ada'][1][5120:6144][None, :]),
            "gainT": np.ascontiguousarray(inputs['ffn_gain'][1].reshape(8, 128).T),
            "fgain": np.ascontiguousarray(inputs['final_gain'][None, :]),
            "wr": np.ascontiguousarray(inputs['moe_w_router'][0]),
            "wg": np.ascontiguousarray(inputs['moe_w_gate'][0]),
            "wu": np.ascontiguousarray(inputs['moe_w_up'][0]),
            "wd": np.ascontiguousarray(inputs['moe_w_down'][0]),
        })
    return maps


def _run(nc, maps):
    res = run_bass_kernel_spmd(nc, maps, core_ids=list(range(8)))
    return res.results


def _kernel_unfused_impl(**inputs):
    inputs = {k: np.asarray(v) for k, v in inputs.items()}
    r0 = _run(build_att0(), att0_inputs(inputs))
    hmid0 = [np.asarray(r0[c]["hmid"]) for c in range(8)]
    r1 = _run(build_ffn0(), ffn0_inputs(inputs, hmid0))
    h1c = [np.asarray(r1[c]["hout"]) for c in range(8)]
    h1_full = gather_blocks(r1, "hout")
    r2 = _run(build_kv1(), kv1_inputs(inputs, h1_full))
    kv_res = [{k: np.asarray(r2[c][k]) for k in ["kslcT", "kwinT", "vslc", "vwin", "kcT", "vc"]} for c in range(8)]
    r3 = _run(build_att1(), att1_inputs(inputs, h1c, kv_res))
    hmid1 = [np.asarray(r3[c]["hmid"]) for c in range(8)]
    r4 = _run(build_ffn1(), ffn1_inputs(inputs, hmid1))
    return gather_blocks(r4, "hout").astype(np.float32)


def build_fused(stop_after=None):
    nc = bass.Bass("TRN2", target_bir_lowering=False)

    def din(name, shape, dt=F32):
        return nc.dram_tensor(name, shape, dt, kind="ExternalInput").ap()

    def dint(name, shape, dt=F32):
        return nc.dram_tensor(name, shape, dt, kind="Internal").ap()
    xk = din("xk", [NKC * 128, 1024])
    posk = din("posk", [1, NKC * 128], I32)
    posq = din("posq", [1, NQB * 128], I32)
    cst = din("cst", [128, NCST1])
    cT = din("cT", [128, 8])
    w_ada = din("w_ada", [2, 1024, 6144])
    b_adaT = din("b_adaT", [2, 128, 48])
    b_row = din("b_row", [2, 1, 6144])
    agT = din("agT", [2, 128, 8])
    fgT = din("fgT", [2, 128, 8])
    kvgT = din("kvgT", [128, 8])
    w_kv_ada = din("w_kv_ada", [1024, 2048])
    b_kvT = din("b_kvT", [128, 16])
    a_w_in = din("a_w_in", [1024, 2120])
    a_w_out = din("a_w_out", [1024, 1024])
    b_w_q = din("b_w_q", [1024, 1072])
    b_w_out = din("b_w_out", [1024, 1024])
    w_kv = din("w_kv", [1024, 1536])
    w1k = din("w1k", [2048, 256])
    w1v = din("w1v", [2048, 256])
    w2k = din("w2k", [256, 64])
    w2v = din("w2v", [256, 64])
    peTk = din("peTk", [64, 32])
    peTv = din("peTv", [64, 32])
    fwg = din("fwg", [1024, 2816])
    fwu = din("fwu", [1024, 2816])
    fwd = din("fwd", [2816, 1024])
    mwr = din("mwr", [1024, 8])
    if stop_after is None:
        mwg = din("mwg", [8, 1024, 3584])
        mwu = din("mwu", [8, 1024, 3584])
        mwd = din("mwd", [8, 3584, 1024])
    fgain = din("fgain", [1, 1024])
    cmpMb = din("cmpMb", [NQB, 128, 256], BF16)
    selvm = din("selvm", [NQB, 128, 64])
    selva = din("selva", [NQB, 128, 64])
    agg = din("agg", [256, 64], BF16)
    ridx = din("ridx", [128, NKC], I32)
    out = nc.dram_tensor("out", [NQB * 128, 1024], F32, kind="ExternalOutput").ap()
    hmid0 = dint("hmid0", [NQB * 128, 1024])
    h1own = dint("h1own", [NQB * 128, 1024])
    h1pair = dint("h1pair", [2 * NQB * 128, 1024])
    hmid1 = dint("hmid1", [NQB * 128, 1024])
    i_kslcT = dint("i_kslcT", [64, 4 * NKC * 128], BF16)
    i_kwinT = dint("i_kwinT", [64, 4 * NKC * 128], BF16)
    i_vslc = dint("i_vslc", [NKC * 128, 256], BF16)
    i_vwin = dint("i_vwin", [NKC * 128, 256], BF16)
    i_kcT = dint("i_kcT", [64, 1024], BF16)
    i_vc = dint("i_vc", [256, 256], BF16)

    X = setup_ctx(nc)
    P = X.P
    with X.st:
        def dbg_out(src, rows):
            dbg = nc.dram_tensor("dbg", [rows, 1024], F32, kind="ExternalOutput").ap()
            X.carve(15000)
            tl = X.a32.alloc(1024)
            for r in range(rows // 128):
                Bt = Buf('dbg')
                P.dma('sp', [(tl, src[r * 128:(r + 1) * 128, :])], writes=[Bt])
                P.dma('sp', [(dbg[r * 128:(r + 1) * 128, :], tl)], reads=[Bt], writes=[Buf('o')], sem_buf=Bt)
                P.barrier()
            P.emit()
            return nc
        phase_att0(X, dict(xk=xk, posk=posk, cst_d=cst, cT=cT, w_ada=w_ada[0][:, 0:3072], b_adaT=b_adaT[0][:, 0:16],
                           b_gate=b_row[0][:, 2048:3072], gainT=agT[0], w_in=a_w_in, w_out=a_w_out, hmid=hmid0))
        P.emit()
        if stop_after == 'att0':
            return dbg_out(hmid0, NQB * 128)
        if stop_after in ('att0q1', 'att0q2', 'att0q3'):
            X.carve(20000)
            tq = X.a32.alloc(4096)
            if stop_after == 'att0q1':
                P.dma('sp', [(tq[:, 0:1024], b_row[0][:, 5120:6144].partition_broadcast(128))], writes=[Buf('q')])
            elif stop_after == 'att0q2':
                P.dma('sp', [(tq.rearrange("p (k n) -> p k n", k=8), w_ada[0][:, 3072:3584].rearrange("(k p) n -> p k n", p=128))], writes=[Buf('q')])
            else:
                P.dma('sp', [(tq[:, 0:8], cT[:, :]), (tq[:, 8:24], b_adaT[0][:, 24:40]), (tq[:, 24:32], fgT[0][:, :])], writes=[Buf('q')])
            P.barrier()
            stop_after = 'att0r'
        if stop_after == 'att0m':
            for i in range(0, NBIG, 2000):
                P.op('pool', lambda e, i=i: e.memset(X.big[:, i:min(i + 2000, NBIG)], 12345.0), writes=[Buf('z')])
            P.barrier()
            stop_after = 'att0r'
        if stop_after == 'att0p':
            dbgA = nc.dram_tensor("dbgA", [NQB * 128, 1024], F32, kind="ExternalOutput").ap()
            X.carve(52000)
            X.a32.alloc(34000)
            hrA = X.a32.alloc(16 * 1024).rearrange("p (t n) -> p t n", t=16)
            BsA = [Buf('rA%d' % t) for t in range(16)]
            for t in range(16):
                P.dma('sp', [(hrA[:, t, :], hmid0[t * 128:(t + 1) * 128, :])], writes=[BsA[t]])
            for t in range(16):
                P.dma('sp', [(dbgA[t * 128:(t + 1) * 128, :], hrA[:, t, :])], reads=[BsA[t]], writes=[Buf('o')], sem_buf=BsA[t])
            P.barrier()
            X.carve(15000)
            load_consts(X, cst)
            X.eps_col = X.a32.alloc(1)
            X.Bcst2 = Buf('cst2')
            P.op('pool', lambda e: e.memset(X.eps_col, 1e-6), writes=[X.Bcst2])
            ffn_phase0(X, cT, w_ada[0][:, 3072:6144], b_adaT[0][:, 24:40], b_row[0][:, 5120:6144], fgT[0])
            P.barrier()
            stop_after = 'att0r'
        if stop_after == 'att0r':
            dbg = nc.dram_tensor("dbg", [NQB * 128, 1024], F32, kind="ExternalOutput").ap()
            X.carve(52000)
            X.a32.alloc(34000)
            hr = X.a32.alloc(16 * 1024).rearrange("p (t n) -> p t n", t=16)
            Bs = [Buf('r%d' % t) for t in range(16)]
            for t in range(16):
                P.dma('sp', [(hr[:, t, :], hmid0[t * 128:(t + 1) * 128, :])], writes=[Bs[t]])
            for t in range(16):
                P.dma('sp', [(dbg[t * 128:(t + 1) * 128, :], hr[:, t, :])], reads=[Bs[t]], writes=[Buf('o')], sem_buf=Bs[t])
            P.barrier()
            P.emit()
            return nc
        if stop_after == 'att0s':
            X.carve(15000)
            Wt = X.a16.alloc(22 * 1024).rearrange("p (f n) -> p f n", f=22)
            P.dma('pool', [(Wt, fwd.rearrange("(f p) n -> p f n", p=128))], writes=[Buf('wt')])
            P.barrier()
            return dbg_out(hmid0, NQB * 128)
        phase_ffn0(X, dict(hin=(hmid0 if stop_after != 'ffn0y' else din('hin_dbg', [NQB * 128, 1024])), cst_d=cst, cT=cT, w_ada=w_ada[0][:, 3072:6144], b_adaT=b_adaT[0][:, 24:40],
                           b_gate=b_row[0][:, 5120:6144], gainT=fgT[0], wg=fwg, wu=fwu, wd=fwd,
                           hout=(h1own if stop_after not in ('ffn0x', 'ffn0y') else nc.dram_tensor("dbg", [NQB * 128, 1024], F32, kind="ExternalOutput").ap())))
        P.emit()
        if stop_after in ('ffn0x', 'ffn0y'):
            return nc
        if stop_after == 'ffn0':
            return dbg_out(h1own, NQB * 128)
        Bcc = Buf('cc')
        P.dma('pool', None, writes=[Bcc], inc=1,
              fns=[lambda e, q=q: e.collective_compute("AllGather", ALU.bypass, replica_groups=[[0, 1], [2, 3], [4, 5], [6, 7]],
                                                       ins=[h1own[q * 512:(q + 1) * 512, :]], outs=[h1pair[q * 1024:(q + 1) * 1024, :]])
                   for q in range(4)])
        P.barrier()
        P.emit()
        if stop_after == 'cc':
            return dbg_out(h1pair, 2 * NQB * 128)
        phase_kv1(X, dict(hk=h1pair, posk=posk, cst_d=cst, cT=cT, w_ada=w_kv_ada, b_adaT=b_kvT, gainT=kvgT, w_kv=w_kv,
                          w1k=w1k, w1v=w1v, w2k=w2k, w2v=w2v, peTk=peTk, peTv=peTv, ridx=ridx,
                          o_kslcT=i_kslcT, o_kwinT=i_kwinT, o_vslc=i_vslc, o_vwin=i_vwin, o_kcT=i_kcT, o_vc=i_vc))
        P.emit()
        phase_att1(X, dict(hq=h1own, posq=posq, cst_d=cst, cT=cT, w_ada=w_ada[1][:, 0:3072], b_adaT=b_adaT[1][:, 0:16],
                           b_gate=b_row[1][:, 2048:3072], gainT=agT[1], w_q=b_w_q, w_out=b_w_out,
                           kslcT_d=i_kslcT, kwinT_d=i_kwinT, vslc_d=i_vslc, vwin_d=i_vwin, kcT_d=i_kcT, vc_d=i_vc,
                           cmpMb_d=cmpMb, selvm_d=selvm, selva_d=selva, agg_d=agg, hmid=hmid1))
        P.emit()
        phase_ffn1(X, dict(hin=hmid1, cst_d=cst, cT=cT, w_ada=w_ada[1][:, 3072:6144], b_adaT=b_adaT[1][:, 24:40],
                           b_gate=b_row[1][:, 5120:6144], gainT=fgT[1], fgain=fgain, wr=mwr, wg=mwg, wu=mwu, wd=mwd, hout=out))
        P.emit()
    return nc


def fused_inputs(inputs):
    A = lambda a: np.ascontiguousarray(a)
    maps = []
    shared = {
        "w_ada": A(inputs['w_ada']),
        "b_adaT": A(inputs['b_ada'].reshape(2, 48, 128).transpose(0, 2, 1)),
        "b_row": A(inputs['b_ada'][:, None, :]),
        "agT": A(inputs['attn_gain'].reshape(2, 8, 128).transpose(0, 2, 1)),
        "fgT": A(inputs['ffn_gain'].reshape(2, 8, 128).transpose(0, 2, 1)),
        "kvgT": A(inputs['kv_gain'].reshape(8, 128).T),
        "w_kv_ada": A(inputs['w_kv_ada']),
        "b_kvT": A(inputs['b_kv_ada'].reshape(16, 128).T),
        "a_w_in": A(inputs['a_w_in'][0]), "a_w_out": A(inputs['a_w_out'][0]),
        "b_w_q": A(inputs['b_w_q'][0]), "b_w_out": A(inputs['b_w_out'][0]),
        "w_kv": A(inputs['w_kv']),
        "w1k": A(inputs['cmp_w1_k']), "w1v": A(inputs['cmp_w1_v']), "w2k": A(inputs['cmp_w2_k']), "w2v": A(inputs['cmp_w2_v']),
        "peTk": A(inputs['cmp_pe_k'].T), "peTv": A(inputs['cmp_pe_v'].T),
        "fwg": A(inputs['ffn_w_gate'][0]), "fwu": A(inputs['ffn_w_up'][0]), "fwd": A(inputs['ffn_w_down'][0]),
        "mwr": A(inputs['moe_w_router'][0]), "mwg": A(inputs['moe_w_gate'][0]), "mwu": A(inputs['moe_w_up'][0]),
        "mwd": A(inputs['moe_w_down'][0]),
        "fgain": A(inputs['final_gain'][None, :]),
    }
    tabs = [make_tables1(0), make_tables1(1)]
    for c in range(8):
        b, j = c // 2, c % 2
        pos = np.asarray(inputs['positions'][b], np.int32)
        posq = np.concatenate([pos[(2 * m + j) * 128:(2 * m + j + 1) * 128] for m in range(NQB)])
        ridx = np.zeros((128, NKC), np.int32)
        for sc in range(NKC):
            gc = max(0, sc - (1 - j))
            o_ = (gc // 2) * 128 + np.arange(128)
            ridx[:, sc] = (o_ // 512) * 1024 + (gc % 2) * 512 + (o_ % 512)
        mp = dict(shared)
        mp.update({
            "xk": storage_order(np.asarray(inputs['x'][b], np.float32), j),
            "posk": A(storage_order(pos, j)[None, :]),
            "posq": A(posq[None, :]),
            "cst": make_consts1(j),
            "cT": A(inputs['c'][b].reshape(8, 128).T),
            "ridx": ridx,
        })
        mp.update(tabs[j])
        maps.append(mp)
    return maps


def kernel_unfused(**inputs):
    return _kernel_unfused_impl(**inputs)


def kernel(**inputs):
    inputs = {k: np.asarray(v) for k, v in inputs.items()}
    res = _run(build_fused(), fused_inputs(inputs))
    return gather_blocks(res, "out").astype(np.float32)
```
